# Optimizing a Trainium2 kernel written in Bass

```python
import math
import jax, jax.numpy as jnp
from jax import lax
import numpy as np

D_MODEL = 1024
BATCH = 8
SEQ = 8192
DEPTH = 4

CHUNK = 64
QBLK = 128
N_MIXERS = 4
D_MIX = D_MODEL
GROUP = D_MIX // N_MIXERS
HEAD_DIM = 64
N_HEADS = GROUP // HEAD_DIM
ROPE_THETA = 10000.0
EPS = 1e-6
MLA_Q_LORA = GROUP
MLA_KV_LORA = GROUP // 2
MLA_NOPE = HEAD_DIM
MLA_ROPE = HEAD_DIM // 2
MLA_V = HEAD_DIM
MLA_QK = MLA_NOPE + MLA_ROPE
DSA_TOPK_MAX = 256
IDX_HEADS = 4
IDX_DIM = 64
DIFF_DK = HEAD_DIM // 2
DIFF_DV = HEAD_DIM

IN_SIZES = (GROUP, GROUP, GROUP, GROUP,
            MLA_Q_LORA, MLA_KV_LORA, MLA_ROPE, GROUP,
            GROUP, HEAD_DIM, HEAD_DIM, GROUP, IDX_HEADS * IDX_DIM, IDX_DIM, IDX_HEADS,
            GROUP, GROUP, GROUP, GROUP)
IN_COLS = sum(IN_SIZES)

kernel_name = "hybrid_parallel_heads_stickbreak_mla_dsa_diff"


def rms_norm(x, g):
    xf = x.astype(jnp.float32)
    y = xf * lax.rsqrt(jnp.mean(xf * xf, axis=-1, keepdims=True) + EPS)
    return (y * g.astype(jnp.float32)).astype(x.dtype)


def apply_rope(x):
    S, d = x.shape[1], x.shape[-1]
    inv = ROPE_THETA ** (-jnp.arange(0, d, 2, dtype=jnp.float32) / d)
    ang = jnp.arange(S, dtype=jnp.float32)[:, None] * inv[None, :]
    cos = jnp.cos(ang)[None, :, None, :].astype(x.dtype)
    sin = jnp.sin(ang)[None, :, None, :].astype(x.dtype)
    x1, x2 = x[..., : d // 2], x[..., d // 2:]
    return jnp.concatenate([x1 * cos - x2 * sin, x1 * sin + x2 * cos], axis=-1)


def sweep(block_fn, S, q_args, kv_args):
    outs = []
    for i in range(S // QBLK):
        lo, hi = i * QBLK, (i + 1) * QBLK
        outs.append(block_fn(i, *[t[:, lo:hi] for t in q_args], *[t[:, :hi] for t in kv_args]))
    return jnp.concatenate(outs, axis=1)


def chunk_visible(i, Lk):
    tq = i * QBLK + jnp.arange(QBLK)
    chunk_end = (tq // CHUNK + 1) * CHUNK
    return jnp.arange(Lk)[None, :] < chunk_end[:, None]


def reverse_cumsum(x):
    L = x.shape[-1]
    n = L // QBLK
    xb = x.reshape(*x.shape[:-1], n, QBLK)
    r = jnp.arange(QBLK)
    upper = (r[:, None] >= r[None, :]).astype(x.dtype)
    within = jnp.einsum('...nj,js->...ns', xb, upper, precision=lax.Precision.HIGHEST)
    m = jnp.arange(n)
    later = (m[:, None] > m[None, :]).astype(x.dtype)
    after = jnp.einsum('...m,mn->...n', jnp.sum(xb, axis=-1), later, precision=lax.Precision.HIGHEST)
    return (within + after[..., None]).reshape(x.shape)


def stick_breaking(q, k, v):
    S, d = q.shape[1], q.shape[-1]
    scale = d ** -0.5

    def block(i, qb, kk, vv):
        Lk = kk.shape[1]
        tq = i * QBLK + jnp.arange(QBLK)
        strict = jnp.arange(Lk)[None, :] < tq[:, None]
        z = jnp.einsum('bqhd,bshd->bhqs', qb, kk).astype(jnp.float32) * scale
        log_1mb = jnp.where(strict, jax.nn.log_sigmoid(-z), 0.0)
        between = reverse_cumsum(log_1mb) - log_1mb
        a = jnp.where(strict, jnp.exp(jax.nn.log_sigmoid(z) + between), 0.0)
        return jnp.einsum('bhqs,bshd->bqhd', a.astype(vv.dtype), vv)

    return sweep(block, S, (q,), (k, v))


def softmax_attention(q, k, v, scale):
    S = q.shape[1]

    def block(i, qb, kk, vv):
        vis = chunk_visible(i, kk.shape[1])
        s = jnp.einsum('bqhd,bshd->bhqs', qb, kk).astype(jnp.float32) * scale
        p = jax.nn.softmax(jnp.where(vis, s, -jnp.inf), axis=-1)
        return jnp.einsum('bhqs,bshd->bqhd', p.astype(vv.dtype), vv)

    return sweep(block, S, (q,), (k, v))


def diff_attention(q1, q2, k1, k2, v, lam, scale):
    S = q1.shape[1]

    def block(i, q1b, q2b, k1b, k2b, vv):
        vis = chunk_visible(i, vv.shape[1])

        def probs(qb, kk):
            s = jnp.einsum('bqhd,bshd->bhqs', qb, kk).astype(jnp.float32) * scale
            return jax.nn.softmax(jnp.where(vis, s, -jnp.inf), axis=-1)

        w = probs(q1b, k1b) - lam * probs(q2b, k2b)
        return jnp.einsum('bhqs,bshd->bqhd', w.astype(vv.dtype), vv)

    return sweep(block, S, (q1, q2), (k1, k2, v))


def dsa_attention(q, k, v, iq, ik, iw, topk):
    S, d = q.shape[1], q.shape[-1]
    scale = d ** -0.5
    idx_scale = IDX_DIM ** -0.5

    def block(i, qb, iqb, iwb, kk, vv, ikk):
        Lk = kk.shape[1]
        kn = min(topk, Lk)
        adm = chunk_visible(i, Lk)
        logits = jnp.einsum('bqhe,bse->bqhs', iqb, ikk).astype(jnp.float32) * idx_scale
        score = jnp.einsum('bqh,bqhs->bqs', iwb.astype(jnp.float32), jax.nn.relu(logits))
        score = jnp.where(adm[None], score, -jnp.inf)
        top_val, top_idx = lax.top_k(score, kn)
        valid = jnp.isfinite(top_val)
        kg = jax.vmap(lambda a, ii: a[ii])(kk, top_idx)
        vg = jax.vmap(lambda a, ii: a[ii])(vv, top_idx)
        s = jnp.einsum('bqhd,bqkd->bhqk', qb, kg).astype(jnp.float32) * scale
        p = jax.nn.softmax(jnp.where(valid[:, None], s, -jnp.inf), axis=-1)
        return jnp.einsum('bhqk,bqkd->bqhd', p.astype(vg.dtype), vg)

    return sweep(block, S, (q, iq, iw), (k, v, ik))


def hybrid_layer(x, layer, ln_g, w_in, mla_q_norm_g, mla_kv_norm_g, mla_w_uq, mla_w_ukv, mla_q_g, mla_k_g,
                 dsa_q_g, dsa_k_g, diff_q_g, diff_k_g, diff_lq1, diff_lk1, diff_lq2, diff_lk2, diff_subln_g,
                 w_out, topk):
    B, S, _ = x.shape
    h = rms_norm(x, ln_g)
    proj = jnp.einsum('bsd,dn->bsn', h, w_in)
    split_points = np.cumsum(np.array(IN_SIZES))[:-1].tolist()
    (a_q, a_k, a_v, a_g,
     b_cq, b_ckv, b_kr, b_g,
     c_q, c_k, c_v, c_g, c_iq, c_ik, c_iw,
     d_q, d_k, d_v, d_g) = jnp.split(proj, split_points, axis=-1)

    def heads(t, n):
        return t.reshape(B, S, n, -1)

    y_a = stick_breaking(heads(a_q, N_HEADS), heads(a_k, N_HEADS), heads(a_v, N_HEADS))

    cq = rms_norm(b_cq, mla_q_norm_g)
    ckv = rms_norm(b_ckv, mla_kv_norm_g)
    qb = heads(jnp.einsum('bsr,rn->bsn', cq, mla_w_uq), N_HEADS)
    kv = heads(jnp.einsum('bsr,rn->bsn', ckv, mla_w_ukv), N_HEADS)
    q_nope = rms_norm(qb[..., :MLA_NOPE], mla_q_g[:MLA_NOPE])
    q_rope = apply_rope(rms_norm(qb[..., MLA_NOPE:], mla_q_g[MLA_NOPE:]))
    k_nope = rms_norm(kv[..., :MLA_NOPE], mla_k_g[:MLA_NOPE])
    v_b = kv[..., MLA_NOPE:]
    k_rope = apply_rope(rms_norm(b_kr, mla_k_g[MLA_NOPE:])[:, :, None, :])
    k_rope = jnp.broadcast_to(k_rope, (B, S, N_HEADS, MLA_ROPE))
    y_b = softmax_attention(jnp.concatenate([q_nope, q_rope], -1),
                            jnp.concatenate([k_nope, k_rope], -1), v_b, MLA_QK ** -0.5)

    qc = apply_rope(rms_norm(heads(c_q, N_HEADS), dsa_q_g))
    kc = apply_rope(rms_norm(c_k, dsa_k_g)[:, :, None, :])[:, :, 0, :]
    iq = apply_rope(heads(c_iq, IDX_HEADS))
    ik = apply_rope(c_ik[:, :, None, :])[:, :, 0, :]
    iw = c_iw * (IDX_HEADS ** -0.5)
    y_c = dsa_attention(qc, kc, c_v, iq, ik, iw, topk)

    lam_init = 0.8 - 0.6 * math.exp(-0.3 * layer)
    lam = (jnp.exp(jnp.sum(diff_lq1.astype(jnp.float32) * diff_lk1.astype(jnp.float32)))
           - jnp.exp(jnp.sum(diff_lq2.astype(jnp.float32) * diff_lk2.astype(jnp.float32))) + lam_init)
    qd = apply_rope(rms_norm(heads(d_q, 2 * N_HEADS), diff_q_g)).reshape(B, S, N_HEADS, 2, DIFF_DK)
    kd = apply_rope(rms_norm(heads(d_k, 2 * N_HEADS), diff_k_g)).reshape(B, S, N_HEADS, 2, DIFF_DK)
    od = diff_attention(qd[..., 0, :], qd[..., 1, :], kd[..., 0, :], kd[..., 1, :],
                        heads(d_v, N_HEADS), lam, DIFF_DK ** -0.5)
    y_d = rms_norm(od, diff_subln_g) * (1.0 - lam_init)

    y = jnp.concatenate([
        jax.nn.silu(a_g) * y_a.reshape(B, S, GROUP),
        jax.nn.silu(b_g) * y_b.reshape(B, S, GROUP),
        jax.nn.silu(c_g) * y_c.reshape(B, S, GROUP),
        jax.nn.silu(d_g) * y_d.reshape(B, S, GROUP),
    ], axis=-1)
    return x + jnp.einsum('bsn,nd->bsd', y, w_out)


def setup_inputs(seed: int = 0) -> dict:
    key = jax.random.key(seed)
    ks = jax.random.split(key, 20)
    f32 = jnp.float32

    def normal(k, shape, scale):
        return jax.random.normal(k, shape, f32) * scale

    def gain(k, n):
        return 1.0 + 0.02 * jax.random.normal(k, (DEPTH, n), f32)

    return {
        "x": normal(ks[0], (BATCH, SEQ, D_MODEL), 1.0),
        "ln_g": gain(ks[1], D_MODEL),
        "w_in": normal(ks[2], (DEPTH, D_MODEL, IN_COLS), D_MODEL ** -0.5),
        "mla_q_norm_g": gain(ks[3], MLA_Q_LORA),
        "mla_kv_norm_g": gain(ks[4], MLA_KV_LORA),
        "mla_w_uq": normal(ks[5], (DEPTH, MLA_Q_LORA, N_HEADS * MLA_QK), MLA_Q_LORA ** -0.5),
        "mla_w_ukv": normal(ks[6], (DEPTH, MLA_KV_LORA, N_HEADS * (MLA_NOPE + MLA_V)), MLA_KV_LORA ** -0.5),
        "mla_q_g": gain(ks[7], MLA_QK),
        "mla_k_g": gain(ks[8], MLA_QK),
        "dsa_q_g": gain(ks[9], HEAD_DIM),
        "dsa_k_g": gain(ks[10], HEAD_DIM),
        "diff_q_g": gain(ks[11], DIFF_DK),
        "diff_k_g": gain(ks[12], DIFF_DK),
        "diff_lq1": normal(ks[13], (DEPTH, DIFF_DK), 0.1),
        "diff_lk1": normal(ks[14], (DEPTH, DIFF_DK), 0.1),
        "diff_lq2": normal(ks[15], (DEPTH, DIFF_DK), 0.1),
        "diff_lk2": normal(ks[16], (DEPTH, DIFF_DK), 0.1),
        "diff_subln_g": gain(ks[17], DIFF_DV),
        "w_out": normal(ks[18], (DEPTH, D_MIX, D_MODEL), D_MIX ** -0.5),
    }


def reference(x, ln_g, w_in, mla_q_norm_g, mla_kv_norm_g, mla_w_uq, mla_w_ukv, mla_q_g, mla_k_g,
              dsa_q_g, dsa_k_g, diff_q_g, diff_k_g, diff_lq1, diff_lk1, diff_lq2, diff_lk2, diff_subln_g,
              w_out):
    topk = min(DSA_TOPK_MAX, x.shape[1] // 4)
    for l in range(DEPTH):
        x = hybrid_layer(x, l, ln_g[l], w_in[l], mla_q_norm_g[l], mla_kv_norm_g[l], mla_w_uq[l], mla_w_ukv[l],
                         mla_q_g[l], mla_k_g[l], dsa_q_g[l], dsa_k_g[l], diff_q_g[l], diff_k_g[l],
                         diff_lq1[l], diff_lk1[l], diff_lq2[l], diff_lk2[l], diff_subln_g[l], w_out[l], topk)
    return x
```

```python
import numpy as np
import ml_dtypes
from contextlib import ExitStack
import concourse.bass as bass
import concourse.mybir as mybir
from concourse.bass_utils import run_bass_kernel_spmd

F32 = mybir.dt.float32
BF16 = mybir.dt.bfloat16
AF = mybir.ActivationFunctionType
ALU = mybir.AluOpType
AX = mybir.AxisListType

D_MODEL = 1024
IN_COLS = 3684
EPS = 1e-6
NEGBIG = -1.0e30

O_AQ, O_AK, O_AV, O_AG = 0, 256, 512, 768
O_BCQ, O_BCKV, O_BKR, O_BG = 1024, 1280, 1408, 1440
O_CQ, O_CK, O_CV, O_CG, O_CIQ, O_CIK, O_CIW = 1696, 1952, 2016, 2080, 2336, 2592, 2656
O_DQ, O_DK, O_DV, O_DG = 2660, 2916, 3172, 3428


def _perm():
    r = lambda a, n: list(range(a, a + n))
    p = []
    p += r(O_AQ, 256) + r(O_AK, 256)
    p += r(O_AG, 256) + r(O_BG, 256) + r(O_CG, 256) + r(O_DG, 256)
    p += r(O_AV, 256) + r(O_DV, 256) + r(O_CV, 64)
    p += r(O_CQ, 256) + r(O_CK, 64) + r(O_CIQ, 256) + r(O_CIK, 64)
    p += r(O_DQ, 256) + r(O_DK, 256) + r(O_BKR, 32)
    p += r(O_BCQ, 256) + r(O_BCKV, 128)
    p += r(O_CIW, 4)
    assert len(p) == IN_COLS and sorted(p) == list(range(IN_COLS))
    return np.array(p)


PERM = _perm()
T0 = 1536
NTM = IN_COLS - T0
P_AV, P_DV, P_CV = 0, 256, 512
P_X64 = 576
P_Y32 = 1216
P_CQ = 1760
P_CKV = 2016
P_IW = 2144
NB_FT = 26


class Sched:
    ENG = ("pe", "act", "dve", "pool", "sp")

    def __init__(self, nc, es):
        self.nc = nc
        self.es = es
        self.e = dict(pe=nc.tensor, act=nc.scalar, dve=nc.vector, pool=nc.gpsimd, sp=nc.sync)
        self.semobj = {}
        self.cnt = {}
        for k in ("pe", "act", "dve", "pool"):
            self.semobj[k] = es.enter_context(nc.semaphore("s_" + k))
            self.cnt[k] = 0
        self.known = {k: {} for k in self.ENG}
        self.lastw = {}
        self.reads = {}
        self.n_ins = 0

    def _dsem(self, slot):
        name = "d_" + slot
        if name not in self.semobj:
            self.semobj[name] = self.es.enter_context(self.nc.semaphore(name))
            self.cnt[name] = 0
        return name

    def _waits(self, E, r, w):
        need = {}

        def add(tok, kind):
            sname, val, prod = tok
            if prod == E:
                if E == "pe" or kind == "war":
                    return
            if self.known[E].get(sname, 0) >= val:
                return
            if need.get(sname, 0) < val:
                need[sname] = val

        for k in r:
            t = self.lastw.get(k)
            if t is not None:
                add(t, "raw")
        for k in w:
            t = self.lastw.get(k)
            if t is not None:
                add(t, "waw")
            for t in self.reads.get(k, {}).values():
                add(t, "war")
        for sname, val in need.items():
            self.e[E].wait_ge(self.semobj[sname], val)
            self.known[E][sname] = val
            self.n_ins += 1

    def _record(self, tok, r, w):
        for k in r:
            d = self.reads.setdefault(k, {})
            d[tok[0]] = tok
        for k in w:
            self.lastw[k] = tok
            self.reads[k] = {}

    def op(self, E, fn, r=(), w=()):
        self._waits(E, r, w)
        ins = fn(self.e[E])
        self.cnt[E] += 1
        ins.then_inc(self.semobj[E], 1)
        tok = (E, self.cnt[E], E)
        self._record(tok, r, w)
        self.n_ins += 1
        return tok

    def dma(self, out, in_, r=(), w=(), slot=None, Q="sp"):
        self._waits(Q, r, w)
        sname = self._dsem(slot)
        ins = self.e[Q].dma_start(out=out, in_=in_)
        self.cnt[sname] += 16
        ins.then_inc(self.semobj[sname], 16)
        tok = (sname, self.cnt[sname], "dma")
        self._record(tok, r, w)
        self.n_ins += 1
        return tok

    def barrier(self, engines=None):
        for E in (engines or self.ENG):
            for sname, c in self.cnt.items():
                if c == 0 or self.known[E].get(sname, 0) >= c:
                    continue
                if sname == E and E == "pe":
                    continue
                self.e[E].wait_ge(self.semobj[sname], c)
                self.known[E][sname] = c
                self.n_ins += 1
        if engines is None:
            self.lastw = {}
            self.reads = {}


def bc(ap, shape, axis):
    return ap.unsqueeze(axis).to_broadcast(list(shape))


def build(S, DEPTH, debug=False, lam_inits=None, MIX="ABCD", DO_OUT=True):
    TOPK = min(256, S // 4)
    NT = S // 128
    NST = S // 512
    nc = bass.Bass("TRN2", target_bir_lowering=False)

    def din(name, shape, dt=F32):
        return nc.dram_tensor(name, list(shape), dt, kind="ExternalInput").ap()

    x_in = din("x", [S, D_MODEL])
    w_in = din("w_in", [DEPTH, D_MODEL, IN_COLS])
    w_out = din("w_out", [DEPTH, D_MODEL, D_MODEL])
    w_uq = din("w_uq", [DEPTH, 256, 384])
    w_ukv = din("w_ukv", [DEPTH, 128, 512])
    ln_g = din("ln_g", [DEPTH, 128, 8])
    qn_g = din("qn_g", [DEPTH, 128, 2])
    kvn_g = din("kvn_g", [DEPTH, 128, 1])
    g64 = din("g64", [DEPTH, 5 * 64])
    g32 = din("g32", [DEPTH, 17 * 32])
    gq96 = din("gq96", [DEPTH, 96])
    gk64 = din("gk64", [DEPTH, 64])
    subln = din("subln", [DEPTH, 64, 1])
    lvec = din("lvec", [DEPTH, 4 * 32])
    c_ident = din("c_ident", [128, 128], BF16)
    c_cs64 = din("c_cs64", [S, 64])
    c_cs32 = din("c_cs32", [S, 32])
    c_mask = din("c_mask", [128, 8 * 512], BF16)
    c_adm = din("c_adm", [128, 2 * 128])
    c_tri = din("c_tri", [128, 128], BF16)
    c_pow2 = din("c_pow2", [128, 32])

    okind = "ExternalOutput"
    y_out = nc.dram_tensor("y", [S, D_MODEL], F32, kind=okind).ap()
    dk = okind if debug else "Internal"
    FT = nc.dram_tensor("FT", [NB_FT, 128, S], BF16, kind=dk).ap()
    GT = nc.dram_tensor("GT", [8, 128, S], F32, kind=dk).ap()
    VA = nc.dram_tensor("VA", [S, 13, 128], BF16, kind=dk).ap()
    IW = nc.dram_tensor("IW", [S, 4], F32, kind=dk).ap()
    YT = nc.dram_tensor("YT", [8, 128, S], BF16, kind=dk).ap()
    XR = nc.dram_tensor("XR", [S, D_MODEL], F32, kind="Internal").ap()
    if debug:
        DSEL = nc.dram_tensor("DSEL", [S // 512, 128, S // 128, 512], BF16, kind=okind).ap()
        DSC = nc.dram_tensor("DSC", [S // 128, 128, S], F32, kind=okind).ap()
        DBS = nc.dram_tensor("DBS", [S // 128, 128, 64], F32, kind=okind).ap()

    with ExitStack() as es:
        sc = Sched(nc, es)
        op, dma = sc.op, sc.dma

        uniq = [0]

        def sb(st, name, shape, dt=F32):
            uniq[0] += 1
            return st.enter_context(nc.sbuf_tensor("%s_%d" % (name, uniq[0]), list(shape), dt))

        def ps(st, name, shape, dt=F32):
            return st.enter_context(nc.psum_tensor(name, list(shape), dt))

        ident = sb(es, "ident", [128, 128], BF16)
        masks = sb(es, "masks", [128, 8, 512], BF16)
        adm = sb(es, "adm", [128, 2, 128])
        tri = sb(es, "tri", [128, 128], BF16)
        ones_bf = sb(es, "ones_bf", [128, 128], BF16)
        ones_f = sb(es, "ones_f", [128, 128])
        pow2 = sb(es, "pow2", [128, 32])
        neghalf = sb(es, "neghalf", [128, 32])
        nh512 = sb(es, "nh512", [128, 512])
        dma(ident[:], c_ident[:, :], w=["ident"], slot="k_ident")
        dma(masks[:].rearrange("p a b -> p (a b)"), c_mask[:, :], w=["masks"], slot="k_masks")
        dma(adm[:].rearrange("p a b -> p (a b)"), c_adm[:, :], w=["adm"], slot="k_adm")
        dma(tri[:], c_tri[:, :], w=["tri"], slot="k_tri")
        dma(pow2[:], c_pow2[:, :], w=["pow2"], slot="k_pow2")
        op("dve", lambda e: e.memset(ones_bf[:], 1.0), w=["ones_bf"])
        op("dve", lambda e: e.memset(ones_f[:], 1.0), w=["ones_f"])
        op("dve", lambda e: e.memset(neghalf[:], -0.5), w=["neghalf"])
        op("dve", lambda e: e.memset(nh512[:], -0.5), w=["nh512"])

        pf = [ps(es, "pf%d" % i, [128, 512]) for i in range(6)]
        pb = [ps(es, "pb%d" % i, [128, 1024], BF16) for i in range(2)]

        for l in range(DEPTH):
            x_src = x_in if l == 0 else XR
            x_dst = y_out if l == DEPTH - 1 else XR
            lam_init = float(lam_inits[l])
            with ExitStack() as ls:
                sublnt = sb(ls, "sublnt", [64, 1])
                lv = sb(ls, "lv", [1, 4, 32])
                lam_bc = sb(ls, "lam_bc", [128, 2])
                dma(sublnt[:], subln[l, :, :], w=["sublnt"], slot="k_sublnt")
                dma(lv[:].rearrange("p a b -> p (a b)"), lvec[l:l + 1, :], w=["lv"], slot="k_lv")
                op("dve", lambda e: e.tensor_scalar(out=sublnt[:], in0=sublnt[:], scalar1=1.0 - lam_init, scalar2=None,
                                                    op0=ALU.mult), r=["sublnt"], w=["sublnt"])
                lt = sb(ls, "lt", [1, 2, 32])
                l2 = sb(ls, "l2", [1, 4])
                op("dve", lambda e: e.tensor_tensor(out=lt[:, 0, :], in0=lv[:, 0, :], in1=lv[:, 1, :], op=ALU.mult), r=["lv"], w=["lt"])
                op("dve", lambda e: e.tensor_tensor(out=lt[:, 1, :], in0=lv[:, 2, :], in1=lv[:, 3, :], op=ALU.mult), r=["lv", "lt"], w=["lt"])
                op("dve", lambda e: e.tensor_reduce(out=l2[:, 0:2], in_=lt[:], axis=AX.X, op=ALU.add), r=["lt"], w=["l2"])
                op("act", lambda e: e.activation(out=l2[:, 0:2], in_=l2[:, 0:2], func=AF.Exp), r=["l2"], w=["l2"])
                op("dve", lambda e: e.tensor_tensor(out=l2[:, 2:3], in0=l2[:, 0:1], in1=l2[:, 1:2], op=ALU.subtract), r=["l2"], w=["l2"])
                op("dve", lambda e: e.tensor_scalar(out=l2[:, 3:4], in0=l2[:, 2:3], scalar1=lam_init, scalar2=None, op0=ALU.add), r=["l2"], w=["l2"])
                op("dve", lambda e: e.tensor_scalar(out=l2[:, 2:3], in0=l2[:, 3:4], scalar1=-1.0, scalar2=None, op0=ALU.mult), r=["l2"], w=["l2"])
                op("pe", lambda e: e.matmul(pf[0][:, 0:2], lhsT=ones_f[0:1, :], rhs=l2[0:1, 2:4], start=True, stop=True),
                   r=["l2", "ones_f"], w=["pf0"])
                op("dve", lambda e: e.tensor_copy(out=lam_bc[:], in_=pf[0][:, 0:2]), r=["pf0"], w=["lam_bc"])

                with ExitStack() as p1:
                    w_sb = sb(p1, "w_sb", [128, 8, IN_COLS], BF16)
                    g64t = sb(p1, "g64t", [128, 5, 64])
                    g32t = sb(p1, "g32t", [128, 17, 32])
                    gq96t = sb(p1, "gq96t", [128, 96])
                    gk64t = sb(p1, "gk64t", [128, 64])
                    dma(g64t[:].rearrange("p a b -> p (a b)"), g64[l:l + 1, :].to_broadcast([128, 320]), w=["g64t"], slot="k_g64t")
                    dma(g32t[:].rearrange("p a b -> p (a b)"), g32[l:l + 1, :].to_broadcast([128, 544]), w=["g32t"], slot="k_g32t")
                    dma(gq96t[:], gq96[l:l + 1, :].to_broadcast([128, 96]), w=["gq96t"], slot="k_gq96t")
                    dma(gk64t[:], gk64[l:l + 1, :].to_broadcast([128, 64]), w=["gk64t"], slot="k_gk64t")
                    wuq_sb = sb(p1, "wuq_sb", [128, 2, 384], BF16)
                    wukv_sb = sb(p1, "wukv_sb", [128, 512], BF16)
                    lng = sb(p1, "lng", [128, 8])
                    qng = sb(p1, "qng", [128, 2])
                    kvng = sb(p1, "kvng", [128, 1])
                    dma(lng[:], ln_g[l, :, :], w=["lng"], slot="k_lng")
                    dma(qng[:], qn_g[l, :, :], w=["qng"], slot="k_qng")
                    dma(kvng[:], kvn_g[l, :, :], w=["kvng"], slot="k_kvng")
                    with ExitStack() as p0:
                        wst = [sb(p0, "wst%d" % i, [128, IN_COLS]) for i in range(2)]
                        for c in range(8):
                            k = "wst%d" % (c % 2)
                            dma(wst[c % 2][:], w_in[l, c * 128:(c + 1) * 128, :], w=[k], slot=k)
                            eng = "dve" if c % 2 == 0 else "pool"
                            op(eng, lambda e, c=c: e.tensor_scalar(out=w_sb[:, c, :], in0=wst[c % 2][:], scalar1=lng[:, c:c + 1],
                                                                    scalar2=None, op0=ALU.mult), r=[k, "lng"], w=["w_sb"])
                        for c in range(2):
                            dma(wst[c][:, 0:384], w_uq[l, c * 128:(c + 1) * 128, :], w=["wst%d" % c], slot="wst%d" % c)
                            op("dve", lambda e, c=c: e.tensor_scalar(out=wuq_sb[:, c, :], in0=wst[c][:, 0:384], scalar1=qng[:, c:c + 1],
                                                                      scalar2=None, op0=ALU.mult), r=["wst%d" % c, "qng"], w=["wuq_sb"])
                        dma(wst[0][:, 0:512], w_ukv[l, :, :], w=["wst0"], slot="wst0")
                        op("dve", lambda e: e.tensor_scalar(out=wukv_sb[:], in0=wst[0][:, 0:512], scalar1=kvng[:, 0:1],
                                                            scalar2=None, op0=ALU.mult), r=["wst0", "kvng"], w=["wukv_sb"])
                        sc.barrier()
                    xt = [sb(p1, "xt%d" % i, [128, D_MODEL]) for i in range(2)]
                    junk = sb(p1, "junk", [128, D_MODEL], BF16)
                    hb = sb(p1, "hb", [128, D_MODEL], BF16)
                    hT = sb(p1, "hT", [128, 8, 512], BF16)
                    proj = sb(p1, "proj", [128, NTM])
                    tmp = sb(p1, "tmp", [128, 640])
                    tmp2 = sb(p1, "tmp2", [128, 640])
                    st8 = sb(p1, "st8", [128, 64])
                    cs64 = [sb(p1, "cs64_%d" % i, [128, 64]) for i in range(2)]
                    cs32 = [sb(p1, "cs32_%d" % i, [128, 32]) for i in range(2)]
                    TT = sb(p1, "TT", [128, 22, 128], BF16)
                    VS = sb(p1, "VS", [128, 13, 128], BF16)
                    IWs = sb(p1, "IWs", [128, 4])
                    QB = sb(p1, "QB", [128, 4, 96])
                    KV = sb(p1, "KV", [128, 4, 128])
                    cqn = sb(p1, "cqn", [128, 384], BF16)
                    cqT = sb(p1, "cqT", [128, 3, 128], BF16)
                    FTs = sb(p1, "FTs", [128, NB_FT, 512], BF16)
                    GS = sb(p1, "GS", [128, 8, 512])
                    op("pool", lambda e: e.memset(TT[:], 0.0), w=["TT"])
                    op("pool", lambda e: e.memset(VS[:], 1.0), w=["VS"])

                    def load_x(t):
                        k = "xt%d" % (t % 2)
                        dma(xt[t % 2][:], x_src[t * 128:(t + 1) * 128, :], w=[k], slot=k)
                        dma(cs64[t % 2][:], c_cs64[t * 128:(t + 1) * 128, :], w=["cs64_%d" % (t % 2)], slot="cs64_%d" % (t % 2))
                        dma(cs32[t % 2][:], c_cs32[t * 128:(t + 1) * 128, :], w=["cs32_%d" % (t % 2)], slot="cs32_%d" % (t % 2))

                    def rstd_of(ss_ap, n, d, key):
                        op("dve", lambda e: e.tensor_scalar(out=ss_ap, in0=ss_ap, scalar1=1.0 / d, scalar2=EPS, op0=ALU.mult, op1=ALU.add),
                           r=[key], w=[key])
                        op("pool", lambda e: e.tensor_tensor(out=ss_ap, in0=ss_ap, in1=neghalf[:, 0:n], op=ALU.pow), r=[key, "neghalf"], w=[key])

                    load_x(0)
                    for stl in range(NST):
                        for sub in range(4):
                            t = stl * 4 + sub
                            if t + 1 < NT:
                                load_x(t + 1)
                            xk = "xt%d" % (t % 2)
                            X = xt[t % 2]
                            c64k, c32k = "cs64_%d" % (t % 2), "cs32_%d" % (t % 2)
                            C64, C32 = cs64[t % 2], cs32[t % 2]
                            op("act", lambda e: e.activation(out=junk[:], in_=X[:], func=AF.Square, accum_out=st8[:, 0:1]),
                               r=[xk], w=["junk", "st_x"])
                            rstd_of(st8[:, 0:1], 1, D_MODEL, "st_x")
                            op("dve", lambda e: e.tensor_scalar(out=hb[:], in0=X[:], scalar1=st8[:, 0:1], scalar2=None, op0=ALU.mult),
                               r=[xk, "st_x"], w=["hb"])
                            for c in range(8):
                                op("pe", lambda e, c=c: e.transpose(pb[0][:, c * 128:(c + 1) * 128], hb[:, c * 128:(c + 1) * 128], ident[:]),
                                   r=["hb", "ident"], w=["pb0"])
                            op("act", lambda e: e.activation(out=hT[:, :, sub * 128:(sub + 1) * 128],
                                                             in_=pb[0][:].rearrange("p (c t) -> p c t", c=8), func=AF.Copy),
                               r=["pb0"], w=["hT%d" % sub])
                            col = 0
                            ci = 0
                            while col < NTM:
                                n = min(512, NTM - col)
                                bank = 1 + (ci % 2)
                                for c in range(8):
                                    op("pe", lambda e, c=c, col=col, n=n, bank=bank: e.matmul(
                                        pf[bank][:, 0:n], lhsT=hT[:, c, sub * 128:(sub + 1) * 128], rhs=w_sb[:, c, T0 + col:T0 + col + n],
                                        start=(c == 0), stop=(c == 7)), r=["hT%d" % sub, "w_sb"], w=["pf%d" % bank])
                                eng = "act" if ci % 2 == 0 else "dve"
                                if eng == "act":
                                    op("act", lambda e, col=col, n=n, bank=bank: e.activation(out=proj[:, col:col + n], in_=pf[bank][:, 0:n], func=AF.Copy),
                                       r=["pf%d" % bank], w=["proj%d" % ci])
                                else:
                                    op("dve", lambda e, col=col, n=n, bank=bank: e.tensor_copy(out=proj[:, col:col + n], in_=pf[bank][:, 0:n]),
                                       r=["pf%d" % bank], w=["proj%d" % ci])
                                col += n
                                ci += 1
                            PJ = ["proj%d" % i for i in range(ci)]
                            op("pool", lambda e: e.tensor_copy(out=VS[:, 0:4, 0:64], in_=proj[:, P_AV:P_AV + 256].rearrange("p (h d) -> p h d", h=4)), r=PJ, w=["VS"])
                            op("pool", lambda e: e.tensor_copy(out=VS[:, 9:13, 0:64], in_=proj[:, P_DV:P_DV + 256].rearrange("p (h d) -> p h d", h=4)), r=PJ, w=["VS"])
                            op("pool", lambda e: e.tensor_copy(out=VS[:, 8, 0:64], in_=proj[:, P_CV:P_CV + 64]), r=PJ, w=["VS"])
                            op("pool", lambda e: e.tensor_scalar(out=IWs[:], in0=proj[:, P_IW:P_IW + 4], scalar1=0.0625, scalar2=None, op0=ALU.mult), r=PJ, w=["IWs"])
                            X64 = proj[:, P_X64:P_X64 + 640].rearrange("p (h d) -> p h d", h=10)
                            T3 = tmp[:].rearrange("p (h d) -> p h d", h=10)
                            T3b = tmp2[:].rearrange("p (h d) -> p h d", h=10)
                            op("act", lambda e: e.activation(out=T3[:, 0:5, :], in_=X64[:, 0:5, :], func=AF.Square), r=PJ, w=["tmp"])
                            op("dve", lambda e: e.tensor_reduce(out=st8[:, 8:13], in_=T3[:, 0:5, :], axis=AX.X, op=ALU.add), r=["tmp"], w=["st_a"])
                            rstd_of(st8[:, 8:13], 5, 64, "st_a")
                            op("dve", lambda e: e.tensor_tensor(out=X64[:, 0:5, :], in0=X64[:, 0:5, :], in1=bc(st8[:, 8:13], [128, 5, 64], 2), op=ALU.mult),
                               r=PJ + ["st_a"], w=PJ)
                            op("dve", lambda e: e.tensor_tensor(out=X64[:, 0:5, :], in0=X64[:, 0:5, :], in1=g64t[:], op=ALU.mult), r=PJ + ["g64t"], w=PJ)
                            cosb = bc(C64[:, 0:32], [128, 10, 32], 1)
                            sinb = bc(C64[:, 32:64], [128, 10, 32], 1)
                            op("dve", lambda e: e.tensor_tensor(out=T3[:, :, 0:32], in0=X64[:, :, 0:32], in1=cosb, op=ALU.mult), r=PJ + [c64k, "tmp"], w=["tmp"])
                            op("pool", lambda e: e.tensor_tensor(out=T3b[:, :, 0:32], in0=X64[:, :, 32:64], in1=sinb, op=ALU.mult), r=PJ + [c64k], w=["tmp2"])
                            op("dve", lambda e: e.tensor_tensor(out=T3[:, :, 32:64], in0=X64[:, :, 0:32], in1=sinb, op=ALU.mult), r=PJ + [c64k, "tmp"], w=["tmp"])
                            op("pool", lambda e: e.tensor_tensor(out=T3b[:, :, 32:64], in0=X64[:, :, 32:64], in1=cosb, op=ALU.mult), r=PJ + [c64k, "tmp2"], w=["tmp2"])
                            TU = TT[:].rearrange("p b (u d) -> p (b u) d", u=2)
                            for (h0, h1, u0) in ((0, 5, 16), (5, 10, 22)):
                                op("dve", lambda e, h0=h0, h1=h1, u0=u0: e.tensor_tensor(out=TU[:, u0:u0 + 5, 0:32], in0=T3[:, h0:h1, 0:32], in1=T3b[:, h0:h1, 0:32], op=ALU.subtract),
                                   r=["tmp", "tmp2"], w=["TT"])
                                op("dve", lambda e, h0=h0, h1=h1, u0=u0: e.tensor_tensor(out=TU[:, u0:u0 + 5, 32:64], in0=T3[:, h0:h1, 32:64], in1=T3b[:, h0:h1, 32:64], op=ALU.add),
                                   r=["tmp", "tmp2"], w=["TT"])
                            op("pool", lambda e: e.tensor_copy(out=TU[:, 21, :], in_=TU[:, 20, :]), r=["TT"], w=["TT"])
                            op("pool", lambda e: e.tensor_copy(out=TU[:, 27, :], in_=TU[:, 26, :]), r=["TT"], w=["TT"])
                            Y32 = proj[:, P_Y32:P_Y32 + 544].rearrange("p (h d) -> p h d", h=17)
                            U3 = tmp[:, 0:544].rearrange("p (h d) -> p h d", h=17)
                            U3b = tmp2[:, 0:544].rearrange("p (h d) -> p h d", h=17)
                            op("act", lambda e: e.activation(out=U3[:], in_=Y32, func=AF.Square), r=PJ + ["TT"], w=["tmp"])
                            op("dve", lambda e: e.tensor_reduce(out=st8[:, 16:33], in_=U3[:], axis=AX.X, op=ALU.add), r=["tmp"], w=["st_b"])
                            rstd_of(st8[:, 16:33], 17, 32, "st_b")
                            op("dve", lambda e: e.tensor_tensor(out=Y32, in0=Y32, in1=bc(st8[:, 16:33], [128, 17, 32], 2), op=ALU.mult), r=PJ + ["st_b"], w=PJ)
                            op("dve", lambda e: e.tensor_tensor(out=Y32, in0=Y32, in1=g32t[:], op=ALU.mult), r=PJ + ["g32t"], w=PJ)
                            cosb = bc(C32[:, 0:16], [128, 17, 16], 1)
                            sinb = bc(C32[:, 16:32], [128, 17, 16], 1)
                            op("dve", lambda e: e.tensor_tensor(out=U3[:, :, 0:16], in0=Y32[:, :, 0:16], in1=cosb, op=ALU.mult), r=PJ + [c32k, "tmp"], w=["tmp"])
                            op("pool", lambda e: e.tensor_tensor(out=U3b[:, :, 0:16], in0=Y32[:, :, 16:32], in1=sinb, op=ALU.mult), r=PJ + [c32k, "TT"], w=["tmp2"])
                            op("dve", lambda e: e.tensor_tensor(out=U3[:, :, 16:32], in0=Y32[:, :, 0:16], in1=sinb, op=ALU.mult), r=PJ + [c32k, "tmp"], w=["tmp"])
                            op("pool", lambda e: e.tensor_tensor(out=U3b[:, :, 16:32], in0=Y32[:, :, 16:32], in1=cosb, op=ALU.mult), r=PJ + [c32k, "tmp2"], w=["tmp2"])
                            TQ = TT[:].rearrange("p b (u d) -> p (b u) d", u=4)
                            op("dve", lambda e: e.tensor_tensor(out=TQ[:, 56:88:2, 0:16], in0=U3[:, 0:16, 0:16], in1=U3b[:, 0:16, 0:16], op=ALU.subtract), r=["tmp", "tmp2"], w=["TT"])
                            op("dve", lambda e: e.tensor_tensor(out=TQ[:, 56:88:2, 16:32], in0=U3[:, 0:16, 16:32], in1=U3b[:, 0:16, 16:32], op=ALU.add), r=["tmp", "tmp2"], w=["TT"])
                            for hh in range(4):
                                u = (4 + hh) * 4 + 2
                                op("dve", lambda e, u=u: e.tensor_tensor(out=TQ[:, u, 0:16], in0=U3[:, 16, 0:16], in1=U3b[:, 16, 0:16], op=ALU.subtract), r=["tmp", "tmp2"], w=["TT"])
                                op("dve", lambda e, u=u: e.tensor_tensor(out=TQ[:, u, 16:32], in0=U3[:, 16, 16:32], in1=U3b[:, 16, 16:32], op=ALU.add), r=["tmp", "tmp2"], w=["TT"])
                            CQ = proj[:, P_CQ:P_CQ + 256]
                            CKV = proj[:, P_CKV:P_CKV + 128]
                            op("act", lambda e: e.activation(out=tmp[:, 0:256], in_=CQ, func=AF.Square, accum_out=st8[:, 40:41]), r=PJ + ["tmp", "TT"], w=["tmp", "st_c"])
                            op("act", lambda e: e.activation(out=tmp[:, 256:384], in_=CKV, func=AF.Square, accum_out=st8[:, 41:42]), r=PJ + ["tmp"], w=["tmp", "st_c"])
                            op("dve", lambda e: e.tensor_scalar(out=st8[:, 41:42], in0=st8[:, 41:42], scalar1=2.0, scalar2=None, op0=ALU.mult), r=["st_c"], w=["st_c"])
                            rstd_of(st8[:, 40:42], 2, 256, "st_c")
                            op("dve", lambda e: e.tensor_scalar(out=cqn[:, 0:256], in0=CQ, scalar1=st8[:, 40:41], scalar2=None, op0=ALU.mult), r=PJ + ["st_c"], w=["cqn"])
                            op("dve", lambda e: e.tensor_scalar(out=cqn[:, 256:384], in0=CKV, scalar1=st8[:, 41:42], scalar2=None, op0=ALU.mult), r=PJ + ["st_c", "cqn"], w=["cqn"])
                            for c in range(3):
                                op("pe", lambda e, c=c: e.transpose(pb[1][:, c * 128:(c + 1) * 128], cqn[:, c * 128:(c + 1) * 128], ident[:]), r=["cqn", "ident"], w=["pb1"])
                            op("act", lambda e: e.activation(out=cqT[:].rearrange("p c t -> p (c t)"), in_=pb[1][:, 0:384], func=AF.Copy), r=["pb1"], w=["cqT"])
                            for c in range(2):
                                op("pe", lambda e, c=c: e.matmul(pf[3][:, 0:384], lhsT=cqT[:, c, :], rhs=wuq_sb[:, c, :], start=(c == 0), stop=(c == 1)),
                                   r=["cqT", "wuq_sb"], w=["pf3"])
                            op("pe", lambda e: e.matmul(pf[4][:, 0:512], lhsT=cqT[:, 2, :], rhs=wukv_sb[:], start=True, stop=True), r=["cqT", "wukv_sb"], w=["pf4"])
                            op("act", lambda e: e.activation(out=QB[:].rearrange("p h d -> p (h d)"), in_=pf[3][:, 0:384], func=AF.Copy), r=["pf3"], w=["QB"])
                            op("dve", lambda e: e.tensor_copy(out=KV[:].rearrange("p h d -> p (h d)"), in_=pf[4][:, 0:512]), r=["pf4"], w=["KV"])
                            Q3 = tmp[:, 0:384].rearrange("p (h d) -> p h d", h=4)
                            K3 = tmp2[:, 0:256].rearrange("p (h d) -> p h d", h=4)
                            op("act", lambda e: e.activation(out=Q3, in_=QB[:], func=AF.Square), r=["QB", "tmp"], w=["tmp"])
                            op("act", lambda e: e.activation(out=K3, in_=KV[:, :, 0:64], func=AF.Square), r=["KV", "tmp2", "TT"], w=["tmp2"])
                            op("dve", lambda e: e.tensor_reduce(out=st8[:, 44:48], in_=Q3[:, :, 0:64], axis=AX.X, op=ALU.add), r=["tmp"], w=["st_d"])
                            op("dve", lambda e: e.tensor_reduce(out=st8[:, 48:52], in_=K3, axis=AX.X, op=ALU.add), r=["tmp2"], w=["st_d"])
                            op("dve", lambda e: e.tensor_reduce(out=st8[:, 52:56], in_=Q3[:, :, 64:96], axis=AX.X, op=ALU.add), r=["tmp"], w=["st_e"])
                            rstd_of(st8[:, 44:52], 8, 64, "st_d")
                            rstd_of(st8[:, 52:56], 4, 32, "st_e")
                            T128 = TT[:]
                            op("dve", lambda e: e.tensor_tensor(out=QB[:, :, 0:64], in0=QB[:, :, 0:64], in1=bc(st8[:, 44:48], [128, 4, 64], 2), op=ALU.mult), r=["QB", "st_d"], w=["QB"])
                            op("dve", lambda e: e.tensor_tensor(out=T128[:, 0:4, 0:64], in0=QB[:, :, 0:64], in1=bc(gq96t[:, 0:64], [128, 4, 64], 1), op=ALU.mult), r=["QB", "gq96t"], w=["TT"])
                            op("dve", lambda e: e.tensor_tensor(out=KV[:, :, 0:64], in0=KV[:, :, 0:64], in1=bc(st8[:, 48:52], [128, 4, 64], 2), op=ALU.mult), r=["KV", "st_d"], w=["KV"])
                            op("dve", lambda e: e.tensor_tensor(out=T128[:, 4:8, 0:64], in0=KV[:, :, 0:64], in1=bc(gk64t[:], [128, 4, 64], 1), op=ALU.mult), r=["KV", "gk64t"], w=["TT"])
                            op("pool", lambda e: e.tensor_copy(out=VS[:, 4:8, 0:64], in_=KV[:, :, 64:128]), r=["KV"], w=["VS"])
                            op("dve", lambda e: e.tensor_tensor(out=QB[:, :, 64:96], in0=QB[:, :, 64:96], in1=bc(st8[:, 52:56], [128, 4, 32], 2), op=ALU.mult), r=["QB", "st_e"], w=["QB"])
                            op("dve", lambda e: e.tensor_tensor(out=QB[:, :, 64:96], in0=QB[:, :, 64:96], in1=bc(gq96t[:, 64:96], [128, 4, 32], 1), op=ALU.mult), r=["QB", "gq96t"], w=["QB"])
                            cosb = bc(C32[:, 0:16], [128, 4, 16], 1)
                            sinb = bc(C32[:, 16:32], [128, 4, 16], 1)
                            R3 = tmp[:, 0:128].rearrange("p (h d) -> p h d", h=4)
                            R3b = tmp2[:, 0:128].rearrange("p (h d) -> p h d", h=4)
                            op("dve", lambda e: e.tensor_tensor(out=R3[:, :, 0:16], in0=QB[:, :, 64:80], in1=cosb, op=ALU.mult), r=["QB", c32k, "tmp"], w=["tmp"])
                            op("pool", lambda e: e.tensor_tensor(out=R3b[:, :, 0:16], in0=QB[:, :, 80:96], in1=sinb, op=ALU.mult), r=["QB", c32k, "tmp2"], w=["tmp2"])
                            op("dve", lambda e: e.tensor_tensor(out=R3[:, :, 16:32], in0=QB[:, :, 64:80], in1=sinb, op=ALU.mult), r=["QB", c32k, "tmp"], w=["tmp"])
                            op("pool", lambda e: e.tensor_tensor(out=R3b[:, :, 16:32], in0=QB[:, :, 80:96], in1=cosb, op=ALU.mult), r=["QB", c32k, "tmp2"], w=["tmp2"])
                            op("dve", lambda e: e.tensor_tensor(out=T128[:, 0:4, 64:80], in0=R3[:, :, 0:16], in1=R3b[:, :, 0:16], op=ALU.subtract), r=["tmp", "tmp2"], w=["TT"])
                            op("dve", lambda e: e.tensor_tensor(out=T128[:, 0:4, 80:96], in0=R3[:, :, 16:32], in1=R3b[:, :, 16:32], op=ALU.add), r=["tmp", "tmp2"], w=["TT"])
                            for g0, g1, bank in ((0, 8, 0), (8, 16, 1), (16, 22, 0)):
                                for b in range(g0, g1):
                                    op("pe", lambda e, b=b, g0=g0, bank=bank: e.transpose(pb[bank][:, (b - g0) * 128:(b - g0 + 1) * 128], TT[:, b, :], ident[:]),
                                       r=["TT", "ident"], w=["pb%d" % bank])
                                nb = g1 - g0
                                eng = "act" if bank == 0 else "dve"
                                if eng == "act":
                                    op("act", lambda e, g0=g0, g1=g1, nb=nb, bank=bank: e.activation(
                                        out=FTs[:, 4 + g0:4 + g1, sub * 128:(sub + 1) * 128], in_=pb[bank][:, 0:nb * 128].rearrange("p (b t) -> p b t", b=nb), func=AF.Copy),
                                       r=["pb%d" % bank], w=["FTs"])
                                else:
                                    op("dve", lambda e, g0=g0, g1=g1, nb=nb, bank=bank: e.tensor_copy(
                                        out=FTs[:, 4 + g0:4 + g1, sub * 128:(sub + 1) * 128], in_=pb[bank][:, 0:nb * 128].rearrange("p (b t) -> p b t", b=nb)),
                                       r=["pb%d" % bank], w=["FTs"])
                            dma(VA[t * 128:(t + 1) * 128, :, :], VS[:], r=["VS"], slot="VS")
                            dma(IW[t * 128:(t + 1) * 128, :], IWs[:], r=["IWs"], slot="IWs")
                        HT = ["hT%d" % i for i in range(4)]
                        for ct in range(12):
                            bank = 3 + (ct % 2)
                            for c in range(8):
                                op("pe", lambda e, c=c, ct=ct, bank=bank: e.matmul(pf[bank][:, :], lhsT=w_sb[:, c, ct * 128:(ct + 1) * 128], rhs=hT[:, c, :],
                                                                                  start=(c == 0), stop=(c == 7)), r=HT + ["w_sb"], w=["pf%d" % bank])
                            if ct < 4:
                                op("dve", lambda e, ct=ct, bank=bank: e.tensor_copy(out=FTs[:, ct, :], in_=pf[bank][:, :]), r=["pf%d" % bank], w=["FTs"])
                            else:
                                op("act", lambda e, ct=ct, bank=bank: e.activation(out=GS[:, ct - 4, :], in_=pf[bank][:, :], func=AF.Silu), r=["pf%d" % bank], w=["GS"])
                        q0 = stl * 512
                        for g in range(0, NB_FT, 7):
                            g1 = min(NB_FT, g + 7)
                            dma(FT[g:g1, :, q0:q0 + 512].rearrange("b p t -> p b t"), FTs[:, g:g1, :], r=["FTs"], slot="FTs")
                        for g in range(0, 8, 4):
                            dma(GT[g:g + 4, :, q0:q0 + 512].rearrange("b p t -> p b t"), GS[:, g:g + 4, :], r=["GS"], slot="GS")
                    sc.barrier()
                NQ = S // 512
                NIT = 20

                def load_V(st, name, h0, nh):
                    V = sb(st, name, [128, NT, nh, 128], BF16)
                    for g in range(0, NT, 16):
                        g1 = min(NT, g + 16)
                        dma(V[:, g:g1, :, :], VA[g * 128:g1 * 128, h0:h0 + nh, :].rearrange("(k p) h d -> p k h d", p=128), w=[name], slot=name)
                    return V

                def load_K(st, name, b0, nb):
                    Kt = sb(st, name, [128, nb, S], BF16)
                    for b in range(nb):
                        dma(Kt[:, b, :], FT[b0 + b, :, :], w=[name], slot=name)
                    return Kt

                def softmax_mixer(mname, kb0, nkb, qb0, nqb, vh0, nvh, gb0, heads, scale, diag_mask, selT_fn=None, post=None, pre_q=None):
                    with ExitStack() as ms:
                        Kt = load_K(ms, "Kt", kb0, nkb)
                        V = load_V(ms, "Vr", vh0, nvh)
                        Qs = [sb(ms, "Qs%d" % i, [128, nqb, 512], BF16) for i in range(2)]
                        Gs = [sb(ms, "Gs%d" % i, [128, 2, 512]) for i in range(1)]
                        PT = [sb(ms, "PT%d" % i, [128, 512], BF16) for i in range(3)]
                        RC = sb(ms, "RC", [128, 2 if post is not None else 1, 512])
                        OD = sb(ms, "OD", [128, 3 if post is not None else 1, 512])
                        YS = [sb(ms, "YS%d" % i, [128, 2, 512], BF16) for i in range(2)]
                        extra = pre_q(ms) if pre_q is not None else None
                        ctr = {"s": 0, "p": 0, "o": 0}

                        def load_q(j):
                            q0 = j * 512
                            dma(Qs[j % 2][:], FT[qb0:qb0 + nqb, :, q0:q0 + 512].rearrange("b p t -> p b t"), w=["Qs%d" % (j % 2)], slot="Qs%d" % (j % 2))

                        def load_g(j):
                            q0 = j * 512
                            dma(Gs[0][:], GT[gb0:gb0 + 2, :, q0:q0 + 512].rearrange("b p t -> p b t"), w=["Gs0"], slot="Gs0")

                        load_q(0)
                        for j in range(NQ):
                            if j + 1 < NQ:
                                load_q(j + 1)
                            load_g(j)
                            Qk, Gk = "Qs%d" % (j % 2), "Gs0"
                            Q, G = Qs[j % 2], Gs[0]
                            nkt = 4 * (j + 1)
                            selT = selT_fn(j, extra, Q, Qk) if selT_fn is not None else None
                            ysk = "YS%d" % (j % 2)
                            for hi, (maps, vh) in enumerate(heads):
                                obanks = []
                                for m in maps:
                                    obanks.append(2 + (ctr["o"] % 4))
                                    ctr["o"] += 1
                                seq = [(kt, mi) for kt in range(nkt) for mi in range(len(maps))]

                                def emit_qk(idx):
                                    kt, mi = seq[idx]
                                    kblk, qblk, base, dkk = maps[mi]
                                    sbank = ctr["s"] % 2
                                    ctr["s"] += 1
                                    op("pe", lambda e: e.matmul(pf[sbank][:, :], lhsT=Kt[base:base + dkk, kblk, kt * 128:(kt + 1) * 128],
                                                                rhs=Q[base:base + dkk, qblk, :], start=True, stop=True),
                                       r=["Kt", Qk], w=["pf%d" % sbank])
                                    return sbank

                                pend = emit_qk(0)
                                for idx in range(len(seq)):
                                    kt, mi = seq[idx]
                                    sbank = pend
                                    if idx + 1 < len(seq):
                                        pend = emit_qk(idx + 1)
                                    pi = ctr["p"] % 3
                                    ctr["p"] += 1
                                    P = PT[pi]
                                    pk = "PT%d" % pi
                                    op("act", lambda e: e.activation(out=P[:], in_=pf[sbank][:, :], func=AF.Exp, scale=scale), r=["pf%d" % sbank], w=[pk])
                                    r_ = kt - (nkt - 4)
                                    if diag_mask is not None and r_ >= 0:
                                        op("dve", lambda e: e.tensor_tensor(out=P[:], in0=P[:], in1=masks[:, diag_mask + r_, :], op=ALU.mult), r=[pk, "masks"], w=[pk])
                                    if selT is not None:
                                        eng = "dve" if (idx % 2 == 0) else "pool"
                                        op(eng, lambda e: e.tensor_tensor(out=P[:], in0=P[:], in1=selT[:, kt, :], op=ALU.mult), r=[pk, "selT"], w=[pk])
                                    ob = obanks[mi]
                                    op("pe", lambda e: e.matmul(pf[ob][:, :], lhsT=V[:, kt, vh, :], rhs=P[:], start=(kt == 0), stop=(kt == nkt - 1)),
                                       r=["Vr", pk], w=["pf%d" % ob])
                                blk, hb_ = hi // 2, (hi % 2) * 64
                                for mi, ob in enumerate(obanks):
                                    op("dve", lambda e, mi=mi, ob=ob: e.reciprocal(out=RC[64:128, mi, :], in_=pf[ob][64:128, :]), r=["pf%d" % ob], w=["RC"])
                                if post is None:
                                    ob = obanks[0]
                                    op("dve", lambda e: e.tensor_tensor(out=OD[hb_:hb_ + 64, 0, :], in0=pf[ob][0:64, :], in1=RC[64:128, 0, :], op=ALU.mult),
                                       r=["pf%d" % ob, "RC"], w=["OD"])
                                    op("dve", lambda e: e.tensor_tensor(out=YS[j % 2][hb_:hb_ + 64, blk, :], in0=OD[hb_:hb_ + 64, 0, :], in1=G[hb_:hb_ + 64, blk, :], op=ALU.mult),
                                       r=["OD", Gk], w=[ysk])
                                else:
                                    post(obanks, RC, OD, G, Gk, YS[j % 2], ysk, blk, hb_)
                            q0 = j * 512
                            dma(YT[gb0:gb0 + 2, :, q0:q0 + 512].rearrange("b p t -> p b t"), YS[j % 2][:], r=[ysk], slot=ysk)
                        sc.barrier()

                if "B" in MIX:
                    softmax_mixer("B", 8, 4, 4, 4, 4, 4, 2, [([(h, h, 0, 96)], h) for h in range(4)], 96.0 ** -0.5, 0)

                def post_D(obanks, RC, OD, G, Gk, YSt, ysk, blk, hb_):
                    a, b = obanks
                    op("dve", lambda e: e.tensor_tensor(out=OD[0:64, 0, :], in0=pf[a][0:64, :], in1=RC[64:128, 0, :], op=ALU.mult), r=["pf%d" % a, "RC"], w=["OD"])
                    op("dve", lambda e: e.tensor_tensor(out=OD[0:64, 1, :], in0=pf[b][0:64, :], in1=RC[64:128, 1, :], op=ALU.mult), r=["pf%d" % b, "RC", "OD"], w=["OD"])
                    op("dve", lambda e: e.scalar_tensor_tensor(out=OD[0:64, 0, :], in0=OD[0:64, 1, :], scalar=lam_bc[0:64, 0:1], in1=OD[0:64, 0, :], op0=ALU.mult, op1=ALU.add),
                       r=["OD", "lam_bc"], w=["OD"])
                    op("act", lambda e: e.activation(out=OD[0:64, 1, :], in_=OD[0:64, 0, :], func=AF.Square), r=["OD"], w=["OD"])
                    op("pe", lambda e: e.matmul(pf[a][0:64, :], lhsT=ones_f[0:64, 0:64], rhs=OD[0:64, 1, :], start=True, stop=True), r=["OD", "ones_f"], w=["pf%d" % a])
                    op("dve", lambda e: e.tensor_scalar(out=OD[0:64, 1, :], in0=pf[a][0:64, :], scalar1=1.0 / 64, scalar2=EPS, op0=ALU.mult, op1=ALU.add), r=["pf%d" % a, "OD"], w=["OD"])
                    op("pool", lambda e: e.tensor_tensor(out=OD[0:64, 1, :], in0=OD[0:64, 1, :], in1=nh512[0:64, :], op=ALU.pow), r=["OD", "nh512"], w=["OD"])
                    op("dve", lambda e: e.scalar_tensor_tensor(out=OD[hb_:hb_ + 64, 2, :], in0=OD[0:64, 0, :], scalar=sublnt[0:64, 0:1], in1=OD[0:64, 1, :], op0=ALU.mult, op1=ALU.mult),
                       r=["OD", "sublnt"], w=["OD"])
                    op("dve", lambda e: e.tensor_tensor(out=YSt[hb_:hb_ + 64, blk, :], in0=OD[hb_:hb_ + 64, 2, :], in1=G[hb_:hb_ + 64, blk, :], op=ALU.mult), r=["OD", Gk], w=[ysk])

                if "D" in MIX:
                    softmax_mixer("D", 22, 4, 18, 4, 9, 4, 6,
                                  [([(h, h, c * 64, 32) for c in range(2)], h) for h in range(4)], 32.0 ** -0.5, 0, post=post_D)

                def pre_C(ms):
                    ex = {}
                    ex["IKc"] = [sb(ms, "IKc%d" % i, [128, 512], BF16) for i in range(3)]
                    ex["IQ"] = [sb(ms, "IQ%d" % i, [128, 2, 512], BF16) for i in range(1)]
                    ex["IWt"] = [sb(ms, "IWt%d" % i, [128, 4, 4]) for i in range(2)]
                    ex["SC"] = sb(ms, "SC", [128, S])
                    ex["JK"] = sb(ms, "JK", [128, S], BF16)
                    ex["BD"] = sb(ms, "BD", [128, S], BF16)
                    ex["selT"] = sb(ms, "selT", [128, NT, 512], BF16)
                    ex["RL"] = [sb(ms, "RL%d" % i, [128, 512]) for i in range(2)]
                    ex["bs"] = sb(ms, "bs", [128, 64])
                    return ex

                def selT_C(j, ex, Q, Qk):
                    q0 = j * 512
                    IQ, IWt = ex["IQ"][0], ex["IWt"][j % 2]
                    iqk, iwk = "IQ0", "IWt%d" % (j % 2)
                    SC, JK, selT, RL, bs, IKc = ex["SC"], ex["JK"], ex["selT"], ex["RL"], ex["bs"], ex["IKc"]
                    BD = ex["BD"]
                    dma(IQ[:], FT[15:17, :, q0:q0 + 512].rearrange("b p t -> p b t"), w=[iqk], slot=iqk)
                    dma(IWt[:], IW[q0:q0 + 512, :].rearrange("(a p) h -> p a h", p=128), w=[iwk], slot=iwk)
                    op("pool", lambda e: e.memset(selT[:, 4 * j:4 * j + 4, :], 0.0), w=["selT"])
                    rl = 0
                    chunks = [(qi, c0) for qi in range(4) for c0 in range(0, (4 * j + qi + 1) * 128, 512)]

                    def load_ik(ci):
                        qi_, c0_ = chunks[ci]
                        n_ = min(512, (4 * j + qi_ + 1) * 128 - c0_)
                        kk_ = "IKc%d" % (ci % 3)
                        dma(IKc[ci % 3][:, 0:n_], FT[17, :, c0_:c0_ + n_], w=[kk_], slot=kk_)

                    load_ik(0)
                    ci = 0
                    for qi in range(4):
                        i = 4 * j + qi
                        Lk = (i + 1) * 128
                        for c0 in range(0, Lk, 512):
                            n = min(512, Lk - c0)
                            if ci + 1 < len(chunks):
                                load_ik(ci + 1)
                            IKb, ikk = IKc[ci % 3], "IKc%d" % (ci % 3)
                            ci += 1
                            for h in range(4):
                                blk, base = h // 2, (h % 2) * 64
                                bank = h % 2
                                op("pe", lambda e: e.matmul(pf[bank][:, 0:n], lhsT=IQ[base:base + 64, blk, qi * 128:(qi + 1) * 128], rhs=IKb[base:base + 64, 0:n],
                                                            start=True, stop=True), r=[iqk, ikk], w=["pf%d" % bank])
                                R = RL[rl % 2]
                                rk = "RL%d" % (rl % 2)
                                rl += 1
                                op("act", lambda e: e.activation(out=R[:, 0:n], in_=pf[bank][:, 0:n], func=AF.Relu), r=["pf%d" % bank], w=[rk])
                                if h == 0:
                                    op("dve", lambda e: e.tensor_scalar(out=SC[:, c0:c0 + n], in0=R[:, 0:n], scalar1=IWt[:, qi, 0:1], scalar2=None, op0=ALU.mult),
                                       r=[rk, iwk], w=["SC"])
                                else:
                                    op("dve", lambda e: e.scalar_tensor_tensor(out=SC[:, c0:c0 + n], in0=R[:, 0:n], scalar=IWt[:, qi, h:h + 1], in1=SC[:, c0:c0 + n],
                                                                                op0=ALU.mult, op1=ALU.add), r=[rk, iwk, "SC"], w=["SC"])
                        op("dve", lambda e: e.tensor_tensor(out=SC[:, Lk - 128:Lk], in0=SC[:, Lk - 128:Lk], in1=adm[:, 0, :], op=ALU.mult), r=["SC", "adm"], w=["SC"])
                        op("dve", lambda e: e.tensor_tensor(out=SC[:, Lk - 128:Lk], in0=SC[:, Lk - 128:Lk], in1=adm[:, 1, :], op=ALU.add), r=["SC", "adm"], w=["SC"])
                        if Lk <= TOPK:
                            op("dve", lambda e: e.tensor_scalar(out=JK[:, 0:Lk], in0=SC[:, 0:Lk], scalar1=-1.0e29, scalar2=None, op0=ALU.is_ge), r=["SC"], w=["JK"])
                        else:
                            op("dve", lambda e: e.max(out=bs[:, 0:8], in_=SC[:, 0:Lk]), r=["SC"], w=["bs_hi"])
                            op("dve", lambda e: e.tensor_reduce(out=bs[:, 8:9], in_=SC[:, 0:Lk - 128], axis=AX.X, op=ALU.min), r=["SC"], w=["bs_lo"])
                            op("dve", lambda e: e.tensor_tensor(out=bs[:, 9:10], in0=bs[:, 0:1], in1=bs[:, 8:9], op=ALU.subtract), r=["bs_hi", "bs_lo"], w=["bs_d"])
                            op("dve", lambda e: e.tensor_scalar(out=bs[:, 9:10], in0=bs[:, 9:10], scalar1=1.001, scalar2=1.0e-20, op0=ALU.mult, op1=ALU.add), r=["bs_d"], w=["bs_d"])
                            op("dve", lambda e: e.tensor_scalar(out=bs[:, 32:32 + NIT], in0=pow2[:, 0:NIT], scalar1=bs[:, 9:10], scalar2=None, op0=ALU.mult), r=["bs_d", "pow2"], w=["bs_dl"])
                            for k in range(NIT):
                                op("dve", lambda e, k=k: e.tensor_tensor(out=bs[:, 10:11], in0=bs[:, 8:9], in1=bs[:, 32 + k:33 + k], op=ALU.add), r=["bs_lo", "bs_dl"], w=["bs_mid"])
                                op("dve", lambda e: e.tensor_scalar(out=JK[:, 0:Lk], in0=SC[:, 0:Lk], scalar1=bs[:, 10:11], scalar2=0.0, op0=ALU.is_ge, op1=ALU.add,
                                                                    accum_out=bs[:, 11:12]), r=["SC", "bs_mid"], w=["JK", "bs_cnt"])
                                op("dve", lambda e, k=k: e.tensor_scalar(out=bs[:, 12:13], in0=bs[:, 11:12], scalar1=TOPK - 0.5, scalar2=bs[:, 32 + k:33 + k], op0=ALU.is_ge, op1=ALU.mult),
                                   r=["bs_cnt", "bs_dl"], w=["bs_t"])
                                op("dve", lambda e: e.tensor_tensor(out=bs[:, 8:9], in0=bs[:, 8:9], in1=bs[:, 12:13], op=ALU.add), r=["bs_lo", "bs_t"], w=["bs_lo"])
                            op("dve", lambda e: e.tensor_tensor(out=bs[:, 13:14], in0=bs[:, 8:9], in1=bs[:, 31 + NIT:32 + NIT], op=ALU.add), r=["bs_lo", "bs_dl"], w=["bs_hf"])
                            op("dve", lambda e: e.tensor_scalar(out=JK[:, 0:Lk], in0=SC[:, 0:Lk], scalar1=bs[:, 8:9], scalar2=0.0, op0=ALU.is_ge, op1=ALU.add,
                                                                accum_out=bs[:, 14:15]), r=["SC", "bs_lo"], w=["JK", "bs_cl"])
                            op("dve", lambda e: e.scalar_tensor_tensor(out=BD[:, 0:Lk], in0=SC[:, 0:Lk], scalar=bs[:, 13:14], in1=JK[:, 0:Lk], op0=ALU.is_lt, op1=ALU.mult,
                                                                        accum_out=bs[:, 15:16]), r=["SC", "JK", "bs_hf"], w=["BD", "bs_nb"])
                            op("dve", lambda e: e.tensor_tensor(out=bs[:, 16:17], in0=bs[:, 15:16], in1=bs[:, 14:15], op=ALU.subtract), r=["bs_nb", "bs_cl"], w=["bs_m"])
                            op("dve", lambda e: e.tensor_scalar(out=bs[:, 16:17], in0=bs[:, 16:17], scalar1=float(TOPK) + 0.5, scalar2=None, op0=ALU.add), r=["bs_m"], w=["bs_m"])
                            op("dve", lambda e: e.tensor_tensor_scan(out=SC[:, 0:Lk], data0=ones_bf[:, 0:1].to_broadcast([128, Lk]), data1=BD[:, 0:Lk], initial=0.0, op0=ALU.mult, op1=ALU.add),
                               r=["BD", "ones_bf", "SC"], w=["SC"])
                            op("dve", lambda e: e.scalar_tensor_tensor(out=BD[:, 0:Lk], in0=SC[:, 0:Lk], scalar=bs[:, 16:17], in1=BD[:, 0:Lk], op0=ALU.is_gt, op1=ALU.mult),
                               r=["SC", "BD", "bs_m"], w=["BD"])
                            op("pool", lambda e: e.tensor_tensor(out=JK[:, 0:Lk], in0=JK[:, 0:Lk], in1=BD[:, 0:Lk], op=ALU.subtract), r=["JK", "BD"], w=["JK"])
                        if debug:
                            dma(DSC[i, :, :], SC[:, :], r=["SC"], slot="dbg1")
                            dma(DBS[i, :, :], bs[:, :], r=["bs_lo", "bs_hi", "bs_d", "bs_dl", "bs_mid", "bs_cnt", "bs_t", "bs_m", "bs_cl", "bs_nb", "bs_hf"], slot="dbg2")
                        for g in range(0, i + 1, 8):
                            g1 = min(i + 1, g + 8)
                            bank = (g // 8) % 2
                            for kb in range(g, g1):
                                op("pe", lambda e, kb=kb: e.transpose(pb[bank][:, (kb - g) * 128:(kb - g + 1) * 128], JK[:, kb * 128:(kb + 1) * 128], ident[:]),
                                   r=["JK", "ident"], w=["pb%d" % bank])
                            nb_ = g1 - g
                            if bank == 0:
                                op("act", lambda e: e.activation(out=selT[:, g:g1, qi * 128:(qi + 1) * 128], in_=pb[bank][:, 0:nb_ * 128].rearrange("p (b t) -> p b t", b=nb_), func=AF.Copy),
                                   r=["pb%d" % bank], w=["selT"])
                            else:
                                op("pool" if False else "dve", lambda e: e.tensor_copy(out=selT[:, g:g1, qi * 128:(qi + 1) * 128], in_=pb[bank][:, 0:nb_ * 128].rearrange("p (b t) -> p b t", b=nb_)),
                                   r=["pb%d" % bank], w=["selT"])
                    if debug:
                        dma(DSEL[j, :, :, :], selT[:, :, :], r=["selT"], slot="dbg3")
                    return selT

                if "C" in MIX:
                    softmax_mixer("C", 14, 1, 12, 2, 8, 1, 4, [([(0, h // 2, (h % 2) * 64, 64)], 0) for h in range(4)], 0.125, None, selT_fn=selT_C, pre_q=pre_C)

                if "A" in MIX:
                    with ExitStack() as ms:
                        Kt = load_K(ms, "Kt", 2, 2)
                        V = load_V(ms, "Vr", 0, 4)
                        nK = sb(ms, "nK", [128, 2, S], BF16)
                        for b in range(2):
                            op("dve" if b == 0 else "pool", lambda e, b=b: e.tensor_scalar(out=nK[:, b, :], in0=Kt[:, b, :], scalar1=-0.125, scalar2=None, op0=ALU.mult), r=["Kt"], w=["nK"])
                        Qs = [sb(ms, "Qs%d" % i, [128, 2, 512], BF16) for i in range(2)]
                        Gs = [sb(ms, "Gs%d" % i, [128, 2, 512]) for i in range(2)]
                        Et = [sb(ms, "Et%d" % i, [128, 512]) for i in range(2)]
                        SPt = [sb(ms, "SPt%d" % i, [128, 512]) for i in range(2)]
                        SPh = [sb(ms, "SPh%d" % i, [128, 512], BF16) for i in range(2)]
                        SPl = [sb(ms, "SPl%d" % i, [128, 512], BF16) for i in range(2)]
                        Cf = sb(ms, "Cf", [128, 512])
                        Ch = [sb(ms, "Ch%d" % i, [128, 512], BF16) for i in range(2)]
                        Cl = [sb(ms, "Cl%d" % i, [128, 512], BF16) for i in range(2)]
                        PT = [sb(ms, "PT%d" % i, [128, 512], BF16) for i in range(2)]
                        YS = [sb(ms, "YS%d" % i, [128, 2, 512], BF16) for i in range(2)]
                        cn = 0

                        def load_qA(j):
                            q0 = j * 512
                            dma(Qs[j % 2][:], FT[0:2, :, q0:q0 + 512].rearrange("b p t -> p b t"), w=["Qs%d" % (j % 2)], slot="Qs%d" % (j % 2))
                            dma(Gs[j % 2][:], GT[0:2, :, q0:q0 + 512].rearrange("b p t -> p b t"), w=["Gs%d" % (j % 2)], slot="Gs%d" % (j % 2))

                        load_qA(0)
                        for j in range(NQ):
                            if j + 1 < NQ:
                                load_qA(j + 1)
                            Qk, Gk = "Qs%d" % (j % 2), "Gs%d" % (j % 2)
                            Q, G = Qs[j % 2], Gs[j % 2]
                            nkt = 4 * (j + 1)
                            ysk = "YS%d" % (j % 2)
                            for h in range(4):
                                blk, base = h // 2, (h % 2) * 64
                                ob = 4 + (h % 2)
                                first = True
                                for kt in range(nkt - 1, -1, -1):
                                    u = cn % 2
                                    cn += 1
                                    zb, rb = u, 2 + u
                                    r_ = kt - (nkt - 4)
                                    ksl = slice(kt * 128, (kt + 1) * 128)
                                    op("pe", lambda e: e.matmul(pf[zb][:, :], lhsT=Kt[base:base + 64, blk, ksl], rhs=Q[base:base + 64, blk, :], start=True, stop=True),
                                       r=["Kt", Qk], w=["pf%d" % zb])
                                    op("act", lambda e: e.activation(out=Et[u][:], in_=pf[zb][:, :], func=AF.Exp, scale=0.125), r=["pf%d" % zb], w=["Et%d" % u])
                                    op("act", lambda e: e.activation(out=SPt[u][:], in_=Et[u][:], func=AF.Ln, bias=1.0, scale=1.0), r=["Et%d" % u], w=["SPt%d" % u])
                                    if r_ >= 0:
                                        op("dve", lambda e: e.tensor_tensor(out=SPt[u][:], in0=SPt[u][:], in1=masks[:, 4 + r_, :], op=ALU.mult), r=["SPt%d" % u, "masks"], w=["SPt%d" % u])
                                    op("dve", lambda e: e.tensor_copy(out=SPh[u][:], in_=SPt[u][:]), r=["SPt%d" % u], w=["SPh%d" % u])
                                    op("dve", lambda e: e.tensor_tensor(out=SPl[u][:], in0=SPt[u][:], in1=SPh[u][:], op=ALU.subtract), r=["SPt%d" % u, "SPh%d" % u], w=["SPl%d" % u])
                                    op("pe", lambda e: e.matmul(pf[rb][:, :], lhsT=tri[:], rhs=SPh[u][:], start=True, stop=False), r=["tri", "SPh%d" % u], w=["pf%d" % rb])
                                    op("pe", lambda e: e.matmul(pf[rb][:, :], lhsT=tri[:], rhs=SPl[u][:], start=False, stop=False), r=["tri", "SPl%d" % u], w=["pf%d" % rb])
                                    if not first:
                                        cu = (cn) % 2
                                        op("pe", lambda e: e.matmul(pf[rb][:, :], lhsT=ones_bf[:], rhs=Ch[cu][:], start=False, stop=False), r=["ones_bf", "Ch%d" % cu], w=["pf%d" % rb])
                                        op("pe", lambda e: e.matmul(pf[rb][:, :], lhsT=ones_bf[:], rhs=Cl[cu][:], start=False, stop=False), r=["ones_bf", "Cl%d" % cu], w=["pf%d" % rb])
                                    op("pe", lambda e: e.matmul(pf[rb][:, :], lhsT=nK[base:base + 64, blk, ksl], rhs=Q[base:base + 64, blk, :], start=False, stop=True),
                                       r=["nK", Qk], w=["pf%d" % rb])
                                    if kt > 0:
                                        cw = (cn + 1) % 2
                                        if first:
                                            op("pool", lambda e: e.tensor_copy(out=Cf[:], in_=SPt[u][:]), r=["SPt%d" % u], w=["Cf"])
                                        else:
                                            op("pool", lambda e: e.tensor_tensor(out=Cf[:], in0=Cf[:], in1=SPt[u][:], op=ALU.add), r=["SPt%d" % u, "Cf"], w=["Cf"])
                                        op("pool", lambda e: e.tensor_copy(out=Ch[cw][:], in_=Cf[:]), r=["Cf"], w=["Ch%d" % cw])
                                        op("pool", lambda e: e.tensor_tensor(out=Cl[cw][:], in0=Cf[:], in1=Ch[cw][:], op=ALU.subtract), r=["Cf", "Ch%d" % cw], w=["Cl%d" % cw])
                                    op("act", lambda e: e.activation(out=PT[u][:], in_=pf[rb][:, :], func=AF.Exp, scale=-1.0), r=["pf%d" % rb], w=["PT%d" % u])
                                    if r_ >= 0:
                                        op("dve", lambda e: e.tensor_tensor(out=PT[u][:], in0=PT[u][:], in1=masks[:, 4 + r_, :], op=ALU.mult), r=["PT%d" % u, "masks"], w=["PT%d" % u])
                                    op("pe", lambda e: e.matmul(pf[ob][0:64, :], lhsT=V[:, kt, h, 0:64], rhs=PT[u][:], start=first, stop=(kt == 0)), r=["Vr", "PT%d" % u], w=["pf%d" % ob])
                                    first = False
                                op("dve", lambda e: e.tensor_tensor(out=YS[j % 2][base:base + 64, blk, :], in0=pf[ob][0:64, :], in1=G[base:base + 64, blk, :], op=ALU.mult),
                                   r=["pf%d" % ob, Gk], w=[ysk])
                            q0 = j * 512
                            dma(YT[0:2, :, q0:q0 + 512].rearrange("b p t -> p b t"), YS[j % 2][:], r=[ysk], slot=ysk)
                        sc.barrier()

                if DO_OUT:
                    with ExitStack() as ms:
                        wo = sb(ms, "wo", [128, 8, D_MODEL], BF16)
                        wos = [sb(ms, "wos%d" % i, [128, D_MODEL]) for i in range(2)]
                        for c in range(8):
                            k = "wos%d" % (c % 2)
                            dma(wos[c % 2][:], w_out[l, c * 128:(c + 1) * 128, :], w=[k], slot=k)
                            op("dve" if c % 2 == 0 else "pool", lambda e, c=c: e.tensor_copy(out=wo[:, c, :], in_=wos[c % 2][:]), r=[k], w=["wo"])
                        YTs = [sb(ms, "YTs%d" % i, [128, 8, 512], BF16) for i in range(2)]
                        xs = [sb(ms, "xs%d" % i, [128, D_MODEL]) for i in range(2)]
                        xo = [sb(ms, "xo%d" % i, [128, D_MODEL]) for i in range(2)]

                        def load_y(stl):
                            q0 = stl * 512
                            for g in range(0, 8, 4):
                                dma(YTs[stl % 2][:, g:g + 4, :], YT[g:g + 4, :, q0:q0 + 512].rearrange("b p t -> p b t"), w=["YTs%d" % (stl % 2)], slot="YTs%d" % (stl % 2))

                        load_y(0)
                        for stl in range(NST):
                            if stl + 1 < NST:
                                load_y(stl + 1)
                            for sub in range(4):
                                t = stl * 4 + sub
                                xk, ok = "xs%d" % (t % 2), "xo%d" % (t % 2)
                                dma(xs[t % 2][:], x_src[t * 128:(t + 1) * 128, :], w=[xk], slot=xk)
                                for half in range(2):
                                    bank = (t * 2 + half) % 2
                                    for c in range(8):
                                        op("pe", lambda e, c=c: e.matmul(pf[bank][:, :], lhsT=YTs[stl % 2][:, c, sub * 128:(sub + 1) * 128], rhs=wo[:, c, half * 512:(half + 1) * 512],
                                                                         start=(c == 0), stop=(c == 7)), r=["YTs%d" % (stl % 2), "wo"], w=["pf%d" % bank])
                                    op("dve", lambda e: e.tensor_tensor(out=xo[t % 2][:, half * 512:(half + 1) * 512], in0=pf[bank][:, :], in1=xs[t % 2][:, half * 512:(half + 1) * 512], op=ALU.add),
                                       r=["pf%d" % bank, xk], w=[ok])
                                dma(x_dst[t * 128:(t + 1) * 128, :], xo[t % 2][:], r=[ok], slot=ok)
                        sc.barrier()
        sc.barrier()
        print("instructions:", sc.n_ins)
    return nc


def host_consts(S):
    bf = ml_dtypes.bfloat16
    c = {}
    c["c_ident"] = np.eye(128, dtype=np.float32).astype(bf)

    def cs(d):
        inv = (10000.0 ** (-np.arange(0, d, 2, dtype=np.float32) / np.float32(d))).astype(np.float32)
        ang = (np.arange(S, dtype=np.float32)[:, None] * inv[None, :]).astype(np.float32)
        return np.concatenate([np.cos(ang), np.sin(ang)], axis=1).astype(np.float32)

    c["c_cs64"] = cs(64)
    c["c_cs32"] = cs(32)
    kk = np.arange(128)[:, None]
    qq = np.arange(512)[None, :]
    m = []
    for r in range(4):
        m.append(((r * 128 + kk) < ((qq // 64) + 1) * 64))
    for r in range(4):
        m.append(((r * 128 + kk) < qq))
    c["c_mask"] = np.concatenate(m, axis=1).astype(np.float32).astype(bf)
    q1 = np.arange(128)[:, None]
    k1 = np.arange(128)[None, :]
    m01 = (k1 < ((q1 // 64) + 1) * 64).astype(np.float32)
    c["c_adm"] = np.concatenate([m01, (1.0 - m01) * NEGBIG], axis=1).astype(np.float32)
    c["c_tri"] = (np.arange(128)[:, None] >= np.arange(128)[None, :]).astype(np.float32).astype(bf)
    c["c_pow2"] = np.tile((2.0 ** -(np.arange(32) + 1.0))[None, :], (128, 1)).astype(np.float32)
    return c


def host_params(inp, DEPTH):
    f = lambda a: np.ascontiguousarray(np.asarray(a, dtype=np.float32)[:DEPTH])
    p = {}
    p["w_in"] = np.ascontiguousarray(f(inp["w_in"])[:, :, PERM])
    p["w_out"] = f(inp["w_out"])
    p["w_uq"] = f(inp["mla_w_uq"])
    p["w_ukv"] = f(inp["mla_w_ukv"])
    p["ln_g"] = np.ascontiguousarray(f(inp["ln_g"]).reshape(-1, 8, 128).transpose(0, 2, 1))
    p["qn_g"] = np.ascontiguousarray(f(inp["mla_q_norm_g"]).reshape(-1, 2, 128).transpose(0, 2, 1))
    p["kvn_g"] = np.ascontiguousarray(f(inp["mla_kv_norm_g"]).reshape(-1, 128, 1))
    dq, dkk = f(inp["dsa_q_g"]), f(inp["dsa_k_g"])
    p["g64"] = np.ascontiguousarray(np.concatenate([dq] * 4 + [dkk], axis=1))
    fq, fk, mk = f(inp["diff_q_g"]), f(inp["diff_k_g"]), f(inp["mla_k_g"])
    p["g32"] = np.ascontiguousarray(np.concatenate([fq] * 8 + [fk] * 8 + [mk[:, 64:96]], axis=1))
    p["gq96"] = f(inp["mla_q_g"])
    p["gk64"] = np.ascontiguousarray(mk[:, :64])
    p["subln"] = np.ascontiguousarray(f(inp["diff_subln_g"]).reshape(-1, 64, 1))
    p["lvec"] = np.ascontiguousarray(np.concatenate([f(inp["diff_lq1"]), f(inp["diff_lk1"]), f(inp["diff_lq2"]), f(inp["diff_lk2"])], axis=1))
    return p


_CACHE = {}


def run(inputs, S, DEPTH, debug=False):
    import math
    key = (S, DEPTH, debug)
    lam_inits = [0.8 - 0.6 * math.exp(-0.3 * l) for l in range(DEPTH)]
    if key not in _CACHE:
        _CACHE[key] = build(S, DEPTH, debug, lam_inits, MIX=globals().get("MIXSEL", "ABCD"))
    nc = _CACHE[key]
    x = np.asarray(inputs["x"], dtype=np.float32)
    B = x.shape[0]
    shared = {}
    shared.update(host_consts(S))
    shared.update(host_params(inputs, DEPTH))
    in_maps = []
    for b in range(B):
        m = dict(shared)
        m["x"] = np.ascontiguousarray(x[b, :S])
        in_maps.append(m)
    res = run_bass_kernel_spmd(nc, in_maps, core_ids=list(range(B)))
    return res


def kernel(**inputs):
    res = run(inputs, 8192, 4)
    return np.stack([np.asarray(r["y"], dtype=np.float32) for r in res.results], axis=0)
```

```python
import numpy as np
import ml_dtypes
from contextlib import ExitStack
import concourse.bass as bass
import concourse.mybir as mybir
from concourse.bass_utils import run_bass_kernel_spmd

F32 = mybir.dt.float32
BF16 = mybir.dt.bfloat16
AF = mybir.ActivationFunctionType
ALU = mybir.AluOpType
AX = mybir.AxisListType

D_MODEL = 1024
IN_COLS = 3684
EPS = 1e-6
NEGBIG = -1.0e30

O_AQ, O_AK, O_AV, O_AG = 0, 256, 512, 768
O_BCQ, O_BCKV, O_BKR, O_BG = 1024, 1280, 1408, 1440
O_CQ, O_CK, O_CV, O_CG, O_CIQ, O_CIK, O_CIW = 1696, 1952, 2016, 2080, 2336, 2592, 2656
O_DQ, O_DK, O_DV, O_DG = 2660, 2916, 3172, 3428


def _perm():
    r = lambda a, n: list(range(a, a + n))
    p = []
    p += r(O_AQ, 256) + r(O_AK, 256)
    p += r(O_AG, 256) + r(O_BG, 256) + r(O_CG, 256) + r(O_DG, 256)
    p += r(O_AV, 256) + r(O_DV, 256) + r(O_CV, 64)
    p += r(O_CQ, 256) + r(O_CK, 64) + r(O_CIQ, 256) + r(O_CIK, 64)
    p += r(O_DQ, 256) + r(O_DK, 256) + r(O_BKR, 32)
    p += r(O_BCQ, 256) + r(O_BCKV, 128)
    p += r(O_CIW, 4)
    assert len(p) == IN_COLS and sorted(p) == list(range(IN_COLS))
    return np.array(p)


PERM = _perm()
T0 = 1536
NTM = IN_COLS - T0
P_AV, P_DV, P_CV = 0, 256, 512
P_X64 = 576
P_Y32 = 1216
P_CQ = 1760
P_CKV = 2016
P_IW = 2144
NB_FT = 26


class Sched:
    ENG = ("pe", "act", "dve", "pool", "sp")

    def __init__(self, nc, es):
        self.nc = nc
        self.es = es
        self.e = dict(pe=nc.tensor, act=nc.scalar, dve=nc.vector, pool=nc.gpsimd, sp=nc.sync)
        self.semobj = {}
        self.cnt = {}
        for k in ("pe", "act", "dve", "pool"):
            self.semobj[k] = es.enter_context(nc.semaphore("s_" + k))
            self.cnt[k] = 0
        self.known = {k: {} for k in self.ENG}
        self.lastw = {}
        self.reads = {}
        self.n_ins = 0

    def _dsem(self, slot):
        name = "d_" + slot
        if name not in self.semobj:
            self.semobj[name] = self.es.enter_context(self.nc.semaphore(name))
            self.cnt[name] = 0
        return name

    def _waits(self, E, r, w, attach_ok=False):
        need = {}

        def add(tok, kind):
            sname, val, prod = tok
            if prod == E:
                if E == "pe" or kind == "war":
                    return
            if self.known[E].get(sname, 0) >= val:
                return
            if need.get(sname, 0) < val:
                need[sname] = val

        for k in r:
            t = self.lastw.get(k)
            if t is not None:
                add(t, "raw")
        for k in w:
            t = self.lastw.get(k)
            if t is not None:
                add(t, "waw")
            for t in self.reads.get(k, {}).values():
                add(t, "war")
        items = list(need.items())
        attach = None
        if attach_ok and items:
            attach = items.pop()
        for sname, val in items:
            self.e[E].wait_ge(self.semobj[sname], val)
            self.known[E][sname] = val
            self.n_ins += 1
        if attach is not None:
            self.known[E][attach[0]] = attach[1]
        return attach

    def _record(self, tok, r, w):
        for k in r:
            d = self.reads.setdefault(k, {})
            d[tok[0]] = tok
        for k in w:
            self.lastw[k] = tok
            self.reads[k] = {}

    def op(self, E, fn, r=(), w=()):
        att = self._waits(E, r, w, attach_ok=True)
        ins = fn(self.e[E])
        if att is not None:
            ins._wait_ge(self.semobj[att[0]], att[1])
        self.cnt[E] += 1
        ins.then_inc(self.semobj[E], 1)
        tok = (E, self.cnt[E], E)
        self._record(tok, r, w)
        self.n_ins += 1
        return tok

    def dma(self, out, in_, r=(), w=(), slot=None, Q="sp"):
        self._waits(Q, r, w)
        sname = self._dsem(slot)
        ins = self.e[Q].dma_start(out=out, in_=in_)
        self.cnt[sname] += 16
        ins.then_inc(self.semobj[sname], 16)
        tok = (sname, self.cnt[sname], "dma")
        self._record(tok, r, w)
        self.n_ins += 1
        return tok

    def barrier(self, engines=None):
        for E in (engines or self.ENG):
            for sname, c in self.cnt.items():
                if c == 0 or self.known[E].get(sname, 0) >= c:
                    continue
                if sname == E and E == "pe":
                    continue
                self.e[E].wait_ge(self.semobj[sname], c)
                self.known[E][sname] = c
                self.n_ins += 1
        if engines is None:
            self.lastw = {}
            self.reads = {}


def bc(ap, shape, axis):
    return ap.unsqueeze(axis).to_broadcast(list(shape))


def build(S, DEPTH, debug=False, lam_inits=None, MIX="ABCD", DO_OUT=True):
    TOPK = min(256, S // 4)
    NT = S // 128
    NST = S // 512
    nc = bass.Bass("TRN2", target_bir_lowering=False)

    def din(name, shape, dt=F32):
        return nc.dram_tensor(name, list(shape), dt, kind="ExternalInput").ap()

    x_in = din("x", [S, D_MODEL])
    w_in = din("w_in", [DEPTH, D_MODEL, IN_COLS])
    w_out = din("w_out", [DEPTH, D_MODEL, D_MODEL])
    w_uq = din("w_uq", [DEPTH, 256, 384])
    w_ukv = din("w_ukv", [DEPTH, 128, 512])
    ln_g = din("ln_g", [DEPTH, 128, 8])
    qn_g = din("qn_g", [DEPTH, 128, 2])
    kvn_g = din("kvn_g", [DEPTH, 128, 1])
    g64 = din("g64", [DEPTH, 5 * 64])
    g32 = din("g32", [DEPTH, 17 * 32])
    gq96 = din("gq96", [DEPTH, 96])
    gk64 = din("gk64", [DEPTH, 64])
    subln = din("subln", [DEPTH, 64, 1])
    lvec = din("lvec", [DEPTH, 4 * 32])
    c_ident = din("c_ident", [128, 128], BF16)
    c_cs64 = din("c_cs64", [S, 64])
    c_cs32 = din("c_cs32", [S, 32])
    c_mask = din("c_mask", [128, 8 * 512], BF16)
    c_adm = din("c_adm", [128, 2 * 128])
    c_tri = din("c_tri", [128, 128], BF16)
    c_pow2 = din("c_pow2", [128, 32])

    okind = "ExternalOutput"
    y_out = nc.dram_tensor("y", [S, D_MODEL], F32, kind=okind).ap()
    dk = okind if debug else "Internal"
    FT = nc.dram_tensor("FT", [NB_FT, 128, S], BF16, kind=dk).ap()
    GT = nc.dram_tensor("GT", [8, 128, S], F32, kind=dk).ap()
    VA = nc.dram_tensor("VA", [S, 13, 128], BF16, kind=dk).ap()
    IW = nc.dram_tensor("IW", [S, 4], F32, kind=dk).ap()
    YT = nc.dram_tensor("YT", [8, 128, S], BF16, kind=dk).ap()
    XR = nc.dram_tensor("XR", [S, D_MODEL], F32, kind="Internal").ap()
    if debug:
        DSEL = nc.dram_tensor("DSEL", [S // 512, 128, S // 128, 512], BF16, kind=okind).ap()
        DSC = nc.dram_tensor("DSC", [S // 128, 128, S], F32, kind=okind).ap()
        DBS = nc.dram_tensor("DBS", [S // 128, 128, 64], F32, kind=okind).ap()

    with ExitStack() as es:
        sc = Sched(nc, es)
        op, dma = sc.op, sc.dma

        uniq = [0]

        def sb(st, name, shape, dt=F32):
            uniq[0] += 1
            return st.enter_context(nc.sbuf_tensor("%s_%d" % (name, uniq[0]), list(shape), dt))

        def ps(st, name, shape, dt=F32):
            return st.enter_context(nc.psum_tensor(name, list(shape), dt))

        ident = sb(es, "ident", [128, 128], BF16)
        masks = sb(es, "masks", [128, 8, 512], BF16)
        adm = sb(es, "adm", [128, 2, 128])
        tri = sb(es, "tri", [128, 128], BF16)
        ones_bf = sb(es, "ones_bf", [128, 128], BF16)
        ones_f = sb(es, "ones_f", [128, 128])
        pow2 = sb(es, "pow2", [128, 32])
        neghalf = sb(es, "neghalf", [128, 32])
        epst = sb(es, "epst", [128, 1])
        nh512 = sb(es, "nh512", [128, 512])
        dma(ident[:], c_ident[:, :], w=["ident"], slot="k_ident")
        dma(masks[:].rearrange("p a b -> p (a b)"), c_mask[:, :], w=["masks"], slot="k_masks")
        dma(adm[:].rearrange("p a b -> p (a b)"), c_adm[:, :], w=["adm"], slot="k_adm")
        dma(tri[:], c_tri[:, :], w=["tri"], slot="k_tri")
        dma(pow2[:], c_pow2[:, :], w=["pow2"], slot="k_pow2")
        op("dve", lambda e: e.memset(ones_bf[:], 1.0), w=["ones_bf"])
        op("dve", lambda e: e.memset(ones_f[:], 1.0), w=["ones_f"])
        op("dve", lambda e: e.memset(neghalf[:], -0.5), w=["neghalf"])
        op("dve", lambda e: e.memset(epst[:], EPS), w=["epst"])
        op("dve", lambda e: e.memset(nh512[:], -0.5), w=["nh512"])

        pf = [ps(es, "pf%d" % i, [128, 512]) for i in range(6)]
        pb = [ps(es, "pb%d" % i, [128, 1024], BF16) for i in range(2)]

        for l in range(DEPTH):
            x_src = x_in if l == 0 else XR
            x_dst = y_out if l == DEPTH - 1 else XR
            lam_init = float(lam_inits[l])
            with ExitStack() as ls:
                sublnt = sb(ls, "sublnt", [64, 1])
                lv = sb(ls, "lv", [1, 4, 32])
                lam_bc = sb(ls, "lam_bc", [128, 2])
                dma(sublnt[:], subln[l, :, :], w=["sublnt"], slot="k_sublnt")
                dma(lv[:].rearrange("p a b -> p (a b)"), lvec[l:l + 1, :], w=["lv"], slot="k_lv")
                op("dve", lambda e: e.tensor_scalar(out=sublnt[:], in0=sublnt[:], scalar1=1.0 - lam_init, scalar2=None,
                                                    op0=ALU.mult), r=["sublnt"], w=["sublnt"])
                lt = sb(ls, "lt", [1, 2, 32])
                l2 = sb(ls, "l2", [1, 4])
                op("dve", lambda e: e.tensor_tensor(out=lt[:, 0, :], in0=lv[:, 0, :], in1=lv[:, 1, :], op=ALU.mult), r=["lv"], w=["lt"])
                op("dve", lambda e: e.tensor_tensor(out=lt[:, 1, :], in0=lv[:, 2, :], in1=lv[:, 3, :], op=ALU.mult), r=["lv", "lt"], w=["lt"])
                op("dve", lambda e: e.tensor_reduce(out=l2[:, 0:2], in_=lt[:], axis=AX.X, op=ALU.add), r=["lt"], w=["l2"])
                op("act", lambda e: e.activation(out=l2[:, 0:2], in_=l2[:, 0:2], func=AF.Exp), r=["l2"], w=["l2"])
                op("dve", lambda e: e.tensor_tensor(out=l2[:, 2:3], in0=l2[:, 0:1], in1=l2[:, 1:2], op=ALU.subtract), r=["l2"], w=["l2"])
                op("dve", lambda e: e.tensor_scalar(out=l2[:, 3:4], in0=l2[:, 2:3], scalar1=lam_init, scalar2=None, op0=ALU.add), r=["l2"], w=["l2"])
                op("dve", lambda e: e.tensor_scalar(out=l2[:, 2:3], in0=l2[:, 3:4], scalar1=-1.0, scalar2=None, op0=ALU.mult), r=["l2"], w=["l2"])
                op("pe", lambda e: e.matmul(pf[0][:, 0:2], lhsT=ones_f[0:1, :], rhs=l2[0:1, 2:4], start=True, stop=True),
                   r=["l2", "ones_f"], w=["pf0"])
                op("dve", lambda e: e.tensor_copy(out=lam_bc[:], in_=pf[0][:, 0:2]), r=["pf0"], w=["lam_bc"])

                with ExitStack() as p1:
                    w_sb = sb(p1, "w_sb", [128, 8, IN_COLS], BF16)
                    g64t = sb(p1, "g64t", [128, 5, 64])
                    g32t = sb(p1, "g32t", [128, 17, 32])
                    gq96t = sb(p1, "gq96t", [128, 96])
                    gk64t = sb(p1, "gk64t", [128, 64])
                    dma(g64t[:].rearrange("p a b -> p (a b)"), g64[l:l + 1, :].to_broadcast([128, 320]), w=["g64t"], slot="k_g64t")
                    dma(g32t[:].rearrange("p a b -> p (a b)"), g32[l:l + 1, :].to_broadcast([128, 544]), w=["g32t"], slot="k_g32t")
                    dma(gq96t[:], gq96[l:l + 1, :].to_broadcast([128, 96]), w=["gq96t"], slot="k_gq96t")
                    dma(gk64t[:], gk64[l:l + 1, :].to_broadcast([128, 64]), w=["gk64t"], slot="k_gk64t")
                    wuq_sb = sb(p1, "wuq_sb", [128, 2, 384], BF16)
                    wukv_sb = sb(p1, "wukv_sb", [128, 512], BF16)
                    lng = sb(p1, "lng", [128, 8])
                    qng = sb(p1, "qng", [128, 2])
                    kvng = sb(p1, "kvng", [128, 1])
                    dma(lng[:], ln_g[l, :, :], w=["lng"], slot="k_lng")
                    dma(qng[:], qn_g[l, :, :], w=["qng"], slot="k_qng")
                    dma(kvng[:], kvn_g[l, :, :], w=["kvng"], slot="k_kvng")
                    with ExitStack() as p0:
                        wst = [sb(p0, "wst%d" % i, [128, IN_COLS]) for i in range(2)]
                        for c in range(8):
                            k = "wst%d" % (c % 2)
                            dma(wst[c % 2][:], w_in[l, c * 128:(c + 1) * 128, :], w=[k], slot=k)
                            eng = "dve" if c % 2 == 0 else "pool"
                            op(eng, lambda e, c=c: e.tensor_scalar(out=w_sb[:, c, :], in0=wst[c % 2][:], scalar1=lng[:, c:c + 1],
                                                                    scalar2=0.0, op0=ALU.mult, op1=ALU.add), r=[k, "lng"], w=["w_sb"])
                        for c in range(2):
                            dma(wst[c][:, 0:384], w_uq[l, c * 128:(c + 1) * 128, :], w=["wst%d" % c], slot="wst%d" % c)
                            op("dve", lambda e, c=c: e.tensor_scalar(out=wuq_sb[:, c, :], in0=wst[c][:, 0:384], scalar1=qng[:, c:c + 1],
                                                                      scalar2=None, op0=ALU.mult), r=["wst%d" % c, "qng"], w=["wuq_sb"])
                        dma(wst[0][:, 0:512], w_ukv[l, :, :], w=["wst0"], slot="wst0")
                        op("dve", lambda e: e.tensor_scalar(out=wukv_sb[:], in0=wst[0][:, 0:512], scalar1=kvng[:, 0:1],
                                                            scalar2=None, op0=ALU.mult), r=["wst0", "kvng"], w=["wukv_sb"])
                        sc.barrier()
                    xt = [sb(p1, "xt%d" % i, [128, D_MODEL]) for i in range(2)]
                    junk = sb(p1, "junk", [128, D_MODEL], BF16)
                    hb = sb(p1, "hb", [128, D_MODEL], BF16)
                    hT = sb(p1, "hT", [128, 8, 512], BF16)
                    proj = sb(p1, "proj", [128, NTM])
                    tmp = sb(p1, "tmp", [128, 640])
                    tmp2 = sb(p1, "tmp2", [128, 640])
                    st8 = sb(p1, "st8", [128, 64])
                    cs64 = [sb(p1, "cs64_%d" % i, [128, 64]) for i in range(2)]
                    cs32 = [sb(p1, "cs32_%d" % i, [128, 32]) for i in range(2)]
                    TT = sb(p1, "TT", [128, 22, 128], BF16)
                    VS = sb(p1, "VS", [128, 13, 128], BF16)
                    IWs = sb(p1, "IWs", [128, 4])
                    QB = sb(p1, "QB", [128, 4, 96])
                    KV = sb(p1, "KV", [128, 4, 128])
                    cqn = sb(p1, "cqn", [128, 384], BF16)
                    cqT = sb(p1, "cqT", [128, 3, 128], BF16)
                    FTs = sb(p1, "FTs", [128, NB_FT, 512], BF16)
                    GS = sb(p1, "GS", [128, 8, 512])
                    op("pool", lambda e: e.memset(TT[:], 0.0), w=["TT"])
                    op("pool", lambda e: e.memset(VS[:], 1.0), w=["VS"])

                    def load_x(t):
                        k = "xt%d" % (t % 2)
                        dma(xt[t % 2][:], x_src[t * 128:(t + 1) * 128, :], w=[k], slot=k)
                        dma(cs64[t % 2][:], c_cs64[t * 128:(t + 1) * 128, :], w=["cs64_%d" % (t % 2)], slot="cs64_%d" % (t % 2))
                        dma(cs32[t % 2][:], c_cs32[t * 128:(t + 1) * 128, :], w=["cs32_%d" % (t % 2)], slot="cs32_%d" % (t % 2))

                    def rstd_of(ss_ap, n, d, key):
                        op("act", lambda e: e.activation(out=ss_ap, in_=ss_ap, func=AF.Ln, scale=1.0 / d, bias=epst[:, 0:1]), r=[key, "epst"], w=[key])
                        op("act", lambda e: e.activation(out=ss_ap, in_=ss_ap, func=AF.Exp, scale=-0.5), r=[key], w=[key])

                    load_x(0)
                    for stl in range(NST):
                        for sub in range(4):
                            t = stl * 4 + sub
                            if t + 1 < NT:
                                load_x(t + 1)
                            xk = "xt%d" % (t % 2)
                            X = xt[t % 2]
                            c64k, c32k = "cs64_%d" % (t % 2), "cs32_%d" % (t % 2)
                            C64, C32 = cs64[t % 2], cs32[t % 2]
                            op("act", lambda e: e.activation(out=junk[:], in_=X[:], func=AF.Square, accum_out=st8[:, 0:1]),
                               r=[xk], w=["junk", "st_x"])
                            rstd_of(st8[:, 0:1], 1, D_MODEL, "st_x")
                            op("dve", lambda e: e.tensor_scalar(out=hb[:], in0=X[:], scalar1=st8[:, 0:1], scalar2=None, op0=ALU.mult),
                               r=[xk, "st_x"], w=["hb"])
                            for c in range(8):
                                op("pe", lambda e, c=c: e.transpose(pb[0][:, c * 128:(c + 1) * 128], hb[:, c * 128:(c + 1) * 128], ident[:]),
                                   r=["hb", "ident"], w=["pb0"])
                            op("act", lambda e: e.activation(out=hT[:, :, sub * 128:(sub + 1) * 128],
                                                             in_=pb[0][:].rearrange("p (c t) -> p c t", c=8), func=AF.Copy),
                               r=["pb0"], w=["hT%d" % sub])
                            col = 0
                            ci = 0
                            while col < NTM:
                                n = min(512, NTM - col)
                                bank = 1 + (ci % 2)
                                for c in range(8):
                                    op("pe", lambda e, c=c, col=col, n=n, bank=bank: e.matmul(
                                        pf[bank][:, 0:n], lhsT=hT[:, c, sub * 128:(sub + 1) * 128], rhs=w_sb[:, c, T0 + col:T0 + col + n],
                                        start=(c == 0), stop=(c == 7)), r=["hT%d" % sub, "w_sb"], w=["pf%d" % bank])
                                eng = "act" if ci % 2 == 0 else "dve"
                                if eng == "act":
                                    op("act", lambda e, col=col, n=n, bank=bank: e.activation(out=proj[:, col:col + n], in_=pf[bank][:, 0:n], func=AF.Copy),
                                       r=["pf%d" % bank], w=["proj%d" % ci])
                                else:
                                    op("dve", lambda e, col=col, n=n, bank=bank: e.tensor_copy(out=proj[:, col:col + n], in_=pf[bank][:, 0:n]),
                                       r=["pf%d" % bank], w=["proj%d" % ci])
                                col += n
                                ci += 1
                            PJ = ["proj%d" % i for i in range(ci)]
                            op("pool", lambda e: e.tensor_copy(out=VS[:, 0:4, 0:64], in_=proj[:, P_AV:P_AV + 256].rearrange("p (h d) -> p h d", h=4)), r=PJ, w=["VS"])
                            op("pool", lambda e: e.tensor_copy(out=VS[:, 9:13, 0:64], in_=proj[:, P_DV:P_DV + 256].rearrange("p (h d) -> p h d", h=4)), r=PJ, w=["VS"])
                            op("pool", lambda e: e.tensor_copy(out=VS[:, 8, 0:64], in_=proj[:, P_CV:P_CV + 64]), r=PJ, w=["VS"])
                            op("pool", lambda e: e.tensor_scalar(out=IWs[:], in0=proj[:, P_IW:P_IW + 4], scalar1=0.0625, scalar2=0.0, op0=ALU.mult, op1=ALU.add), r=PJ, w=["IWs"])
                            X64 = proj[:, P_X64:P_X64 + 640].rearrange("p (h d) -> p h d", h=10)
                            T3 = tmp[:].rearrange("p (h d) -> p h d", h=10)
                            T3b = tmp2[:].rearrange("p (h d) -> p h d", h=10)
                            op("act", lambda e: e.activation(out=T3[:, 0:5, :], in_=X64[:, 0:5, :], func=AF.Square), r=PJ, w=["tmp"])
                            op("dve", lambda e: e.tensor_reduce(out=st8[:, 8:13], in_=T3[:, 0:5, :], axis=AX.X, op=ALU.add), r=["tmp"], w=["st_a"])
                            rstd_of(st8[:, 8:13], 5, 64, "st_a")
                            op("dve", lambda e: e.tensor_tensor(out=X64[:, 0:5, :], in0=X64[:, 0:5, :], in1=bc(st8[:, 8:13], [128, 5, 64], 2), op=ALU.mult),
                               r=PJ + ["st_a"], w=PJ)
                            op("dve", lambda e: e.tensor_tensor(out=X64[:, 0:5, :], in0=X64[:, 0:5, :], in1=g64t[:], op=ALU.mult), r=PJ + ["g64t"], w=PJ)
                            cosb = bc(C64[:, 0:32], [128, 10, 32], 1)
                            sinb = bc(C64[:, 32:64], [128, 10, 32], 1)
                            op("dve", lambda e: e.tensor_tensor(out=T3[:, :, 0:32], in0=X64[:, :, 0:32], in1=cosb, op=ALU.mult), r=PJ + [c64k, "tmp"], w=["tmp"])
                            op("pool", lambda e: e.tensor_tensor(out=T3b[:, :, 0:32], in0=X64[:, :, 32:64], in1=sinb, op=ALU.mult), r=PJ + [c64k], w=["tmp2"])
                            op("dve", lambda e: e.tensor_tensor(out=T3[:, :, 32:64], in0=X64[:, :, 0:32], in1=sinb, op=ALU.mult), r=PJ + [c64k, "tmp"], w=["tmp"])
                            op("pool", lambda e: e.tensor_tensor(out=T3b[:, :, 32:64], in0=X64[:, :, 32:64], in1=cosb, op=ALU.mult), r=PJ + [c64k, "tmp2"], w=["tmp2"])
                            TU = TT[:].rearrange("p b (u d) -> p (b u) d", u=2)
                            for (h0, h1, u0) in ((0, 5, 16), (5, 10, 22)):
                                op("dve", lambda e, h0=h0, h1=h1, u0=u0: e.tensor_tensor(out=TU[:, u0:u0 + 5, 0:32], in0=T3[:, h0:h1, 0:32], in1=T3b[:, h0:h1, 0:32], op=ALU.subtract),
                                   r=["tmp", "tmp2"], w=["TT"])
                                op("dve", lambda e, h0=h0, h1=h1, u0=u0: e.tensor_tensor(out=TU[:, u0:u0 + 5, 32:64], in0=T3[:, h0:h1, 32:64], in1=T3b[:, h0:h1, 32:64], op=ALU.add),
                                   r=["tmp", "tmp2"], w=["TT"])
                            op("pool", lambda e: e.tensor_copy(out=TU[:, 21, :], in_=TU[:, 20, :]), r=["TT"], w=["TT"])
                            op("pool", lambda e: e.tensor_copy(out=TU[:, 27, :], in_=TU[:, 26, :]), r=["TT"], w=["TT"])
                            Y32 = proj[:, P_Y32:P_Y32 + 544].rearrange("p (h d) -> p h d", h=17)
                            U3 = tmp[:, 0:544].rearrange("p (h d) -> p h d", h=17)
                            U3b = tmp2[:, 0:544].rearrange("p (h d) -> p h d", h=17)
                            op("act", lambda e: e.activation(out=U3[:], in_=Y32, func=AF.Square), r=PJ + ["TT"], w=["tmp"])
                            op("dve", lambda e: e.tensor_reduce(out=st8[:, 16:33], in_=U3[:], axis=AX.X, op=ALU.add), r=["tmp"], w=["st_b"])
                            rstd_of(st8[:, 16:33], 17, 32, "st_b")
                            op("dve", lambda e: e.tensor_tensor(out=Y32, in0=Y32, in1=bc(st8[:, 16:33], [128, 17, 32], 2), op=ALU.mult), r=PJ + ["st_b"], w=PJ)
                            op("dve", lambda e: e.tensor_tensor(out=Y32, in0=Y32, in1=g32t[:], op=ALU.mult), r=PJ + ["g32t"], w=PJ)
                            cosb = bc(C32[:, 0:16], [128, 17, 16], 1)
                            sinb = bc(C32[:, 16:32], [128, 17, 16], 1)
                            op("dve", lambda e: e.tensor_tensor(out=U3[:, :, 0:16], in0=Y32[:, :, 0:16], in1=cosb, op=ALU.mult), r=PJ + [c32k, "tmp"], w=["tmp"])
                            op("pool", lambda e: e.tensor_tensor(out=U3b[:, :, 0:16], in0=Y32[:, :, 16:32], in1=sinb, op=ALU.mult), r=PJ + [c32k, "TT"], w=["tmp2"])
                            op("dve", lambda e: e.tensor_tensor(out=U3[:, :, 16:32], in0=Y32[:, :, 0:16], in1=sinb, op=ALU.mult), r=PJ + [c32k, "tmp"], w=["tmp"])
                            op("pool", lambda e: e.tensor_tensor(out=U3b[:, :, 16:32], in0=Y32[:, :, 16:32], in1=cosb, op=ALU.mult), r=PJ + [c32k, "tmp2"], w=["tmp2"])
                            TQ = TT[:].rearrange("p b (u d) -> p (b u) d", u=4)
                            op("dve", lambda e: e.tensor_tensor(out=TQ[:, 56:88:2, 0:16], in0=U3[:, 0:16, 0:16], in1=U3b[:, 0:16, 0:16], op=ALU.subtract), r=["tmp", "tmp2"], w=["TT"])
                            op("dve", lambda e: e.tensor_tensor(out=TQ[:, 56:88:2, 16:32], in0=U3[:, 0:16, 16:32], in1=U3b[:, 0:16, 16:32], op=ALU.add), r=["tmp", "tmp2"], w=["TT"])
                            for hh in range(4):
                                u = (4 + hh) * 4 + 2
                                op("dve", lambda e, u=u: e.tensor_tensor(out=TQ[:, u, 0:16], in0=U3[:, 16, 0:16], in1=U3b[:, 16, 0:16], op=ALU.subtract), r=["tmp", "tmp2"], w=["TT"])
                                op("dve", lambda e, u=u: e.tensor_tensor(out=TQ[:, u, 16:32], in0=U3[:, 16, 16:32], in1=U3b[:, 16, 16:32], op=ALU.add), r=["tmp", "tmp2"], w=["TT"])
                            CQ = proj[:, P_CQ:P_CQ + 256]
                            CKV = proj[:, P_CKV:P_CKV + 128]
                            op("act", lambda e: e.activation(out=tmp[:, 0:256], in_=CQ, func=AF.Square, accum_out=st8[:, 40:41]), r=PJ + ["tmp", "TT"], w=["tmp", "st_c"])
                            op("act", lambda e: e.activation(out=tmp[:, 256:384], in_=CKV, func=AF.Square, accum_out=st8[:, 41:42]), r=PJ + ["tmp"], w=["tmp", "st_c"])
                            op("dve", lambda e: e.tensor_scalar(out=st8[:, 41:42], in0=st8[:, 41:42], scalar1=2.0, scalar2=None, op0=ALU.mult), r=["st_c"], w=["st_c"])
                            rstd_of(st8[:, 40:42], 2, 256, "st_c")
                            op("dve", lambda e: e.tensor_scalar(out=cqn[:, 0:256], in0=CQ, scalar1=st8[:, 40:41], scalar2=None, op0=ALU.mult), r=PJ + ["st_c"], w=["cqn"])
                            op("dve", lambda e: e.tensor_scalar(out=cqn[:, 256:384], in0=CKV, scalar1=st8[:, 41:42], scalar2=None, op0=ALU.mult), r=PJ + ["st_c", "cqn"], w=["cqn"])
                            for c in range(3):
                                op("pe", lambda e, c=c: e.transpose(pb[1][:, c * 128:(c + 1) * 128], cqn[:, c * 128:(c + 1) * 128], ident[:]), r=["cqn", "ident"], w=["pb1"])
                            op("act", lambda e: e.activation(out=cqT[:].rearrange("p c t -> p (c t)"), in_=pb[1][:, 0:384], func=AF.Copy), r=["pb1"], w=["cqT"])
                            for c in range(2):
                                op("pe", lambda e, c=c: e.matmul(pf[3][:, 0:384], lhsT=cqT[:, c, :], rhs=wuq_sb[:, c, :], start=(c == 0), stop=(c == 1)),
                                   r=["cqT", "wuq_sb"], w=["pf3"])
                            op("pe", lambda e: e.matmul(pf[4][:, 0:512], lhsT=cqT[:, 2, :], rhs=wukv_sb[:], start=True, stop=True), r=["cqT", "wukv_sb"], w=["pf4"])
                            op("act", lambda e: e.activation(out=QB[:].rearrange("p h d -> p (h d)"), in_=pf[3][:, 0:384], func=AF.Copy), r=["pf3"], w=["QB"])
                            op("dve", lambda e: e.tensor_copy(out=KV[:].rearrange("p h d -> p (h d)"), in_=pf[4][:, 0:512]), r=["pf4"], w=["KV"])
                            Q3 = tmp[:, 0:384].rearrange("p (h d) -> p h d", h=4)
                            K3 = tmp2[:, 0:256].rearrange("p (h d) -> p h d", h=4)
                            op("act", lambda e: e.activation(out=Q3, in_=QB[:], func=AF.Square), r=["QB", "tmp"], w=["tmp"])
                            op("act", lambda e: e.activation(out=K3, in_=KV[:, :, 0:64], func=AF.Square), r=["KV", "tmp2", "TT"], w=["tmp2"])
                            op("dve", lambda e: e.tensor_reduce(out=st8[:, 44:48], in_=Q3[:, :, 0:64], axis=AX.X, op=ALU.add), r=["tmp"], w=["st_d"])
                            op("dve", lambda e: e.tensor_reduce(out=st8[:, 48:52], in_=K3, axis=AX.X, op=ALU.add), r=["tmp2"], w=["st_d"])
                            op("dve", lambda e: e.tensor_reduce(out=st8[:, 52:56], in_=Q3[:, :, 64:96], axis=AX.X, op=ALU.add), r=["tmp"], w=["st_e"])
                            rstd_of(st8[:, 44:52], 8, 64, "st_d")
                            rstd_of(st8[:, 52:56], 4, 32, "st_e")
                            T128 = TT[:]
                            op("dve", lambda e: e.tensor_tensor(out=QB[:, :, 0:64], in0=QB[:, :, 0:64], in1=bc(st8[:, 44:48], [128, 4, 64], 2), op=ALU.mult), r=["QB", "st_d"], w=["QB"])
                            op("dve", lambda e: e.tensor_tensor(out=T128[:, 0:4, 0:64], in0=QB[:, :, 0:64], in1=bc(gq96t[:, 0:64], [128, 4, 64], 1), op=ALU.mult), r=["QB", "gq96t"], w=["TT"])
                            op("dve", lambda e: e.tensor_tensor(out=KV[:, :, 0:64], in0=KV[:, :, 0:64], in1=bc(st8[:, 48:52], [128, 4, 64], 2), op=ALU.mult), r=["KV", "st_d"], w=["KV"])
                            op("dve", lambda e: e.tensor_tensor(out=T128[:, 4:8, 0:64], in0=KV[:, :, 0:64], in1=bc(gk64t[:], [128, 4, 64], 1), op=ALU.mult), r=["KV", "gk64t"], w=["TT"])
                            op("pool", lambda e: e.tensor_copy(out=VS[:, 4:8, 0:64], in_=KV[:, :, 64:128]), r=["KV"], w=["VS"])
                            op("dve", lambda e: e.tensor_tensor(out=QB[:, :, 64:96], in0=QB[:, :, 64:96], in1=bc(st8[:, 52:56], [128, 4, 32], 2), op=ALU.mult), r=["QB", "st_e"], w=["QB"])
                            op("dve", lambda e: e.tensor_tensor(out=QB[:, :, 64:96], in0=QB[:, :, 64:96], in1=bc(gq96t[:, 64:96], [128, 4, 32], 1), op=ALU.mult), r=["QB", "gq96t"], w=["QB"])
                            cosb = bc(C32[:, 0:16], [128, 4, 16], 1)
                            sinb = bc(C32[:, 16:32], [128, 4, 16], 1)
                            R3 = tmp[:, 0:128].rearrange("p (h d) -> p h d", h=4)
                            R3b = tmp2[:, 0:128].rearrange("p (h d) -> p h d", h=4)
                            op("dve", lambda e: e.tensor_tensor(out=R3[:, :, 0:16], in0=QB[:, :, 64:80], in1=cosb, op=ALU.mult), r=["QB", c32k, "tmp"], w=["tmp"])
                            op("pool", lambda e: e.tensor_tensor(out=R3b[:, :, 0:16], in0=QB[:, :, 80:96], in1=sinb, op=ALU.mult), r=["QB", c32k, "tmp2"], w=["tmp2"])
                            op("dve", lambda e: e.tensor_tensor(out=R3[:, :, 16:32], in0=QB[:, :, 64:80], in1=sinb, op=ALU.mult), r=["QB", c32k, "tmp"], w=["tmp"])
                            op("pool", lambda e: e.tensor_tensor(out=R3b[:, :, 16:32], in0=QB[:, :, 80:96], in1=cosb, op=ALU.mult), r=["QB", c32k, "tmp2"], w=["tmp2"])
                            op("dve", lambda e: e.tensor_tensor(out=T128[:, 0:4, 64:80], in0=R3[:, :, 0:16], in1=R3b[:, :, 0:16], op=ALU.subtract), r=["tmp", "tmp2"], w=["TT"])
                            op("dve", lambda e: e.tensor_tensor(out=T128[:, 0:4, 80:96], in0=R3[:, :, 16:32], in1=R3b[:, :, 16:32], op=ALU.add), r=["tmp", "tmp2"], w=["TT"])
                            for g0, g1, bank in ((0, 8, 0), (8, 16, 1), (16, 22, 0)):
                                for b in range(g0, g1):
                                    op("pe", lambda e, b=b, g0=g0, bank=bank: e.transpose(pb[bank][:, (b - g0) * 128:(b - g0 + 1) * 128], TT[:, b, :], ident[:]),
                                       r=["TT", "ident"], w=["pb%d" % bank])
                                nb = g1 - g0
                                eng = "act" if bank == 0 else "dve"
                                if eng == "act":
                                    op("act", lambda e, g0=g0, g1=g1, nb=nb, bank=bank: e.activation(
                                        out=FTs[:, 4 + g0:4 + g1, sub * 128:(sub + 1) * 128], in_=pb[bank][:, 0:nb * 128].rearrange("p (b t) -> p b t", b=nb), func=AF.Copy),
                                       r=["pb%d" % bank], w=["FTs"])
                                else:
                                    op("dve", lambda e, g0=g0, g1=g1, nb=nb, bank=bank: e.tensor_copy(
                                        out=FTs[:, 4 + g0:4 + g1, sub * 128:(sub + 1) * 128], in_=pb[bank][:, 0:nb * 128].rearrange("p (b t) -> p b t", b=nb)),
                                       r=["pb%d" % bank], w=["FTs"])
                            dma(VA[t * 128:(t + 1) * 128, :, :], VS[:], r=["VS"], slot="VS")
                            dma(IW[t * 128:(t + 1) * 128, :], IWs[:], r=["IWs"], slot="IWs")
                        HT = ["hT%d" % i for i in range(4)]
                        for ct in range(12):
                            bank = 3 + (ct % 2)
                            for c in range(8):
                                op("pe", lambda e, c=c, ct=ct, bank=bank: e.matmul(pf[bank][:, :], lhsT=w_sb[:, c, ct * 128:(ct + 1) * 128], rhs=hT[:, c, :],
                                                                                  start=(c == 0), stop=(c == 7)), r=HT + ["w_sb"], w=["pf%d" % bank])
                            if ct < 4:
                                op("dve", lambda e, ct=ct, bank=bank: e.tensor_copy(out=FTs[:, ct, :], in_=pf[bank][:, :]), r=["pf%d" % bank], w=["FTs"])
                            else:
                                op("act", lambda e, ct=ct, bank=bank: e.activation(out=GS[:, ct - 4, :], in_=pf[bank][:, :], func=AF.Silu), r=["pf%d" % bank], w=["GS"])
                        q0 = stl * 512
                        for g in range(0, NB_FT, 7):
                            g1 = min(NB_FT, g + 7)
                            dma(FT[g:g1, :, q0:q0 + 512].rearrange("b p t -> p b t"), FTs[:, g:g1, :], r=["FTs"], slot="FTs")
                        for g in range(0, 8, 4):
                            dma(GT[g:g + 4, :, q0:q0 + 512].rearrange("b p t -> p b t"), GS[:, g:g + 4, :], r=["GS"], slot="GS")
                    sc.barrier()
                NQ = S // 512
                NIT = 16

                def load_V(st, name, h0, nh):
                    V = sb(st, name, [128, NT, nh, 128], BF16)
                    for g in range(0, NT, 8):
                        g1 = min(NT, g + 8)
                        dma(V[:, g:g1, :, :], VA[g * 128:g1 * 128, h0:h0 + nh, :].rearrange("(k p) h d -> p k h d", p=128), w=[name], slot=name)
                    return V

                def load_K(st, name, b0, nb):
                    Kt = sb(st, name, [128, nb, S], BF16)
                    for b in range(nb):
                        dma(Kt[:, b, :], FT[b0 + b, :, :], w=[name], slot=name)
                    return Kt

                def softmax_mixer(mname, kb0, nkb, qb0, nqb, vh0, nvh, gb0, heads, scale, diag_mask, selT_fn=None, post=None, pre_q=None):
                    with ExitStack() as ms:
                        Kt = load_K(ms, "Kt", kb0, nkb)
                        V = load_V(ms, "Vr", vh0, nvh)
                        Qs = [sb(ms, "Qs%d" % i, [128, nqb, 512], BF16) for i in range(2)]
                        Gs = [sb(ms, "Gs%d" % i, [128, 2, 512]) for i in range(1)]
                        PT = [sb(ms, "PT%d" % i, [128, 512], BF16) for i in range(3)]
                        RC = sb(ms, "RC", [128, 2 if post is not None else 1, 512])
                        OD = sb(ms, "OD", [128, 3 if post is not None else 1, 512])
                        YS = [sb(ms, "YS%d" % i, [128, 2, 512], BF16) for i in range(2)]
                        extra = pre_q(ms) if pre_q is not None else None
                        ctr = {"s": 0, "p": 0, "o": 0}

                        def load_q(j):
                            q0 = j * 512
                            dma(Qs[j % 2][:], FT[qb0:qb0 + nqb, :, q0:q0 + 512].rearrange("b p t -> p b t"), w=["Qs%d" % (j % 2)], slot="Qs%d" % (j % 2))

                        def load_g(j):
                            q0 = j * 512
                            dma(Gs[0][:], GT[gb0:gb0 + 2, :, q0:q0 + 512].rearrange("b p t -> p b t"), w=["Gs0"], slot="Gs0")

                        load_q(0)
                        for j in range(NQ):
                            if j + 1 < NQ:
                                load_q(j + 1)
                            load_g(j)
                            Qk, Gk = "Qs%d" % (j % 2), "Gs0"
                            Q, G = Qs[j % 2], Gs[0]
                            nkt = 4 * (j + 1)
                            selT = selT_fn(j, extra, Q, Qk) if selT_fn is not None else None
                            ysk = "YS%d" % (j % 2)
                            for hi, (maps, vh) in enumerate(heads):
                                obanks = []
                                for m in maps:
                                    obanks.append(2 + (ctr["o"] % 4))
                                    ctr["o"] += 1
                                seq = [(kt, mi) for kt in range(nkt) for mi in range(len(maps))]

                                def emit_qk(idx):
                                    kt, mi = seq[idx]
                                    kblk, qblk, base, dkk = maps[mi]
                                    sbank = ctr["s"] % 2
                                    ctr["s"] += 1
                                    op("pe", lambda e: e.matmul(pf[sbank][:, :], lhsT=Kt[base:base + dkk, kblk, kt * 128:(kt + 1) * 128],
                                                                rhs=Q[base:base + dkk, qblk, :], start=True, stop=True),
                                       r=["Kt", Qk], w=["pf%d" % sbank])
                                    return sbank

                                pend = emit_qk(0)
                                for idx in range(len(seq)):
                                    kt, mi = seq[idx]
                                    sbank = pend
                                    if idx + 1 < len(seq):
                                        pend = emit_qk(idx + 1)
                                    pi = ctr["p"] % 3
                                    ctr["p"] += 1
                                    P = PT[pi]
                                    pk = "PT%d" % pi
                                    op("act", lambda e: e.activation(out=P[:], in_=pf[sbank][:, :], func=AF.Exp, scale=scale), r=["pf%d" % sbank], w=[pk])
                                    r_ = kt - (nkt - 4)
                                    if diag_mask is not None and r_ >= 0:
                                        op("dve", lambda e: e.tensor_tensor(out=P[:], in0=P[:], in1=masks[:, diag_mask + r_, :], op=ALU.mult), r=[pk, "masks"], w=[pk])
                                    if selT is not None:
                                        eng = "dve" if (idx % 2 == 0) else "pool"
                                        op(eng, lambda e: e.tensor_tensor(out=P[:], in0=P[:], in1=selT[:, kt, :], op=ALU.mult), r=[pk, "selT"], w=[pk])
                                    ob = obanks[mi]
                                    op("pe", lambda e: e.matmul(pf[ob][:, :], lhsT=V[:, kt, vh, :], rhs=P[:], start=(kt == 0), stop=(kt == nkt - 1)),
                                       r=["Vr", pk], w=["pf%d" % ob])
                                blk, hb_ = hi // 2, (hi % 2) * 64
                                for mi, ob in enumerate(obanks):
                                    op("dve", lambda e, mi=mi, ob=ob: e.reciprocal(out=RC[64:128, mi, :], in_=pf[ob][64:128, :]), r=["pf%d" % ob], w=["RC"])
                                if post is None:
                                    ob = obanks[0]
                                    op("dve", lambda e: e.tensor_tensor(out=OD[hb_:hb_ + 64, 0, :], in0=pf[ob][0:64, :], in1=RC[64:128, 0, :], op=ALU.mult),
                                       r=["pf%d" % ob, "RC"], w=["OD"])
                                    op("dve", lambda e: e.tensor_tensor(out=YS[j % 2][hb_:hb_ + 64, blk, :], in0=OD[hb_:hb_ + 64, 0, :], in1=G[hb_:hb_ + 64, blk, :], op=ALU.mult),
                                       r=["OD", Gk], w=[ysk])
                                else:
                                    post(obanks, RC, OD, G, Gk, YS[j % 2], ysk, blk, hb_)
                            q0 = j * 512
                            dma(YT[gb0:gb0 + 2, :, q0:q0 + 512].rearrange("b p t -> p b t"), YS[j % 2][:], r=[ysk], slot=ysk)
                        sc.barrier()

                if "B" in MIX:
                    softmax_mixer("B", 8, 4, 4, 4, 4, 4, 2, [([(h, h, 0, 96)], h) for h in range(4)], 96.0 ** -0.5, 0)

                def post_D(obanks, RC, OD, G, Gk, YSt, ysk, blk, hb_):
                    a, b = obanks
                    op("dve", lambda e: e.tensor_tensor(out=OD[0:64, 0, :], in0=pf[a][0:64, :], in1=RC[64:128, 0, :], op=ALU.mult), r=["pf%d" % a, "RC"], w=["OD"])
                    op("dve", lambda e: e.tensor_tensor(out=OD[0:64, 1, :], in0=pf[b][0:64, :], in1=RC[64:128, 1, :], op=ALU.mult), r=["pf%d" % b, "RC", "OD"], w=["OD"])
                    op("dve", lambda e: e.scalar_tensor_tensor(out=OD[0:64, 0, :], in0=OD[0:64, 1, :], scalar=lam_bc[0:64, 0:1], in1=OD[0:64, 0, :], op0=ALU.mult, op1=ALU.add),
                       r=["OD", "lam_bc"], w=["OD"])
                    op("act", lambda e: e.activation(out=OD[0:64, 1, :], in_=OD[0:64, 0, :], func=AF.Square), r=["OD"], w=["OD"])
                    op("pe", lambda e: e.matmul(pf[a][0:64, :], lhsT=ones_f[0:64, 0:64], rhs=OD[0:64, 1, :], start=True, stop=True), r=["OD", "ones_f"], w=["pf%d" % a])
                    op("act", lambda e: e.activation(out=OD[0:64, 1, :], in_=pf[a][0:64, :], func=AF.Ln, scale=1.0 / 64, bias=epst[0:64, 0:1]), r=["pf%d" % a, "OD", "epst"], w=["OD"])
                    op("act", lambda e: e.activation(out=OD[0:64, 1, :], in_=OD[0:64, 1, :], func=AF.Exp, scale=-0.5), r=["OD"], w=["OD"])
                    op("dve", lambda e: e.scalar_tensor_tensor(out=OD[hb_:hb_ + 64, 2, :], in0=OD[0:64, 0, :], scalar=sublnt[0:64, 0:1], in1=OD[0:64, 1, :], op0=ALU.mult, op1=ALU.mult),
                       r=["OD", "sublnt"], w=["OD"])
                    op("dve", lambda e: e.tensor_tensor(out=YSt[hb_:hb_ + 64, blk, :], in0=OD[hb_:hb_ + 64, 2, :], in1=G[hb_:hb_ + 64, blk, :], op=ALU.mult), r=["OD", Gk], w=[ysk])

                if "D" in MIX:
                    softmax_mixer("D", 22, 4, 18, 4, 9, 4, 6,
                                  [([(h, h, c * 64, 32) for c in range(2)], h) for h in range(4)], 32.0 ** -0.5, 0, post=post_D)

                def pre_C(ms):
                    ex = {}
                    ex["IKc"] = [sb(ms, "IKc%d" % i, [128, 512], BF16) for i in range(3)]
                    ex["IQ"] = [sb(ms, "IQ%d" % i, [128, 2, 512], BF16) for i in range(1)]
                    ex["IWt"] = [sb(ms, "IWt%d" % i, [128, 4, 4]) for i in range(2)]
                    ex["SC"] = sb(ms, "SC", [128, S])
                    ex["JK"] = sb(ms, "JK", [128, S], BF16)
                    ex["BD"] = sb(ms, "BD", [128, S], BF16)
                    ex["selT"] = sb(ms, "selT", [128, NT, 512], BF16)
                    ex["RL"] = [sb(ms, "RL%d" % i, [128, 512]) for i in range(2)]
                    ex["bs"] = sb(ms, "bs", [128, 64])
                    return ex

                def selT_C(j, ex, Q, Qk):
                    q0 = j * 512
                    IQ, IWt = ex["IQ"][0], ex["IWt"][j % 2]
                    iqk, iwk = "IQ0", "IWt%d" % (j % 2)
                    SC, JK, selT, RL, bs, IKc = ex["SC"], ex["JK"], ex["selT"], ex["RL"], ex["bs"], ex["IKc"]
                    BD = ex["BD"]
                    dma(IQ[:], FT[15:17, :, q0:q0 + 512].rearrange("b p t -> p b t"), w=[iqk], slot=iqk)
                    dma(IWt[:], IW[q0:q0 + 512, :].rearrange("(a p) h -> p a h", p=128), w=[iwk], slot=iwk)
                    op("pool", lambda e: e.memset(selT[:, 4 * j:4 * j + 4, :], 0.0), w=["selT"])
                    rl = 0
                    chunks = [(qi, c0) for qi in range(4) for c0 in range(0, (4 * j + qi + 1) * 128, 512)]

                    def load_ik(ci):
                        qi_, c0_ = chunks[ci]
                        n_ = min(512, (4 * j + qi_ + 1) * 128 - c0_)
                        kk_ = "IKc%d" % (ci % 3)
                        dma(IKc[ci % 3][:, 0:n_], FT[17, :, c0_:c0_ + n_], w=[kk_], slot=kk_)

                    load_ik(0)
                    ci = 0
                    for qi in range(4):
                        i = 4 * j + qi
                        Lk = (i + 1) * 128
                        for c0 in range(0, Lk, 512):
                            n = min(512, Lk - c0)
                            if ci + 1 < len(chunks):
                                load_ik(ci + 1)
                            IKb, ikk = IKc[ci % 3], "IKc%d" % (ci % 3)
                            ci += 1
                            for h in range(4):
                                blk, base = h // 2, (h % 2) * 64
                                bank = h % 2
                                op("pe", lambda e: e.matmul(pf[bank][:, 0:n], lhsT=IQ[base:base + 64, blk, qi * 128:(qi + 1) * 128], rhs=IKb[base:base + 64, 0:n],
                                                            start=True, stop=True), r=[iqk, ikk], w=["pf%d" % bank])
                                R = RL[rl % 2]
                                rk = "RL%d" % (rl % 2)
                                rl += 1
                                op("act", lambda e: e.activation(out=R[:, 0:n], in_=pf[bank][:, 0:n], func=AF.Relu), r=["pf%d" % bank], w=[rk])
                                if h == 0:
                                    op("dve", lambda e: e.tensor_scalar(out=SC[:, c0:c0 + n], in0=R[:, 0:n], scalar1=IWt[:, qi, 0:1], scalar2=None, op0=ALU.mult),
                                       r=[rk, iwk], w=["SC"])
                                else:
                                    op("dve", lambda e: e.scalar_tensor_tensor(out=SC[:, c0:c0 + n], in0=R[:, 0:n], scalar=IWt[:, qi, h:h + 1], in1=SC[:, c0:c0 + n],
                                                                                op0=ALU.mult, op1=ALU.add), r=[rk, iwk, "SC"], w=["SC"])
                        op("dve", lambda e: e.tensor_tensor(out=SC[:, Lk - 128:Lk], in0=SC[:, Lk - 128:Lk], in1=adm[:, 0, :], op=ALU.mult), r=["SC", "adm"], w=["SC"])
                        op("dve", lambda e: e.tensor_tensor(out=SC[:, Lk - 128:Lk], in0=SC[:, Lk - 128:Lk], in1=adm[:, 1, :], op=ALU.add), r=["SC", "adm"], w=["SC"])
                        if Lk <= TOPK:
                            op("dve", lambda e: e.tensor_scalar(out=JK[:, 0:Lk], in0=SC[:, 0:Lk], scalar1=-1.0e29, scalar2=None, op0=ALU.is_ge), r=["SC"], w=["JK"])
                        else:
                            op("dve", lambda e: e.max(out=bs[:, 0:8], in_=SC[:, 0:Lk]), r=["SC"], w=["bs_hi"])
                            op("dve", lambda e: e.tensor_reduce(out=bs[:, 8:9], in_=SC[:, 0:Lk - 128], axis=AX.X, op=ALU.min), r=["SC"], w=["bs_lo"])
                            op("dve", lambda e: e.tensor_tensor(out=bs[:, 9:10], in0=bs[:, 0:1], in1=bs[:, 8:9], op=ALU.subtract), r=["bs_hi", "bs_lo"], w=["bs_d"])
                            op("dve", lambda e: e.tensor_scalar(out=bs[:, 9:10], in0=bs[:, 9:10], scalar1=1.001, scalar2=1.0e-20, op0=ALU.mult, op1=ALU.add), r=["bs_d"], w=["bs_d"])
                            op("dve", lambda e: e.tensor_scalar(out=bs[:, 32:32 + NIT], in0=pow2[:, 0:NIT], scalar1=bs[:, 9:10], scalar2=None, op0=ALU.mult), r=["bs_d", "pow2"], w=["bs_dl"])
                            for k in range(NIT):
                                op("dve", lambda e, k=k: e.tensor_tensor(out=bs[:, 10:11], in0=bs[:, 8:9], in1=bs[:, 32 + k:33 + k], op=ALU.add), r=["bs_lo", "bs_dl"], w=["bs_mid"])
                                op("dve", lambda e: e.tensor_scalar(out=JK[:, 0:Lk], in0=SC[:, 0:Lk], scalar1=bs[:, 10:11], scalar2=0.0, op0=ALU.is_ge, op1=ALU.add,
                                                                    accum_out=bs[:, 11:12]), r=["SC", "bs_mid"], w=["JK", "bs_cnt"])
                                op("dve", lambda e, k=k: e.tensor_scalar(out=bs[:, 12:13], in0=bs[:, 11:12], scalar1=TOPK - 0.5, scalar2=bs[:, 32 + k:33 + k], op0=ALU.is_ge, op1=ALU.mult),
                                   r=["bs_cnt", "bs_dl"], w=["bs_t"])
                                op("dve", lambda e: e.tensor_tensor(out=bs[:, 8:9], in0=bs[:, 8:9], in1=bs[:, 12:13], op=ALU.add), r=["bs_lo", "bs_t"], w=["bs_lo"])
                            op("dve", lambda e: e.tensor_tensor(out=bs[:, 13:14], in0=bs[:, 8:9], in1=bs[:, 31 + NIT:32 + NIT], op=ALU.add), r=["bs_lo", "bs_dl"], w=["bs_hf"])
                            op("dve", lambda e: e.tensor_scalar(out=JK[:, 0:Lk], in0=SC[:, 0:Lk], scalar1=bs[:, 8:9], scalar2=0.0, op0=ALU.is_ge, op1=ALU.add,
                                                                accum_out=bs[:, 14:15]), r=["SC", "bs_lo"], w=["JK", "bs_cl"])
                            op("dve", lambda e: e.scalar_tensor_tensor(out=BD[:, 0:Lk], in0=SC[:, 0:Lk], scalar=bs[:, 13:14], in1=JK[:, 0:Lk], op0=ALU.is_lt, op1=ALU.mult,
                                                                        accum_out=bs[:, 15:16]), r=["SC", "JK", "bs_hf"], w=["BD", "bs_nb"])
                            op("dve", lambda e: e.tensor_tensor(out=bs[:, 16:17], in0=bs[:, 15:16], in1=bs[:, 14:15], op=ALU.subtract), r=["bs_nb", "bs_cl"], w=["bs_m"])
                            op("dve", lambda e: e.tensor_scalar(out=bs[:, 16:17], in0=bs[:, 16:17], scalar1=float(TOPK) + 0.5, scalar2=None, op0=ALU.add), r=["bs_m"], w=["bs_m"])
                            op("dve", lambda e: e.tensor_tensor_scan(out=SC[:, 0:Lk], data0=ones_bf[:, 0:1].to_broadcast([128, Lk]), data1=BD[:, 0:Lk], initial=0.0, op0=ALU.mult, op1=ALU.add),
                               r=["BD", "ones_bf", "SC"], w=["SC"])
                            op("dve", lambda e: e.scalar_tensor_tensor(out=BD[:, 0:Lk], in0=SC[:, 0:Lk], scalar=bs[:, 16:17], in1=BD[:, 0:Lk], op0=ALU.is_gt, op1=ALU.mult),
                               r=["SC", "BD", "bs_m"], w=["BD"])
                            op("pool", lambda e: e.tensor_tensor(out=JK[:, 0:Lk], in0=JK[:, 0:Lk], in1=BD[:, 0:Lk], op=ALU.subtract), r=["JK", "BD"], w=["JK"])
                        if debug:
                            dma(DSC[i, :, :], SC[:, :], r=["SC"], slot="dbg1")
                            dma(DBS[i, :, :], bs[:, :], r=["bs_lo", "bs_hi", "bs_d", "bs_dl", "bs_mid", "bs_cnt", "bs_t", "bs_m", "bs_cl", "bs_nb", "bs_hf"], slot="dbg2")
                        for g in range(0, i + 1, 8):
                            g1 = min(i + 1, g + 8)
                            bank = (g // 8) % 2
                            for kb in range(g, g1):
                                op("pe", lambda e, kb=kb: e.transpose(pb[bank][:, (kb - g) * 128:(kb - g + 1) * 128], JK[:, kb * 128:(kb + 1) * 128], ident[:]),
                                   r=["JK", "ident"], w=["pb%d" % bank])
                            nb_ = g1 - g
                            if bank == 0:
                                op("act", lambda e: e.activation(out=selT[:, g:g1, qi * 128:(qi + 1) * 128], in_=pb[bank][:, 0:nb_ * 128].rearrange("p (b t) -> p b t", b=nb_), func=AF.Copy),
                                   r=["pb%d" % bank], w=["selT"])
                            else:
                                op("pool" if False else "dve", lambda e: e.tensor_copy(out=selT[:, g:g1, qi * 128:(qi + 1) * 128], in_=pb[bank][:, 0:nb_ * 128].rearrange("p (b t) -> p b t", b=nb_)),
                                   r=["pb%d" % bank], w=["selT"])
                    if debug:
                        dma(DSEL[j, :, :, :], selT[:, :, :], r=["selT"], slot="dbg3")
                    return selT

                if "C" in MIX:
                    softmax_mixer("C", 14, 1, 12, 2, 8, 1, 4, [([(0, h // 2, (h % 2) * 64, 64)], 0) for h in range(4)], 0.125, None, selT_fn=selT_C, pre_q=pre_C)

                if "A" in MIX:
                    with ExitStack() as ms:
                        Kt = load_K(ms, "Kt", 2, 2)
                        V = load_V(ms, "Vr", 0, 4)
                        nK = sb(ms, "nK", [128, 2, S], BF16)
                        for b in range(2):
                            op("dve" if b == 0 else "pool", lambda e, b=b: e.tensor_scalar(out=nK[:, b, :], in0=Kt[:, b, :], scalar1=-0.125, scalar2=0.0, op0=ALU.mult, op1=ALU.add), r=["Kt"], w=["nK"])
                        Qs = [sb(ms, "Qs%d" % i, [128, 2, 512], BF16) for i in range(2)]
                        Gs = [sb(ms, "Gs%d" % i, [128, 2, 512]) for i in range(2)]
                        Et = [sb(ms, "Et%d" % i, [128, 512]) for i in range(2)]
                        SPt = [sb(ms, "SPt%d" % i, [128, 512]) for i in range(2)]
                        SPh = [sb(ms, "SPh%d" % i, [128, 512], BF16) for i in range(2)]
                        SPl = [sb(ms, "SPl%d" % i, [128, 512], BF16) for i in range(2)]
                        Cf = sb(ms, "Cf", [128, 512])
                        Ch = [sb(ms, "Ch%d" % i, [128, 512], BF16) for i in range(2)]
                        Cl = [sb(ms, "Cl%d" % i, [128, 512], BF16) for i in range(2)]
                        PT = [sb(ms, "PT%d" % i, [128, 512], BF16) for i in range(2)]
                        YS = [sb(ms, "YS%d" % i, [128, 2, 512], BF16) for i in range(2)]
                        cn = 0

                        def load_qA(j):
                            q0 = j * 512
                            dma(Qs[j % 2][:], FT[0:2, :, q0:q0 + 512].rearrange("b p t -> p b t"), w=["Qs%d" % (j % 2)], slot="Qs%d" % (j % 2))
                            dma(Gs[j % 2][:], GT[0:2, :, q0:q0 + 512].rearrange("b p t -> p b t"), w=["Gs%d" % (j % 2)], slot="Gs%d" % (j % 2))

                        load_qA(0)
                        for j in range(NQ):
                            if j + 1 < NQ:
                                load_qA(j + 1)
                            Qk, Gk = "Qs%d" % (j % 2), "Gs%d" % (j % 2)
                            Q, G = Qs[j % 2], Gs[j % 2]
                            nkt = 4 * (j + 1)
                            ysk = "YS%d" % (j % 2)
                            for h in range(4):
                                blk, base = h // 2, (h % 2) * 64
                                ob = 4 + (h % 2)
                                first = True
                                for kt in range(nkt - 1, -1, -1):
                                    u = cn % 2
                                    cn += 1
                                    zb, rb = u, 2 + u
                                    r_ = kt - (nkt - 4)
                                    ksl = slice(kt * 128, (kt + 1) * 128)
                                    op("pe", lambda e: e.matmul(pf[zb][:, :], lhsT=Kt[base:base + 64, blk, ksl], rhs=Q[base:base + 64, blk, :], start=True, stop=True),
                                       r=["Kt", Qk], w=["pf%d" % zb])
                                    op("act", lambda e: e.activation(out=Et[u][:], in_=pf[zb][:, :], func=AF.Exp, scale=0.125), r=["pf%d" % zb], w=["Et%d" % u])
                                    op("act", lambda e: e.activation(out=SPt[u][:], in_=Et[u][:], func=AF.Ln, bias=1.0, scale=1.0), r=["Et%d" % u], w=["SPt%d" % u])
                                    if r_ >= 0:
                                        op("dve", lambda e: e.tensor_tensor(out=SPt[u][:], in0=SPt[u][:], in1=masks[:, 4 + r_, :], op=ALU.mult), r=["SPt%d" % u, "masks"], w=["SPt%d" % u])
                                    op("dve", lambda e: e.tensor_copy(out=SPh[u][:], in_=SPt[u][:]), r=["SPt%d" % u], w=["SPh%d" % u])
                                    op("dve", lambda e: e.tensor_tensor(out=SPl[u][:], in0=SPt[u][:], in1=SPh[u][:], op=ALU.subtract), r=["SPt%d" % u, "SPh%d" % u], w=["SPl%d" % u])
                                    op("pe", lambda e: e.matmul(pf[rb][:, :], lhsT=tri[:], rhs=SPh[u][:], start=True, stop=False), r=["tri", "SPh%d" % u], w=["pf%d" % rb])
                                    op("pe", lambda e: e.matmul(pf[rb][:, :], lhsT=tri[:], rhs=SPl[u][:], start=False, stop=False), r=["tri", "SPl%d" % u], w=["pf%d" % rb])
                                    if not first:
                                        cu = (cn) % 2
                                        op("pe", lambda e: e.matmul(pf[rb][:, :], lhsT=ones_bf[:], rhs=Ch[cu][:], start=False, stop=False), r=["ones_bf", "Ch%d" % cu], w=["pf%d" % rb])
                                        op("pe", lambda e: e.matmul(pf[rb][:, :], lhsT=ones_bf[:], rhs=Cl[cu][:], start=False, stop=False), r=["ones_bf", "Cl%d" % cu], w=["pf%d" % rb])
                                    op("pe", lambda e: e.matmul(pf[rb][:, :], lhsT=nK[base:base + 64, blk, ksl], rhs=Q[base:base + 64, blk, :], start=False, stop=True),
                                       r=["nK", Qk], w=["pf%d" % rb])
                                    if kt > 0:
                                        cw = (cn + 1) % 2
                                        if first:
                                            op("pool", lambda e: e.tensor_copy(out=Cf[:], in_=SPt[u][:]), r=["SPt%d" % u], w=["Cf"])
                                        else:
                                            op("pool", lambda e: e.tensor_tensor(out=Cf[:], in0=Cf[:], in1=SPt[u][:], op=ALU.add), r=["SPt%d" % u, "Cf"], w=["Cf"])
                                        op("pool", lambda e: e.tensor_copy(out=Ch[cw][:], in_=Cf[:]), r=["Cf"], w=["Ch%d" % cw])
                                        op("pool", lambda e: e.tensor_tensor(out=Cl[cw][:], in0=Cf[:], in1=Ch[cw][:], op=ALU.subtract), r=["Cf", "Ch%d" % cw], w=["Cl%d" % cw])
                                    op("act", lambda e: e.activation(out=PT[u][:], in_=pf[rb][:, :], func=AF.Exp, scale=-1.0), r=["pf%d" % rb], w=["PT%d" % u])
                                    if r_ >= 0:
                                        op("dve", lambda e: e.tensor_tensor(out=PT[u][:], in0=PT[u][:], in1=masks[:, 4 + r_, :], op=ALU.mult), r=["PT%d" % u, "masks"], w=["PT%d" % u])
                                    op("pe", lambda e: e.matmul(pf[ob][0:64, :], lhsT=V[:, kt, h, 0:64], rhs=PT[u][:], start=first, stop=(kt == 0)), r=["Vr", "PT%d" % u], w=["pf%d" % ob])
                                    first = False
                                op("dve", lambda e: e.tensor_tensor(out=YS[j % 2][base:base + 64, blk, :], in0=pf[ob][0:64, :], in1=G[base:base + 64, blk, :], op=ALU.mult),
                                   r=["pf%d" % ob, Gk], w=[ysk])
                            q0 = j * 512
                            dma(YT[0:2, :, q0:q0 + 512].rearrange("b p t -> p b t"), YS[j % 2][:], r=[ysk], slot=ysk)
                        sc.barrier()

                if DO_OUT:
                    with ExitStack() as ms:
                        wo = sb(ms, "wo", [128, 8, D_MODEL], BF16)
                        wos = [sb(ms, "wos%d" % i, [128, D_MODEL]) for i in range(2)]
                        for c in range(8):
                            k = "wos%d" % (c % 2)
                            dma(wos[c % 2][:], w_out[l, c * 128:(c + 1) * 128, :], w=[k], slot=k)
                            op("dve" if c % 2 == 0 else "pool", lambda e, c=c: e.tensor_copy(out=wo[:, c, :], in_=wos[c % 2][:]), r=[k], w=["wo"])
                        YTs = [sb(ms, "YTs%d" % i, [128, 8, 512], BF16) for i in range(2)]
                        xs = [sb(ms, "xs%d" % i, [128, D_MODEL]) for i in range(2)]
                        xo = [sb(ms, "xo%d" % i, [128, D_MODEL]) for i in range(2)]

                        def load_y(stl):
                            q0 = stl * 512
                            for g in range(0, 8, 4):
                                dma(YTs[stl % 2][:, g:g + 4, :], YT[g:g + 4, :, q0:q0 + 512].rearrange("b p t -> p b t"), w=["YTs%d" % (stl % 2)], slot="YTs%d" % (stl % 2))

                        load_y(0)
                        for stl in range(NST):
                            if stl + 1 < NST:
                                load_y(stl + 1)
                            for sub in range(4):
                                t = stl * 4 + sub
                                xk, ok = "xs%d" % (t % 2), "xo%d" % (t % 2)
                                dma(xs[t % 2][:], x_src[t * 128:(t + 1) * 128, :], w=[xk], slot=xk)
                                for half in range(2):
                                    bank = (t * 2 + half) % 2
                                    for c in range(8):
                                        op("pe", lambda e, c=c: e.matmul(pf[bank][:, :], lhsT=YTs[stl % 2][:, c, sub * 128:(sub + 1) * 128], rhs=wo[:, c, half * 512:(half + 1) * 512],
                                                                         start=(c == 0), stop=(c == 7)), r=["YTs%d" % (stl % 2), "wo"], w=["pf%d" % bank])
                                    op("dve", lambda e: e.tensor_tensor(out=xo[t % 2][:, half * 512:(half + 1) * 512], in0=pf[bank][:, :], in1=xs[t % 2][:, half * 512:(half + 1) * 512], op=ALU.add),
                                       r=["pf%d" % bank, xk], w=[ok])
                                dma(x_dst[t * 128:(t + 1) * 128, :], xo[t % 2][:], r=[ok], slot=ok)
                        sc.barrier()
        sc.barrier()
        print("instructions:", sc.n_ins)
    return nc


def host_consts(S):
    bf = ml_dtypes.bfloat16
    c = {}
    c["c_ident"] = np.eye(128, dtype=np.float32).astype(bf)

    def cs(d):
        inv = (10000.0 ** (-np.arange(0, d, 2, dtype=np.float32) / np.float32(d))).astype(np.float32)
        ang = (np.arange(S, dtype=np.float32)[:, None] * inv[None, :]).astype(np.float32)
        return np.concatenate([np.cos(ang), np.sin(ang)], axis=1).astype(np.float32)

    c["c_cs64"] = cs(64)
    c["c_cs32"] = cs(32)
    kk = np.arange(128)[:, None]
    qq = np.arange(512)[None, :]
    m = []
    for r in range(4):
        m.append(((r * 128 + kk) < ((qq // 64) + 1) * 64))
    for r in range(4):
        m.append(((r * 128 + kk) < qq))
    c["c_mask"] = np.concatenate(m, axis=1).astype(np.float32).astype(bf)
    q1 = np.arange(128)[:, None]
    k1 = np.arange(128)[None, :]
    m01 = (k1 < ((q1 // 64) + 1) * 64).astype(np.float32)
    c["c_adm"] = np.concatenate([m01, (1.0 - m01) * NEGBIG], axis=1).astype(np.float32)
    c["c_tri"] = (np.arange(128)[:, None] >= np.arange(128)[None, :]).astype(np.float32).astype(bf)
    c["c_pow2"] = np.tile((2.0 ** -(np.arange(32) + 1.0))[None, :], (128, 1)).astype(np.float32)
    return c


def host_params(inp, DEPTH):
    f = lambda a: np.ascontiguousarray(np.asarray(a, dtype=np.float32)[:DEPTH])
    p = {}
    p["w_in"] = np.ascontiguousarray(f(inp["w_in"])[:, :, PERM])
    p["w_out"] = f(inp["w_out"])
    p["w_uq"] = f(inp["mla_w_uq"])
    p["w_ukv"] = f(inp["mla_w_ukv"])
    p["ln_g"] = np.ascontiguousarray(f(inp["ln_g"]).reshape(-1, 8, 128).transpose(0, 2, 1))
    p["qn_g"] = np.ascontiguousarray(f(inp["mla_q_norm_g"]).reshape(-1, 2, 128).transpose(0, 2, 1))
    p["kvn_g"] = np.ascontiguousarray(f(inp["mla_kv_norm_g"]).reshape(-1, 128, 1))
    dq, dkk = f(inp["dsa_q_g"]), f(inp["dsa_k_g"])
    p["g64"] = np.ascontiguousarray(np.concatenate([dq] * 4 + [dkk], axis=1))
    fq, fk, mk = f(inp["diff_q_g"]), f(inp["diff_k_g"]), f(inp["mla_k_g"])
    p["g32"] = np.ascontiguousarray(np.concatenate([fq] * 8 + [fk] * 8 + [mk[:, 64:96]], axis=1))
    p["gq96"] = f(inp["mla_q_g"])
    p["gk64"] = np.ascontiguousarray(mk[:, :64])
    p["subln"] = np.ascontiguousarray(f(inp["diff_subln_g"]).reshape(-1, 64, 1))
    p["lvec"] = np.ascontiguousarray(np.concatenate([f(inp["diff_lq1"]), f(inp["diff_lk1"]), f(inp["diff_lq2"]), f(inp["diff_lk2"])], axis=1))
    return p


_CACHE = {}


def run(inputs, S, DEPTH, debug=False):
    import math
    key = (S, DEPTH, debug)
    lam_inits = [0.8 - 0.6 * math.exp(-0.3 * l) for l in range(DEPTH)]
    if key not in _CACHE:
        _CACHE[key] = build(S, DEPTH, debug, lam_inits, MIX=globals().get("MIXSEL", "ABCD"))
    nc = _CACHE[key]
    x = np.asarray(inputs["x"], dtype=np.float32)
    B = x.shape[0]
    shared = {}
    shared.update(host_consts(S))
    shared.update(host_params(inputs, DEPTH))
    in_maps = []
    for b in range(B):
        m = dict(shared)
        m["x"] = np.ascontiguousarray(x[b, :S])
        in_maps.append(m)
    res = run_bass_kernel_spmd(nc, in_maps, core_ids=list(range(B)))
    return res


def kernel(**inputs):
    res = run(inputs, 8192, 4)
    return np.stack([np.asarray(r["y"], dtype=np.float32) for r in res.results], axis=0)
```

```python
import numpy as np
import ml_dtypes
from contextlib import ExitStack
import concourse.bass as bass
import concourse.mybir as mybir
from concourse.bass_utils import run_bass_kernel_spmd

F32 = mybir.dt.float32
BF16 = mybir.dt.bfloat16
AF = mybir.ActivationFunctionType
ALU = mybir.AluOpType
AX = mybir.AxisListType

D_MODEL = 1024
IN_COLS = 3684
EPS = 1e-6
NEGBIG = -1.0e30

O_AQ, O_AK, O_AV, O_AG = 0, 256, 512, 768
O_BCQ, O_BCKV, O_BKR, O_BG = 1024, 1280, 1408, 1440
O_CQ, O_CK, O_CV, O_CG, O_CIQ, O_CIK, O_CIW = 1696, 1952, 2016, 2080, 2336, 2592, 2656
O_DQ, O_DK, O_DV, O_DG = 2660, 2916, 3172, 3428


def _perm():
    r = lambda a, n: list(range(a, a + n))
    p = []
    p += r(O_AQ, 256) + r(O_AK, 256)
    p += r(O_AG, 256) + r(O_BG, 256) + r(O_CG, 256) + r(O_DG, 256)
    p += r(O_AV, 256) + r(O_DV, 256) + r(O_CV, 64)
    p += r(O_CQ, 256) + r(O_CK, 64) + r(O_CIQ, 256) + r(O_CIK, 64)
    p += r(O_DQ, 256) + r(O_DK, 256) + r(O_BKR, 32)
    p += r(O_BCQ, 256) + r(O_BCKV, 128)
    p += r(O_CIW, 4)
    assert len(p) == IN_COLS and sorted(p) == list(range(IN_COLS))
    return np.array(p)


PERM = _perm()
T0 = 1536
NTM = IN_COLS - T0
P_AV, P_DV, P_CV = 0, 256, 512
P_X64 = 576
P_Y32 = 1216
P_CQ = 1760
P_CKV = 2016
P_IW = 2144
NB_FT = 26


class Sched:
    ENG = ("pe", "act", "dve", "pool", "sp")

    def __init__(self, nc, es):
        self.nc = nc
        self.es = es
        self.e = dict(pe=nc.tensor, act=nc.scalar, dve=nc.vector, pool=nc.gpsimd, sp=nc.sync)
        self.semobj = {}
        self.cnt = {}
        for k in ("pe", "act", "dve", "pool"):
            self.semobj[k] = es.enter_context(nc.semaphore("s_" + k))
            self.cnt[k] = 0
        self.known = {k: {} for k in self.ENG}
        self.lastw = {}
        self.reads = {}
        self.n_ins = 0

    def _dsem(self, slot):
        name = "d_" + slot
        if name not in self.semobj:
            self.semobj[name] = self.es.enter_context(self.nc.semaphore(name))
            self.cnt[name] = 0
        return name

    def _waits(self, E, r, w, attach_ok=False):
        need = {}

        def add(tok, kind):
            sname, val, prod = tok
            if prod == E:
                if E == "pe" or kind == "war":
                    return
            if self.known[E].get(sname, 0) >= val:
                return
            if need.get(sname, 0) < val:
                need[sname] = val

        for k in r:
            t = self.lastw.get(k)
            if t is not None:
                add(t, "raw")
        for k in w:
            t = self.lastw.get(k)
            if t is not None:
                add(t, "waw")
            for t in self.reads.get(k, {}).values():
                add(t, "war")
        items = list(need.items())
        attach = None
        if attach_ok and items:
            attach = items.pop()
        for sname, val in items:
            self.e[E].wait_ge(self.semobj[sname], val)
            self.known[E][sname] = val
            self.n_ins += 1
        if attach is not None:
            self.known[E][attach[0]] = attach[1]
        return attach

    def _record(self, tok, r, w):
        for k in r:
            d = self.reads.setdefault(k, {})
            d[tok[0]] = tok
        for k in w:
            self.lastw[k] = tok
            self.reads[k] = {}

    def op(self, E, fn, r=(), w=()):
        att = self._waits(E, r, w, attach_ok=True)
        ins = fn(self.e[E])
        if att is not None:
            ins._wait_ge(self.semobj[att[0]], att[1])
        self.cnt[E] += 1
        ins.then_inc(self.semobj[E], 1)
        tok = (E, self.cnt[E], E)
        self._record(tok, r, w)
        self.n_ins += 1
        return tok

    def dma(self, out, in_, r=(), w=(), slot=None, Q="sp"):
        self._waits(Q, r, w)
        sname = self._dsem(slot)
        ins = self.e[Q].dma_start(out=out, in_=in_)
        self.cnt[sname] += 16
        ins.then_inc(self.semobj[sname], 16)
        tok = (sname, self.cnt[sname], "dma")
        self._record(tok, r, w)
        self.n_ins += 1
        return tok

    def barrier(self, engines=None):
        for E in (engines or self.ENG):
            for sname, c in self.cnt.items():
                if c == 0 or self.known[E].get(sname, 0) >= c:
                    continue
                if sname == E and E == "pe":
                    continue
                self.e[E].wait_ge(self.semobj[sname], c)
                self.known[E][sname] = c
                self.n_ins += 1
        if engines is None:
            self.lastw = {}
            self.reads = {}


def bc(ap, shape, axis):
    return ap.unsqueeze(axis).to_broadcast(list(shape))


def build(S, DEPTH, debug=False, lam_inits=None, MIX="ABCD", DO_OUT=True):
    TOPK = min(256, S // 4)
    NT = S // 128
    NST = S // 512
    nc = bass.Bass("TRN2", target_bir_lowering=False)

    def din(name, shape, dt=F32):
        return nc.dram_tensor(name, list(shape), dt, kind="ExternalInput").ap()

    x_in = din("x", [S, D_MODEL])
    w_in = din("w_in", [DEPTH, D_MODEL, IN_COLS])
    w_out = din("w_out", [DEPTH, D_MODEL, D_MODEL])
    w_uq = din("w_uq", [DEPTH, 256, 384])
    w_ukv = din("w_ukv", [DEPTH, 128, 512])
    ln_g = din("ln_g", [DEPTH, 128, 8])
    qn_g = din("qn_g", [DEPTH, 128, 2])
    kvn_g = din("kvn_g", [DEPTH, 128, 1])
    g64 = din("g64", [DEPTH, 5 * 64])
    g32 = din("g32", [DEPTH, 17 * 32])
    gq96 = din("gq96", [DEPTH, 96])
    gk64 = din("gk64", [DEPTH, 64])
    subln = din("subln", [DEPTH, 64, 1])
    lvec = din("lvec", [DEPTH, 4 * 32])
    c_ident = din("c_ident", [128, 128], BF16)
    c_cs64 = din("c_cs64", [S, 64])
    c_cs32 = din("c_cs32", [S, 32])
    c_mask = din("c_mask", [128, 8 * 512], BF16)
    c_adm = din("c_adm", [128, 2 * 128])
    c_tri = din("c_tri", [128, 128], BF16)
    c_pow2 = din("c_pow2", [128, 32])

    okind = "ExternalOutput"
    y_out = nc.dram_tensor("y", [S, D_MODEL], F32, kind=okind).ap()
    dk = okind if debug else "Internal"
    FT = nc.dram_tensor("FT", [NB_FT, 128, S], BF16, kind=dk).ap()
    GT = nc.dram_tensor("GT", [8, 128, S], F32, kind=dk).ap()
    VA = nc.dram_tensor("VA", [S, 13, 128], BF16, kind=dk).ap()
    IW = nc.dram_tensor("IW", [S, 4], F32, kind=dk).ap()
    YT = nc.dram_tensor("YT", [8, 128, S], BF16, kind=dk).ap()
    XR = nc.dram_tensor("XR", [S, D_MODEL], F32, kind="Internal").ap()
    if debug:
        DSEL = nc.dram_tensor("DSEL", [S // 512, 128, S // 128, 512], BF16, kind=okind).ap()
        DSC = nc.dram_tensor("DSC", [S // 128, 128, S], F32, kind=okind).ap()
        DBS = nc.dram_tensor("DBS", [S // 128, 128, 64], F32, kind=okind).ap()

    with ExitStack() as es:
        sc = Sched(nc, es)
        op, dma = sc.op, sc.dma

        uniq = [0]

        def sb(st, name, shape, dt=F32):
            uniq[0] += 1
            return st.enter_context(nc.sbuf_tensor("%s_%d" % (name, uniq[0]), list(shape), dt))

        def ps(st, name, shape, dt=F32):
            return st.enter_context(nc.psum_tensor(name, list(shape), dt))

        ident = sb(es, "ident", [128, 128], BF16)
        masks = sb(es, "masks", [128, 8, 512], BF16)
        adm = sb(es, "adm", [128, 2, 128])
        tri = sb(es, "tri", [128, 128], BF16)
        ones_bf = sb(es, "ones_bf", [128, 128], BF16)
        ones_f = sb(es, "ones_f", [128, 128])
        pow2 = sb(es, "pow2", [128, 32])
        neghalf = sb(es, "neghalf", [128, 32])
        epst = sb(es, "epst", [128, 1])
        nh512 = sb(es, "nh512", [128, 512])
        dma(ident[:], c_ident[:, :], w=["ident"], slot="k_ident")
        dma(masks[:].rearrange("p a b -> p (a b)"), c_mask[:, :], w=["masks"], slot="k_masks")
        dma(adm[:].rearrange("p a b -> p (a b)"), c_adm[:, :], w=["adm"], slot="k_adm")
        dma(tri[:], c_tri[:, :], w=["tri"], slot="k_tri")
        dma(pow2[:], c_pow2[:, :], w=["pow2"], slot="k_pow2")
        op("dve", lambda e: e.memset(ones_bf[:], 1.0), w=["ones_bf"])
        op("dve", lambda e: e.memset(ones_f[:], 1.0), w=["ones_f"])
        op("dve", lambda e: e.memset(neghalf[:], -0.5), w=["neghalf"])
        op("dve", lambda e: e.memset(epst[:], EPS), w=["epst"])
        op("dve", lambda e: e.memset(nh512[:], -0.5), w=["nh512"])

        pf = [ps(es, "pf%d" % i, [128, 512]) for i in range(6)]
        pb = [ps(es, "pb%d" % i, [128, 1024], BF16) for i in range(2)]

        for l in range(DEPTH):
            x_src = x_in if l == 0 else XR
            x_dst = y_out if l == DEPTH - 1 else XR
            lam_init = float(lam_inits[l])
            with ExitStack() as ls:
                sublnt = sb(ls, "sublnt", [64, 1])
                lv = sb(ls, "lv", [1, 4, 32])
                lam_bc = sb(ls, "lam_bc", [128, 2])
                dma(sublnt[:], subln[l, :, :], w=["sublnt"], slot="k_sublnt")
                dma(lv[:].rearrange("p a b -> p (a b)"), lvec[l:l + 1, :], w=["lv"], slot="k_lv")
                op("dve", lambda e: e.tensor_scalar(out=sublnt[:], in0=sublnt[:], scalar1=1.0 - lam_init, scalar2=None,
                                                    op0=ALU.mult), r=["sublnt"], w=["sublnt"])
                lt = sb(ls, "lt", [1, 2, 32])
                l2 = sb(ls, "l2", [1, 4])
                op("dve", lambda e: e.tensor_tensor(out=lt[:, 0, :], in0=lv[:, 0, :], in1=lv[:, 1, :], op=ALU.mult), r=["lv"], w=["lt"])
                op("dve", lambda e: e.tensor_tensor(out=lt[:, 1, :], in0=lv[:, 2, :], in1=lv[:, 3, :], op=ALU.mult), r=["lv", "lt"], w=["lt"])
                op("dve", lambda e: e.tensor_reduce(out=l2[:, 0:2], in_=lt[:], axis=AX.X, op=ALU.add), r=["lt"], w=["l2"])
                op("act", lambda e: e.activation(out=l2[:, 0:2], in_=l2[:, 0:2], func=AF.Exp), r=["l2"], w=["l2"])
                op("dve", lambda e: e.tensor_tensor(out=l2[:, 2:3], in0=l2[:, 0:1], in1=l2[:, 1:2], op=ALU.subtract), r=["l2"], w=["l2"])
                op("dve", lambda e: e.tensor_scalar(out=l2[:, 3:4], in0=l2[:, 2:3], scalar1=lam_init, scalar2=None, op0=ALU.add), r=["l2"], w=["l2"])
                op("dve", lambda e: e.tensor_scalar(out=l2[:, 2:3], in0=l2[:, 3:4], scalar1=-1.0, scalar2=None, op0=ALU.mult), r=["l2"], w=["l2"])
                op("pe", lambda e: e.matmul(pf[0][:, 0:2], lhsT=ones_f[0:1, :], rhs=l2[0:1, 2:4], start=True, stop=True),
                   r=["l2", "ones_f"], w=["pf0"])
                op("dve", lambda e: e.tensor_copy(out=lam_bc[:], in_=pf[0][:, 0:2]), r=["pf0"], w=["lam_bc"])

                with ExitStack() as p1:
                    w_sb = sb(p1, "w_sb", [128, 8, IN_COLS], BF16)
                    g64t = sb(p1, "g64t", [128, 5, 64])
                    g32t = sb(p1, "g32t", [128, 17, 32])
                    gq96t = sb(p1, "gq96t", [128, 96])
                    gk64t = sb(p1, "gk64t", [128, 64])
                    dma(g64t[:].rearrange("p a b -> p (a b)"), g64[l:l + 1, :].to_broadcast([128, 320]), w=["g64t"], slot="k_g64t")
                    dma(g32t[:].rearrange("p a b -> p (a b)"), g32[l:l + 1, :].to_broadcast([128, 544]), w=["g32t"], slot="k_g32t")
                    dma(gq96t[:], gq96[l:l + 1, :].to_broadcast([128, 96]), w=["gq96t"], slot="k_gq96t")
                    dma(gk64t[:], gk64[l:l + 1, :].to_broadcast([128, 64]), w=["gk64t"], slot="k_gk64t")
                    wuq_sb = sb(p1, "wuq_sb", [128, 2, 384], BF16)
                    wukv_sb = sb(p1, "wukv_sb", [128, 512], BF16)
                    lng = sb(p1, "lng", [128, 8])
                    qng = sb(p1, "qng", [128, 2])
                    kvng = sb(p1, "kvng", [128, 1])
                    dma(lng[:], ln_g[l, :, :], w=["lng"], slot="k_lng")
                    dma(qng[:], qn_g[l, :, :], w=["qng"], slot="k_qng")
                    dma(kvng[:], kvn_g[l, :, :], w=["kvng"], slot="k_kvng")
                    with ExitStack() as p0:
                        wst = [sb(p0, "wst%d" % i, [128, IN_COLS]) for i in range(2)]
                        for c in range(8):
                            k = "wst%d" % (c % 2)
                            dma(wst[c % 2][:], w_in[l, c * 128:(c + 1) * 128, :], w=[k], slot=k)
                            eng = "dve" if c % 2 == 0 else "pool"
                            op(eng, lambda e, c=c: e.tensor_scalar(out=w_sb[:, c, :], in0=wst[c % 2][:], scalar1=lng[:, c:c + 1],
                                                                    scalar2=0.0, op0=ALU.mult, op1=ALU.add), r=[k, "lng"], w=["w_sb"])
                        for c in range(2):
                            dma(wst[c][:, 0:384], w_uq[l, c * 128:(c + 1) * 128, :], w=["wst%d" % c], slot="wst%d" % c)
                            op("dve", lambda e, c=c: e.tensor_scalar(out=wuq_sb[:, c, :], in0=wst[c][:, 0:384], scalar1=qng[:, c:c + 1],
                                                                      scalar2=None, op0=ALU.mult), r=["wst%d" % c, "qng"], w=["wuq_sb"])
                        dma(wst[0][:, 0:512], w_ukv[l, :, :], w=["wst0"], slot="wst0")
                        op("dve", lambda e: e.tensor_scalar(out=wukv_sb[:], in0=wst[0][:, 0:512], scalar1=kvng[:, 0:1],
                                                            scalar2=None, op0=ALU.mult), r=["wst0", "kvng"], w=["wukv_sb"])
                        sc.barrier()
                    xt = [sb(p1, "xt%d" % i, [128, D_MODEL]) for i in range(2)]
                    junk = sb(p1, "junk", [128, D_MODEL], BF16)
                    hb = sb(p1, "hb", [128, D_MODEL], BF16)
                    hT = sb(p1, "hT", [128, 8, 512], BF16)
                    proj = sb(p1, "proj", [128, NTM])
                    tmp = sb(p1, "tmp", [128, 640])
                    tmp2 = sb(p1, "tmp2", [128, 640])
                    st8 = sb(p1, "st8", [128, 64])
                    cs64 = [sb(p1, "cs64_%d" % i, [128, 64]) for i in range(2)]
                    cs32 = [sb(p1, "cs32_%d" % i, [128, 32]) for i in range(2)]
                    TT = sb(p1, "TT", [128, 22, 128], BF16)
                    VS = sb(p1, "VS", [128, 13, 128], BF16)
                    IWs = sb(p1, "IWs", [128, 4])
                    QB = sb(p1, "QB", [128, 4, 96])
                    KV = sb(p1, "KV", [128, 4, 128])
                    cqn = sb(p1, "cqn", [128, 384], BF16)
                    cqT = sb(p1, "cqT", [128, 3, 128], BF16)
                    FTs = sb(p1, "FTs", [128, NB_FT, 512], BF16)
                    GS = sb(p1, "GS", [128, 8, 512])
                    op("pool", lambda e: e.memset(TT[:], 0.0), w=["TT"])
                    op("pool", lambda e: e.memset(VS[:], 1.0), w=["VS"])

                    def load_x(t):
                        k = "xt%d" % (t % 2)
                        dma(xt[t % 2][:], x_src[t * 128:(t + 1) * 128, :], w=[k], slot=k)
                        dma(cs64[t % 2][:], c_cs64[t * 128:(t + 1) * 128, :], w=["cs64_%d" % (t % 2)], slot="cs64_%d" % (t % 2))
                        dma(cs32[t % 2][:], c_cs32[t * 128:(t + 1) * 128, :], w=["cs32_%d" % (t % 2)], slot="cs32_%d" % (t % 2))

                    def rstd_of(ss_ap, n, d, key):
                        op("act", lambda e: e.activation(out=ss_ap, in_=ss_ap, func=AF.Ln, scale=1.0 / d, bias=epst[:, 0:1]), r=[key, "epst"], w=[key])
                        op("act", lambda e: e.activation(out=ss_ap, in_=ss_ap, func=AF.Exp, scale=-0.5), r=[key], w=[key])

                    load_x(0)
                    for stl in range(NST):
                        for sub in range(4):
                            t = stl * 4 + sub
                            if t + 1 < NT:
                                load_x(t + 1)
                            xk = "xt%d" % (t % 2)
                            X = xt[t % 2]
                            c64k, c32k = "cs64_%d" % (t % 2), "cs32_%d" % (t % 2)
                            C64, C32 = cs64[t % 2], cs32[t % 2]
                            op("act", lambda e: e.activation(out=junk[:], in_=X[:], func=AF.Square, accum_out=st8[:, 0:1]),
                               r=[xk], w=["junk", "st_x"])
                            rstd_of(st8[:, 0:1], 1, D_MODEL, "st_x")
                            op("dve", lambda e: e.tensor_scalar(out=hb[:], in0=X[:], scalar1=st8[:, 0:1], scalar2=None, op0=ALU.mult),
                               r=[xk, "st_x"], w=["hb"])
                            for c in range(8):
                                op("pe", lambda e, c=c: e.transpose(pb[0][:, c * 128:(c + 1) * 128], hb[:, c * 128:(c + 1) * 128], ident[:]),
                                   r=["hb", "ident"], w=["pb0"])
                            op("act", lambda e: e.activation(out=hT[:, :, sub * 128:(sub + 1) * 128],
                                                             in_=pb[0][:].rearrange("p (c t) -> p c t", c=8), func=AF.Copy),
                               r=["pb0"], w=["hT%d" % sub])
                            col = 0
                            ci = 0
                            while col < NTM:
                                n = min(512, NTM - col)
                                bank = 1 + (ci % 2)
                                for c in range(8):
                                    op("pe", lambda e, c=c, col=col, n=n, bank=bank: e.matmul(
                                        pf[bank][:, 0:n], lhsT=hT[:, c, sub * 128:(sub + 1) * 128], rhs=w_sb[:, c, T0 + col:T0 + col + n],
                                        start=(c == 0), stop=(c == 7)), r=["hT%d" % sub, "w_sb"], w=["pf%d" % bank])
                                eng = "act" if ci % 2 == 0 else "dve"
                                if eng == "act":
                                    op("act", lambda e, col=col, n=n, bank=bank: e.activation(out=proj[:, col:col + n], in_=pf[bank][:, 0:n], func=AF.Copy),
                                       r=["pf%d" % bank], w=["proj%d" % ci])
                                else:
                                    op("dve", lambda e, col=col, n=n, bank=bank: e.tensor_copy(out=proj[:, col:col + n], in_=pf[bank][:, 0:n]),
                                       r=["pf%d" % bank], w=["proj%d" % ci])
                                col += n
                                ci += 1
                            PJ = ["proj%d" % i for i in range(ci)]
                            op("pool", lambda e: e.tensor_copy(out=VS[:, 0:4, 0:64], in_=proj[:, P_AV:P_AV + 256].rearrange("p (h d) -> p h d", h=4)), r=PJ, w=["VS"])
                            op("pool", lambda e: e.tensor_copy(out=VS[:, 9:13, 0:64], in_=proj[:, P_DV:P_DV + 256].rearrange("p (h d) -> p h d", h=4)), r=PJ, w=["VS"])
                            op("pool", lambda e: e.tensor_copy(out=VS[:, 8, 0:64], in_=proj[:, P_CV:P_CV + 64]), r=PJ, w=["VS"])
                            op("pool", lambda e: e.tensor_scalar(out=IWs[:], in0=proj[:, P_IW:P_IW + 4], scalar1=0.0625, scalar2=0.0, op0=ALU.mult, op1=ALU.add), r=PJ, w=["IWs"])
                            X64 = proj[:, P_X64:P_X64 + 640].rearrange("p (h d) -> p h d", h=10)
                            T3 = tmp[:].rearrange("p (h d) -> p h d", h=10)
                            T3b = tmp2[:].rearrange("p (h d) -> p h d", h=10)
                            op("act", lambda e: e.activation(out=T3[:, 0:5, :], in_=X64[:, 0:5, :], func=AF.Square), r=PJ, w=["tmp"])
                            op("dve", lambda e: e.tensor_reduce(out=st8[:, 8:13], in_=T3[:, 0:5, :], axis=AX.X, op=ALU.add), r=["tmp"], w=["st_a"])
                            rstd_of(st8[:, 8:13], 5, 64, "st_a")
                            op("dve", lambda e: e.tensor_tensor(out=X64[:, 0:5, :], in0=X64[:, 0:5, :], in1=bc(st8[:, 8:13], [128, 5, 64], 2), op=ALU.mult),
                               r=PJ + ["st_a"], w=PJ)
                            op("dve", lambda e: e.tensor_tensor(out=X64[:, 0:5, :], in0=X64[:, 0:5, :], in1=g64t[:], op=ALU.mult), r=PJ + ["g64t"], w=PJ)
                            cosb = bc(C64[:, 0:32], [128, 10, 32], 1)
                            sinb = bc(C64[:, 32:64], [128, 10, 32], 1)
                            op("dve", lambda e: e.tensor_tensor(out=T3[:, :, 0:32], in0=X64[:, :, 0:32], in1=cosb, op=ALU.mult), r=PJ + [c64k, "tmp"], w=["tmp"])
                            op("pool", lambda e: e.tensor_tensor(out=T3b[:, :, 0:32], in0=X64[:, :, 32:64], in1=sinb, op=ALU.mult), r=PJ + [c64k], w=["tmp2"])
                            op("dve", lambda e: e.tensor_tensor(out=T3[:, :, 32:64], in0=X64[:, :, 0:32], in1=sinb, op=ALU.mult), r=PJ + [c64k, "tmp"], w=["tmp"])
                            op("pool", lambda e: e.tensor_tensor(out=T3b[:, :, 32:64], in0=X64[:, :, 32:64], in1=cosb, op=ALU.mult), r=PJ + [c64k, "tmp2"], w=["tmp2"])
                            TU = TT[:].rearrange("p b (u d) -> p (b u) d", u=2)
                            for (h0, h1, u0) in ((0, 5, 16), (5, 10, 22)):
                                op("dve", lambda e, h0=h0, h1=h1, u0=u0: e.tensor_tensor(out=TU[:, u0:u0 + 5, 0:32], in0=T3[:, h0:h1, 0:32], in1=T3b[:, h0:h1, 0:32], op=ALU.subtract),
                                   r=["tmp", "tmp2"], w=["TT"])
                                op("dve", lambda e, h0=h0, h1=h1, u0=u0: e.tensor_tensor(out=TU[:, u0:u0 + 5, 32:64], in0=T3[:, h0:h1, 32:64], in1=T3b[:, h0:h1, 32:64], op=ALU.add),
                                   r=["tmp", "tmp2"], w=["TT"])
                            op("pool", lambda e: e.tensor_copy(out=TU[:, 21, :], in_=TU[:, 20, :]), r=["TT"], w=["TT"])
                            op("pool", lambda e: e.tensor_copy(out=TU[:, 27, :], in_=TU[:, 26, :]), r=["TT"], w=["TT"])
                            Y32 = proj[:, P_Y32:P_Y32 + 544].rearrange("p (h d) -> p h d", h=17)
                            U3 = tmp[:, 0:544].rearrange("p (h d) -> p h d", h=17)
                            U3b = tmp2[:, 0:544].rearrange("p (h d) -> p h d", h=17)
                            op("act", lambda e: e.activation(out=U3[:], in_=Y32, func=AF.Square), r=PJ + ["TT"], w=["tmp"])
                            op("dve", lambda e: e.tensor_reduce(out=st8[:, 16:33], in_=U3[:], axis=AX.X, op=ALU.add), r=["tmp"], w=["st_b"])
                            rstd_of(st8[:, 16:33], 17, 32, "st_b")
                            op("dve", lambda e: e.tensor_tensor(out=Y32, in0=Y32, in1=bc(st8[:, 16:33], [128, 17, 32], 2), op=ALU.mult), r=PJ + ["st_b"], w=PJ)
                            op("dve", lambda e: e.tensor_tensor(out=Y32, in0=Y32, in1=g32t[:], op=ALU.mult), r=PJ + ["g32t"], w=PJ)
                            cosb = bc(C32[:, 0:16], [128, 17, 16], 1)
                            sinb = bc(C32[:, 16:32], [128, 17, 16], 1)
                            op("dve", lambda e: e.tensor_tensor(out=U3[:, :, 0:16], in0=Y32[:, :, 0:16], in1=cosb, op=ALU.mult), r=PJ + [c32k, "tmp"], w=["tmp"])
                            op("pool", lambda e: e.tensor_tensor(out=U3b[:, :, 0:16], in0=Y32[:, :, 16:32], in1=sinb, op=ALU.mult), r=PJ + [c32k, "TT"], w=["tmp2"])
                            op("dve", lambda e: e.tensor_tensor(out=U3[:, :, 16:32], in0=Y32[:, :, 0:16], in1=sinb, op=ALU.mult), r=PJ + [c32k, "tmp"], w=["tmp"])
                            op("pool", lambda e: e.tensor_tensor(out=U3b[:, :, 16:32], in0=Y32[:, :, 16:32], in1=cosb, op=ALU.mult), r=PJ + [c32k, "tmp2"], w=["tmp2"])
                            TQ = TT[:].rearrange("p b (u d) -> p (b u) d", u=4)
                            op("dve", lambda e: e.tensor_tensor(out=TQ[:, 56:88:2, 0:16], in0=U3[:, 0:16, 0:16], in1=U3b[:, 0:16, 0:16], op=ALU.subtract), r=["tmp", "tmp2"], w=["TT"])
                            op("dve", lambda e: e.tensor_tensor(out=TQ[:, 56:88:2, 16:32], in0=U3[:, 0:16, 16:32], in1=U3b[:, 0:16, 16:32], op=ALU.add), r=["tmp", "tmp2"], w=["TT"])
                            for hh in range(4):
                                u = (4 + hh) * 4 + 2
                                op("dve", lambda e, u=u: e.tensor_tensor(out=TQ[:, u, 0:16], in0=U3[:, 16, 0:16], in1=U3b[:, 16, 0:16], op=ALU.subtract), r=["tmp", "tmp2"], w=["TT"])
                                op("dve", lambda e, u=u: e.tensor_tensor(out=TQ[:, u, 16:32], in0=U3[:, 16, 16:32], in1=U3b[:, 16, 16:32], op=ALU.add), r=["tmp", "tmp2"], w=["TT"])
                            CQ = proj[:, P_CQ:P_CQ + 256]
                            CKV = proj[:, P_CKV:P_CKV + 128]
                            op("act", lambda e: e.activation(out=tmp[:, 0:256], in_=CQ, func=AF.Square, accum_out=st8[:, 40:41]), r=PJ + ["tmp", "TT"], w=["tmp", "st_c"])
                            op("act", lambda e: e.activation(out=tmp[:, 256:384], in_=CKV, func=AF.Square, accum_out=st8[:, 41:42]), r=PJ + ["tmp"], w=["tmp", "st_c"])
                            op("dve", lambda e: e.tensor_scalar(out=st8[:, 41:42], in0=st8[:, 41:42], scalar1=2.0, scalar2=None, op0=ALU.mult), r=["st_c"], w=["st_c"])
                            rstd_of(st8[:, 40:42], 2, 256, "st_c")
                            op("dve", lambda e: e.tensor_scalar(out=cqn[:, 0:256], in0=CQ, scalar1=st8[:, 40:41], scalar2=None, op0=ALU.mult), r=PJ + ["st_c"], w=["cqn"])
                            op("dve", lambda e: e.tensor_scalar(out=cqn[:, 256:384], in0=CKV, scalar1=st8[:, 41:42], scalar2=None, op0=ALU.mult), r=PJ + ["st_c", "cqn"], w=["cqn"])
                            for c in range(3):
                                op("pe", lambda e, c=c: e.transpose(pb[1][:, c * 128:(c + 1) * 128], cqn[:, c * 128:(c + 1) * 128], ident[:]), r=["cqn", "ident"], w=["pb1"])
                            op("act", lambda e: e.activation(out=cqT[:].rearrange("p c t -> p (c t)"), in_=pb[1][:, 0:384], func=AF.Copy), r=["pb1"], w=["cqT"])
                            for c in range(2):
                                op("pe", lambda e, c=c: e.matmul(pf[3][:, 0:384], lhsT=cqT[:, c, :], rhs=wuq_sb[:, c, :], start=(c == 0), stop=(c == 1)),
                                   r=["cqT", "wuq_sb"], w=["pf3"])
                            op("pe", lambda e: e.matmul(pf[4][:, 0:512], lhsT=cqT[:, 2, :], rhs=wukv_sb[:], start=True, stop=True), r=["cqT", "wukv_sb"], w=["pf4"])
                            op("act", lambda e: e.activation(out=QB[:].rearrange("p h d -> p (h d)"), in_=pf[3][:, 0:384], func=AF.Copy), r=["pf3"], w=["QB"])
                            op("dve", lambda e: e.tensor_copy(out=KV[:].rearrange("p h d -> p (h d)"), in_=pf[4][:, 0:512]), r=["pf4"], w=["KV"])
                            Q3 = tmp[:, 0:384].rearrange("p (h d) -> p h d", h=4)
                            K3 = tmp2[:, 0:256].rearrange("p (h d) -> p h d", h=4)
                            op("act", lambda e: e.activation(out=Q3, in_=QB[:], func=AF.Square), r=["QB", "tmp"], w=["tmp"])
                            op("act", lambda e: e.activation(out=K3, in_=KV[:, :, 0:64], func=AF.Square), r=["KV", "tmp2", "TT"], w=["tmp2"])
                            op("dve", lambda e: e.tensor_reduce(out=st8[:, 44:48], in_=Q3[:, :, 0:64], axis=AX.X, op=ALU.add), r=["tmp"], w=["st_d"])
                            op("dve", lambda e: e.tensor_reduce(out=st8[:, 48:52], in_=K3, axis=AX.X, op=ALU.add), r=["tmp2"], w=["st_d"])
                            op("dve", lambda e: e.tensor_reduce(out=st8[:, 52:56], in_=Q3[:, :, 64:96], axis=AX.X, op=ALU.add), r=["tmp"], w=["st_e"])
                            rstd_of(st8[:, 44:52], 8, 64, "st_d")
                            rstd_of(st8[:, 52:56], 4, 32, "st_e")
                            T128 = TT[:]
                            op("dve", lambda e: e.tensor_tensor(out=QB[:, :, 0:64], in0=QB[:, :, 0:64], in1=bc(st8[:, 44:48], [128, 4, 64], 2), op=ALU.mult), r=["QB", "st_d"], w=["QB"])
                            op("dve", lambda e: e.tensor_tensor(out=T128[:, 0:4, 0:64], in0=QB[:, :, 0:64], in1=bc(gq96t[:, 0:64], [128, 4, 64], 1), op=ALU.mult), r=["QB", "gq96t"], w=["TT"])
                            op("dve", lambda e: e.tensor_tensor(out=KV[:, :, 0:64], in0=KV[:, :, 0:64], in1=bc(st8[:, 48:52], [128, 4, 64], 2), op=ALU.mult), r=["KV", "st_d"], w=["KV"])
                            op("dve", lambda e: e.tensor_tensor(out=T128[:, 4:8, 0:64], in0=KV[:, :, 0:64], in1=bc(gk64t[:], [128, 4, 64], 1), op=ALU.mult), r=["KV", "gk64t"], w=["TT"])
                            op("pool", lambda e: e.tensor_copy(out=VS[:, 4:8, 0:64], in_=KV[:, :, 64:128]), r=["KV"], w=["VS"])
                            op("dve", lambda e: e.tensor_tensor(out=QB[:, :, 64:96], in0=QB[:, :, 64:96], in1=bc(st8[:, 52:56], [128, 4, 32], 2), op=ALU.mult), r=["QB", "st_e"], w=["QB"])
                            op("dve", lambda e: e.tensor_tensor(out=QB[:, :, 64:96], in0=QB[:, :, 64:96], in1=bc(gq96t[:, 64:96], [128, 4, 32], 1), op=ALU.mult), r=["QB", "gq96t"], w=["QB"])
                            cosb = bc(C32[:, 0:16], [128, 4, 16], 1)
                            sinb = bc(C32[:, 16:32], [128, 4, 16], 1)
                            R3 = tmp[:, 0:128].rearrange("p (h d) -> p h d", h=4)
                            R3b = tmp2[:, 0:128].rearrange("p (h d) -> p h d", h=4)
                            op("dve", lambda e: e.tensor_tensor(out=R3[:, :, 0:16], in0=QB[:, :, 64:80], in1=cosb, op=ALU.mult), r=["QB", c32k, "tmp"], w=["tmp"])
                            op("pool", lambda e: e.tensor_tensor(out=R3b[:, :, 0:16], in0=QB[:, :, 80:96], in1=sinb, op=ALU.mult), r=["QB", c32k, "tmp2"], w=["tmp2"])
                            op("dve", lambda e: e.tensor_tensor(out=R3[:, :, 16:32], in0=QB[:, :, 64:80], in1=sinb, op=ALU.mult), r=["QB", c32k, "tmp"], w=["tmp"])
                            op("pool", lambda e: e.tensor_tensor(out=R3b[:, :, 16:32], in0=QB[:, :, 80:96], in1=cosb, op=ALU.mult), r=["QB", c32k, "tmp2"], w=["tmp2"])
                            op("dve", lambda e: e.tensor_tensor(out=T128[:, 0:4, 64:80], in0=R3[:, :, 0:16], in1=R3b[:, :, 0:16], op=ALU.subtract), r=["tmp", "tmp2"], w=["TT"])
                            op("dve", lambda e: e.tensor_tensor(out=T128[:, 0:4, 80:96], in0=R3[:, :, 16:32], in1=R3b[:, :, 16:32], op=ALU.add), r=["tmp", "tmp2"], w=["TT"])
                            for g0, g1, bank in ((0, 8, 0), (8, 16, 1), (16, 22, 0)):
                                for b in range(g0, g1):
                                    op("pe", lambda e, b=b, g0=g0, bank=bank: e.transpose(pb[bank][:, (b - g0) * 128:(b - g0 + 1) * 128], TT[:, b, :], ident[:]),
                                       r=["TT", "ident"], w=["pb%d" % bank])
                                nb = g1 - g0
                                eng = "act" if bank == 0 else "dve"
                                if eng == "act":
                                    op("act", lambda e, g0=g0, g1=g1, nb=nb, bank=bank: e.activation(
                                        out=FTs[:, 4 + g0:4 + g1, sub * 128:(sub + 1) * 128], in_=pb[bank][:, 0:nb * 128].rearrange("p (b t) -> p b t", b=nb), func=AF.Copy),
                                       r=["pb%d" % bank], w=["FTs"])
                                else:
                                    op("dve", lambda e, g0=g0, g1=g1, nb=nb, bank=bank: e.tensor_copy(
                                        out=FTs[:, 4 + g0:4 + g1, sub * 128:(sub + 1) * 128], in_=pb[bank][:, 0:nb * 128].rearrange("p (b t) -> p b t", b=nb)),
                                       r=["pb%d" % bank], w=["FTs"])
                            dma(VA[t * 128:(t + 1) * 128, :, :], VS[:], r=["VS"], slot="VS")
                            dma(IW[t * 128:(t + 1) * 128, :], IWs[:], r=["IWs"], slot="IWs")
                        HT = ["hT%d" % i for i in range(4)]
                        for ct in range(12):
                            bank = 3 + (ct % 2)
                            for c in range(8):
                                op("pe", lambda e, c=c, ct=ct, bank=bank: e.matmul(pf[bank][:, :], lhsT=w_sb[:, c, ct * 128:(ct + 1) * 128], rhs=hT[:, c, :],
                                                                                  start=(c == 0), stop=(c == 7)), r=HT + ["w_sb"], w=["pf%d" % bank])
                            if ct < 4:
                                op("dve", lambda e, ct=ct, bank=bank: e.tensor_copy(out=FTs[:, ct, :], in_=pf[bank][:, :]), r=["pf%d" % bank], w=["FTs"])
                            else:
                                op("act", lambda e, ct=ct, bank=bank: e.activation(out=GS[:, ct - 4, :], in_=pf[bank][:, :], func=AF.Silu), r=["pf%d" % bank], w=["GS"])
                        q0 = stl * 512
                        for g in range(0, NB_FT, 7):
                            g1 = min(NB_FT, g + 7)
                            dma(FT[g:g1, :, q0:q0 + 512].rearrange("b p t -> p b t"), FTs[:, g:g1, :], r=["FTs"], slot="FTs")
                        for g in range(0, 8, 4):
                            dma(GT[g:g + 4, :, q0:q0 + 512].rearrange("b p t -> p b t"), GS[:, g:g + 4, :], r=["GS"], slot="GS")
                    sc.barrier()
                NQ = S // 512
                NIT = 16

                def load_V(st, name, h0, nh):
                    V = sb(st, name, [128, NT, nh, 128], BF16)
                    for g in range(0, NT, 8):
                        g1 = min(NT, g + 8)
                        dma(V[:, g:g1, :, :], VA[g * 128:g1 * 128, h0:h0 + nh, :].rearrange("(k p) h d -> p k h d", p=128), w=[name], slot=name)
                    return V

                def load_K(st, name, b0, nb):
                    Kt = sb(st, name, [128, nb, S], BF16)
                    for b in range(nb):
                        dma(Kt[:, b, :], FT[b0 + b, :, :], w=[name], slot=name)
                    return Kt

                def softmax_mixer(mname, kb0, nkb, qb0, nqb, vh0, nvh, gb0, heads, scale, diag_mask, selT_fn=None, post=None, pre_q=None):
                    with ExitStack() as ms:
                        Kt = load_K(ms, "Kt", kb0, nkb)
                        V = load_V(ms, "Vr", vh0, nvh)
                        Qs = [sb(ms, "Qs%d" % i, [128, nqb, 512], BF16) for i in range(2)]
                        Gs = [sb(ms, "Gs%d" % i, [128, 2, 512]) for i in range(1)]
                        PT = [sb(ms, "PT%d" % i, [128, 512], BF16) for i in range(3)]
                        RC = sb(ms, "RC", [128, 2 if post is not None else 1, 512])
                        OD = sb(ms, "OD", [128, 3 if post is not None else 1, 512])
                        YS = [sb(ms, "YS%d" % i, [128, 2, 512], BF16) for i in range(2)]
                        extra = pre_q(ms) if pre_q is not None else None
                        ctr = {"s": 0, "p": 0, "o": 0}

                        def load_q(j):
                            q0 = j * 512
                            dma(Qs[j % 2][:], FT[qb0:qb0 + nqb, :, q0:q0 + 512].rearrange("b p t -> p b t"), w=["Qs%d" % (j % 2)], slot="Qs%d" % (j % 2))

                        def load_g(j):
                            q0 = j * 512
                            dma(Gs[0][:], GT[gb0:gb0 + 2, :, q0:q0 + 512].rearrange("b p t -> p b t"), w=["Gs0"], slot="Gs0")

                        load_q(0)
                        for j in range(NQ):
                            if j + 1 < NQ:
                                load_q(j + 1)
                            load_g(j)
                            Qk, Gk = "Qs%d" % (j % 2), "Gs0"
                            Q, G = Qs[j % 2], Gs[0]
                            nkt = 4 * (j + 1)
                            selT = selT_fn(j, extra, Q, Qk) if selT_fn is not None else None
                            ysk = "YS%d" % (j % 2)
                            for hi, (maps, vh) in enumerate(heads):
                                obanks = []
                                for m in maps:
                                    obanks.append(2 + (ctr["o"] % 4))
                                    ctr["o"] += 1
                                seq = [(kt, mi) for kt in range(nkt) for mi in range(len(maps))]

                                def emit_qk(idx):
                                    kt, mi = seq[idx]
                                    kblk, qblk, base, dkk = maps[mi]
                                    sbank = ctr["s"] % 2
                                    ctr["s"] += 1
                                    op("pe", lambda e: e.matmul(pf[sbank][:, :], lhsT=Kt[base:base + dkk, kblk, kt * 128:(kt + 1) * 128],
                                                                rhs=Q[base:base + dkk, qblk, :], start=True, stop=True),
                                       r=["Kt", Qk], w=["pf%d" % sbank])
                                    return sbank

                                pend = emit_qk(0)
                                for idx in range(len(seq)):
                                    kt, mi = seq[idx]
                                    sbank = pend
                                    if idx + 1 < len(seq):
                                        pend = emit_qk(idx + 1)
                                    pi = ctr["p"] % 3
                                    ctr["p"] += 1
                                    P = PT[pi]
                                    pk = "PT%d" % pi
                                    op("act", lambda e: e.activation(out=P[:], in_=pf[sbank][:, :], func=AF.Exp, scale=scale), r=["pf%d" % sbank], w=[pk])
                                    r_ = kt - (nkt - 4)
                                    if diag_mask is not None and r_ >= 0:
                                        op("dve", lambda e: e.tensor_tensor(out=P[:], in0=P[:], in1=masks[:, diag_mask + r_, :], op=ALU.mult), r=[pk, "masks"], w=[pk])
                                    if selT is not None:
                                        eng = "dve" if (idx % 2 == 0) else "pool"
                                        op(eng, lambda e: e.tensor_tensor(out=P[:], in0=P[:], in1=selT[:, kt, :], op=ALU.mult), r=[pk, "selT"], w=[pk])
                                    ob = obanks[mi]
                                    op("pe", lambda e: e.matmul(pf[ob][:, :], lhsT=V[:, kt, vh, :], rhs=P[:], start=(kt == 0), stop=(kt == nkt - 1)),
                                       r=["Vr", pk], w=["pf%d" % ob])
                                blk, hb_ = hi // 2, (hi % 2) * 64
                                for mi, ob in enumerate(obanks):
                                    op("dve", lambda e, mi=mi, ob=ob: e.reciprocal(out=RC[64:128, mi, :], in_=pf[ob][64:128, :]), r=["pf%d" % ob], w=["RC"])
                                if post is None:
                                    ob = obanks[0]
                                    op("dve", lambda e: e.tensor_tensor(out=OD[hb_:hb_ + 64, 0, :], in0=pf[ob][0:64, :], in1=RC[64:128, 0, :], op=ALU.mult),
                                       r=["pf%d" % ob, "RC"], w=["OD"])
                                    op("dve", lambda e: e.tensor_tensor(out=YS[j % 2][hb_:hb_ + 64, blk, :], in0=OD[hb_:hb_ + 64, 0, :], in1=G[hb_:hb_ + 64, blk, :], op=ALU.mult),
                                       r=["OD", Gk], w=[ysk])
                                else:
                                    post(obanks, RC, OD, G, Gk, YS[j % 2], ysk, blk, hb_)
                            q0 = j * 512
                            dma(YT[gb0:gb0 + 2, :, q0:q0 + 512].rearrange("b p t -> p b t"), YS[j % 2][:], r=[ysk], slot=ysk)
                        sc.barrier()

                if "B" in MIX:
                    softmax_mixer("B", 8, 4, 4, 4, 4, 4, 2, [([(h, h, 0, 96)], h) for h in range(4)], 96.0 ** -0.5, 0)

                def post_D(obanks, RC, OD, G, Gk, YSt, ysk, blk, hb_):
                    a, b = obanks
                    op("dve", lambda e: e.tensor_tensor(out=OD[0:64, 0, :], in0=pf[a][0:64, :], in1=RC[64:128, 0, :], op=ALU.mult), r=["pf%d" % a, "RC"], w=["OD"])
                    op("dve", lambda e: e.tensor_tensor(out=OD[0:64, 1, :], in0=pf[b][0:64, :], in1=RC[64:128, 1, :], op=ALU.mult), r=["pf%d" % b, "RC", "OD"], w=["OD"])
                    op("dve", lambda e: e.scalar_tensor_tensor(out=OD[0:64, 0, :], in0=OD[0:64, 1, :], scalar=lam_bc[0:64, 0:1], in1=OD[0:64, 0, :], op0=ALU.mult, op1=ALU.add),
                       r=["OD", "lam_bc"], w=["OD"])
                    op("act", lambda e: e.activation(out=OD[0:64, 1, :], in_=OD[0:64, 0, :], func=AF.Square), r=["OD"], w=["OD"])
                    op("pe", lambda e: e.matmul(pf[a][0:64, :], lhsT=ones_f[0:64, 0:64], rhs=OD[0:64, 1, :], start=True, stop=True), r=["OD", "ones_f"], w=["pf%d" % a])
                    op("act", lambda e: e.activation(out=OD[0:64, 1, :], in_=pf[a][0:64, :], func=AF.Ln, scale=1.0 / 64, bias=epst[0:64, 0:1]), r=["pf%d" % a, "OD", "epst"], w=["OD"])
                    op("act", lambda e: e.activation(out=OD[0:64, 1, :], in_=OD[0:64, 1, :], func=AF.Exp, scale=-0.5), r=["OD"], w=["OD"])
                    op("dve", lambda e: e.scalar_tensor_tensor(out=OD[hb_:hb_ + 64, 2, :], in0=OD[0:64, 0, :], scalar=sublnt[0:64, 0:1], in1=OD[0:64, 1, :], op0=ALU.mult, op1=ALU.mult),
                       r=["OD", "sublnt"], w=["OD"])
                    op("dve", lambda e: e.tensor_tensor(out=YSt[hb_:hb_ + 64, blk, :], in0=OD[hb_:hb_ + 64, 2, :], in1=G[hb_:hb_ + 64, blk, :], op=ALU.mult), r=["OD", Gk], w=[ysk])

                if "D" in MIX:
                    softmax_mixer("D", 22, 4, 18, 4, 9, 4, 6,
                                  [([(h, h, c * 64, 32) for c in range(2)], h) for h in range(4)], 32.0 ** -0.5, 0, post=post_D)

                def pre_C(ms):
                    ex = {}
                    ex["IKc"] = [sb(ms, "IKc%d" % i, [128, 512], BF16) for i in range(3)]
                    ex["IQ"] = [sb(ms, "IQ%d" % i, [128, 2, 512], BF16) for i in range(1)]
                    ex["IWt"] = [sb(ms, "IWt%d" % i, [128, 4, 4]) for i in range(2)]
                    ex["SC"] = sb(ms, "SC", [128, S])
                    ex["JK"] = sb(ms, "JK", [128, S], BF16)
                    ex["BD"] = sb(ms, "BD", [128, S], BF16)
                    ex["selT"] = sb(ms, "selT", [128, NT, 512], BF16)
                    ex["RL"] = [sb(ms, "RL%d" % i, [128, 512]) for i in range(2)]
                    ex["bs"] = sb(ms, "bs", [128, 64])
                    return ex

                def selT_C(j, ex, Q, Qk):
                    q0 = j * 512
                    IQ, IWt = ex["IQ"][0], ex["IWt"][j % 2]
                    iqk, iwk = "IQ0", "IWt%d" % (j % 2)
                    SC, JK, selT, RL, bs, IKc = ex["SC"], ex["JK"], ex["selT"], ex["RL"], ex["bs"], ex["IKc"]
                    BD = ex["BD"]
                    dma(IQ[:], FT[15:17, :, q0:q0 + 512].rearrange("b p t -> p b t"), w=[iqk], slot=iqk)
                    dma(IWt[:], IW[q0:q0 + 512, :].rearrange("(a p) h -> p a h", p=128), w=[iwk], slot=iwk)
                    op("pool", lambda e: e.memset(selT[:, 4 * j:4 * j + 4, :], 0.0), w=["selT"])
                    rl = 0
                    chunks = [(qi, c0) for qi in range(4) for c0 in range(0, (4 * j + qi + 1) * 128, 512)]

                    def load_ik(ci):
                        qi_, c0_ = chunks[ci]
                        n_ = min(512, (4 * j + qi_ + 1) * 128 - c0_)
                        kk_ = "IKc%d" % (ci % 3)
                        dma(IKc[ci % 3][:, 0:n_], FT[17, :, c0_:c0_ + n_], w=[kk_], slot=kk_)

                    load_ik(0)
                    ci = 0
                    for qi in range(4):
                        i = 4 * j + qi
                        Lk = (i + 1) * 128
                        for c0 in range(0, Lk, 512):
                            n = min(512, Lk - c0)
                            if ci + 1 < len(chunks):
                                load_ik(ci + 1)
                            IKb, ikk = IKc[ci % 3], "IKc%d" % (ci % 3)
                            ci += 1
                            for h in range(4):
                                blk, base = h // 2, (h % 2) * 64
                                bank = h % 2
                                op("pe", lambda e: e.matmul(pf[bank][:, 0:n], lhsT=IQ[base:base + 64, blk, qi * 128:(qi + 1) * 128], rhs=IKb[base:base + 64, 0:n],
                                                            start=True, stop=True), r=[iqk, ikk], w=["pf%d" % bank])
                                R = RL[rl % 2]
                                rk = "RL%d" % (rl % 2)
                                rl += 1
                                op("act", lambda e: e.activation(out=R[:, 0:n], in_=pf[bank][:, 0:n], func=AF.Relu), r=["pf%d" % bank], w=[rk])
                                if h == 0:
                                    op("dve", lambda e: e.tensor_scalar(out=SC[:, c0:c0 + n], in0=R[:, 0:n], scalar1=IWt[:, qi, 0:1], scalar2=None, op0=ALU.mult),
                                       r=[rk, iwk], w=["SC"])
                                else:
                                    op("dve", lambda e: e.scalar_tensor_tensor(out=SC[:, c0:c0 + n], in0=R[:, 0:n], scalar=IWt[:, qi, h:h + 1], in1=SC[:, c0:c0 + n],
                                                                                op0=ALU.mult, op1=ALU.add), r=[rk, iwk, "SC"], w=["SC"])
                        op("dve", lambda e: e.tensor_tensor(out=SC[:, Lk - 128:Lk], in0=SC[:, Lk - 128:Lk], in1=adm[:, 0, :], op=ALU.mult), r=["SC", "adm"], w=["SC"])
                        op("dve", lambda e: e.tensor_tensor(out=SC[:, Lk - 128:Lk], in0=SC[:, Lk - 128:Lk], in1=adm[:, 1, :], op=ALU.add), r=["SC", "adm"], w=["SC"])
                        if Lk <= TOPK:
                            op("dve", lambda e: e.tensor_scalar(out=JK[:, 0:Lk], in0=SC[:, 0:Lk], scalar1=-1.0e29, scalar2=None, op0=ALU.is_ge), r=["SC"], w=["JK"])
                        else:
                            op("dve", lambda e: e.max(out=bs[:, 0:8], in_=SC[:, 0:Lk]), r=["SC"], w=["bs_hi"])
                            op("dve", lambda e: e.tensor_reduce(out=bs[:, 8:9], in_=SC[:, 0:Lk - 128], axis=AX.X, op=ALU.min), r=["SC"], w=["bs_lo"])
                            op("dve", lambda e: e.tensor_tensor(out=bs[:, 9:10], in0=bs[:, 0:1], in1=bs[:, 8:9], op=ALU.subtract), r=["bs_hi", "bs_lo"], w=["bs_d"])
                            op("dve", lambda e: e.tensor_scalar(out=bs[:, 9:10], in0=bs[:, 9:10], scalar1=1.001, scalar2=1.0e-20, op0=ALU.mult, op1=ALU.add), r=["bs_d"], w=["bs_d"])
                            op("dve", lambda e: e.tensor_scalar(out=bs[:, 32:32 + NIT], in0=pow2[:, 0:NIT], scalar1=bs[:, 9:10], scalar2=None, op0=ALU.mult), r=["bs_d", "pow2"], w=["bs_dl"])
                            for k in range(NIT):
                                op("dve", lambda e, k=k: e.tensor_tensor(out=bs[:, 10:11], in0=bs[:, 8:9], in1=bs[:, 32 + k:33 + k], op=ALU.add), r=["bs_lo", "bs_dl"], w=["bs_mid"])
                                op("dve", lambda e: e.tensor_scalar(out=JK[:, 0:Lk], in0=SC[:, 0:Lk], scalar1=bs[:, 10:11], scalar2=0.0, op0=ALU.is_ge, op1=ALU.add,
                                                                    accum_out=bs[:, 11:12]), r=["SC", "bs_mid"], w=["JK", "bs_cnt"])
                                op("dve", lambda e, k=k: e.tensor_scalar(out=bs[:, 12:13], in0=bs[:, 11:12], scalar1=TOPK - 0.5, scalar2=bs[:, 32 + k:33 + k], op0=ALU.is_ge, op1=ALU.mult),
                                   r=["bs_cnt", "bs_dl"], w=["bs_t"])
                                op("dve", lambda e: e.tensor_tensor(out=bs[:, 8:9], in0=bs[:, 8:9], in1=bs[:, 12:13], op=ALU.add), r=["bs_lo", "bs_t"], w=["bs_lo"])
                            op("dve", lambda e: e.tensor_tensor(out=bs[:, 13:14], in0=bs[:, 8:9], in1=bs[:, 31 + NIT:32 + NIT], op=ALU.add), r=["bs_lo", "bs_dl"], w=["bs_hf"])
                            op("dve", lambda e: e.tensor_scalar(out=JK[:, 0:Lk], in0=SC[:, 0:Lk], scalar1=bs[:, 8:9], scalar2=0.0, op0=ALU.is_ge, op1=ALU.add,
                                                                accum_out=bs[:, 14:15]), r=["SC", "bs_lo"], w=["JK", "bs_cl"])
                            op("dve", lambda e: e.scalar_tensor_tensor(out=BD[:, 0:Lk], in0=SC[:, 0:Lk], scalar=bs[:, 13:14], in1=JK[:, 0:Lk], op0=ALU.is_lt, op1=ALU.mult,
                                                                        accum_out=bs[:, 15:16]), r=["SC", "JK", "bs_hf"], w=["BD", "bs_nb"])
                            op("dve", lambda e: e.tensor_tensor(out=bs[:, 16:17], in0=bs[:, 15:16], in1=bs[:, 14:15], op=ALU.subtract), r=["bs_nb", "bs_cl"], w=["bs_m"])
                            op("dve", lambda e: e.tensor_scalar(out=bs[:, 16:17], in0=bs[:, 16:17], scalar1=float(TOPK) + 0.5, scalar2=None, op0=ALU.add), r=["bs_m"], w=["bs_m"])
                            op("dve", lambda e: e.tensor_tensor_scan(out=SC[:, 0:Lk], data0=ones_bf[:, 0:1].to_broadcast([128, Lk]), data1=BD[:, 0:Lk], initial=0.0, op0=ALU.mult, op1=ALU.add),
                               r=["BD", "ones_bf", "SC"], w=["SC"])
                            op("dve", lambda e: e.scalar_tensor_tensor(out=BD[:, 0:Lk], in0=SC[:, 0:Lk], scalar=bs[:, 16:17], in1=BD[:, 0:Lk], op0=ALU.is_gt, op1=ALU.mult),
                               r=["SC", "BD", "bs_m"], w=["BD"])
                            op("pool", lambda e: e.tensor_tensor(out=JK[:, 0:Lk], in0=JK[:, 0:Lk], in1=BD[:, 0:Lk], op=ALU.subtract), r=["JK", "BD"], w=["JK"])
                        if debug:
                            dma(DSC[i, :, :], SC[:, :], r=["SC"], slot="dbg1")
                            dma(DBS[i, :, :], bs[:, :], r=["bs_lo", "bs_hi", "bs_d", "bs_dl", "bs_mid", "bs_cnt", "bs_t", "bs_m", "bs_cl", "bs_nb", "bs_hf"], slot="dbg2")
                        for g in range(0, i + 1, 8):
                            g1 = min(i + 1, g + 8)
                            bank = (g // 8) % 2
                            for kb in range(g, g1):
                                op("pe", lambda e, kb=kb: e.transpose(pb[bank][:, (kb - g) * 128:(kb - g + 1) * 128], JK[:, kb * 128:(kb + 1) * 128], ident[:]),
                                   r=["JK", "ident"], w=["pb%d" % bank])
                            nb_ = g1 - g
                            if bank == 0:
                                op("act", lambda e: e.activation(out=selT[:, g:g1, qi * 128:(qi + 1) * 128], in_=pb[bank][:, 0:nb_ * 128].rearrange("p (b t) -> p b t", b=nb_), func=AF.Copy),
                                   r=["pb%d" % bank], w=["selT"])
                            else:
                                op("pool" if False else "dve", lambda e: e.tensor_copy(out=selT[:, g:g1, qi * 128:(qi + 1) * 128], in_=pb[bank][:, 0:nb_ * 128].rearrange("p (b t) -> p b t", b=nb_)),
                                   r=["pb%d" % bank], w=["selT"])
                    if debug:
                        dma(DSEL[j, :, :, :], selT[:, :, :], r=["selT"], slot="dbg3")
                    return selT

                if "C" in MIX:
                    softmax_mixer("C", 14, 1, 12, 2, 8, 1, 4, [([(0, h // 2, (h % 2) * 64, 64)], 0) for h in range(4)], 0.125, None, selT_fn=selT_C, pre_q=pre_C)

                if "A" in MIX:
                    with ExitStack() as ms:
                        Kt = load_K(ms, "Kt", 2, 2)
                        V = load_V(ms, "Vr", 0, 4)
                        nK = sb(ms, "nK", [128, 2, S], BF16)
                        for b in range(2):
                            op("dve" if b == 0 else "pool", lambda e, b=b: e.tensor_scalar(out=nK[:, b, :], in0=Kt[:, b, :], scalar1=-0.125, scalar2=0.0, op0=ALU.mult, op1=ALU.add), r=["Kt"], w=["nK"])
                        Qs = [sb(ms, "Qs%d" % i, [128, 2, 512], BF16) for i in range(2)]
                        Gs = [sb(ms, "Gs%d" % i, [128, 2, 512]) for i in range(2)]
                        NBUF = 3
                        Et = [sb(ms, "Et%d" % i, [128, 512]) for i in range(NBUF)]
                        SPt = [sb(ms, "SPt%d" % i, [128, 512]) for i in range(NBUF)]
                        SPh = [sb(ms, "SPh%d" % i, [128, 512], BF16) for i in range(NBUF)]
                        SPl = [sb(ms, "SPl%d" % i, [128, 512], BF16) for i in range(NBUF)]
                        Cf = [sb(ms, "Cf%d" % i, [128, 512]) for i in range(2)]
                        Ch = [[sb(ms, "Ch%d_%d" % (i, k), [128, 512], BF16) for k in range(2)] for i in range(2)]
                        Cl = [[sb(ms, "Cl%d_%d" % (i, k), [128, 512], BF16) for k in range(2)] for i in range(2)]
                        PT = [sb(ms, "PT%d" % i, [128, 512], BF16) for i in range(3)]
                        YS = [sb(ms, "YS%d" % i, [128, 2, 512], BF16) for i in range(2)]
                        gc = {"g1": 0, "g2": 0}

                        def load_qA(j):
                            q0 = j * 512
                            dma(Qs[j % 2][:], FT[0:2, :, q0:q0 + 512].rearrange("b p t -> p b t"), w=["Qs%d" % (j % 2)], slot="Qs%d" % (j % 2))
                            dma(Gs[j % 2][:], GT[0:2, :, q0:q0 + 512].rearrange("b p t -> p b t"), w=["Gs%d" % (j % 2)], slot="Gs%d" % (j % 2))

                        load_qA(0)
                        for j in range(NQ):
                            if j + 1 < NQ:
                                load_qA(j + 1)
                            Qk, Gk = "Qs%d" % (j % 2), "Gs%d" % (j % 2)
                            Q, G = Qs[j % 2], Gs[j % 2]
                            nkt = 4 * (j + 1)
                            ysk = "YS%d" % (j % 2)
                            for pair in range(2):
                                seq = [(kt, sl) for kt in range(nkt - 1, -1, -1) for sl in range(2)]
                                info = {}
                                cidx = [0, 0]

                                def stage1(idx):
                                    kt, sl = seq[idx]
                                    h = 2 * pair + sl
                                    blk, base = h // 2, (h % 2) * 64
                                    b1 = gc["g1"] % NBUF
                                    zb = gc["g1"] % 2
                                    gc["g1"] += 1
                                    info[idx] = b1
                                    r_ = kt - (nkt - 4)
                                    ksl = slice(kt * 128, (kt + 1) * 128)
                                    op("pe", lambda e: e.matmul(pf[zb][:, :], lhsT=Kt[base:base + 64, blk, ksl], rhs=Q[base:base + 64, blk, :], start=True, stop=True),
                                       r=["Kt", Qk], w=["pf%d" % zb])
                                    op("act", lambda e: e.activation(out=Et[b1][:], in_=pf[zb][:, :], func=AF.Exp, scale=0.125), r=["pf%d" % zb], w=["Et%d" % b1])
                                    op("act", lambda e: e.activation(out=SPt[b1][:], in_=Et[b1][:], func=AF.Ln, bias=1.0, scale=1.0), r=["Et%d" % b1], w=["SPt%d" % b1])
                                    if r_ >= 0:
                                        op("dve", lambda e: e.tensor_tensor(out=SPt[b1][:], in0=SPt[b1][:], in1=masks[:, 4 + r_, :], op=ALU.mult), r=["SPt%d" % b1, "masks"], w=["SPt%d" % b1])
                                    op("dve", lambda e: e.tensor_copy(out=SPh[b1][:], in_=SPt[b1][:]), r=["SPt%d" % b1], w=["SPh%d" % b1])
                                    op("dve", lambda e: e.tensor_tensor(out=SPl[b1][:], in0=SPt[b1][:], in1=SPh[b1][:], op=ALU.subtract), r=["SPt%d" % b1, "SPh%d" % b1], w=["SPl%d" % b1])

                                def stage2(idx):
                                    kt, sl = seq[idx]
                                    h = 2 * pair + sl
                                    blk, base = h // 2, (h % 2) * 64
                                    b1 = info.pop(idx)
                                    rb = 2 + gc["g2"] % 2
                                    p2 = gc["g2"] % 3
                                    gc["g2"] += 1
                                    ob = 4 + sl
                                    first = (kt == nkt - 1)
                                    r_ = kt - (nkt - 4)
                                    ksl = slice(kt * 128, (kt + 1) * 128)
                                    op("pe", lambda e: e.matmul(pf[rb][:, :], lhsT=tri[:], rhs=SPh[b1][:], start=True, stop=False), r=["tri", "SPh%d" % b1], w=["pf%d" % rb])
                                    op("pe", lambda e: e.matmul(pf[rb][:, :], lhsT=tri[:], rhs=SPl[b1][:], start=False, stop=False), r=["tri", "SPl%d" % b1], w=["pf%d" % rb])
                                    if not first:
                                        cu = cidx[sl] % 2
                                        op("pe", lambda e: e.matmul(pf[rb][:, :], lhsT=ones_bf[:], rhs=Ch[sl][cu][:], start=False, stop=False), r=["ones_bf", "Ch%d_%d" % (sl, cu)], w=["pf%d" % rb])
                                        op("pe", lambda e: e.matmul(pf[rb][:, :], lhsT=ones_bf[:], rhs=Cl[sl][cu][:], start=False, stop=False), r=["ones_bf", "Cl%d_%d" % (sl, cu)], w=["pf%d" % rb])
                                    op("pe", lambda e: e.matmul(pf[rb][:, :], lhsT=nK[base:base + 64, blk, ksl], rhs=Q[base:base + 64, blk, :], start=False, stop=True),
                                       r=["nK", Qk], w=["pf%d" % rb])
                                    if kt > 0:
                                        cidx[sl] += 1
                                        cw = cidx[sl] % 2
                                        if first:
                                            op("pool", lambda e: e.tensor_copy(out=Cf[sl][:], in_=SPt[b1][:]), r=["SPt%d" % b1], w=["Cf%d" % sl])
                                        else:
                                            op("pool", lambda e: e.tensor_tensor(out=Cf[sl][:], in0=Cf[sl][:], in1=SPt[b1][:], op=ALU.add), r=["SPt%d" % b1, "Cf%d" % sl], w=["Cf%d" % sl])
                                        op("act", lambda e: e.activation(out=Ch[sl][cw][:], in_=Cf[sl][:], func=AF.Copy), r=["Cf%d" % sl], w=["Ch%d_%d" % (sl, cw)])
                                        op("pool", lambda e: e.tensor_tensor(out=Cl[sl][cw][:], in0=Cf[sl][:], in1=Ch[sl][cw][:], op=ALU.subtract), r=["Cf%d" % sl, "Ch%d_%d" % (sl, cw)], w=["Cl%d_%d" % (sl, cw)])
                                    op("act", lambda e: e.activation(out=PT[p2][:], in_=pf[rb][:, :], func=AF.Exp, scale=-1.0), r=["pf%d" % rb], w=["PT%d" % p2])
                                    if r_ >= 0:
                                        op("dve", lambda e: e.tensor_tensor(out=PT[p2][:], in0=PT[p2][:], in1=masks[:, 4 + r_, :], op=ALU.mult), r=["PT%d" % p2, "masks"], w=["PT%d" % p2])
                                    op("pe", lambda e: e.matmul(pf[ob][0:64, :], lhsT=V[:, kt, h, 0:64], rhs=PT[p2][:], start=first, stop=(kt == 0)), r=["Vr", "PT%d" % p2], w=["pf%d" % ob])

                                LA = 2
                                for idx in range(len(seq) + LA):
                                    if idx < len(seq):
                                        stage1(idx)
                                    if idx >= LA:
                                        stage2(idx - LA)
                                for sl in range(2):
                                    h = 2 * pair + sl
                                    blk, base = h // 2, (h % 2) * 64
                                    ob = 4 + sl
                                    op("dve", lambda e: e.tensor_tensor(out=YS[j % 2][base:base + 64, blk, :], in0=pf[ob][0:64, :], in1=G[base:base + 64, blk, :], op=ALU.mult),
                                       r=["pf%d" % ob, Gk], w=[ysk])
                            q0 = j * 512
                            dma(YT[0:2, :, q0:q0 + 512].rearrange("b p t -> p b t"), YS[j % 2][:], r=[ysk], slot=ysk)
                        sc.barrier()

                if DO_OUT:
                    with ExitStack() as ms:
                        wo = sb(ms, "wo", [128, 8, D_MODEL], BF16)
                        wos = [sb(ms, "wos%d" % i, [128, D_MODEL]) for i in range(2)]
                        for c in range(8):
                            k = "wos%d" % (c % 2)
                            dma(wos[c % 2][:], w_out[l, c * 128:(c + 1) * 128, :], w=[k], slot=k)
                            op("dve" if c % 2 == 0 else "pool", lambda e, c=c: e.tensor_copy(out=wo[:, c, :], in_=wos[c % 2][:]), r=[k], w=["wo"])
                        YTs = [sb(ms, "YTs%d" % i, [128, 8, 512], BF16) for i in range(2)]
                        xs = [sb(ms, "xs%d" % i, [128, D_MODEL]) for i in range(2)]
                        xo = [sb(ms, "xo%d" % i, [128, D_MODEL]) for i in range(2)]

                        def load_y(stl):
                            q0 = stl * 512
                            for g in range(0, 8, 4):
                                dma(YTs[stl % 2][:, g:g + 4, :], YT[g:g + 4, :, q0:q0 + 512].rearrange("b p t -> p b t"), w=["YTs%d" % (stl % 2)], slot="YTs%d" % (stl % 2))

                        load_y(0)
                        for stl in range(NST):
                            if stl + 1 < NST:
                                load_y(stl + 1)
                            for sub in range(4):
                                t = stl * 4 + sub
                                xk, ok = "xs%d" % (t % 2), "xo%d" % (t % 2)
                                dma(xs[t % 2][:], x_src[t * 128:(t + 1) * 128, :], w=[xk], slot=xk)
                                for half in range(2):
                                    bank = (t * 2 + half) % 2
                                    for c in range(8):
                                        op("pe", lambda e, c=c: e.matmul(pf[bank][:, :], lhsT=YTs[stl % 2][:, c, sub * 128:(sub + 1) * 128], rhs=wo[:, c, half * 512:(half + 1) * 512],
                                                                         start=(c == 0), stop=(c == 7)), r=["YTs%d" % (stl % 2), "wo"], w=["pf%d" % bank])
                                    op("dve", lambda e: e.tensor_tensor(out=xo[t % 2][:, half * 512:(half + 1) * 512], in0=pf[bank][:, :], in1=xs[t % 2][:, half * 512:(half + 1) * 512], op=ALU.add),
                                       r=["pf%d" % bank, xk], w=[ok])
                                dma(x_dst[t * 128:(t + 1) * 128, :], xo[t % 2][:], r=[ok], slot=ok)
                        sc.barrier()
        sc.barrier()
        print("instructions:", sc.n_ins)
    return nc


def host_consts(S):
    bf = ml_dtypes.bfloat16
    c = {}
    c["c_ident"] = np.eye(128, dtype=np.float32).astype(bf)

    def cs(d):
        inv = (10000.0 ** (-np.arange(0, d, 2, dtype=np.float32) / np.float32(d))).astype(np.float32)
        ang = (np.arange(S, dtype=np.float32)[:, None] * inv[None, :]).astype(np.float32)
        return np.concatenate([np.cos(ang), np.sin(ang)], axis=1).astype(np.float32)

    c["c_cs64"] = cs(64)
    c["c_cs32"] = cs(32)
    kk = np.arange(128)[:, None]
    qq = np.arange(512)[None, :]
    m = []
    for r in range(4):
        m.append(((r * 128 + kk) < ((qq // 64) + 1) * 64))
    for r in range(4):
        m.append(((r * 128 + kk) < qq))
    c["c_mask"] = np.concatenate(m, axis=1).astype(np.float32).astype(bf)
    q1 = np.arange(128)[:, None]
    k1 = np.arange(128)[None, :]
    m01 = (k1 < ((q1 // 64) + 1) * 64).astype(np.float32)
    c["c_adm"] = np.concatenate([m01, (1.0 - m01) * NEGBIG], axis=1).astype(np.float32)
    c["c_tri"] = (np.arange(128)[:, None] >= np.arange(128)[None, :]).astype(np.float32).astype(bf)
    c["c_pow2"] = np.tile((2.0 ** -(np.arange(32) + 1.0))[None, :], (128, 1)).astype(np.float32)
    return c


def host_params(inp, DEPTH):
    f = lambda a: np.ascontiguousarray(np.asarray(a, dtype=np.float32)[:DEPTH])
    p = {}
    p["w_in"] = np.ascontiguousarray(f(inp["w_in"])[:, :, PERM])
    p["w_out"] = f(inp["w_out"])
    p["w_uq"] = f(inp["mla_w_uq"])
    p["w_ukv"] = f(inp["mla_w_ukv"])
    p["ln_g"] = np.ascontiguousarray(f(inp["ln_g"]).reshape(-1, 8, 128).transpose(0, 2, 1))
    p["qn_g"] = np.ascontiguousarray(f(inp["mla_q_norm_g"]).reshape(-1, 2, 128).transpose(0, 2, 1))
    p["kvn_g"] = np.ascontiguousarray(f(inp["mla_kv_norm_g"]).reshape(-1, 128, 1))
    dq, dkk = f(inp["dsa_q_g"]), f(inp["dsa_k_g"])
    p["g64"] = np.ascontiguousarray(np.concatenate([dq] * 4 + [dkk], axis=1))
    fq, fk, mk = f(inp["diff_q_g"]), f(inp["diff_k_g"]), f(inp["mla_k_g"])
    p["g32"] = np.ascontiguousarray(np.concatenate([fq] * 8 + [fk] * 8 + [mk[:, 64:96]], axis=1))
    p["gq96"] = f(inp["mla_q_g"])
    p["gk64"] = np.ascontiguousarray(mk[:, :64])
    p["subln"] = np.ascontiguousarray(f(inp["diff_subln_g"]).reshape(-1, 64, 1))
    p["lvec"] = np.ascontiguousarray(np.concatenate([f(inp["diff_lq1"]), f(inp["diff_lk1"]), f(inp["diff_lq2"]), f(inp["diff_lk2"])], axis=1))
    return p


_CACHE = {}


def run(inputs, S, DEPTH, debug=False):
    import math
    key = (S, DEPTH, debug)
    lam_inits = [0.8 - 0.6 * math.exp(-0.3 * l) for l in range(DEPTH)]
    if key not in _CACHE:
        _CACHE[key] = build(S, DEPTH, debug, lam_inits, MIX=globals().get("MIXSEL", "ABCD"))
    nc = _CACHE[key]
    x = np.asarray(inputs["x"], dtype=np.float32)
    B = x.shape[0]
    shared = {}
    shared.update(host_consts(S))
    shared.update(host_params(inputs, DEPTH))
    in_maps = []
    for b in range(B):
        m = dict(shared)
        m["x"] = np.ascontiguousarray(x[b, :S])
        in_maps.append(m)
    res = run_bass_kernel_spmd(nc, in_maps, core_ids=list(range(B)))
    return res


def kernel(**inputs):
    res = run(inputs, 8192, 4)
    return np.stack([np.asarray(r["y"], dtype=np.float32) for r in res.results], axis=0)
```

```python
import numpy as np
import ml_dtypes
from contextlib import ExitStack
import concourse.bass as bass
import concourse.mybir as mybir
from concourse.bass_utils import run_bass_kernel_spmd

F32 = mybir.dt.float32
BF16 = mybir.dt.bfloat16
AF = mybir.ActivationFunctionType
ALU = mybir.AluOpType
AX = mybir.AxisListType

D_MODEL = 1024
IN_COLS = 3684
EPS = 1e-6
NEGBIG = -1.0e30

O_AQ, O_AK, O_AV, O_AG = 0, 256, 512, 768
O_BCQ, O_BCKV, O_BKR, O_BG = 1024, 1280, 1408, 1440
O_CQ, O_CK, O_CV, O_CG, O_CIQ, O_CIK, O_CIW = 1696, 1952, 2016, 2080, 2336, 2592, 2656
O_DQ, O_DK, O_DV, O_DG = 2660, 2916, 3172, 3428


def _perm():
    r = lambda a, n: list(range(a, a + n))
    p = []
    p += r(O_AQ, 256) + r(O_AK, 256)
    p += r(O_AG, 256) + r(O_BG, 256) + r(O_CG, 256) + r(O_DG, 256)
    p += r(O_AV, 256) + r(O_DV, 256) + r(O_CV, 64)
    p += r(O_CQ, 256) + r(O_CK, 64) + r(O_CIQ, 256) + r(O_CIK, 64)
    p += r(O_DQ, 256) + r(O_DK, 256) + r(O_BKR, 32)
    p += r(O_BCQ, 256) + r(O_BCKV, 128)
    p += r(O_CIW, 4)
    assert len(p) == IN_COLS and sorted(p) == list(range(IN_COLS))
    return np.array(p)


PERM = _perm()
T0 = 1536
NTM = IN_COLS - T0
P_AV, P_DV, P_CV = 0, 256, 512
P_X64 = 576
P_Y32 = 1216
P_CQ = 1760
P_CKV = 2016
P_IW = 2144
NB_FT = 26


class Sched:
    ENG = ("pe", "act", "dve", "pool", "sp")

    def __init__(self, nc, es):
        self.nc = nc
        self.es = es
        self.e = dict(pe=nc.tensor, act=nc.scalar, dve=nc.vector, pool=nc.gpsimd, sp=nc.sync)
        self.semobj = {}
        self.cnt = {}
        for k in ("pe", "act", "dve", "pool"):
            self.semobj[k] = es.enter_context(nc.semaphore("s_" + k))
            self.cnt[k] = 0
        self.known = {k: {} for k in self.ENG}
        self.lastw = {}
        self.reads = {}
        self.n_ins = 0

    def _dsem(self, slot):
        name = "d_" + slot
        if name not in self.semobj:
            self.semobj[name] = self.es.enter_context(self.nc.semaphore(name))
            self.cnt[name] = 0
        return name

    def _waits(self, E, r, w, attach_ok=False):
        need = {}

        def add(tok, kind):
            sname, val, prod = tok
            if prod == E:
                if E == "pe" or kind == "war":
                    return
            if self.known[E].get(sname, 0) >= val:
                return
            if need.get(sname, 0) < val:
                need[sname] = val

        for k in r:
            t = self.lastw.get(k)
            if t is not None:
                add(t, "raw")
        for k in w:
            t = self.lastw.get(k)
            if t is not None:
                add(t, "waw")
            for t in self.reads.get(k, {}).values():
                add(t, "war")
        items = list(need.items())
        attach = None
        if attach_ok and items:
            attach = items.pop()
        for sname, val in items:
            self.e[E].wait_ge(self.semobj[sname], val)
            self.known[E][sname] = val
            self.n_ins += 1
        if attach is not None:
            self.known[E][attach[0]] = attach[1]
        return attach

    def _record(self, tok, r, w):
        for k in r:
            d = self.reads.setdefault(k, {})
            d[tok[0]] = tok
        for k in w:
            self.lastw[k] = tok
            self.reads[k] = {}

    def op(self, E, fn, r=(), w=()):
        att = self._waits(E, r, w, attach_ok=True)
        ins = fn(self.e[E])
        if att is not None:
            ins._wait_ge(self.semobj[att[0]], att[1])
        self.cnt[E] += 1
        ins.then_inc(self.semobj[E], 1)
        tok = (E, self.cnt[E], E)
        self._record(tok, r, w)
        self.n_ins += 1
        return tok

    def dma(self, out, in_, r=(), w=(), slot=None, Q="sp"):
        self._waits(Q, r, w)
        sname = self._dsem(slot)
        ins = self.e[Q].dma_start(out=out, in_=in_)
        self.cnt[sname] += 16
        ins.then_inc(self.semobj[sname], 16)
        tok = (sname, self.cnt[sname], "dma")
        self._record(tok, r, w)
        self.n_ins += 1
        return tok

    def barrier(self, engines=None):
        for E in (engines or self.ENG):
            for sname, c in self.cnt.items():
                if c == 0 or self.known[E].get(sname, 0) >= c:
                    continue
                if sname == E and E == "pe":
                    continue
                self.e[E].wait_ge(self.semobj[sname], c)
                self.known[E][sname] = c
                self.n_ins += 1
        if engines is None:
            self.lastw = {}
            self.reads = {}


def bc(ap, shape, axis):
    return ap.unsqueeze(axis).to_broadcast(list(shape))


def build(S, DEPTH, debug=False, lam_inits=None, MIX="ABCD", DO_OUT=True):
    TOPK = min(256, S // 4)
    NT = S // 128
    NST = S // 512
    nc = bass.Bass("TRN2", target_bir_lowering=False)

    def din(name, shape, dt=F32):
        return nc.dram_tensor(name, list(shape), dt, kind="ExternalInput").ap()

    x_in = din("x", [S, D_MODEL])
    w_in = din("w_in", [DEPTH, D_MODEL, IN_COLS])
    w_out = din("w_out", [DEPTH, D_MODEL, D_MODEL])
    w_uq = din("w_uq", [DEPTH, 256, 384])
    w_ukv = din("w_ukv", [DEPTH, 128, 512])
    ln_g = din("ln_g", [DEPTH, 128, 8])
    qn_g = din("qn_g", [DEPTH, 128, 2])
    kvn_g = din("kvn_g", [DEPTH, 128, 1])
    g64 = din("g64", [DEPTH, 5 * 64])
    g32 = din("g32", [DEPTH, 17 * 32])
    gq96 = din("gq96", [DEPTH, 96])
    gk64 = din("gk64", [DEPTH, 64])
    subln = din("subln", [DEPTH, 64, 1])
    lvec = din("lvec", [DEPTH, 4 * 32])
    c_ident = din("c_ident", [128, 128], BF16)
    c_cs64 = din("c_cs64", [S, 64])
    c_cs32 = din("c_cs32", [S, 32])
    c_mask = din("c_mask", [128, 8 * 512], BF16)
    c_adm = din("c_adm", [128, 2 * 128])
    c_tri = din("c_tri", [128, 128], BF16)
    c_pow2 = din("c_pow2", [128, 32])

    okind = "ExternalOutput"
    y_out = nc.dram_tensor("y", [S, D_MODEL], F32, kind=okind).ap()
    dk = okind if debug else "Internal"
    FT = nc.dram_tensor("FT", [NB_FT, 128, S], BF16, kind=dk).ap()
    GT = nc.dram_tensor("GT", [8, 128, S], F32, kind=dk).ap()
    VA = nc.dram_tensor("VA", [S, 13, 128], BF16, kind=dk).ap()
    IW = nc.dram_tensor("IW", [S, 4], F32, kind=dk).ap()
    YT = nc.dram_tensor("YT", [8, 128, S], BF16, kind=dk).ap()
    XR = nc.dram_tensor("XR", [S, D_MODEL], F32, kind="Internal").ap()
    if debug:
        DSEL = nc.dram_tensor("DSEL", [S // 512, 128, S // 128, 512], BF16, kind=okind).ap()
        DSC = nc.dram_tensor("DSC", [S // 128, 128, S], F32, kind=okind).ap()
        DBS = nc.dram_tensor("DBS", [S // 128, 128, 64], F32, kind=okind).ap()

    with ExitStack() as es:
        sc = Sched(nc, es)
        op, dma = sc.op, sc.dma

        uniq = [0]

        def sb(st, name, shape, dt=F32):
            uniq[0] += 1
            return st.enter_context(nc.sbuf_tensor("%s_%d" % (name, uniq[0]), list(shape), dt))

        def ps(st, name, shape, dt=F32):
            return st.enter_context(nc.psum_tensor(name, list(shape), dt))

        ident = sb(es, "ident", [128, 128], BF16)
        masks = sb(es, "masks", [128, 8, 512], BF16)
        adm = sb(es, "adm", [128, 2, 128])
        tri = sb(es, "tri", [128, 128], BF16)
        ones_bf = sb(es, "ones_bf", [128, 128], BF16)
        ones_f = sb(es, "ones_f", [128, 128])
        pow2 = sb(es, "pow2", [128, 32])
        neghalf = sb(es, "neghalf", [128, 32])
        epst = sb(es, "epst", [128, 1])
        nh512 = sb(es, "nh512", [128, 512])
        dma(ident[:], c_ident[:, :], w=["ident"], slot="k_ident")
        dma(masks[:].rearrange("p a b -> p (a b)"), c_mask[:, :], w=["masks"], slot="k_masks")
        dma(adm[:].rearrange("p a b -> p (a b)"), c_adm[:, :], w=["adm"], slot="k_adm")
        dma(tri[:], c_tri[:, :], w=["tri"], slot="k_tri")
        dma(pow2[:], c_pow2[:, :], w=["pow2"], slot="k_pow2")
        op("dve", lambda e: e.memset(ones_bf[:], 1.0), w=["ones_bf"])
        op("dve", lambda e: e.memset(ones_f[:], 1.0), w=["ones_f"])
        op("dve", lambda e: e.memset(neghalf[:], -0.5), w=["neghalf"])
        op("dve", lambda e: e.memset(epst[:], EPS), w=["epst"])
        op("dve", lambda e: e.memset(nh512[:], -0.5), w=["nh512"])

        pf = [ps(es, "pf%d" % i, [128, 512]) for i in range(6)]
        pb = [ps(es, "pb%d" % i, [128, 1024], BF16) for i in range(2)]

        for l in range(DEPTH):
            x_src = x_in if l == 0 else XR
            x_dst = y_out if l == DEPTH - 1 else XR
            lam_init = float(lam_inits[l])
            with ExitStack() as ls:
                sublnt = sb(ls, "sublnt", [64, 1])
                lv = sb(ls, "lv", [1, 4, 32])
                lam_bc = sb(ls, "lam_bc", [128, 2])
                dma(sublnt[:], subln[l, :, :], w=["sublnt"], slot="k_sublnt")
                dma(lv[:].rearrange("p a b -> p (a b)"), lvec[l:l + 1, :], w=["lv"], slot="k_lv")
                op("dve", lambda e: e.tensor_scalar(out=sublnt[:], in0=sublnt[:], scalar1=1.0 - lam_init, scalar2=None,
                                                    op0=ALU.mult), r=["sublnt"], w=["sublnt"])
                lt = sb(ls, "lt", [1, 2, 32])
                l2 = sb(ls, "l2", [1, 4])
                op("dve", lambda e: e.tensor_tensor(out=lt[:, 0, :], in0=lv[:, 0, :], in1=lv[:, 1, :], op=ALU.mult), r=["lv"], w=["lt"])
                op("dve", lambda e: e.tensor_tensor(out=lt[:, 1, :], in0=lv[:, 2, :], in1=lv[:, 3, :], op=ALU.mult), r=["lv", "lt"], w=["lt"])
                op("dve", lambda e: e.tensor_reduce(out=l2[:, 0:2], in_=lt[:], axis=AX.X, op=ALU.add), r=["lt"], w=["l2"])
                op("act", lambda e: e.activation(out=l2[:, 0:2], in_=l2[:, 0:2], func=AF.Exp), r=["l2"], w=["l2"])
                op("dve", lambda e: e.tensor_tensor(out=l2[:, 2:3], in0=l2[:, 0:1], in1=l2[:, 1:2], op=ALU.subtract), r=["l2"], w=["l2"])
                op("dve", lambda e: e.tensor_scalar(out=l2[:, 3:4], in0=l2[:, 2:3], scalar1=lam_init, scalar2=None, op0=ALU.add), r=["l2"], w=["l2"])
                op("dve", lambda e: e.tensor_scalar(out=l2[:, 2:3], in0=l2[:, 3:4], scalar1=-1.0, scalar2=None, op0=ALU.mult), r=["l2"], w=["l2"])
                op("pe", lambda e: e.matmul(pf[0][:, 0:2], lhsT=ones_f[0:1, :], rhs=l2[0:1, 2:4], start=True, stop=True),
                   r=["l2", "ones_f"], w=["pf0"])
                op("dve", lambda e: e.tensor_copy(out=lam_bc[:], in_=pf[0][:, 0:2]), r=["pf0"], w=["lam_bc"])

                with ExitStack() as p1:
                    w_sb = sb(p1, "w_sb", [128, 8, IN_COLS], BF16)
                    g64t = sb(p1, "g64t", [128, 5, 64])
                    g32t = sb(p1, "g32t", [128, 17, 32])
                    gq96t = sb(p1, "gq96t", [128, 96])
                    gk64t = sb(p1, "gk64t", [128, 64])
                    dma(g64t[:].rearrange("p a b -> p (a b)"), g64[l:l + 1, :].to_broadcast([128, 320]), w=["g64t"], slot="k_g64t")
                    dma(g32t[:].rearrange("p a b -> p (a b)"), g32[l:l + 1, :].to_broadcast([128, 544]), w=["g32t"], slot="k_g32t")
                    dma(gq96t[:], gq96[l:l + 1, :].to_broadcast([128, 96]), w=["gq96t"], slot="k_gq96t")
                    dma(gk64t[:], gk64[l:l + 1, :].to_broadcast([128, 64]), w=["gk64t"], slot="k_gk64t")
                    wuq_sb = sb(p1, "wuq_sb", [128, 2, 384], BF16)
                    wukv_sb = sb(p1, "wukv_sb", [128, 512], BF16)
                    lng = sb(p1, "lng", [128, 8])
                    qng = sb(p1, "qng", [128, 2])
                    kvng = sb(p1, "kvng", [128, 1])
                    dma(lng[:], ln_g[l, :, :], w=["lng"], slot="k_lng")
                    dma(qng[:], qn_g[l, :, :], w=["qng"], slot="k_qng")
                    dma(kvng[:], kvn_g[l, :, :], w=["kvng"], slot="k_kvng")
                    with ExitStack() as p0:
                        wst = [sb(p0, "wst%d" % i, [128, IN_COLS]) for i in range(2)]
                        for c in range(8):
                            k = "wst%d" % (c % 2)
                            dma(wst[c % 2][:], w_in[l, c * 128:(c + 1) * 128, :], w=[k], slot=k)
                            eng = "dve" if c % 2 == 0 else "pool"
                            op(eng, lambda e, c=c: e.tensor_scalar(out=w_sb[:, c, :], in0=wst[c % 2][:], scalar1=lng[:, c:c + 1],
                                                                    scalar2=0.0, op0=ALU.mult, op1=ALU.add), r=[k, "lng"], w=["w_sb"])
                        for c in range(2):
                            dma(wst[c][:, 0:384], w_uq[l, c * 128:(c + 1) * 128, :], w=["wst%d" % c], slot="wst%d" % c)
                            op("dve", lambda e, c=c: e.tensor_scalar(out=wuq_sb[:, c, :], in0=wst[c][:, 0:384], scalar1=qng[:, c:c + 1],
                                                                      scalar2=None, op0=ALU.mult), r=["wst%d" % c, "qng"], w=["wuq_sb"])
                        dma(wst[0][:, 0:512], w_ukv[l, :, :], w=["wst0"], slot="wst0")
                        op("dve", lambda e: e.tensor_scalar(out=wukv_sb[:], in0=wst[0][:, 0:512], scalar1=kvng[:, 0:1],
                                                            scalar2=None, op0=ALU.mult), r=["wst0", "kvng"], w=["wukv_sb"])
                        sc.barrier()
                    xt = [sb(p1, "xt%d" % i, [128, D_MODEL]) for i in range(2)]
                    junk = sb(p1, "junk", [128, D_MODEL], BF16)
                    hb = sb(p1, "hb", [128, D_MODEL], BF16)
                    hT = sb(p1, "hT", [128, 8, 512], BF16)
                    proj = sb(p1, "proj", [128, NTM])
                    tmp = sb(p1, "tmp", [128, 640])
                    tmp2 = sb(p1, "tmp2", [128, 640])
                    st8 = sb(p1, "st8", [128, 64])
                    cs64 = [sb(p1, "cs64_%d" % i, [128, 64]) for i in range(2)]
                    cs32 = [sb(p1, "cs32_%d" % i, [128, 32]) for i in range(2)]
                    TT = sb(p1, "TT", [128, 22, 128], BF16)
                    VS = sb(p1, "VS", [128, 13, 128], BF16)
                    IWs = sb(p1, "IWs", [128, 4])
                    QB = sb(p1, "QB", [128, 4, 96])
                    KV = sb(p1, "KV", [128, 4, 128])
                    cqn = sb(p1, "cqn", [128, 384], BF16)
                    cqT = sb(p1, "cqT", [128, 3, 128], BF16)
                    FTs = sb(p1, "FTs", [128, NB_FT, 512], BF16)
                    GS = sb(p1, "GS", [128, 8, 512])
                    op("pool", lambda e: e.memset(TT[:], 0.0), w=["TT"])
                    op("pool", lambda e: e.memset(VS[:], 1.0), w=["VS"])

                    def load_x(t):
                        k = "xt%d" % (t % 2)
                        dma(xt[t % 2][:], x_src[t * 128:(t + 1) * 128, :], w=[k], slot=k)
                        dma(cs64[t % 2][:], c_cs64[t * 128:(t + 1) * 128, :], w=["cs64_%d" % (t % 2)], slot="cs64_%d" % (t % 2))
                        dma(cs32[t % 2][:], c_cs32[t * 128:(t + 1) * 128, :], w=["cs32_%d" % (t % 2)], slot="cs32_%d" % (t % 2))

                    def rstd_of(ss_ap, n, d, key):
                        op("act", lambda e: e.activation(out=ss_ap, in_=ss_ap, func=AF.Ln, scale=1.0 / d, bias=epst[:, 0:1]), r=[key, "epst"], w=[key])
                        op("act", lambda e: e.activation(out=ss_ap, in_=ss_ap, func=AF.Exp, scale=-0.5), r=[key], w=[key])

                    load_x(0)
                    for stl in range(NST):
                        for sub in range(4):
                            t = stl * 4 + sub
                            if t + 1 < NT:
                                load_x(t + 1)
                            xk = "xt%d" % (t % 2)
                            X = xt[t % 2]
                            c64k, c32k = "cs64_%d" % (t % 2), "cs32_%d" % (t % 2)
                            C64, C32 = cs64[t % 2], cs32[t % 2]
                            op("act", lambda e: e.activation(out=junk[:], in_=X[:], func=AF.Square, accum_out=st8[:, 0:1]),
                               r=[xk], w=["junk", "st_x"])
                            rstd_of(st8[:, 0:1], 1, D_MODEL, "st_x")
                            op("dve", lambda e: e.tensor_scalar(out=hb[:], in0=X[:], scalar1=st8[:, 0:1], scalar2=None, op0=ALU.mult),
                               r=[xk, "st_x"], w=["hb"])
                            for c in range(8):
                                op("pe", lambda e, c=c: e.transpose(pb[0][:, c * 128:(c + 1) * 128], hb[:, c * 128:(c + 1) * 128], ident[:]),
                                   r=["hb", "ident"], w=["pb0"])
                            op("act", lambda e: e.activation(out=hT[:, :, sub * 128:(sub + 1) * 128],
                                                             in_=pb[0][:].rearrange("p (c t) -> p c t", c=8), func=AF.Copy),
                               r=["pb0"], w=["hT%d" % sub])
                            col = 0
                            ci = 0
                            while col < NTM:
                                n = min(512, NTM - col)
                                bank = 1 + (ci % 2)
                                for c in range(8):
                                    op("pe", lambda e, c=c, col=col, n=n, bank=bank: e.matmul(
                                        pf[bank][:, 0:n], lhsT=hT[:, c, sub * 128:(sub + 1) * 128], rhs=w_sb[:, c, T0 + col:T0 + col + n],
                                        start=(c == 0), stop=(c == 7)), r=["hT%d" % sub, "w_sb"], w=["pf%d" % bank])
                                eng = "act" if ci % 2 == 0 else "dve"
                                if eng == "act":
                                    op("act", lambda e, col=col, n=n, bank=bank: e.activation(out=proj[:, col:col + n], in_=pf[bank][:, 0:n], func=AF.Copy),
                                       r=["pf%d" % bank], w=["proj%d" % ci])
                                else:
                                    op("dve", lambda e, col=col, n=n, bank=bank: e.tensor_copy(out=proj[:, col:col + n], in_=pf[bank][:, 0:n]),
                                       r=["pf%d" % bank], w=["proj%d" % ci])
                                col += n
                                ci += 1
                            PJ = ["proj%d" % i for i in range(ci)]
                            op("pool", lambda e: e.tensor_copy(out=VS[:, 0:4, 0:64], in_=proj[:, P_AV:P_AV + 256].rearrange("p (h d) -> p h d", h=4)), r=PJ, w=["VS"])
                            op("pool", lambda e: e.tensor_copy(out=VS[:, 9:13, 0:64], in_=proj[:, P_DV:P_DV + 256].rearrange("p (h d) -> p h d", h=4)), r=PJ, w=["VS"])
                            op("pool", lambda e: e.tensor_copy(out=VS[:, 8, 0:64], in_=proj[:, P_CV:P_CV + 64]), r=PJ, w=["VS"])
                            op("pool", lambda e: e.tensor_scalar(out=IWs[:], in0=proj[:, P_IW:P_IW + 4], scalar1=0.0625, scalar2=0.0, op0=ALU.mult, op1=ALU.add), r=PJ, w=["IWs"])
                            X64 = proj[:, P_X64:P_X64 + 640].rearrange("p (h d) -> p h d", h=10)
                            T3 = tmp[:].rearrange("p (h d) -> p h d", h=10)
                            T3b = tmp2[:].rearrange("p (h d) -> p h d", h=10)
                            op("act", lambda e: e.activation(out=T3[:, 0:5, :], in_=X64[:, 0:5, :], func=AF.Square), r=PJ, w=["tmp"])
                            op("dve", lambda e: e.tensor_reduce(out=st8[:, 8:13], in_=T3[:, 0:5, :], axis=AX.X, op=ALU.add), r=["tmp"], w=["st_a"])
                            rstd_of(st8[:, 8:13], 5, 64, "st_a")
                            op("dve", lambda e: e.tensor_tensor(out=X64[:, 0:5, :], in0=X64[:, 0:5, :], in1=bc(st8[:, 8:13], [128, 5, 64], 2), op=ALU.mult),
                               r=PJ + ["st_a"], w=PJ)
                            op("dve", lambda e: e.tensor_tensor(out=X64[:, 0:5, :], in0=X64[:, 0:5, :], in1=g64t[:], op=ALU.mult), r=PJ + ["g64t"], w=PJ)
                            cosb = bc(C64[:, 0:32], [128, 10, 32], 1)
                            sinb = bc(C64[:, 32:64], [128, 10, 32], 1)
                            op("dve", lambda e: e.tensor_tensor(out=T3[:, :, 0:32], in0=X64[:, :, 0:32], in1=cosb, op=ALU.mult), r=PJ + [c64k, "tmp"], w=["tmp"])
                            op("pool", lambda e: e.tensor_tensor(out=T3b[:, :, 0:32], in0=X64[:, :, 32:64], in1=sinb, op=ALU.mult), r=PJ + [c64k], w=["tmp2"])
                            op("dve", lambda e: e.tensor_tensor(out=T3[:, :, 32:64], in0=X64[:, :, 0:32], in1=sinb, op=ALU.mult), r=PJ + [c64k, "tmp"], w=["tmp"])
                            op("pool", lambda e: e.tensor_tensor(out=T3b[:, :, 32:64], in0=X64[:, :, 32:64], in1=cosb, op=ALU.mult), r=PJ + [c64k, "tmp2"], w=["tmp2"])
                            TU = TT[:].rearrange("p b (u d) -> p (b u) d", u=2)
                            for (h0, h1, u0) in ((0, 5, 16), (5, 10, 22)):
                                op("dve", lambda e, h0=h0, h1=h1, u0=u0: e.tensor_tensor(out=TU[:, u0:u0 + 5, 0:32], in0=T3[:, h0:h1, 0:32], in1=T3b[:, h0:h1, 0:32], op=ALU.subtract),
                                   r=["tmp", "tmp2"], w=["TT"])
                                op("dve", lambda e, h0=h0, h1=h1, u0=u0: e.tensor_tensor(out=TU[:, u0:u0 + 5, 32:64], in0=T3[:, h0:h1, 32:64], in1=T3b[:, h0:h1, 32:64], op=ALU.add),
                                   r=["tmp", "tmp2"], w=["TT"])
                            op("pool", lambda e: e.tensor_copy(out=TU[:, 21, :], in_=TU[:, 20, :]), r=["TT"], w=["TT"])
                            op("pool", lambda e: e.tensor_copy(out=TU[:, 27, :], in_=TU[:, 26, :]), r=["TT"], w=["TT"])
                            Y32 = proj[:, P_Y32:P_Y32 + 544].rearrange("p (h d) -> p h d", h=17)
                            U3 = tmp[:, 0:544].rearrange("p (h d) -> p h d", h=17)
                            U3b = tmp2[:, 0:544].rearrange("p (h d) -> p h d", h=17)
                            op("act", lambda e: e.activation(out=U3[:], in_=Y32, func=AF.Square), r=PJ + ["TT"], w=["tmp"])
                            op("dve", lambda e: e.tensor_reduce(out=st8[:, 16:33], in_=U3[:], axis=AX.X, op=ALU.add), r=["tmp"], w=["st_b"])
                            rstd_of(st8[:, 16:33], 17, 32, "st_b")
                            op("dve", lambda e: e.tensor_tensor(out=Y32, in0=Y32, in1=bc(st8[:, 16:33], [128, 17, 32], 2), op=ALU.mult), r=PJ + ["st_b"], w=PJ)
                            op("dve", lambda e: e.tensor_tensor(out=Y32, in0=Y32, in1=g32t[:], op=ALU.mult), r=PJ + ["g32t"], w=PJ)
                            cosb = bc(C32[:, 0:16], [128, 17, 16], 1)
                            sinb = bc(C32[:, 16:32], [128, 17, 16], 1)
                            op("dve", lambda e: e.tensor_tensor(out=U3[:, :, 0:16], in0=Y32[:, :, 0:16], in1=cosb, op=ALU.mult), r=PJ + [c32k, "tmp"], w=["tmp"])
                            op("pool", lambda e: e.tensor_tensor(out=U3b[:, :, 0:16], in0=Y32[:, :, 16:32], in1=sinb, op=ALU.mult), r=PJ + [c32k, "TT"], w=["tmp2"])
                            op("dve", lambda e: e.tensor_tensor(out=U3[:, :, 16:32], in0=Y32[:, :, 0:16], in1=sinb, op=ALU.mult), r=PJ + [c32k, "tmp"], w=["tmp"])
                            op("pool", lambda e: e.tensor_tensor(out=U3b[:, :, 16:32], in0=Y32[:, :, 16:32], in1=cosb, op=ALU.mult), r=PJ + [c32k, "tmp2"], w=["tmp2"])
                            TQ = TT[:].rearrange("p b (u d) -> p (b u) d", u=4)
                            op("dve", lambda e: e.tensor_tensor(out=TQ[:, 56:88:2, 0:16], in0=U3[:, 0:16, 0:16], in1=U3b[:, 0:16, 0:16], op=ALU.subtract), r=["tmp", "tmp2"], w=["TT"])
                            op("dve", lambda e: e.tensor_tensor(out=TQ[:, 56:88:2, 16:32], in0=U3[:, 0:16, 16:32], in1=U3b[:, 0:16, 16:32], op=ALU.add), r=["tmp", "tmp2"], w=["TT"])
                            for hh in range(4):
                                u = (4 + hh) * 4 + 2
                                op("dve", lambda e, u=u: e.tensor_tensor(out=TQ[:, u, 0:16], in0=U3[:, 16, 0:16], in1=U3b[:, 16, 0:16], op=ALU.subtract), r=["tmp", "tmp2"], w=["TT"])
                                op("dve", lambda e, u=u: e.tensor_tensor(out=TQ[:, u, 16:32], in0=U3[:, 16, 16:32], in1=U3b[:, 16, 16:32], op=ALU.add), r=["tmp", "tmp2"], w=["TT"])
                            CQ = proj[:, P_CQ:P_CQ + 256]
                            CKV = proj[:, P_CKV:P_CKV + 128]
                            op("act", lambda e: e.activation(out=tmp[:, 0:256], in_=CQ, func=AF.Square, accum_out=st8[:, 40:41]), r=PJ + ["tmp", "TT"], w=["tmp", "st_c"])
                            op("act", lambda e: e.activation(out=tmp[:, 256:384], in_=CKV, func=AF.Square, accum_out=st8[:, 41:42]), r=PJ + ["tmp"], w=["tmp", "st_c"])
                            op("dve", lambda e: e.tensor_scalar(out=st8[:, 41:42], in0=st8[:, 41:42], scalar1=2.0, scalar2=None, op0=ALU.mult), r=["st_c"], w=["st_c"])
                            rstd_of(st8[:, 40:42], 2, 256, "st_c")
                            op("dve", lambda e: e.tensor_scalar(out=cqn[:, 0:256], in0=CQ, scalar1=st8[:, 40:41], scalar2=None, op0=ALU.mult), r=PJ + ["st_c"], w=["cqn"])
                            op("dve", lambda e: e.tensor_scalar(out=cqn[:, 256:384], in0=CKV, scalar1=st8[:, 41:42], scalar2=None, op0=ALU.mult), r=PJ + ["st_c", "cqn"], w=["cqn"])
                            for c in range(3):
                                op("pe", lambda e, c=c: e.transpose(pb[1][:, c * 128:(c + 1) * 128], cqn[:, c * 128:(c + 1) * 128], ident[:]), r=["cqn", "ident"], w=["pb1"])
                            op("act", lambda e: e.activation(out=cqT[:].rearrange("p c t -> p (c t)"), in_=pb[1][:, 0:384], func=AF.Copy), r=["pb1"], w=["cqT"])
                            for c in range(2):
                                op("pe", lambda e, c=c: e.matmul(pf[3][:, 0:384], lhsT=cqT[:, c, :], rhs=wuq_sb[:, c, :], start=(c == 0), stop=(c == 1)),
                                   r=["cqT", "wuq_sb"], w=["pf3"])
                            op("pe", lambda e: e.matmul(pf[4][:, 0:512], lhsT=cqT[:, 2, :], rhs=wukv_sb[:], start=True, stop=True), r=["cqT", "wukv_sb"], w=["pf4"])
                            op("act", lambda e: e.activation(out=QB[:].rearrange("p h d -> p (h d)"), in_=pf[3][:, 0:384], func=AF.Copy), r=["pf3"], w=["QB"])
                            op("dve", lambda e: e.tensor_copy(out=KV[:].rearrange("p h d -> p (h d)"), in_=pf[4][:, 0:512]), r=["pf4"], w=["KV"])
                            Q3 = tmp[:, 0:384].rearrange("p (h d) -> p h d", h=4)
                            K3 = tmp2[:, 0:256].rearrange("p (h d) -> p h d", h=4)
                            op("act", lambda e: e.activation(out=Q3, in_=QB[:], func=AF.Square), r=["QB", "tmp"], w=["tmp"])
                            op("act", lambda e: e.activation(out=K3, in_=KV[:, :, 0:64], func=AF.Square), r=["KV", "tmp2", "TT"], w=["tmp2"])
                            op("dve", lambda e: e.tensor_reduce(out=st8[:, 44:48], in_=Q3[:, :, 0:64], axis=AX.X, op=ALU.add), r=["tmp"], w=["st_d"])
                            op("dve", lambda e: e.tensor_reduce(out=st8[:, 48:52], in_=K3, axis=AX.X, op=ALU.add), r=["tmp2"], w=["st_d"])
                            op("dve", lambda e: e.tensor_reduce(out=st8[:, 52:56], in_=Q3[:, :, 64:96], axis=AX.X, op=ALU.add), r=["tmp"], w=["st_e"])
                            rstd_of(st8[:, 44:52], 8, 64, "st_d")
                            rstd_of(st8[:, 52:56], 4, 32, "st_e")
                            T128 = TT[:]
                            op("dve", lambda e: e.tensor_tensor(out=QB[:, :, 0:64], in0=QB[:, :, 0:64], in1=bc(st8[:, 44:48], [128, 4, 64], 2), op=ALU.mult), r=["QB", "st_d"], w=["QB"])
                            op("dve", lambda e: e.tensor_tensor(out=T128[:, 0:4, 0:64], in0=QB[:, :, 0:64], in1=bc(gq96t[:, 0:64], [128, 4, 64], 1), op=ALU.mult), r=["QB", "gq96t"], w=["TT"])
                            op("dve", lambda e: e.tensor_tensor(out=KV[:, :, 0:64], in0=KV[:, :, 0:64], in1=bc(st8[:, 48:52], [128, 4, 64], 2), op=ALU.mult), r=["KV", "st_d"], w=["KV"])
                            op("dve", lambda e: e.tensor_tensor(out=T128[:, 4:8, 0:64], in0=KV[:, :, 0:64], in1=bc(gk64t[:], [128, 4, 64], 1), op=ALU.mult), r=["KV", "gk64t"], w=["TT"])
                            op("pool", lambda e: e.tensor_copy(out=VS[:, 4:8, 0:64], in_=KV[:, :, 64:128]), r=["KV"], w=["VS"])
                            op("dve", lambda e: e.tensor_tensor(out=QB[:, :, 64:96], in0=QB[:, :, 64:96], in1=bc(st8[:, 52:56], [128, 4, 32], 2), op=ALU.mult), r=["QB", "st_e"], w=["QB"])
                            op("dve", lambda e: e.tensor_tensor(out=QB[:, :, 64:96], in0=QB[:, :, 64:96], in1=bc(gq96t[:, 64:96], [128, 4, 32], 1), op=ALU.mult), r=["QB", "gq96t"], w=["QB"])
                            cosb = bc(C32[:, 0:16], [128, 4, 16], 1)
                            sinb = bc(C32[:, 16:32], [128, 4, 16], 1)
                            R3 = tmp[:, 0:128].rearrange("p (h d) -> p h d", h=4)
                            R3b = tmp2[:, 0:128].rearrange("p (h d) -> p h d", h=4)
                            op("dve", lambda e: e.tensor_tensor(out=R3[:, :, 0:16], in0=QB[:, :, 64:80], in1=cosb, op=ALU.mult), r=["QB", c32k, "tmp"], w=["tmp"])
                            op("pool", lambda e: e.tensor_tensor(out=R3b[:, :, 0:16], in0=QB[:, :, 80:96], in1=sinb, op=ALU.mult), r=["QB", c32k, "tmp2"], w=["tmp2"])
                            op("dve", lambda e: e.tensor_tensor(out=R3[:, :, 16:32], in0=QB[:, :, 64:80], in1=sinb, op=ALU.mult), r=["QB", c32k, "tmp"], w=["tmp"])
                            op("pool", lambda e: e.tensor_tensor(out=R3b[:, :, 16:32], in0=QB[:, :, 80:96], in1=cosb, op=ALU.mult), r=["QB", c32k, "tmp2"], w=["tmp2"])
                            op("dve", lambda e: e.tensor_tensor(out=T128[:, 0:4, 64:80], in0=R3[:, :, 0:16], in1=R3b[:, :, 0:16], op=ALU.subtract), r=["tmp", "tmp2"], w=["TT"])
                            op("dve", lambda e: e.tensor_tensor(out=T128[:, 0:4, 80:96], in0=R3[:, :, 16:32], in1=R3b[:, :, 16:32], op=ALU.add), r=["tmp", "tmp2"], w=["TT"])
                            for g0, g1, bank in ((0, 8, 0), (8, 16, 1), (16, 22, 0)):
                                for b in range(g0, g1):
                                    op("pe", lambda e, b=b, g0=g0, bank=bank: e.transpose(pb[bank][:, (b - g0) * 128:(b - g0 + 1) * 128], TT[:, b, :], ident[:]),
                                       r=["TT", "ident"], w=["pb%d" % bank])
                                nb = g1 - g0
                                eng = "act" if bank == 0 else "dve"
                                if eng == "act":
                                    op("act", lambda e, g0=g0, g1=g1, nb=nb, bank=bank: e.activation(
                                        out=FTs[:, 4 + g0:4 + g1, sub * 128:(sub + 1) * 128], in_=pb[bank][:, 0:nb * 128].rearrange("p (b t) -> p b t", b=nb), func=AF.Copy),
                                       r=["pb%d" % bank], w=["FTs"])
                                else:
                                    op("dve", lambda e, g0=g0, g1=g1, nb=nb, bank=bank: e.tensor_copy(
                                        out=FTs[:, 4 + g0:4 + g1, sub * 128:(sub + 1) * 128], in_=pb[bank][:, 0:nb * 128].rearrange("p (b t) -> p b t", b=nb)),
                                       r=["pb%d" % bank], w=["FTs"])
                            dma(VA[t * 128:(t + 1) * 128, :, :], VS[:], r=["VS"], slot="VS")
                            dma(IW[t * 128:(t + 1) * 128, :], IWs[:], r=["IWs"], slot="IWs")
                        HT = ["hT%d" % i for i in range(4)]
                        for ct in range(12):
                            bank = 3 + (ct % 2)
                            for c in range(8):
                                op("pe", lambda e, c=c, ct=ct, bank=bank: e.matmul(pf[bank][:, :], lhsT=w_sb[:, c, ct * 128:(ct + 1) * 128], rhs=hT[:, c, :],
                                                                                  start=(c == 0), stop=(c == 7)), r=HT + ["w_sb"], w=["pf%d" % bank])
                            if ct < 4:
                                op("dve", lambda e, ct=ct, bank=bank: e.tensor_copy(out=FTs[:, ct, :], in_=pf[bank][:, :]), r=["pf%d" % bank], w=["FTs"])
                            else:
                                op("act", lambda e, ct=ct, bank=bank: e.activation(out=GS[:, ct - 4, :], in_=pf[bank][:, :], func=AF.Silu), r=["pf%d" % bank], w=["GS"])
                        q0 = stl * 512
                        for g in range(0, NB_FT, 7):
                            g1 = min(NB_FT, g + 7)
                            dma(FT[g:g1, :, q0:q0 + 512].rearrange("b p t -> p b t"), FTs[:, g:g1, :], r=["FTs"], slot="FTs")
                        for g in range(0, 8, 4):
                            dma(GT[g:g + 4, :, q0:q0 + 512].rearrange("b p t -> p b t"), GS[:, g:g + 4, :], r=["GS"], slot="GS")
                    sc.barrier()
                NQ = S // 512
                NIT = 16

                def load_V(st, name, h0, nh):
                    V = sb(st, name, [128, NT, nh, 128], BF16)
                    for g in range(0, NT, 8):
                        g1 = min(NT, g + 8)
                        dma(V[:, g:g1, :, :], VA[g * 128:g1 * 128, h0:h0 + nh, :].rearrange("(k p) h d -> p k h d", p=128), w=[name], slot=name)
                    return V

                def load_K(st, name, b0, nb):
                    Kt = sb(st, name, [128, nb, S], BF16)
                    for b in range(nb):
                        dma(Kt[:, b, :], FT[b0 + b, :, :], w=[name], slot=name)
                    return Kt

                def softmax_mixer(mname, kb0, nkb, qb0, nqb, vh0, nvh, gb0, heads, scale, diag_mask, selT_fn=None, post=None, pre_q=None):
                    with ExitStack() as ms:
                        Kt = load_K(ms, "Kt", kb0, nkb)
                        V = load_V(ms, "Vr", vh0, nvh)
                        Qs = [sb(ms, "Qs%d" % i, [128, nqb, 512], BF16) for i in range(2)]
                        Gs = [sb(ms, "Gs%d" % i, [128, 2, 512]) for i in range(1)]
                        PT = [sb(ms, "PT%d" % i, [128, 512], BF16) for i in range(3)]
                        RC = sb(ms, "RC", [128, 2 if post is not None else 1, 512])
                        OD = sb(ms, "OD", [128, 3 if post is not None else 1, 512])
                        YS = [sb(ms, "YS%d" % i, [128, 2, 512], BF16) for i in range(2)]
                        extra = pre_q(ms) if pre_q is not None else None
                        ctr = {"s": 0, "p": 0, "o": 0}

                        def load_q(j):
                            q0 = j * 512
                            dma(Qs[j % 2][:], FT[qb0:qb0 + nqb, :, q0:q0 + 512].rearrange("b p t -> p b t"), w=["Qs%d" % (j % 2)], slot="Qs%d" % (j % 2))

                        def load_g(j):
                            q0 = j * 512
                            dma(Gs[0][:], GT[gb0:gb0 + 2, :, q0:q0 + 512].rearrange("b p t -> p b t"), w=["Gs0"], slot="Gs0")

                        def tile_gen(j, selT, selk):
                            load_g(j)
                            Qk, Gk = "Qs%d" % (j % 2), "Gs0"
                            Q, G = Qs[j % 2], Gs[0]
                            nkt = 4 * (j + 1)
                            ysk = "YS%d" % (j % 2)
                            for hi, (maps, vh) in enumerate(heads):
                                obanks = []
                                for m in maps:
                                    obanks.append(2 + (ctr["o"] % (2 if selT_fn is not None else 4)))
                                    ctr["o"] += 1
                                seq = [(kt, mi) for kt in range(nkt) for mi in range(len(maps))]

                                def emit_qk(idx):
                                    kt, mi = seq[idx]
                                    kblk, qblk, base, dkk = maps[mi]
                                    sbank = ctr["s"] % 2
                                    ctr["s"] += 1
                                    op("pe", lambda e: e.matmul(pf[sbank][:, :], lhsT=Kt[base:base + dkk, kblk, kt * 128:(kt + 1) * 128],
                                                                rhs=Q[base:base + dkk, qblk, :], start=True, stop=(selT is None)),
                                       r=["Kt", Qk], w=["pf%d" % sbank])
                                    if selT is not None:
                                        op("pe", lambda e: e.matmul(pf[sbank][:, :], lhsT=ident[:], rhs=selT[:, kt, :], start=False, stop=True),
                                           r=["ident", selk], w=["pf%d" % sbank])
                                    return sbank

                                pend = emit_qk(0)
                                for idx in range(len(seq)):
                                    kt, mi = seq[idx]
                                    sbank = pend
                                    if idx + 1 < len(seq):
                                        pend = emit_qk(idx + 1)
                                    pi = ctr["p"] % 3
                                    ctr["p"] += 1
                                    P = PT[pi]
                                    pk = "PT%d" % pi
                                    op("act", lambda e: e.activation(out=P[:], in_=pf[sbank][:, :], func=AF.Exp, scale=scale), r=["pf%d" % sbank], w=[pk])
                                    r_ = kt - (nkt - 4)
                                    if diag_mask is not None and r_ >= 0:
                                        op("dve", lambda e: e.tensor_tensor(out=P[:], in0=P[:], in1=masks[:, diag_mask + r_, :], op=ALU.mult), r=[pk, "masks"], w=[pk])
                                    ob = obanks[mi]
                                    op("pe", lambda e: e.matmul(pf[ob][:, :], lhsT=V[:, kt, vh, :], rhs=P[:], start=(kt == 0), stop=(kt == nkt - 1)),
                                       r=["Vr", pk], w=["pf%d" % ob])
                                    yield
                                blk, hb_ = hi // 2, (hi % 2) * 64
                                for mi, ob in enumerate(obanks):
                                    op("dve", lambda e, mi=mi, ob=ob: e.reciprocal(out=RC[64:128, mi, :], in_=pf[ob][64:128, :]), r=["pf%d" % ob], w=["RC"])
                                if post is None:
                                    ob = obanks[0]
                                    op("dve", lambda e: e.tensor_tensor(out=OD[hb_:hb_ + 64, 0, :], in0=pf[ob][0:64, :], in1=RC[64:128, 0, :], op=ALU.mult),
                                       r=["pf%d" % ob, "RC"], w=["OD"])
                                    op("dve", lambda e: e.tensor_tensor(out=YS[j % 2][hb_:hb_ + 64, blk, :], in0=OD[hb_:hb_ + 64, 0, :], in1=G[hb_:hb_ + 64, blk, :], op=ALU.mult),
                                       r=["OD", Gk], w=[ysk])
                                else:
                                    post(obanks, RC, OD, G, Gk, YS[j % 2], ysk, blk, hb_)
                            q0 = j * 512
                            dma(YT[gb0:gb0 + 2, :, q0:q0 + 512].rearrange("b p t -> p b t"), YS[j % 2][:], r=[ysk], slot=ysk)
                            yield

                        load_q(0)
                        if selT_fn is None:
                            for j in range(NQ):
                                if j + 1 < NQ:
                                    load_q(j + 1)
                                for _ in tile_gen(j, None, None):
                                    pass
                        else:
                            cur = selT_fn(0, extra, None)
                            for j in range(NQ):
                                if j + 1 < NQ:
                                    load_q(j + 1)
                                g = tile_gen(j, cur[0], cur[1])
                                nxt = None
                                if j + 1 < NQ:
                                    nxt = selT_fn(j + 1, extra, lambda g=g: next(g, None))
                                for _ in g:
                                    pass
                                cur = nxt
                        sc.barrier()

                if "B" in MIX:
                    softmax_mixer("B", 8, 4, 4, 4, 4, 4, 2, [([(h, h, 0, 96)], h) for h in range(4)], 96.0 ** -0.5, 0)

                def post_D(obanks, RC, OD, G, Gk, YSt, ysk, blk, hb_):
                    a, b = obanks
                    op("dve", lambda e: e.tensor_tensor(out=OD[0:64, 0, :], in0=pf[a][0:64, :], in1=RC[64:128, 0, :], op=ALU.mult), r=["pf%d" % a, "RC"], w=["OD"])
                    op("dve", lambda e: e.tensor_tensor(out=OD[0:64, 1, :], in0=pf[b][0:64, :], in1=RC[64:128, 1, :], op=ALU.mult), r=["pf%d" % b, "RC", "OD"], w=["OD"])
                    op("dve", lambda e: e.scalar_tensor_tensor(out=OD[0:64, 0, :], in0=OD[0:64, 1, :], scalar=lam_bc[0:64, 0:1], in1=OD[0:64, 0, :], op0=ALU.mult, op1=ALU.add),
                       r=["OD", "lam_bc"], w=["OD"])
                    op("act", lambda e: e.activation(out=OD[0:64, 1, :], in_=OD[0:64, 0, :], func=AF.Square), r=["OD"], w=["OD"])
                    op("pe", lambda e: e.matmul(pf[a][0:64, :], lhsT=ones_f[0:64, 0:64], rhs=OD[0:64, 1, :], start=True, stop=True), r=["OD", "ones_f"], w=["pf%d" % a])
                    op("act", lambda e: e.activation(out=OD[0:64, 1, :], in_=pf[a][0:64, :], func=AF.Ln, scale=1.0 / 64, bias=epst[0:64, 0:1]), r=["pf%d" % a, "OD", "epst"], w=["OD"])
                    op("act", lambda e: e.activation(out=OD[0:64, 1, :], in_=OD[0:64, 1, :], func=AF.Exp, scale=-0.5), r=["OD"], w=["OD"])
                    op("dve", lambda e: e.scalar_tensor_tensor(out=OD[hb_:hb_ + 64, 2, :], in0=OD[0:64, 0, :], scalar=sublnt[0:64, 0:1], in1=OD[0:64, 1, :], op0=ALU.mult, op1=ALU.mult),
                       r=["OD", "sublnt"], w=["OD"])
                    op("dve", lambda e: e.tensor_tensor(out=YSt[hb_:hb_ + 64, blk, :], in0=OD[hb_:hb_ + 64, 2, :], in1=G[hb_:hb_ + 64, blk, :], op=ALU.mult), r=["OD", Gk], w=[ysk])

                if "D" in MIX:
                    softmax_mixer("D", 22, 4, 18, 4, 9, 4, 6,
                                  [([(h, h, c * 64, 32) for c in range(2)], h) for h in range(4)], 32.0 ** -0.5, 0, post=post_D)

                def pre_C(ms):
                    ex = {}
                    ex["IKc"] = [sb(ms, "IKc%d" % i, [128, 512], BF16) for i in range(3)]
                    ex["IQ"] = [sb(ms, "IQ%d" % i, [128, 2, 512], BF16) for i in range(1)]
                    ex["IWt"] = [sb(ms, "IWt%d" % i, [128, 4, 4]) for i in range(2)]
                    ex["SC"] = sb(ms, "SC", [128, S])
                    ex["JK"] = sb(ms, "JK", [128, S], BF16)
                    ex["BD"] = sb(ms, "BD", [128, S], BF16)
                    ex["selT"] = [sb(ms, "selT" + str(i), [128, NT, 512], mybir.dt.float8e4) for i in range(2)]
                    ex["RL"] = [sb(ms, "RL%d" % i, [128, 512]) for i in range(2)]
                    ex["bs"] = sb(ms, "bs", [128, 64])
                    return ex

                def selT_C(j, ex, pump):
                    pump = pump if pump is not None else (lambda: None)
                    q0 = j * 512
                    IQ, IWt = ex["IQ"][0], ex["IWt"][j % 2]
                    iqk, iwk = "IQ0", "IWt%d" % (j % 2)
                    SC, JK, selT, RL, bs, IKc = ex["SC"], ex["JK"], ex["selT"][j % 2], ex["RL"], ex["bs"], ex["IKc"]
                    selk = "selT" + str(j % 2)
                    BD = ex["BD"]
                    dma(IQ[:], FT[15:17, :, q0:q0 + 512].rearrange("b p t -> p b t"), w=[iqk], slot=iqk)
                    dma(IWt[:], IW[q0:q0 + 512, :].rearrange("(a p) h -> p a h", p=128), w=[iwk], slot=iwk)
                    op("pool", lambda e: e.memset(selT[:, 4 * j:4 * j + 4, :], -240.0), w=[selk])
                    rl = 0
                    chunks = [(qi, c0) for qi in range(4) for c0 in range(0, (4 * j + qi + 1) * 128, 512)]

                    def load_ik(ci):
                        qi_, c0_ = chunks[ci]
                        n_ = min(512, (4 * j + qi_ + 1) * 128 - c0_)
                        kk_ = "IKc%d" % (ci % 3)
                        dma(IKc[ci % 3][:, 0:n_], FT[17, :, c0_:c0_ + n_], w=[kk_], slot=kk_)

                    load_ik(0)
                    ci = 0
                    for qi in range(4):
                        i = 4 * j + qi
                        Lk = (i + 1) * 128
                        for c0 in range(0, Lk, 512):
                            n = min(512, Lk - c0)
                            if ci + 1 < len(chunks):
                                load_ik(ci + 1)
                            IKb, ikk = IKc[ci % 3], "IKc%d" % (ci % 3)
                            ci += 1
                            for h in range(4):
                                blk, base = h // 2, (h % 2) * 64
                                bank = 4 + (h % 2)
                                op("pe", lambda e: e.matmul(pf[bank][:, 0:n], lhsT=IQ[base:base + 64, blk, qi * 128:(qi + 1) * 128], rhs=IKb[base:base + 64, 0:n],
                                                            start=True, stop=True), r=[iqk, ikk], w=["pf%d" % bank])
                                R = RL[rl % 2]
                                rk = "RL%d" % (rl % 2)
                                rl += 1
                                op("act", lambda e: e.activation(out=R[:, 0:n], in_=pf[bank][:, 0:n], func=AF.Relu), r=["pf%d" % bank], w=[rk])
                                if h == 0:
                                    op("dve", lambda e: e.tensor_scalar(out=SC[:, c0:c0 + n], in0=R[:, 0:n], scalar1=IWt[:, qi, 0:1], scalar2=None, op0=ALU.mult),
                                       r=[rk, iwk], w=["SC"])
                                else:
                                    op("dve", lambda e: e.scalar_tensor_tensor(out=SC[:, c0:c0 + n], in0=R[:, 0:n], scalar=IWt[:, qi, h:h + 1], in1=SC[:, c0:c0 + n],
                                                                                op0=ALU.mult, op1=ALU.add), r=[rk, iwk, "SC"], w=["SC"])
                                pump()
                        op("dve", lambda e: e.tensor_tensor(out=SC[:, Lk - 128:Lk], in0=SC[:, Lk - 128:Lk], in1=adm[:, 0, :], op=ALU.mult), r=["SC", "adm"], w=["SC"])
                        op("dve", lambda e: e.tensor_tensor(out=SC[:, Lk - 128:Lk], in0=SC[:, Lk - 128:Lk], in1=adm[:, 1, :], op=ALU.add), r=["SC", "adm"], w=["SC"])
                        if Lk <= TOPK:
                            op("dve", lambda e: e.tensor_scalar(out=JK[:, 0:Lk], in0=SC[:, 0:Lk], scalar1=-1.0e29, scalar2=None, op0=ALU.is_ge), r=["SC"], w=["JK"])
                        else:
                            op("dve", lambda e: e.max(out=bs[:, 0:8], in_=SC[:, 0:Lk]), r=["SC"], w=["bs_hi"])
                            op("dve", lambda e: e.tensor_reduce(out=bs[:, 8:9], in_=SC[:, 0:Lk - 128], axis=AX.X, op=ALU.min), r=["SC"], w=["bs_lo"])
                            op("dve", lambda e: e.tensor_tensor(out=bs[:, 9:10], in0=bs[:, 0:1], in1=bs[:, 8:9], op=ALU.subtract), r=["bs_hi", "bs_lo"], w=["bs_d"])
                            op("dve", lambda e: e.tensor_scalar(out=bs[:, 9:10], in0=bs[:, 9:10], scalar1=1.001, scalar2=1.0e-20, op0=ALU.mult, op1=ALU.add), r=["bs_d"], w=["bs_d"])
                            op("dve", lambda e: e.tensor_scalar(out=bs[:, 32:32 + NIT], in0=pow2[:, 0:NIT], scalar1=bs[:, 9:10], scalar2=None, op0=ALU.mult), r=["bs_d", "pow2"], w=["bs_dl"])
                            for k in range(NIT):
                                op("dve", lambda e, k=k: e.tensor_tensor(out=bs[:, 10:11], in0=bs[:, 8:9], in1=bs[:, 32 + k:33 + k], op=ALU.add), r=["bs_lo", "bs_dl"], w=["bs_mid"])
                                op("dve", lambda e: e.tensor_scalar(out=JK[:, 0:Lk], in0=SC[:, 0:Lk], scalar1=bs[:, 10:11], scalar2=0.0, op0=ALU.is_ge, op1=ALU.add,
                                                                    accum_out=bs[:, 11:12]), r=["SC", "bs_mid"], w=["JK", "bs_cnt"])
                                op("dve", lambda e, k=k: e.tensor_scalar(out=bs[:, 12:13], in0=bs[:, 11:12], scalar1=TOPK - 0.5, scalar2=bs[:, 32 + k:33 + k], op0=ALU.is_ge, op1=ALU.mult),
                                   r=["bs_cnt", "bs_dl"], w=["bs_t"])
                                op("dve", lambda e: e.tensor_tensor(out=bs[:, 8:9], in0=bs[:, 8:9], in1=bs[:, 12:13], op=ALU.add), r=["bs_lo", "bs_t"], w=["bs_lo"])
                                pump()
                                pump()
                            op("dve", lambda e: e.tensor_tensor(out=bs[:, 13:14], in0=bs[:, 8:9], in1=bs[:, 31 + NIT:32 + NIT], op=ALU.add), r=["bs_lo", "bs_dl"], w=["bs_hf"])
                            op("dve", lambda e: e.tensor_scalar(out=JK[:, 0:Lk], in0=SC[:, 0:Lk], scalar1=bs[:, 8:9], scalar2=0.0, op0=ALU.is_ge, op1=ALU.add,
                                                                accum_out=bs[:, 14:15]), r=["SC", "bs_lo"], w=["JK", "bs_cl"])
                            op("dve", lambda e: e.scalar_tensor_tensor(out=BD[:, 0:Lk], in0=SC[:, 0:Lk], scalar=bs[:, 13:14], in1=JK[:, 0:Lk], op0=ALU.is_lt, op1=ALU.mult,
                                                                        accum_out=bs[:, 15:16]), r=["SC", "JK", "bs_hf"], w=["BD", "bs_nb"])
                            op("dve", lambda e: e.tensor_tensor(out=bs[:, 16:17], in0=bs[:, 15:16], in1=bs[:, 14:15], op=ALU.subtract), r=["bs_nb", "bs_cl"], w=["bs_m"])
                            op("dve", lambda e: e.tensor_scalar(out=bs[:, 16:17], in0=bs[:, 16:17], scalar1=float(TOPK) + 0.5, scalar2=None, op0=ALU.add), r=["bs_m"], w=["bs_m"])
                            op("dve", lambda e: e.tensor_tensor_scan(out=SC[:, 0:Lk], data0=ones_bf[:, 0:1].to_broadcast([128, Lk]), data1=BD[:, 0:Lk], initial=0.0, op0=ALU.mult, op1=ALU.add),
                               r=["BD", "ones_bf", "SC"], w=["SC"])
                            op("dve", lambda e: e.scalar_tensor_tensor(out=BD[:, 0:Lk], in0=SC[:, 0:Lk], scalar=bs[:, 16:17], in1=BD[:, 0:Lk], op0=ALU.is_gt, op1=ALU.mult),
                               r=["SC", "BD", "bs_m"], w=["BD"])
                            op("pool", lambda e: e.tensor_tensor(out=JK[:, 0:Lk], in0=JK[:, 0:Lk], in1=BD[:, 0:Lk], op=ALU.subtract), r=["JK", "BD"], w=["JK"])
                        if debug:
                            dma(DSC[i, :, :], SC[:, :], r=["SC"], slot="dbg1")
                            dma(DBS[i, :, :], bs[:, :], r=["bs_lo", "bs_hi", "bs_d", "bs_dl", "bs_mid", "bs_cnt", "bs_t", "bs_m", "bs_cl", "bs_nb", "bs_hf"], slot="dbg2")
                        for g in range(0, i + 1, 8):
                            g1 = min(i + 1, g + 8)
                            bank = (g // 8) % 2
                            for kb in range(g, g1):
                                op("pe", lambda e, kb=kb: e.transpose(pb[bank][:, (kb - g) * 128:(kb - g + 1) * 128], JK[:, kb * 128:(kb + 1) * 128], ident[:]),
                                   r=["JK", "ident"], w=["pb%d" % bank])
                            nb_ = g1 - g
                            op("dve", lambda e: e.tensor_scalar(out=selT[:, g:g1, qi * 128:(qi + 1) * 128], in0=pb[bank][:, 0:nb_ * 128].rearrange("p (b t) -> p b t", b=nb_),
                                                                scalar1=-1.0, scalar2=240.0, op0=ALU.add, op1=ALU.mult, saturate=False), r=["pb%d" % bank], w=[selk])
                            pump()
                    return selT, selk

                if "C" in MIX:
                    softmax_mixer("C", 14, 1, 12, 2, 8, 1, 4, [([(0, h // 2, (h % 2) * 64, 64)], 0) for h in range(4)], 0.125, None, selT_fn=selT_C, pre_q=pre_C)

                if "A" in MIX:
                    with ExitStack() as ms:
                        Kt = load_K(ms, "Kt", 2, 2)
                        V = load_V(ms, "Vr", 0, 4)
                        nK = sb(ms, "nK", [128, 2, S], BF16)
                        for b in range(2):
                            op("dve" if b == 0 else "pool", lambda e, b=b: e.tensor_scalar(out=nK[:, b, :], in0=Kt[:, b, :], scalar1=-0.125, scalar2=0.0, op0=ALU.mult, op1=ALU.add), r=["Kt"], w=["nK"])
                        Qs = [sb(ms, "Qs%d" % i, [128, 2, 512], BF16) for i in range(2)]
                        Gs = [sb(ms, "Gs%d" % i, [128, 2, 512]) for i in range(2)]
                        NBUF = 3
                        Et = [sb(ms, "Et%d" % i, [128, 512]) for i in range(NBUF)]
                        SPt = [sb(ms, "SPt%d" % i, [128, 512]) for i in range(NBUF)]
                        SPh = [sb(ms, "SPh%d" % i, [128, 512], BF16) for i in range(NBUF)]
                        SPl = [sb(ms, "SPl%d" % i, [128, 512], BF16) for i in range(NBUF)]
                        Cf = [sb(ms, "Cf%d" % i, [128, 512]) for i in range(2)]
                        Ch = [[sb(ms, "Ch%d_%d" % (i, k), [128, 512], BF16) for k in range(2)] for i in range(2)]
                        Cl = [[sb(ms, "Cl%d_%d" % (i, k), [128, 512], BF16) for k in range(2)] for i in range(2)]
                        PT = [sb(ms, "PT%d" % i, [128, 512], BF16) for i in range(3)]
                        YS = [sb(ms, "YS%d" % i, [128, 2, 512], BF16) for i in range(2)]
                        gc = {"g1": 0, "g2": 0}

                        def load_qA(j):
                            q0 = j * 512
                            dma(Qs[j % 2][:], FT[0:2, :, q0:q0 + 512].rearrange("b p t -> p b t"), w=["Qs%d" % (j % 2)], slot="Qs%d" % (j % 2))
                            dma(Gs[j % 2][:], GT[0:2, :, q0:q0 + 512].rearrange("b p t -> p b t"), w=["Gs%d" % (j % 2)], slot="Gs%d" % (j % 2))

                        load_qA(0)
                        for j in range(NQ):
                            if j + 1 < NQ:
                                load_qA(j + 1)
                            Qk, Gk = "Qs%d" % (j % 2), "Gs%d" % (j % 2)
                            Q, G = Qs[j % 2], Gs[j % 2]
                            nkt = 4 * (j + 1)
                            ysk = "YS%d" % (j % 2)
                            for pair in range(2):
                                seq = [(kt, sl) for kt in range(nkt - 1, -1, -1) for sl in range(2)]
                                info = {}
                                cidx = [0, 0]

                                def stage1(idx):
                                    kt, sl = seq[idx]
                                    h = 2 * pair + sl
                                    blk, base = h // 2, (h % 2) * 64
                                    b1 = gc["g1"] % NBUF
                                    zb = gc["g1"] % 2
                                    gc["g1"] += 1
                                    info[idx] = b1
                                    r_ = kt - (nkt - 4)
                                    ksl = slice(kt * 128, (kt + 1) * 128)
                                    op("pe", lambda e: e.matmul(pf[zb][:, :], lhsT=Kt[base:base + 64, blk, ksl], rhs=Q[base:base + 64, blk, :], start=True, stop=True),
                                       r=["Kt", Qk], w=["pf%d" % zb])
                                    op("act", lambda e: e.activation(out=Et[b1][:], in_=pf[zb][:, :], func=AF.Exp, scale=0.125), r=["pf%d" % zb], w=["Et%d" % b1])
                                    op("act", lambda e: e.activation(out=SPt[b1][:], in_=Et[b1][:], func=AF.Ln, bias=1.0, scale=1.0), r=["Et%d" % b1], w=["SPt%d" % b1])
                                    if r_ >= 0:
                                        op("dve", lambda e: e.tensor_tensor(out=SPt[b1][:], in0=SPt[b1][:], in1=masks[:, 4 + r_, :], op=ALU.mult), r=["SPt%d" % b1, "masks"], w=["SPt%d" % b1])
                                    op("dve", lambda e: e.tensor_copy(out=SPh[b1][:], in_=SPt[b1][:]), r=["SPt%d" % b1], w=["SPh%d" % b1])
                                    op("dve", lambda e: e.tensor_tensor(out=SPl[b1][:], in0=SPt[b1][:], in1=SPh[b1][:], op=ALU.subtract), r=["SPt%d" % b1, "SPh%d" % b1], w=["SPl%d" % b1])

                                def stage2(idx):
                                    kt, sl = seq[idx]
                                    h = 2 * pair + sl
                                    blk, base = h // 2, (h % 2) * 64
                                    b1 = info.pop(idx)
                                    rb = 2 + gc["g2"] % 2
                                    p2 = gc["g2"] % 3
                                    gc["g2"] += 1
                                    ob = 4 + sl
                                    first = (kt == nkt - 1)
                                    r_ = kt - (nkt - 4)
                                    ksl = slice(kt * 128, (kt + 1) * 128)
                                    op("pe", lambda e: e.matmul(pf[rb][:, :], lhsT=tri[:], rhs=SPh[b1][:], start=True, stop=False), r=["tri", "SPh%d" % b1], w=["pf%d" % rb])
                                    op("pe", lambda e: e.matmul(pf[rb][:, :], lhsT=tri[:], rhs=SPl[b1][:], start=False, stop=False), r=["tri", "SPl%d" % b1], w=["pf%d" % rb])
                                    if not first:
                                        cu = cidx[sl] % 2
                                        op("pe", lambda e: e.matmul(pf[rb][:, :], lhsT=ones_bf[:], rhs=Ch[sl][cu][:], start=False, stop=False), r=["ones_bf", "Ch%d_%d" % (sl, cu)], w=["pf%d" % rb])
                                        op("pe", lambda e: e.matmul(pf[rb][:, :], lhsT=ones_bf[:], rhs=Cl[sl][cu][:], start=False, stop=False), r=["ones_bf", "Cl%d_%d" % (sl, cu)], w=["pf%d" % rb])
                                    op("pe", lambda e: e.matmul(pf[rb][:, :], lhsT=nK[base:base + 64, blk, ksl], rhs=Q[base:base + 64, blk, :], start=False, stop=True),
                                       r=["nK", Qk], w=["pf%d" % rb])
                                    if kt > 0:
                                        cidx[sl] += 1
                                        cw = cidx[sl] % 2
                                        if first:
                                            op("pool", lambda e: e.tensor_copy(out=Cf[sl][:], in_=SPt[b1][:]), r=["SPt%d" % b1], w=["Cf%d" % sl])
                                        else:
                                            op("pool", lambda e: e.tensor_tensor(out=Cf[sl][:], in0=Cf[sl][:], in1=SPt[b1][:], op=ALU.add), r=["SPt%d" % b1, "Cf%d" % sl], w=["Cf%d" % sl])
                                        op("act", lambda e: e.activation(out=Ch[sl][cw][:], in_=Cf[sl][:], func=AF.Copy), r=["Cf%d" % sl], w=["Ch%d_%d" % (sl, cw)])
                                        op("pool", lambda e: e.tensor_tensor(out=Cl[sl][cw][:], in0=Cf[sl][:], in1=Ch[sl][cw][:], op=ALU.subtract), r=["Cf%d" % sl, "Ch%d_%d" % (sl, cw)], w=["Cl%d_%d" % (sl, cw)])
                                    op("act", lambda e: e.activation(out=PT[p2][:], in_=pf[rb][:, :], func=AF.Exp, scale=-1.0), r=["pf%d" % rb], w=["PT%d" % p2])
                                    if r_ >= 0:
                                        op("dve", lambda e: e.tensor_tensor(out=PT[p2][:], in0=PT[p2][:], in1=masks[:, 4 + r_, :], op=ALU.mult), r=["PT%d" % p2, "masks"], w=["PT%d" % p2])
                                    op("pe", lambda e: e.matmul(pf[ob][0:64, :], lhsT=V[:, kt, h, 0:64], rhs=PT[p2][:], start=first, stop=(kt == 0)), r=["Vr", "PT%d" % p2], w=["pf%d" % ob])

                                LA = 2
                                for idx in range(len(seq) + LA):
                                    if idx < len(seq):
                                        stage1(idx)
                                    if idx >= LA:
                                        stage2(idx - LA)
                                for sl in range(2):
                                    h = 2 * pair + sl
                                    blk, base = h // 2, (h % 2) * 64
                                    ob = 4 + sl
                                    op("dve", lambda e: e.tensor_tensor(out=YS[j % 2][base:base + 64, blk, :], in0=pf[ob][0:64, :], in1=G[base:base + 64, blk, :], op=ALU.mult),
                                       r=["pf%d" % ob, Gk], w=[ysk])
                            q0 = j * 512
                            dma(YT[0:2, :, q0:q0 + 512].rearrange("b p t -> p b t"), YS[j % 2][:], r=[ysk], slot=ysk)
                        sc.barrier()

                if DO_OUT:
                    with ExitStack() as ms:
                        wo = sb(ms, "wo", [128, 8, D_MODEL], BF16)
                        wos = [sb(ms, "wos%d" % i, [128, D_MODEL]) for i in range(2)]
                        for c in range(8):
                            k = "wos%d" % (c % 2)
                            dma(wos[c % 2][:], w_out[l, c * 128:(c + 1) * 128, :], w=[k], slot=k)
                            op("dve" if c % 2 == 0 else "pool", lambda e, c=c: e.tensor_copy(out=wo[:, c, :], in_=wos[c % 2][:]), r=[k], w=["wo"])
                        YTs = [sb(ms, "YTs%d" % i, [128, 8, 512], BF16) for i in range(2)]
                        xs = [sb(ms, "xs%d" % i, [128, D_MODEL]) for i in range(2)]
                        xo = [sb(ms, "xo%d" % i, [128, D_MODEL]) for i in range(2)]

                        def load_y(stl):
                            q0 = stl * 512
                            for g in range(0, 8, 4):
                                dma(YTs[stl % 2][:, g:g + 4, :], YT[g:g + 4, :, q0:q0 + 512].rearrange("b p t -> p b t"), w=["YTs%d" % (stl % 2)], slot="YTs%d" % (stl % 2))

                        load_y(0)
                        for stl in range(NST):
                            if stl + 1 < NST:
                                load_y(stl + 1)
                            for sub in range(4):
                                t = stl * 4 + sub
                                xk, ok = "xs%d" % (t % 2), "xo%d" % (t % 2)
                                dma(xs[t % 2][:], x_src[t * 128:(t + 1) * 128, :], w=[xk], slot=xk)
                                for half in range(2):
                                    bank = (t * 2 + half) % 2
                                    for c in range(8):
                                        op("pe", lambda e, c=c: e.matmul(pf[bank][:, :], lhsT=YTs[stl % 2][:, c, sub * 128:(sub + 1) * 128], rhs=wo[:, c, half * 512:(half + 1) * 512],
                                                                         start=(c == 0), stop=(c == 7)), r=["YTs%d" % (stl % 2), "wo"], w=["pf%d" % bank])
                                    op("dve", lambda e: e.tensor_tensor(out=xo[t % 2][:, half * 512:(half + 1) * 512], in0=pf[bank][:, :], in1=xs[t % 2][:, half * 512:(half + 1) * 512], op=ALU.add),
                                       r=["pf%d" % bank, xk], w=[ok])
                                dma(x_dst[t * 128:(t + 1) * 128, :], xo[t % 2][:], r=[ok], slot=ok)
                        sc.barrier()
        sc.barrier()
        print("instructions:", sc.n_ins)
    return nc


def host_consts(S):
    bf = ml_dtypes.bfloat16
    c = {}
    c["c_ident"] = np.eye(128, dtype=np.float32).astype(bf)

    def cs(d):
        inv = (10000.0 ** (-np.arange(0, d, 2, dtype=np.float32) / np.float32(d))).astype(np.float32)
        ang = (np.arange(S, dtype=np.float32)[:, None] * inv[None, :]).astype(np.float32)
        return np.concatenate([np.cos(ang), np.sin(ang)], axis=1).astype(np.float32)

    c["c_cs64"] = cs(64)
    c["c_cs32"] = cs(32)
    kk = np.arange(128)[:, None]
    qq = np.arange(512)[None, :]
    m = []
    for r in range(4):
        m.append(((r * 128 + kk) < ((qq // 64) + 1) * 64))
    for r in range(4):
        m.append(((r * 128 + kk) < qq))
    c["c_mask"] = np.concatenate(m, axis=1).astype(np.float32).astype(bf)
    q1 = np.arange(128)[:, None]
    k1 = np.arange(128)[None, :]
    m01 = (k1 < ((q1 // 64) + 1) * 64).astype(np.float32)
    c["c_adm"] = np.concatenate([m01, (1.0 - m01) * NEGBIG], axis=1).astype(np.float32)
    c["c_tri"] = (np.arange(128)[:, None] >= np.arange(128)[None, :]).astype(np.float32).astype(bf)
    c["c_pow2"] = np.tile((2.0 ** -(np.arange(32) + 1.0))[None, :], (128, 1)).astype(np.float32)
    return c


def host_params(inp, DEPTH):
    f = lambda a: np.ascontiguousarray(np.asarray(a, dtype=np.float32)[:DEPTH])
    p = {}
    p["w_in"] = np.ascontiguousarray(f(inp["w_in"])[:, :, PERM])
    p["w_out"] = f(inp["w_out"])
    p["w_uq"] = f(inp["mla_w_uq"])
    p["w_ukv"] = f(inp["mla_w_ukv"])
    p["ln_g"] = np.ascontiguousarray(f(inp["ln_g"]).reshape(-1, 8, 128).transpose(0, 2, 1))
    p["qn_g"] = np.ascontiguousarray(f(inp["mla_q_norm_g"]).reshape(-1, 2, 128).transpose(0, 2, 1))
    p["kvn_g"] = np.ascontiguousarray(f(inp["mla_kv_norm_g"]).reshape(-1, 128, 1))
    dq, dkk = f(inp["dsa_q_g"]), f(inp["dsa_k_g"])
    p["g64"] = np.ascontiguousarray(np.concatenate([dq] * 4 + [dkk], axis=1))
    fq, fk, mk = f(inp["diff_q_g"]), f(inp["diff_k_g"]), f(inp["mla_k_g"])
    p["g32"] = np.ascontiguousarray(np.concatenate([fq] * 8 + [fk] * 8 + [mk[:, 64:96]], axis=1))
    p["gq96"] = f(inp["mla_q_g"])
    p["gk64"] = np.ascontiguousarray(mk[:, :64])
    p["subln"] = np.ascontiguousarray(f(inp["diff_subln_g"]).reshape(-1, 64, 1))
    p["lvec"] = np.ascontiguousarray(np.concatenate([f(inp["diff_lq1"]), f(inp["diff_lk1"]), f(inp["diff_lq2"]), f(inp["diff_lk2"])], axis=1))
    return p


_CACHE = {}


def run(inputs, S, DEPTH, debug=False):
    import math
    key = (S, DEPTH, debug)
    lam_inits = [0.8 - 0.6 * math.exp(-0.3 * l) for l in range(DEPTH)]
    if key not in _CACHE:
        _CACHE[key] = build(S, DEPTH, debug, lam_inits, MIX=globals().get("MIXSEL", "ABCD"))
    nc = _CACHE[key]
    x = np.asarray(inputs["x"], dtype=np.float32)
    B = x.shape[0]
    shared = {}
    shared.update(host_consts(S))
    shared.update(host_params(inputs, DEPTH))
    in_maps = []
    for b in range(B):
        m = dict(shared)
        m["x"] = np.ascontiguousarray(x[b, :S])
        in_maps.append(m)
    res = run_bass_kernel_spmd(nc, in_maps, core_ids=list(range(B)))
    return res


def kernel(**inputs):
    res = run(inputs, 8192, 4)
    return np.stack([np.asarray(r["y"], dtype=np.float32) for r in res.results], axis=0)
```

```python
import numpy as np
import ml_dtypes
from contextlib import ExitStack
import concourse.bass as bass
import concourse.mybir as mybir
from concourse.bass_utils import run_bass_kernel_spmd

F32 = mybir.dt.float32
BF16 = mybir.dt.bfloat16
AF = mybir.ActivationFunctionType
ALU = mybir.AluOpType
AX = mybir.AxisListType

D_MODEL = 1024
IN_COLS = 3684
EPS = 1e-6
NEGBIG = -1.0e30

O_AQ, O_AK, O_AV, O_AG = 0, 256, 512, 768
O_BCQ, O_BCKV, O_BKR, O_BG = 1024, 1280, 1408, 1440
O_CQ, O_CK, O_CV, O_CG, O_CIQ, O_CIK, O_CIW = 1696, 1952, 2016, 2080, 2336, 2592, 2656
O_DQ, O_DK, O_DV, O_DG = 2660, 2916, 3172, 3428


def _perm():
    r = lambda a, n: list(range(a, a + n))
    p = []
    p += r(O_AQ, 256) + r(O_AK, 256)
    p += r(O_AG, 256) + r(O_BG, 256) + r(O_CG, 256) + r(O_DG, 256)
    p += r(O_AV, 256) + r(O_DV, 256) + r(O_CV, 64)
    p += r(O_CQ, 256) + r(O_CK, 64) + r(O_CIQ, 256) + r(O_CIK, 64)
    p += r(O_DQ, 256) + r(O_DK, 256) + r(O_BKR, 32)
    p += r(O_BCQ, 256) + r(O_BCKV, 128)
    p += r(O_CIW, 4)
    assert len(p) == IN_COLS and sorted(p) == list(range(IN_COLS))
    return np.array(p)


PERM = _perm()
T0 = 1536
NTM = IN_COLS - T0
P_AV, P_DV, P_CV = 0, 256, 512
P_X64 = 576
P_Y32 = 1216
P_CQ = 1760
P_CKV = 2016
P_IW = 2144
NB_FT = 26


class Sched:
    ENG = ("pe", "act", "dve", "pool", "sp")

    def __init__(self, nc, es):
        self.nc = nc
        self.es = es
        self.e = dict(pe=nc.tensor, act=nc.scalar, dve=nc.vector, pool=nc.gpsimd, sp=nc.sync)
        self.semobj = {}
        self.cnt = {}
        for k in ("pe", "act", "dve", "pool"):
            self.semobj[k] = es.enter_context(nc.semaphore("s_" + k))
            self.cnt[k] = 0
        self.known = {k: {} for k in self.ENG}
        self.lastw = {}
        self.reads = {}
        self.n_ins = 0

    def _dsem(self, slot):
        name = "d_" + slot
        if name not in self.semobj:
            self.semobj[name] = self.es.enter_context(self.nc.semaphore(name))
            self.cnt[name] = 0
        return name

    def _waits(self, E, r, w, attach_ok=False):
        need = {}

        def add(tok, kind):
            sname, val, prod = tok
            if prod == E:
                if E == "pe" or kind == "war":
                    return
            if self.known[E].get(sname, 0) >= val:
                return
            if need.get(sname, 0) < val:
                need[sname] = val

        for k in r:
            t = self.lastw.get(k)
            if t is not None:
                add(t, "raw")
        for k in w:
            t = self.lastw.get(k)
            if t is not None:
                add(t, "waw")
            for t in self.reads.get(k, {}).values():
                add(t, "war")
        items = list(need.items())
        attach = None
        if attach_ok and items:
            attach = items.pop()
        for sname, val in items:
            self.e[E].wait_ge(self.semobj[sname], val)
            self.known[E][sname] = val
            self.n_ins += 1
        if attach is not None:
            self.known[E][attach[0]] = attach[1]
        return attach

    def _record(self, tok, r, w):
        for k in r:
            d = self.reads.setdefault(k, {})
            d[tok[0]] = tok
        for k in w:
            self.lastw[k] = tok
            self.reads[k] = {}

    def op(self, E, fn, r=(), w=()):
        att = self._waits(E, r, w, attach_ok=True)
        ins = fn(self.e[E])
        if att is not None:
            ins._wait_ge(self.semobj[att[0]], att[1])
        self.cnt[E] += 1
        ins.then_inc(self.semobj[E], 1)
        tok = (E, self.cnt[E], E)
        self._record(tok, r, w)
        self.n_ins += 1
        return tok

    def dma(self, out, in_, r=(), w=(), slot=None, Q="sp"):
        self._waits(Q, r, w)
        sname = self._dsem(slot)
        ins = self.e[Q].dma_start(out=out, in_=in_)
        self.cnt[sname] += 16
        ins.then_inc(self.semobj[sname], 16)
        tok = (sname, self.cnt[sname], "dma")
        self._record(tok, r, w)
        self.n_ins += 1
        return tok

    def barrier(self, engines=None):
        for E in (engines or self.ENG):
            for sname, c in self.cnt.items():
                if c == 0 or self.known[E].get(sname, 0) >= c:
                    continue
                if sname == E and E == "pe":
                    continue
                self.e[E].wait_ge(self.semobj[sname], c)
                self.known[E][sname] = c
                self.n_ins += 1
        if engines is None:
            self.lastw = {}
            self.reads = {}


def bc(ap, shape, axis):
    return ap.unsqueeze(axis).to_broadcast(list(shape))


def build(S, DEPTH, debug=False, lam_inits=None, MIX="ABCD", DO_OUT=True):
    TOPK = min(256, S // 4)
    NT = S // 128
    NST = S // 512
    nc = bass.Bass("TRN2", target_bir_lowering=False)

    def din(name, shape, dt=F32):
        return nc.dram_tensor(name, list(shape), dt, kind="ExternalInput").ap()

    x_in = din("x", [S, D_MODEL])
    w_in = din("w_in", [DEPTH, D_MODEL, IN_COLS])
    w_out = din("w_out", [DEPTH, D_MODEL, D_MODEL])
    w_uq = din("w_uq", [DEPTH, 256, 384])
    w_ukv = din("w_ukv", [DEPTH, 128, 512])
    ln_g = din("ln_g", [DEPTH, 128, 8])
    qn_g = din("qn_g", [DEPTH, 128, 2])
    kvn_g = din("kvn_g", [DEPTH, 128, 1])
    g64 = din("g64", [DEPTH, 5 * 64])
    g32 = din("g32", [DEPTH, 17 * 32])
    gq96 = din("gq96", [DEPTH, 96])
    gk64 = din("gk64", [DEPTH, 64])
    subln = din("subln", [DEPTH, 64, 1])
    lvec = din("lvec", [DEPTH, 4 * 32])
    c_ident = din("c_ident", [128, 128], BF16)
    c_cs64 = din("c_cs64", [S, 64])
    c_cs32 = din("c_cs32", [S, 32])
    c_mask = din("c_mask", [128, 8 * 512], BF16)
    c_adm = din("c_adm", [128, 2 * 128])
    c_tri = din("c_tri", [128, 128], BF16)
    c_pow2 = din("c_pow2", [128, 32])

    okind = "ExternalOutput"
    y_out = nc.dram_tensor("y", [S, D_MODEL], F32, kind=okind).ap()
    dk = okind if debug else "Internal"
    FT = nc.dram_tensor("FT", [NB_FT, 128, S], BF16, kind=dk).ap()
    GT = nc.dram_tensor("GT", [8, 128, S], F32, kind=dk).ap()
    VA = nc.dram_tensor("VA", [S, 13, 128], BF16, kind=dk).ap()
    IW = nc.dram_tensor("IW", [S, 4], F32, kind=dk).ap()
    YT = nc.dram_tensor("YT", [8, 128, S], BF16, kind=dk).ap()
    XR = nc.dram_tensor("XR", [S, D_MODEL], F32, kind="Internal").ap()
    if debug:
        DSEL = nc.dram_tensor("DSEL", [S // 512, 128, S // 128, 512], BF16, kind=okind).ap()
        DSC = nc.dram_tensor("DSC", [S // 128, 128, S], F32, kind=okind).ap()
        DBS = nc.dram_tensor("DBS", [S // 128, 128, 64], F32, kind=okind).ap()

    with ExitStack() as es:
        sc = Sched(nc, es)
        op, dma = sc.op, sc.dma

        uniq = [0]

        def sb(st, name, shape, dt=F32):
            uniq[0] += 1
            return st.enter_context(nc.sbuf_tensor("%s_%d" % (name, uniq[0]), list(shape), dt))

        def ps(st, name, shape, dt=F32):
            return st.enter_context(nc.psum_tensor(name, list(shape), dt))

        ident = sb(es, "ident", [128, 128], BF16)
        masks = sb(es, "masks", [128, 8, 512], BF16)
        adm = sb(es, "adm", [128, 2, 128])
        tri = sb(es, "tri", [128, 128], BF16)
        ones_bf = sb(es, "ones_bf", [128, 128], BF16)
        ones_f = sb(es, "ones_f", [128, 128])
        pow2 = sb(es, "pow2", [128, 32])
        neghalf = sb(es, "neghalf", [128, 32])
        epst = sb(es, "epst", [128, 1])
        nh512 = sb(es, "nh512", [128, 512])
        dma(ident[:], c_ident[:, :], w=["ident"], slot="k_ident")
        dma(masks[:].rearrange("p a b -> p (a b)"), c_mask[:, :], w=["masks"], slot="k_masks")
        dma(adm[:].rearrange("p a b -> p (a b)"), c_adm[:, :], w=["adm"], slot="k_adm")
        dma(tri[:], c_tri[:, :], w=["tri"], slot="k_tri")
        dma(pow2[:], c_pow2[:, :], w=["pow2"], slot="k_pow2")
        op("dve", lambda e: e.memset(ones_bf[:], 1.0), w=["ones_bf"])
        op("dve", lambda e: e.memset(ones_f[:], 1.0), w=["ones_f"])
        op("dve", lambda e: e.memset(neghalf[:], -0.5), w=["neghalf"])
        op("dve", lambda e: e.memset(epst[:], EPS), w=["epst"])
        op("dve", lambda e: e.memset(nh512[:], -0.5), w=["nh512"])

        pf = [ps(es, "pf%d" % i, [128, 512]) for i in range(6)]
        pb = [ps(es, "pb%d" % i, [128, 1024], BF16) for i in range(2)]

        for l in range(DEPTH):
            x_src = x_in if l == 0 else XR
            x_dst = y_out if l == DEPTH - 1 else XR
            lam_init = float(lam_inits[l])
            with ExitStack() as ls:
                sublnt = sb(ls, "sublnt", [64, 1])
                lv = sb(ls, "lv", [1, 4, 32])
                lam_bc = sb(ls, "lam_bc", [128, 2])
                dma(sublnt[:], subln[l, :, :], w=["sublnt"], slot="k_sublnt")
                dma(lv[:].rearrange("p a b -> p (a b)"), lvec[l:l + 1, :], w=["lv"], slot="k_lv")
                op("dve", lambda e: e.tensor_scalar(out=sublnt[:], in0=sublnt[:], scalar1=1.0 - lam_init, scalar2=None,
                                                    op0=ALU.mult), r=["sublnt"], w=["sublnt"])
                lt = sb(ls, "lt", [1, 2, 32])
                l2 = sb(ls, "l2", [1, 4])
                op("dve", lambda e: e.tensor_tensor(out=lt[:, 0, :], in0=lv[:, 0, :], in1=lv[:, 1, :], op=ALU.mult), r=["lv"], w=["lt"])
                op("dve", lambda e: e.tensor_tensor(out=lt[:, 1, :], in0=lv[:, 2, :], in1=lv[:, 3, :], op=ALU.mult), r=["lv", "lt"], w=["lt"])
                op("dve", lambda e: e.tensor_reduce(out=l2[:, 0:2], in_=lt[:], axis=AX.X, op=ALU.add), r=["lt"], w=["l2"])
                op("act", lambda e: e.activation(out=l2[:, 0:2], in_=l2[:, 0:2], func=AF.Exp), r=["l2"], w=["l2"])
                op("dve", lambda e: e.tensor_tensor(out=l2[:, 2:3], in0=l2[:, 0:1], in1=l2[:, 1:2], op=ALU.subtract), r=["l2"], w=["l2"])
                op("dve", lambda e: e.tensor_scalar(out=l2[:, 3:4], in0=l2[:, 2:3], scalar1=lam_init, scalar2=None, op0=ALU.add), r=["l2"], w=["l2"])
                op("dve", lambda e: e.tensor_scalar(out=l2[:, 2:3], in0=l2[:, 3:4], scalar1=-1.0, scalar2=None, op0=ALU.mult), r=["l2"], w=["l2"])
                op("pe", lambda e: e.matmul(pf[0][:, 0:2], lhsT=ones_f[0:1, :], rhs=l2[0:1, 2:4], start=True, stop=True),
                   r=["l2", "ones_f"], w=["pf0"])
                op("dve", lambda e: e.tensor_copy(out=lam_bc[:], in_=pf[0][:, 0:2]), r=["pf0"], w=["lam_bc"])

                with ExitStack() as p1:
                    w_sb = sb(p1, "w_sb", [128, 8, IN_COLS], BF16)
                    g64t = sb(p1, "g64t", [128, 5, 64])
                    g32t = sb(p1, "g32t", [128, 17, 32])
                    gq96t = sb(p1, "gq96t", [128, 96])
                    gk64t = sb(p1, "gk64t", [128, 64])
                    dma(g64t[:].rearrange("p a b -> p (a b)"), g64[l:l + 1, :].to_broadcast([128, 320]), w=["g64t"], slot="k_g64t")
                    dma(g32t[:].rearrange("p a b -> p (a b)"), g32[l:l + 1, :].to_broadcast([128, 544]), w=["g32t"], slot="k_g32t")
                    dma(gq96t[:], gq96[l:l + 1, :].to_broadcast([128, 96]), w=["gq96t"], slot="k_gq96t")
                    dma(gk64t[:], gk64[l:l + 1, :].to_broadcast([128, 64]), w=["gk64t"], slot="k_gk64t")
                    wuq_sb = sb(p1, "wuq_sb", [128, 2, 384], BF16)
                    wukv_sb = sb(p1, "wukv_sb", [128, 512], BF16)
                    lng = sb(p1, "lng", [128, 8])
                    qng = sb(p1, "qng", [128, 2])
                    kvng = sb(p1, "kvng", [128, 1])
                    dma(lng[:], ln_g[l, :, :], w=["lng"], slot="k_lng")
                    dma(qng[:], qn_g[l, :, :], w=["qng"], slot="k_qng")
                    dma(kvng[:], kvn_g[l, :, :], w=["kvng"], slot="k_kvng")
                    with ExitStack() as p0:
                        wst = [sb(p0, "wst%d" % i, [128, IN_COLS]) for i in range(2)]
                        for c in range(8):
                            k = "wst%d" % (c % 2)
                            dma(wst[c % 2][:], w_in[l, c * 128:(c + 1) * 128, :], w=[k], slot=k)
                            eng = "dve" if c % 2 == 0 else "pool"
                            op(eng, lambda e, c=c: e.tensor_scalar(out=w_sb[:, c, :], in0=wst[c % 2][:], scalar1=lng[:, c:c + 1],
                                                                    scalar2=0.0, op0=ALU.mult, op1=ALU.add), r=[k, "lng"], w=["w_sb"])
                        for c in range(2):
                            dma(wst[c][:, 0:384], w_uq[l, c * 128:(c + 1) * 128, :], w=["wst%d" % c], slot="wst%d" % c)
                            op("dve", lambda e, c=c: e.tensor_scalar(out=wuq_sb[:, c, :], in0=wst[c][:, 0:384], scalar1=qng[:, c:c + 1],
                                                                      scalar2=None, op0=ALU.mult), r=["wst%d" % c, "qng"], w=["wuq_sb"])
                        dma(wst[0][:, 0:512], w_ukv[l, :, :], w=["wst0"], slot="wst0")
                        op("dve", lambda e: e.tensor_scalar(out=wukv_sb[:], in0=wst[0][:, 0:512], scalar1=kvng[:, 0:1],
                                                            scalar2=None, op0=ALU.mult), r=["wst0", "kvng"], w=["wukv_sb"])
                        sc.barrier()
                    xt = [sb(p1, "xt%d" % i, [128, D_MODEL]) for i in range(2)]
                    junk = sb(p1, "junk", [128, D_MODEL], BF16)
                    hb = sb(p1, "hb", [128, D_MODEL], BF16)
                    hT = sb(p1, "hT", [128, 8, 512], BF16)
                    proj = sb(p1, "proj", [128, NTM])
                    tmp = sb(p1, "tmp", [128, 640])
                    tmp2 = sb(p1, "tmp2", [128, 640])
                    st8 = sb(p1, "st8", [128, 64])
                    cs64 = [sb(p1, "cs64_%d" % i, [128, 64]) for i in range(2)]
                    cs32 = [sb(p1, "cs32_%d" % i, [128, 32]) for i in range(2)]
                    TT = sb(p1, "TT", [128, 22, 128], BF16)
                    VS = sb(p1, "VS", [128, 13, 128], BF16)
                    IWs = sb(p1, "IWs", [128, 4])
                    QB = sb(p1, "QB", [128, 4, 96])
                    KV = sb(p1, "KV", [128, 4, 128])
                    cqn = sb(p1, "cqn", [128, 384], BF16)
                    cqT = sb(p1, "cqT", [128, 3, 128], BF16)
                    FTs = sb(p1, "FTs", [128, NB_FT, 512], BF16)
                    GS = sb(p1, "GS", [128, 8, 512])
                    op("pool", lambda e: e.memset(TT[:], 0.0), w=["TT"])
                    op("pool", lambda e: e.memset(VS[:], 1.0), w=["VS"])

                    def load_x(t):
                        k = "xt%d" % (t % 2)
                        dma(xt[t % 2][:], x_src[t * 128:(t + 1) * 128, :], w=[k], slot=k)
                        dma(cs64[t % 2][:], c_cs64[t * 128:(t + 1) * 128, :], w=["cs64_%d" % (t % 2)], slot="cs64_%d" % (t % 2))
                        dma(cs32[t % 2][:], c_cs32[t * 128:(t + 1) * 128, :], w=["cs32_%d" % (t % 2)], slot="cs32_%d" % (t % 2))

                    def rstd_of(ss_ap, n, d, key):
                        op("act", lambda e: e.activation(out=ss_ap, in_=ss_ap, func=AF.Ln, scale=1.0 / d, bias=epst[:, 0:1]), r=[key, "epst"], w=[key])
                        op("act", lambda e: e.activation(out=ss_ap, in_=ss_ap, func=AF.Exp, scale=-0.5), r=[key], w=[key])

                    load_x(0)
                    for stl in range(NST):
                        for sub in range(4):
                            t = stl * 4 + sub
                            if t + 1 < NT:
                                load_x(t + 1)
                            xk = "xt%d" % (t % 2)
                            X = xt[t % 2]
                            c64k, c32k = "cs64_%d" % (t % 2), "cs32_%d" % (t % 2)
                            C64, C32 = cs64[t % 2], cs32[t % 2]
                            op("act", lambda e: e.activation(out=junk[:], in_=X[:], func=AF.Square, accum_out=st8[:, 0:1]),
                               r=[xk], w=["junk", "st_x"])
                            rstd_of(st8[:, 0:1], 1, D_MODEL, "st_x")
                            op("dve", lambda e: e.tensor_scalar(out=hb[:], in0=X[:], scalar1=st8[:, 0:1], scalar2=None, op0=ALU.mult),
                               r=[xk, "st_x"], w=["hb"])
                            for c in range(8):
                                op("pe", lambda e, c=c: e.transpose(pb[0][:, c * 128:(c + 1) * 128], hb[:, c * 128:(c + 1) * 128], ident[:]),
                                   r=["hb", "ident"], w=["pb0"])
                            op("act", lambda e: e.activation(out=hT[:, :, sub * 128:(sub + 1) * 128],
                                                             in_=pb[0][:].rearrange("p (c t) -> p c t", c=8), func=AF.Copy),
                               r=["pb0"], w=["hT%d" % sub])
                            col = 0
                            ci = 0
                            while col < NTM:
                                n = min(512, NTM - col)
                                bank = 1 + (ci % 2)
                                for c in range(8):
                                    op("pe", lambda e, c=c, col=col, n=n, bank=bank: e.matmul(
                                        pf[bank][:, 0:n], lhsT=hT[:, c, sub * 128:(sub + 1) * 128], rhs=w_sb[:, c, T0 + col:T0 + col + n],
                                        start=(c == 0), stop=(c == 7)), r=["hT%d" % sub, "w_sb"], w=["pf%d" % bank])
                                eng = "act" if ci % 2 == 0 else "dve"
                                if eng == "act":
                                    op("act", lambda e, col=col, n=n, bank=bank: e.activation(out=proj[:, col:col + n], in_=pf[bank][:, 0:n], func=AF.Copy),
                                       r=["pf%d" % bank], w=["proj%d" % ci])
                                else:
                                    op("dve", lambda e, col=col, n=n, bank=bank: e.tensor_copy(out=proj[:, col:col + n], in_=pf[bank][:, 0:n]),
                                       r=["pf%d" % bank], w=["proj%d" % ci])
                                col += n
                                ci += 1
                            PJ = ["proj%d" % i for i in range(ci)]
                            op("pool", lambda e: e.tensor_copy(out=VS[:, 0:4, 0:64], in_=proj[:, P_AV:P_AV + 256].rearrange("p (h d) -> p h d", h=4)), r=PJ, w=["VS"])
                            op("pool", lambda e: e.tensor_copy(out=VS[:, 9:13, 0:64], in_=proj[:, P_DV:P_DV + 256].rearrange("p (h d) -> p h d", h=4)), r=PJ, w=["VS"])
                            op("pool", lambda e: e.tensor_copy(out=VS[:, 8, 0:64], in_=proj[:, P_CV:P_CV + 64]), r=PJ, w=["VS"])
                            op("pool", lambda e: e.tensor_scalar(out=IWs[:], in0=proj[:, P_IW:P_IW + 4], scalar1=0.0625, scalar2=0.0, op0=ALU.mult, op1=ALU.add), r=PJ, w=["IWs"])
                            X64 = proj[:, P_X64:P_X64 + 640].rearrange("p (h d) -> p h d", h=10)
                            T3 = tmp[:].rearrange("p (h d) -> p h d", h=10)
                            T3b = tmp2[:].rearrange("p (h d) -> p h d", h=10)
                            op("act", lambda e: e.activation(out=T3[:, 0:5, :], in_=X64[:, 0:5, :], func=AF.Square), r=PJ, w=["tmp"])
                            op("dve", lambda e: e.tensor_reduce(out=st8[:, 8:13], in_=T3[:, 0:5, :], axis=AX.X, op=ALU.add), r=["tmp"], w=["st_a"])
                            rstd_of(st8[:, 8:13], 5, 64, "st_a")
                            op("dve", lambda e: e.tensor_tensor(out=X64[:, 0:5, :], in0=X64[:, 0:5, :], in1=bc(st8[:, 8:13], [128, 5, 64], 2), op=ALU.mult),
                               r=PJ + ["st_a"], w=PJ)
                            op("dve", lambda e: e.tensor_tensor(out=X64[:, 0:5, :], in0=X64[:, 0:5, :], in1=g64t[:], op=ALU.mult), r=PJ + ["g64t"], w=PJ)
                            cosb = bc(C64[:, 0:32], [128, 10, 32], 1)
                            sinb = bc(C64[:, 32:64], [128, 10, 32], 1)
                            op("dve", lambda e: e.tensor_tensor(out=T3[:, :, 0:32], in0=X64[:, :, 0:32], in1=cosb, op=ALU.mult), r=PJ + [c64k, "tmp"], w=["tmp"])
                            op("pool", lambda e: e.tensor_tensor(out=T3b[:, :, 0:32], in0=X64[:, :, 32:64], in1=sinb, op=ALU.mult), r=PJ + [c64k], w=["tmp2"])
                            op("dve", lambda e: e.tensor_tensor(out=T3[:, :, 32:64], in0=X64[:, :, 0:32], in1=sinb, op=ALU.mult), r=PJ + [c64k, "tmp"], w=["tmp"])
                            op("pool", lambda e: e.tensor_tensor(out=T3b[:, :, 32:64], in0=X64[:, :, 32:64], in1=cosb, op=ALU.mult), r=PJ + [c64k, "tmp2"], w=["tmp2"])
                            TU = TT[:].rearrange("p b (u d) -> p (b u) d", u=2)
                            for (h0, h1, u0) in ((0, 5, 16), (5, 10, 22)):
                                op("dve", lambda e, h0=h0, h1=h1, u0=u0: e.tensor_tensor(out=TU[:, u0:u0 + 5, 0:32], in0=T3[:, h0:h1, 0:32], in1=T3b[:, h0:h1, 0:32], op=ALU.subtract),
                                   r=["tmp", "tmp2"], w=["TT"])
                                op("dve", lambda e, h0=h0, h1=h1, u0=u0: e.tensor_tensor(out=TU[:, u0:u0 + 5, 32:64], in0=T3[:, h0:h1, 32:64], in1=T3b[:, h0:h1, 32:64], op=ALU.add),
                                   r=["tmp", "tmp2"], w=["TT"])
                            op("pool", lambda e: e.tensor_copy(out=TU[:, 21, :], in_=TU[:, 20, :]), r=["TT"], w=["TT"])
                            op("pool", lambda e: e.tensor_copy(out=TU[:, 27, :], in_=TU[:, 26, :]), r=["TT"], w=["TT"])
                            Y32 = proj[:, P_Y32:P_Y32 + 544].rearrange("p (h d) -> p h d", h=17)
                            U3 = tmp[:, 0:544].rearrange("p (h d) -> p h d", h=17)
                            U3b = tmp2[:, 0:544].rearrange("p (h d) -> p h d", h=17)
                            op("act", lambda e: e.activation(out=U3[:], in_=Y32, func=AF.Square), r=PJ + ["TT"], w=["tmp"])
                            op("dve", lambda e: e.tensor_reduce(out=st8[:, 16:33], in_=U3[:], axis=AX.X, op=ALU.add), r=["tmp"], w=["st_b"])
                            rstd_of(st8[:, 16:33], 17, 32, "st_b")
                            op("dve", lambda e: e.tensor_tensor(out=Y32, in0=Y32, in1=bc(st8[:, 16:33], [128, 17, 32], 2), op=ALU.mult), r=PJ + ["st_b"], w=PJ)
                            op("dve", lambda e: e.tensor_tensor(out=Y32, in0=Y32, in1=g32t[:], op=ALU.mult), r=PJ + ["g32t"], w=PJ)
                            cosb = bc(C32[:, 0:16], [128, 17, 16], 1)
                            sinb = bc(C32[:, 16:32], [128, 17, 16], 1)
                            op("dve", lambda e: e.tensor_tensor(out=U3[:, :, 0:16], in0=Y32[:, :, 0:16], in1=cosb, op=ALU.mult), r=PJ + [c32k, "tmp"], w=["tmp"])
                            op("pool", lambda e: e.tensor_tensor(out=U3b[:, :, 0:16], in0=Y32[:, :, 16:32], in1=sinb, op=ALU.mult), r=PJ + [c32k, "TT"], w=["tmp2"])
                            op("dve", lambda e: e.tensor_tensor(out=U3[:, :, 16:32], in0=Y32[:, :, 0:16], in1=sinb, op=ALU.mult), r=PJ + [c32k, "tmp"], w=["tmp"])
                            op("pool", lambda e: e.tensor_tensor(out=U3b[:, :, 16:32], in0=Y32[:, :, 16:32], in1=cosb, op=ALU.mult), r=PJ + [c32k, "tmp2"], w=["tmp2"])
                            TQ = TT[:].rearrange("p b (u d) -> p (b u) d", u=4)
                            op("dve", lambda e: e.tensor_tensor(out=TQ[:, 56:88:2, 0:16], in0=U3[:, 0:16, 0:16], in1=U3b[:, 0:16, 0:16], op=ALU.subtract), r=["tmp", "tmp2"], w=["TT"])
                            op("dve", lambda e: e.tensor_tensor(out=TQ[:, 56:88:2, 16:32], in0=U3[:, 0:16, 16:32], in1=U3b[:, 0:16, 16:32], op=ALU.add), r=["tmp", "tmp2"], w=["TT"])
                            for hh in range(4):
                                u = (4 + hh) * 4 + 2
                                op("dve", lambda e, u=u: e.tensor_tensor(out=TQ[:, u, 0:16], in0=U3[:, 16, 0:16], in1=U3b[:, 16, 0:16], op=ALU.subtract), r=["tmp", "tmp2"], w=["TT"])
                                op("dve", lambda e, u=u: e.tensor_tensor(out=TQ[:, u, 16:32], in0=U3[:, 16, 16:32], in1=U3b[:, 16, 16:32], op=ALU.add), r=["tmp", "tmp2"], w=["TT"])
                            CQ = proj[:, P_CQ:P_CQ + 256]
                            CKV = proj[:, P_CKV:P_CKV + 128]
                            op("act", lambda e: e.activation(out=tmp[:, 0:256], in_=CQ, func=AF.Square, accum_out=st8[:, 40:41]), r=PJ + ["tmp", "TT"], w=["tmp", "st_c"])
                            op("act", lambda e: e.activation(out=tmp[:, 256:384], in_=CKV, func=AF.Square, accum_out=st8[:, 41:42]), r=PJ + ["tmp"], w=["tmp", "st_c"])
                            op("dve", lambda e: e.tensor_scalar(out=st8[:, 41:42], in0=st8[:, 41:42], scalar1=2.0, scalar2=None, op0=ALU.mult), r=["st_c"], w=["st_c"])
                            rstd_of(st8[:, 40:42], 2, 256, "st_c")
                            op("dve", lambda e: e.tensor_scalar(out=cqn[:, 0:256], in0=CQ, scalar1=st8[:, 40:41], scalar2=None, op0=ALU.mult), r=PJ + ["st_c"], w=["cqn"])
                            op("dve", lambda e: e.tensor_scalar(out=cqn[:, 256:384], in0=CKV, scalar1=st8[:, 41:42], scalar2=None, op0=ALU.mult), r=PJ + ["st_c", "cqn"], w=["cqn"])
                            for c in range(3):
                                op("pe", lambda e, c=c: e.transpose(pb[1][:, c * 128:(c + 1) * 128], cqn[:, c * 128:(c + 1) * 128], ident[:]), r=["cqn", "ident"], w=["pb1"])
                            op("act", lambda e: e.activation(out=cqT[:].rearrange("p c t -> p (c t)"), in_=pb[1][:, 0:384], func=AF.Copy), r=["pb1"], w=["cqT"])
                            for c in range(2):
                                op("pe", lambda e, c=c: e.matmul(pf[3][:, 0:384], lhsT=cqT[:, c, :], rhs=wuq_sb[:, c, :], start=(c == 0), stop=(c == 1)),
                                   r=["cqT", "wuq_sb"], w=["pf3"])
                            op("pe", lambda e: e.matmul(pf[4][:, 0:512], lhsT=cqT[:, 2, :], rhs=wukv_sb[:], start=True, stop=True), r=["cqT", "wukv_sb"], w=["pf4"])
                            op("act", lambda e: e.activation(out=QB[:].rearrange("p h d -> p (h d)"), in_=pf[3][:, 0:384], func=AF.Copy), r=["pf3"], w=["QB"])
                            op("dve", lambda e: e.tensor_copy(out=KV[:].rearrange("p h d -> p (h d)"), in_=pf[4][:, 0:512]), r=["pf4"], w=["KV"])
                            Q3 = tmp[:, 0:384].rearrange("p (h d) -> p h d", h=4)
                            K3 = tmp2[:, 0:256].rearrange("p (h d) -> p h d", h=4)
                            op("act", lambda e: e.activation(out=Q3, in_=QB[:], func=AF.Square), r=["QB", "tmp"], w=["tmp"])
                            op("act", lambda e: e.activation(out=K3, in_=KV[:, :, 0:64], func=AF.Square), r=["KV", "tmp2", "TT"], w=["tmp2"])
                            op("dve", lambda e: e.tensor_reduce(out=st8[:, 44:48], in_=Q3[:, :, 0:64], axis=AX.X, op=ALU.add), r=["tmp"], w=["st_d"])
                            op("dve", lambda e: e.tensor_reduce(out=st8[:, 48:52], in_=K3, axis=AX.X, op=ALU.add), r=["tmp2"], w=["st_d"])
                            op("dve", lambda e: e.tensor_reduce(out=st8[:, 52:56], in_=Q3[:, :, 64:96], axis=AX.X, op=ALU.add), r=["tmp"], w=["st_e"])
                            rstd_of(st8[:, 44:52], 8, 64, "st_d")
                            rstd_of(st8[:, 52:56], 4, 32, "st_e")
                            T128 = TT[:]
                            op("dve", lambda e: e.tensor_tensor(out=QB[:, :, 0:64], in0=QB[:, :, 0:64], in1=bc(st8[:, 44:48], [128, 4, 64], 2), op=ALU.mult), r=["QB", "st_d"], w=["QB"])
                            op("dve", lambda e: e.tensor_tensor(out=T128[:, 0:4, 0:64], in0=QB[:, :, 0:64], in1=bc(gq96t[:, 0:64], [128, 4, 64], 1), op=ALU.mult), r=["QB", "gq96t"], w=["TT"])
                            op("dve", lambda e: e.tensor_tensor(out=KV[:, :, 0:64], in0=KV[:, :, 0:64], in1=bc(st8[:, 48:52], [128, 4, 64], 2), op=ALU.mult), r=["KV", "st_d"], w=["KV"])
                            op("dve", lambda e: e.tensor_tensor(out=T128[:, 4:8, 0:64], in0=KV[:, :, 0:64], in1=bc(gk64t[:], [128, 4, 64], 1), op=ALU.mult), r=["KV", "gk64t"], w=["TT"])
                            op("pool", lambda e: e.tensor_copy(out=VS[:, 4:8, 0:64], in_=KV[:, :, 64:128]), r=["KV"], w=["VS"])
                            op("dve", lambda e: e.tensor_tensor(out=QB[:, :, 64:96], in0=QB[:, :, 64:96], in1=bc(st8[:, 52:56], [128, 4, 32], 2), op=ALU.mult), r=["QB", "st_e"], w=["QB"])
                            op("dve", lambda e: e.tensor_tensor(out=QB[:, :, 64:96], in0=QB[:, :, 64:96], in1=bc(gq96t[:, 64:96], [128, 4, 32], 1), op=ALU.mult), r=["QB", "gq96t"], w=["QB"])
                            cosb = bc(C32[:, 0:16], [128, 4, 16], 1)
                            sinb = bc(C32[:, 16:32], [128, 4, 16], 1)
                            R3 = tmp[:, 0:128].rearrange("p (h d) -> p h d", h=4)
                            R3b = tmp2[:, 0:128].rearrange("p (h d) -> p h d", h=4)
                            op("dve", lambda e: e.tensor_tensor(out=R3[:, :, 0:16], in0=QB[:, :, 64:80], in1=cosb, op=ALU.mult), r=["QB", c32k, "tmp"], w=["tmp"])
                            op("pool", lambda e: e.tensor_tensor(out=R3b[:, :, 0:16], in0=QB[:, :, 80:96], in1=sinb, op=ALU.mult), r=["QB", c32k, "tmp2"], w=["tmp2"])
                            op("dve", lambda e: e.tensor_tensor(out=R3[:, :, 16:32], in0=QB[:, :, 64:80], in1=sinb, op=ALU.mult), r=["QB", c32k, "tmp"], w=["tmp"])
                            op("pool", lambda e: e.tensor_tensor(out=R3b[:, :, 16:32], in0=QB[:, :, 80:96], in1=cosb, op=ALU.mult), r=["QB", c32k, "tmp2"], w=["tmp2"])
                            op("dve", lambda e: e.tensor_tensor(out=T128[:, 0:4, 64:80], in0=R3[:, :, 0:16], in1=R3b[:, :, 0:16], op=ALU.subtract), r=["tmp", "tmp2"], w=["TT"])
                            op("dve", lambda e: e.tensor_tensor(out=T128[:, 0:4, 80:96], in0=R3[:, :, 16:32], in1=R3b[:, :, 16:32], op=ALU.add), r=["tmp", "tmp2"], w=["TT"])
                            for g0, g1, bank in ((0, 8, 0), (8, 16, 1), (16, 22, 0)):
                                for b in range(g0, g1):
                                    op("pe", lambda e, b=b, g0=g0, bank=bank: e.transpose(pb[bank][:, (b - g0) * 128:(b - g0 + 1) * 128], TT[:, b, :], ident[:]),
                                       r=["TT", "ident"], w=["pb%d" % bank])
                                nb = g1 - g0
                                eng = "act" if bank == 0 else "dve"
                                if eng == "act":
                                    op("act", lambda e, g0=g0, g1=g1, nb=nb, bank=bank: e.activation(
                                        out=FTs[:, 4 + g0:4 + g1, sub * 128:(sub + 1) * 128], in_=pb[bank][:, 0:nb * 128].rearrange("p (b t) -> p b t", b=nb), func=AF.Copy),
                                       r=["pb%d" % bank], w=["FTs"])
                                else:
                                    op("dve", lambda e, g0=g0, g1=g1, nb=nb, bank=bank: e.tensor_copy(
                                        out=FTs[:, 4 + g0:4 + g1, sub * 128:(sub + 1) * 128], in_=pb[bank][:, 0:nb * 128].rearrange("p (b t) -> p b t", b=nb)),
                                       r=["pb%d" % bank], w=["FTs"])
                            dma(VA[t * 128:(t + 1) * 128, :, :], VS[:], r=["VS"], slot="VS")
                            dma(IW[t * 128:(t + 1) * 128, :], IWs[:], r=["IWs"], slot="IWs")
                        HT = ["hT%d" % i for i in range(4)]
                        for ct in range(12):
                            bank = 3 + (ct % 2)
                            for c in range(8):
                                op("pe", lambda e, c=c, ct=ct, bank=bank: e.matmul(pf[bank][:, :], lhsT=w_sb[:, c, ct * 128:(ct + 1) * 128], rhs=hT[:, c, :],
                                                                                  start=(c == 0), stop=(c == 7)), r=HT + ["w_sb"], w=["pf%d" % bank])
                            if ct < 4:
                                op("dve", lambda e, ct=ct, bank=bank: e.tensor_copy(out=FTs[:, ct, :], in_=pf[bank][:, :]), r=["pf%d" % bank], w=["FTs"])
                            else:
                                op("act", lambda e, ct=ct, bank=bank: e.activation(out=GS[:, ct - 4, :], in_=pf[bank][:, :], func=AF.Silu), r=["pf%d" % bank], w=["GS"])
                        q0 = stl * 512
                        for g in range(0, NB_FT, 7):
                            g1 = min(NB_FT, g + 7)
                            dma(FT[g:g1, :, q0:q0 + 512].rearrange("b p t -> p b t"), FTs[:, g:g1, :], r=["FTs"], slot="FTs")
                        for g in range(0, 8, 4):
                            dma(GT[g:g + 4, :, q0:q0 + 512].rearrange("b p t -> p b t"), GS[:, g:g + 4, :], r=["GS"], slot="GS")
                    sc.barrier()
                NQ = S // 512
                NIT = 16

                def load_V(st, name, h0, nh):
                    V = sb(st, name, [128, NT, nh, 128], BF16)
                    for g in range(0, NT, 8):
                        g1 = min(NT, g + 8)
                        dma(V[:, g:g1, :, :], VA[g * 128:g1 * 128, h0:h0 + nh, :].rearrange("(k p) h d -> p k h d", p=128), w=[name], slot=name)
                    return V

                def load_K(st, name, b0, nb):
                    Kt = sb(st, name, [128, nb, S], BF16)
                    for b in range(nb):
                        dma(Kt[:, b, :], FT[b0 + b, :, :], w=[name], slot=name)
                    return Kt

                def softmax_mixer(mname, kb0, nkb, qb0, nqb, vh0, nvh, gb0, heads, scale, diag_mask, selT_fn=None, post=None, pre_q=None):
                    with ExitStack() as ms:
                        Kt = load_K(ms, "Kt", kb0, nkb)
                        V = load_V(ms, "Vr", vh0, nvh)
                        Qs = [sb(ms, "Qs%d" % i, [128, nqb, 512], BF16) for i in range(2)]
                        Gs = [sb(ms, "Gs%d" % i, [128, 2, 512]) for i in range(1)]
                        PT = [sb(ms, "PT%d" % i, [128, 512], BF16) for i in range(3)]
                        RC = sb(ms, "RC", [128, 2 if post is not None else 1, 512])
                        OD = sb(ms, "OD", [128, 3 if post is not None else 1, 512])
                        YS = [sb(ms, "YS%d" % i, [128, 2, 512], BF16) for i in range(2)]
                        extra = pre_q(ms) if pre_q is not None else None
                        ctr = {"s": 0, "p": 0, "o": 0}
                        allmaps = []
                        for (mps, _vh) in heads:
                            for m in mps:
                                if m not in allmaps:
                                    allmaps.append(m)
                        use_qz = (selT_fn is None) and any(m[3] < 96 for m in allmaps)
                        Qz = None
                        if use_qz:
                            Qz = [[sb(ms, "Qz%d_%d" % (i, mi), [128, 512], BF16) for mi in range(len(allmaps))] for i in range(2)]
                            for i in range(2):
                                for mi in range(len(allmaps)):
                                    op("pool", lambda e, i=i, mi=mi: e.memset(Qz[i][mi][:], 0.0), w=["Qz%d_%d" % (i, mi)])

                        def load_q(j):
                            q0 = j * 512
                            dma(Qs[j % 2][:], FT[qb0:qb0 + nqb, :, q0:q0 + 512].rearrange("b p t -> p b t"), w=["Qs%d" % (j % 2)], slot="Qs%d" % (j % 2))
                            if use_qz:
                                for mi, (kblk_, qblk_, base_, dk_) in enumerate(allmaps):
                                    op("pool" if mi % 2 else "dve", lambda e, mi=mi, qblk_=qblk_, base_=base_, dk_=dk_: e.tensor_copy(
                                        out=Qz[j % 2][mi][base_:base_ + dk_, :], in_=Qs[j % 2][base_:base_ + dk_, qblk_, :]),
                                       r=["Qs%d" % (j % 2)], w=["Qz%d_%d" % (j % 2, mi)])

                        def load_g(j):
                            q0 = j * 512
                            dma(Gs[0][:], GT[gb0:gb0 + 2, :, q0:q0 + 512].rearrange("b p t -> p b t"), w=["Gs0"], slot="Gs0")

                        def tile_gen(j, selT, selk):
                            load_g(j)
                            Qk, Gk = "Qs%d" % (j % 2), "Gs0"
                            Q, G = Qs[j % 2], Gs[0]
                            nkt = 4 * (j + 1)
                            ysk = "YS%d" % (j % 2)
                            for hi, (maps, vh) in enumerate(heads):
                                obanks = []
                                for m in maps:
                                    obanks.append(2 + (ctr["o"] % (2 if selT_fn is not None else 4)))
                                    ctr["o"] += 1
                                seq = [(kt, mi) for kt in range(nkt) for mi in range(len(maps))]

                                def emit_qk(idx):
                                    kt, mi = seq[idx]
                                    kblk, qblk, base, dkk = maps[mi]
                                    sbank = ctr["s"] % 2
                                    ctr["s"] += 1
                                    if use_qz:
                                        zi = allmaps.index(maps[mi])
                                        op("pe", lambda e: e.matmul(pf[sbank][:, :], lhsT=Kt[:, kblk, kt * 128:(kt + 1) * 128],
                                                                    rhs=Qz[j % 2][zi][:, :], start=True, stop=(selT is None)),
                                           r=["Kt", "Qz%d_%d" % (j % 2, zi)], w=["pf%d" % sbank])
                                    elif dkk >= 96:
                                        op("pe", lambda e: e.matmul(pf[sbank][:, :], lhsT=Kt[:, kblk, kt * 128:(kt + 1) * 128],
                                                                    rhs=Q[:, qblk, :], start=True, stop=(selT is None)),
                                           r=["Kt", Qk], w=["pf%d" % sbank])
                                    else:
                                        op("pe", lambda e: e.matmul(pf[sbank][:, :], lhsT=Kt[base:base + dkk, kblk, kt * 128:(kt + 1) * 128],
                                                                    rhs=Q[base:base + dkk, qblk, :], start=True, stop=(selT is None)),
                                           r=["Kt", Qk], w=["pf%d" % sbank])
                                    if selT is not None:
                                        op("pe", lambda e: e.matmul(pf[sbank][:, :], lhsT=ident[:], rhs=selT[:, kt, :], start=False, stop=True),
                                           r=["ident", selk], w=["pf%d" % sbank])
                                    return sbank

                                pend = emit_qk(0)
                                for idx in range(len(seq)):
                                    kt, mi = seq[idx]
                                    sbank = pend
                                    if idx + 1 < len(seq):
                                        pend = emit_qk(idx + 1)
                                    pi = ctr["p"] % 3
                                    ctr["p"] += 1
                                    P = PT[pi]
                                    pk = "PT%d" % pi
                                    op("act", lambda e: e.activation(out=P[:], in_=pf[sbank][:, :], func=AF.Exp, scale=scale), r=["pf%d" % sbank], w=[pk])
                                    r_ = kt - (nkt - 4)
                                    if diag_mask is not None and r_ >= 0:
                                        op("dve", lambda e: e.tensor_tensor(out=P[:], in0=P[:], in1=masks[:, diag_mask + r_, :], op=ALU.mult), r=[pk, "masks"], w=[pk])
                                    ob = obanks[mi]
                                    op("pe", lambda e: e.matmul(pf[ob][:, :], lhsT=V[:, kt, vh, :], rhs=P[:], start=(kt == 0), stop=(kt == nkt - 1)),
                                       r=["Vr", pk], w=["pf%d" % ob])
                                    yield
                                blk, hb_ = hi // 2, (hi % 2) * 64
                                for mi, ob in enumerate(obanks):
                                    op("dve", lambda e, mi=mi, ob=ob: e.reciprocal(out=RC[64:128, mi, :], in_=pf[ob][64:128, :]), r=["pf%d" % ob], w=["RC"])
                                if post is None:
                                    ob = obanks[0]
                                    op("dve", lambda e: e.tensor_tensor(out=OD[hb_:hb_ + 64, 0, :], in0=pf[ob][0:64, :], in1=RC[64:128, 0, :], op=ALU.mult),
                                       r=["pf%d" % ob, "RC"], w=["OD"])
                                    op("dve", lambda e: e.tensor_tensor(out=YS[j % 2][hb_:hb_ + 64, blk, :], in0=OD[hb_:hb_ + 64, 0, :], in1=G[hb_:hb_ + 64, blk, :], op=ALU.mult),
                                       r=["OD", Gk], w=[ysk])
                                else:
                                    post(obanks, RC, OD, G, Gk, YS[j % 2], ysk, blk, hb_)
                            q0 = j * 512
                            dma(YT[gb0:gb0 + 2, :, q0:q0 + 512].rearrange("b p t -> p b t"), YS[j % 2][:], r=[ysk], slot=ysk)
                            yield

                        load_q(0)
                        if selT_fn is None:
                            for j in range(NQ):
                                if j + 1 < NQ:
                                    load_q(j + 1)
                                for _ in tile_gen(j, None, None):
                                    pass
                        else:
                            cur = selT_fn(0, extra, None)
                            for j in range(NQ):
                                if j + 1 < NQ:
                                    load_q(j + 1)
                                g = tile_gen(j, cur[0], cur[1])
                                nxt = None
                                if j + 1 < NQ:
                                    nxt = selT_fn(j + 1, extra, lambda g=g: next(g, None))
                                for _ in g:
                                    pass
                                cur = nxt
                        sc.barrier()

                if "B" in MIX:
                    softmax_mixer("B", 8, 4, 4, 4, 4, 4, 2, [([(h, h, 0, 96)], h) for h in range(4)], 96.0 ** -0.5, 0)

                def post_D(obanks, RC, OD, G, Gk, YSt, ysk, blk, hb_):
                    a, b = obanks
                    op("dve", lambda e: e.tensor_tensor(out=OD[0:64, 0, :], in0=pf[a][0:64, :], in1=RC[64:128, 0, :], op=ALU.mult), r=["pf%d" % a, "RC"], w=["OD"])
                    op("dve", lambda e: e.tensor_tensor(out=OD[0:64, 1, :], in0=pf[b][0:64, :], in1=RC[64:128, 1, :], op=ALU.mult), r=["pf%d" % b, "RC", "OD"], w=["OD"])
                    op("dve", lambda e: e.scalar_tensor_tensor(out=OD[0:64, 0, :], in0=OD[0:64, 1, :], scalar=lam_bc[0:64, 0:1], in1=OD[0:64, 0, :], op0=ALU.mult, op1=ALU.add),
                       r=["OD", "lam_bc"], w=["OD"])
                    op("act", lambda e: e.activation(out=OD[0:64, 1, :], in_=OD[0:64, 0, :], func=AF.Square), r=["OD"], w=["OD"])
                    op("pe", lambda e: e.matmul(pf[a][0:64, :], lhsT=ones_f[0:64, 0:64], rhs=OD[0:64, 1, :], start=True, stop=True), r=["OD", "ones_f"], w=["pf%d" % a])
                    op("act", lambda e: e.activation(out=OD[0:64, 1, :], in_=pf[a][0:64, :], func=AF.Ln, scale=1.0 / 64, bias=epst[0:64, 0:1]), r=["pf%d" % a, "OD", "epst"], w=["OD"])
                    op("act", lambda e: e.activation(out=OD[0:64, 1, :], in_=OD[0:64, 1, :], func=AF.Exp, scale=-0.5), r=["OD"], w=["OD"])
                    op("dve", lambda e: e.scalar_tensor_tensor(out=OD[hb_:hb_ + 64, 2, :], in0=OD[0:64, 0, :], scalar=sublnt[0:64, 0:1], in1=OD[0:64, 1, :], op0=ALU.mult, op1=ALU.mult),
                       r=["OD", "sublnt"], w=["OD"])
                    op("dve", lambda e: e.tensor_tensor(out=YSt[hb_:hb_ + 64, blk, :], in0=OD[hb_:hb_ + 64, 2, :], in1=G[hb_:hb_ + 64, blk, :], op=ALU.mult), r=["OD", Gk], w=[ysk])

                if "D" in MIX:
                    softmax_mixer("D", 22, 4, 18, 4, 9, 4, 6,
                                  [([(h, h, c * 64, 32) for c in range(2)], h) for h in range(4)], 32.0 ** -0.5, 0, post=post_D)

                def pre_C(ms):
                    ex = {}
                    ex["IKc"] = [sb(ms, "IKc%d" % i, [128, 512], BF16) for i in range(3)]
                    ex["IQ"] = [sb(ms, "IQ%d" % i, [128, 2, 512], BF16) for i in range(1)]
                    ex["IWt"] = [sb(ms, "IWt%d" % i, [128, 4, 4]) for i in range(2)]
                    ex["SC"] = sb(ms, "SC", [128, S])
                    ex["JK"] = sb(ms, "JK", [128, S], BF16)
                    ex["BD"] = sb(ms, "BD", [128, S], BF16)
                    ex["selT"] = [sb(ms, "selT" + str(i), [128, NT, 512], mybir.dt.float8e4) for i in range(2)]
                    ex["RL"] = [sb(ms, "RL%d" % i, [128, 512]) for i in range(2)]
                    ex["bs"] = sb(ms, "bs", [128, 64])
                    return ex

                def selT_C(j, ex, pump):
                    pump = pump if pump is not None else (lambda: None)
                    q0 = j * 512
                    IQ, IWt = ex["IQ"][0], ex["IWt"][j % 2]
                    iqk, iwk = "IQ0", "IWt%d" % (j % 2)
                    SC, JK, selT, RL, bs, IKc = ex["SC"], ex["JK"], ex["selT"][j % 2], ex["RL"], ex["bs"], ex["IKc"]
                    selk = "selT" + str(j % 2)
                    BD = ex["BD"]
                    dma(IQ[:], FT[15:17, :, q0:q0 + 512].rearrange("b p t -> p b t"), w=[iqk], slot=iqk)
                    dma(IWt[:], IW[q0:q0 + 512, :].rearrange("(a p) h -> p a h", p=128), w=[iwk], slot=iwk)
                    op("pool", lambda e: e.memset(selT[:, 4 * j:4 * j + 4, :], -240.0), w=[selk])
                    rl = 0
                    chunks = [(qi, c0) for qi in range(4) for c0 in range(0, (4 * j + qi + 1) * 128, 512)]

                    def load_ik(ci):
                        qi_, c0_ = chunks[ci]
                        n_ = min(512, (4 * j + qi_ + 1) * 128 - c0_)
                        kk_ = "IKc%d" % (ci % 3)
                        dma(IKc[ci % 3][:, 0:n_], FT[17, :, c0_:c0_ + n_], w=[kk_], slot=kk_)

                    load_ik(0)
                    ci = 0
                    for qi in range(4):
                        i = 4 * j + qi
                        Lk = (i + 1) * 128
                        for c0 in range(0, Lk, 512):
                            n = min(512, Lk - c0)
                            if ci + 1 < len(chunks):
                                load_ik(ci + 1)
                            IKb, ikk = IKc[ci % 3], "IKc%d" % (ci % 3)
                            ci += 1
                            for h in range(4):
                                blk, base = h // 2, (h % 2) * 64
                                bank = 4 + (h % 2)
                                op("pe", lambda e: e.matmul(pf[bank][:, 0:n], lhsT=IQ[base:base + 64, blk, qi * 128:(qi + 1) * 128], rhs=IKb[base:base + 64, 0:n],
                                                            start=True, stop=True), r=[iqk, ikk], w=["pf%d" % bank])
                                R = RL[rl % 2]
                                rk = "RL%d" % (rl % 2)
                                rl += 1
                                op("act", lambda e: e.activation(out=R[:, 0:n], in_=pf[bank][:, 0:n], func=AF.Relu), r=["pf%d" % bank], w=[rk])
                                if h == 0:
                                    op("dve", lambda e: e.tensor_scalar(out=SC[:, c0:c0 + n], in0=R[:, 0:n], scalar1=IWt[:, qi, 0:1], scalar2=None, op0=ALU.mult),
                                       r=[rk, iwk], w=["SC"])
                                else:
                                    op("dve", lambda e: e.scalar_tensor_tensor(out=SC[:, c0:c0 + n], in0=R[:, 0:n], scalar=IWt[:, qi, h:h + 1], in1=SC[:, c0:c0 + n],
                                                                                op0=ALU.mult, op1=ALU.add), r=[rk, iwk, "SC"], w=["SC"])
                                pump()
                        op("dve", lambda e: e.tensor_tensor(out=SC[:, Lk - 128:Lk], in0=SC[:, Lk - 128:Lk], in1=adm[:, 0, :], op=ALU.mult), r=["SC", "adm"], w=["SC"])
                        op("dve", lambda e: e.tensor_tensor(out=SC[:, Lk - 128:Lk], in0=SC[:, Lk - 128:Lk], in1=adm[:, 1, :], op=ALU.add), r=["SC", "adm"], w=["SC"])
                        if Lk <= TOPK:
                            op("dve", lambda e: e.tensor_scalar(out=JK[:, 0:Lk], in0=SC[:, 0:Lk], scalar1=-1.0e29, scalar2=None, op0=ALU.is_ge), r=["SC"], w=["JK"])
                        else:
                            op("dve", lambda e: e.max(out=bs[:, 0:8], in_=SC[:, 0:Lk]), r=["SC"], w=["bs_hi"])
                            op("dve", lambda e: e.tensor_reduce(out=bs[:, 8:9], in_=SC[:, 0:Lk - 128], axis=AX.X, op=ALU.min), r=["SC"], w=["bs_lo"])
                            op("dve", lambda e: e.tensor_tensor(out=bs[:, 9:10], in0=bs[:, 0:1], in1=bs[:, 8:9], op=ALU.subtract), r=["bs_hi", "bs_lo"], w=["bs_d"])
                            op("dve", lambda e: e.tensor_scalar(out=bs[:, 9:10], in0=bs[:, 9:10], scalar1=1.001, scalar2=1.0e-20, op0=ALU.mult, op1=ALU.add), r=["bs_d"], w=["bs_d"])
                            op("dve", lambda e: e.tensor_scalar(out=bs[:, 32:32 + NIT], in0=pow2[:, 0:NIT], scalar1=bs[:, 9:10], scalar2=None, op0=ALU.mult), r=["bs_d", "pow2"], w=["bs_dl"])
                            for k in range(NIT):
                                op("dve", lambda e, k=k: e.tensor_tensor(out=bs[:, 10:11], in0=bs[:, 8:9], in1=bs[:, 32 + k:33 + k], op=ALU.add), r=["bs_lo", "bs_dl"], w=["bs_mid"])
                                op("dve", lambda e: e.tensor_scalar(out=JK[:, 0:Lk], in0=SC[:, 0:Lk], scalar1=bs[:, 10:11], scalar2=0.0, op0=ALU.is_ge, op1=ALU.add,
                                                                    accum_out=bs[:, 11:12]), r=["SC", "bs_mid"], w=["JK", "bs_cnt"])
                                op("dve", lambda e, k=k: e.tensor_scalar(out=bs[:, 12:13], in0=bs[:, 11:12], scalar1=TOPK - 0.5, scalar2=bs[:, 32 + k:33 + k], op0=ALU.is_ge, op1=ALU.mult),
                                   r=["bs_cnt", "bs_dl"], w=["bs_t"])
                                op("dve", lambda e: e.tensor_tensor(out=bs[:, 8:9], in0=bs[:, 8:9], in1=bs[:, 12:13], op=ALU.add), r=["bs_lo", "bs_t"], w=["bs_lo"])
                                pump()
                                pump()
                            op("dve", lambda e: e.tensor_tensor(out=bs[:, 13:14], in0=bs[:, 8:9], in1=bs[:, 31 + NIT:32 + NIT], op=ALU.add), r=["bs_lo", "bs_dl"], w=["bs_hf"])
                            op("dve", lambda e: e.tensor_scalar(out=JK[:, 0:Lk], in0=SC[:, 0:Lk], scalar1=bs[:, 8:9], scalar2=0.0, op0=ALU.is_ge, op1=ALU.add,
                                                                accum_out=bs[:, 14:15]), r=["SC", "bs_lo"], w=["JK", "bs_cl"])
                            op("dve", lambda e: e.scalar_tensor_tensor(out=BD[:, 0:Lk], in0=SC[:, 0:Lk], scalar=bs[:, 13:14], in1=JK[:, 0:Lk], op0=ALU.is_lt, op1=ALU.mult,
                                                                        accum_out=bs[:, 15:16]), r=["SC", "JK", "bs_hf"], w=["BD", "bs_nb"])
                            op("dve", lambda e: e.tensor_tensor(out=bs[:, 16:17], in0=bs[:, 15:16], in1=bs[:, 14:15], op=ALU.subtract), r=["bs_nb", "bs_cl"], w=["bs_m"])
                            op("dve", lambda e: e.tensor_scalar(out=bs[:, 16:17], in0=bs[:, 16:17], scalar1=float(TOPK) + 0.5, scalar2=None, op0=ALU.add), r=["bs_m"], w=["bs_m"])
                            op("dve", lambda e: e.tensor_tensor_scan(out=SC[:, 0:Lk], data0=ones_bf[:, 0:1].to_broadcast([128, Lk]), data1=BD[:, 0:Lk], initial=0.0, op0=ALU.mult, op1=ALU.add),
                               r=["BD", "ones_bf", "SC"], w=["SC"])
                            op("dve", lambda e: e.scalar_tensor_tensor(out=BD[:, 0:Lk], in0=SC[:, 0:Lk], scalar=bs[:, 16:17], in1=BD[:, 0:Lk], op0=ALU.is_gt, op1=ALU.mult),
                               r=["SC", "BD", "bs_m"], w=["BD"])
                            op("pool", lambda e: e.tensor_tensor(out=JK[:, 0:Lk], in0=JK[:, 0:Lk], in1=BD[:, 0:Lk], op=ALU.subtract), r=["JK", "BD"], w=["JK"])
                        if debug:
                            dma(DSC[i, :, :], SC[:, :], r=["SC"], slot="dbg1")
                            dma(DBS[i, :, :], bs[:, :], r=["bs_lo", "bs_hi", "bs_d", "bs_dl", "bs_mid", "bs_cnt", "bs_t", "bs_m", "bs_cl", "bs_nb", "bs_hf"], slot="dbg2")
                        for g in range(0, i + 1, 8):
                            g1 = min(i + 1, g + 8)
                            bank = (g // 8) % 2
                            for kb in range(g, g1):
                                op("pe", lambda e, kb=kb: e.transpose(pb[bank][:, (kb - g) * 128:(kb - g + 1) * 128], JK[:, kb * 128:(kb + 1) * 128], ident[:]),
                                   r=["JK", "ident"], w=["pb%d" % bank])
                            nb_ = g1 - g
                            op("dve", lambda e: e.tensor_scalar(out=selT[:, g:g1, qi * 128:(qi + 1) * 128], in0=pb[bank][:, 0:nb_ * 128].rearrange("p (b t) -> p b t", b=nb_),
                                                                scalar1=-1.0, scalar2=240.0, op0=ALU.add, op1=ALU.mult, saturate=False), r=["pb%d" % bank], w=[selk])
                            pump()
                    return selT, selk

                if "C" in MIX:
                    softmax_mixer("C", 14, 1, 12, 2, 8, 1, 4, [([(0, h // 2, (h % 2) * 64, 64)], 0) for h in range(4)], 0.125, None, selT_fn=selT_C, pre_q=pre_C)

                if "A" in MIX:
                    with ExitStack() as ms:
                        Kt = load_K(ms, "Kt", 2, 2)
                        V = load_V(ms, "Vr", 0, 4)
                        nK = sb(ms, "nK", [128, 2, S], BF16)
                        for b in range(2):
                            op("dve" if b == 0 else "pool", lambda e, b=b: e.tensor_scalar(out=nK[:, b, :], in0=Kt[:, b, :], scalar1=-0.125, scalar2=0.0, op0=ALU.mult, op1=ALU.add), r=["Kt"], w=["nK"])
                        Qs = [sb(ms, "Qs%d" % i, [128, 2, 512], BF16) for i in range(2)]
                        Gs = [sb(ms, "Gs%d" % i, [128, 2, 512]) for i in range(2)]
                        NBUF = 3
                        Et = [sb(ms, "Et%d" % i, [128, 512]) for i in range(NBUF)]
                        SPt = [sb(ms, "SPt%d" % i, [128, 512]) for i in range(NBUF)]
                        SPh = [sb(ms, "SPh%d" % i, [128, 512], BF16) for i in range(NBUF)]
                        SPl = [sb(ms, "SPl%d" % i, [128, 512], BF16) for i in range(NBUF)]
                        Cf = [sb(ms, "Cf%d" % i, [128, 512]) for i in range(2)]
                        Ch = [[sb(ms, "Ch%d_%d" % (i, k), [128, 512], BF16) for k in range(2)] for i in range(2)]
                        Cl = [[sb(ms, "Cl%d_%d" % (i, k), [128, 512], BF16) for k in range(2)] for i in range(2)]
                        PT = [sb(ms, "PT%d" % i, [128, 512], BF16) for i in range(3)]
                        YS = [sb(ms, "YS%d" % i, [128, 2, 512], BF16) for i in range(2)]
                        gc = {"g1": 0, "g2": 0}

                        def load_qA(j):
                            q0 = j * 512
                            dma(Qs[j % 2][:], FT[0:2, :, q0:q0 + 512].rearrange("b p t -> p b t"), w=["Qs%d" % (j % 2)], slot="Qs%d" % (j % 2))
                            dma(Gs[j % 2][:], GT[0:2, :, q0:q0 + 512].rearrange("b p t -> p b t"), w=["Gs%d" % (j % 2)], slot="Gs%d" % (j % 2))

                        load_qA(0)
                        for j in range(NQ):
                            if j + 1 < NQ:
                                load_qA(j + 1)
                            Qk, Gk = "Qs%d" % (j % 2), "Gs%d" % (j % 2)
                            Q, G = Qs[j % 2], Gs[j % 2]
                            nkt = 4 * (j + 1)
                            ysk = "YS%d" % (j % 2)
                            for pair in range(2):
                                seq = [(kt, sl) for kt in range(nkt - 1, -1, -1) for sl in range(2)]
                                info = {}
                                cidx = [0, 0]

                                def stage1(idx):
                                    kt, sl = seq[idx]
                                    h = 2 * pair + sl
                                    blk, base = h // 2, (h % 2) * 64
                                    b1 = gc["g1"] % NBUF
                                    zb = gc["g1"] % 2
                                    gc["g1"] += 1
                                    info[idx] = b1
                                    r_ = kt - (nkt - 4)
                                    ksl = slice(kt * 128, (kt + 1) * 128)
                                    op("pe", lambda e: e.matmul(pf[zb][:, :], lhsT=Kt[base:base + 64, blk, ksl], rhs=Q[base:base + 64, blk, :], start=True, stop=True),
                                       r=["Kt", Qk], w=["pf%d" % zb])
                                    op("act", lambda e: e.activation(out=Et[b1][:], in_=pf[zb][:, :], func=AF.Exp, scale=0.125), r=["pf%d" % zb], w=["Et%d" % b1])
                                    op("act", lambda e: e.activation(out=SPt[b1][:], in_=Et[b1][:], func=AF.Ln, bias=1.0, scale=1.0), r=["Et%d" % b1], w=["SPt%d" % b1])
                                    if r_ >= 0:
                                        op("dve", lambda e: e.tensor_tensor(out=SPt[b1][:], in0=SPt[b1][:], in1=masks[:, 4 + r_, :], op=ALU.mult), r=["SPt%d" % b1, "masks"], w=["SPt%d" % b1])
                                    op("dve", lambda e: e.tensor_copy(out=SPh[b1][:], in_=SPt[b1][:]), r=["SPt%d" % b1], w=["SPh%d" % b1])
                                    op("dve", lambda e: e.tensor_tensor(out=SPl[b1][:], in0=SPt[b1][:], in1=SPh[b1][:], op=ALU.subtract), r=["SPt%d" % b1, "SPh%d" % b1], w=["SPl%d" % b1])

                                def stage2(idx):
                                    kt, sl = seq[idx]
                                    h = 2 * pair + sl
                                    blk, base = h // 2, (h % 2) * 64
                                    b1 = info.pop(idx)
                                    rb = 2 + gc["g2"] % 2
                                    p2 = gc["g2"] % 3
                                    gc["g2"] += 1
                                    ob = 4 + sl
                                    first = (kt == nkt - 1)
                                    r_ = kt - (nkt - 4)
                                    ksl = slice(kt * 128, (kt + 1) * 128)
                                    op("pe", lambda e: e.matmul(pf[rb][:, :], lhsT=tri[:], rhs=SPh[b1][:], start=True, stop=False), r=["tri", "SPh%d" % b1], w=["pf%d" % rb])
                                    op("pe", lambda e: e.matmul(pf[rb][:, :], lhsT=tri[:], rhs=SPl[b1][:], start=False, stop=False), r=["tri", "SPl%d" % b1], w=["pf%d" % rb])
                                    if not first:
                                        cu = cidx[sl] % 2
                                        op("pe", lambda e: e.matmul(pf[rb][:, :], lhsT=ones_bf[:], rhs=Ch[sl][cu][:], start=False, stop=False), r=["ones_bf", "Ch%d_%d" % (sl, cu)], w=["pf%d" % rb])
                                        op("pe", lambda e: e.matmul(pf[rb][:, :], lhsT=ones_bf[:], rhs=Cl[sl][cu][:], start=False, stop=False), r=["ones_bf", "Cl%d_%d" % (sl, cu)], w=["pf%d" % rb])
                                    op("pe", lambda e: e.matmul(pf[rb][:, :], lhsT=nK[base:base + 64, blk, ksl], rhs=Q[base:base + 64, blk, :], start=False, stop=True),
                                       r=["nK", Qk], w=["pf%d" % rb])
                                    if kt > 0:
                                        cidx[sl] += 1
                                        cw = cidx[sl] % 2
                                        if first:
                                            op("pool", lambda e: e.tensor_copy(out=Cf[sl][:], in_=SPt[b1][:]), r=["SPt%d" % b1], w=["Cf%d" % sl])
                                        else:
                                            op("pool", lambda e: e.tensor_tensor(out=Cf[sl][:], in0=Cf[sl][:], in1=SPt[b1][:], op=ALU.add), r=["SPt%d" % b1, "Cf%d" % sl], w=["Cf%d" % sl])
                                        op("act", lambda e: e.activation(out=Ch[sl][cw][:], in_=Cf[sl][:], func=AF.Copy), r=["Cf%d" % sl], w=["Ch%d_%d" % (sl, cw)])
                                        op("pool", lambda e: e.tensor_tensor(out=Cl[sl][cw][:], in0=Cf[sl][:], in1=Ch[sl][cw][:], op=ALU.subtract), r=["Cf%d" % sl, "Ch%d_%d" % (sl, cw)], w=["Cl%d_%d" % (sl, cw)])
                                    op("act", lambda e: e.activation(out=PT[p2][:], in_=pf[rb][:, :], func=AF.Exp, scale=-1.0), r=["pf%d" % rb], w=["PT%d" % p2])
                                    if r_ >= 0:
                                        op("dve", lambda e: e.tensor_tensor(out=PT[p2][:], in0=PT[p2][:], in1=masks[:, 4 + r_, :], op=ALU.mult), r=["PT%d" % p2, "masks"], w=["PT%d" % p2])
                                    op("pe", lambda e: e.matmul(pf[ob][0:64, :], lhsT=V[:, kt, h, 0:64], rhs=PT[p2][:], start=first, stop=(kt == 0)), r=["Vr", "PT%d" % p2], w=["pf%d" % ob])

                                LA = 2
                                for idx in range(len(seq) + LA):
                                    if idx < len(seq):
                                        stage1(idx)
                                    if idx >= LA:
                                        stage2(idx - LA)
                                for sl in range(2):
                                    h = 2 * pair + sl
                                    blk, base = h // 2, (h % 2) * 64
                                    ob = 4 + sl
                                    op("dve", lambda e: e.tensor_tensor(out=YS[j % 2][base:base + 64, blk, :], in0=pf[ob][0:64, :], in1=G[base:base + 64, blk, :], op=ALU.mult),
                                       r=["pf%d" % ob, Gk], w=[ysk])
                            q0 = j * 512
                            dma(YT[0:2, :, q0:q0 + 512].rearrange("b p t -> p b t"), YS[j % 2][:], r=[ysk], slot=ysk)
                        sc.barrier()

                if DO_OUT:
                    with ExitStack() as ms:
                        wo = sb(ms, "wo", [128, 8, D_MODEL], BF16)
                        wos = [sb(ms, "wos%d" % i, [128, D_MODEL]) for i in range(2)]
                        for c in range(8):
                            k = "wos%d" % (c % 2)
                            dma(wos[c % 2][:], w_out[l, c * 128:(c + 1) * 128, :], w=[k], slot=k)
                            op("dve" if c % 2 == 0 else "pool", lambda e, c=c: e.tensor_copy(out=wo[:, c, :], in_=wos[c % 2][:]), r=[k], w=["wo"])
                        YTs = [sb(ms, "YTs%d" % i, [128, 8, 512], BF16) for i in range(2)]
                        xs = [sb(ms, "xs%d" % i, [128, D_MODEL]) for i in range(2)]
                        xo = [sb(ms, "xo%d" % i, [128, D_MODEL]) for i in range(2)]

                        def load_y(stl):
                            q0 = stl * 512
                            for g in range(0, 8, 4):
                                dma(YTs[stl % 2][:, g:g + 4, :], YT[g:g + 4, :, q0:q0 + 512].rearrange("b p t -> p b t"), w=["YTs%d" % (stl % 2)], slot="YTs%d" % (stl % 2))

                        load_y(0)
                        for stl in range(NST):
                            if stl + 1 < NST:
                                load_y(stl + 1)
                            for sub in range(4):
                                t = stl * 4 + sub
                                xk, ok = "xs%d" % (t % 2), "xo%d" % (t % 2)
                                dma(xs[t % 2][:], x_src[t * 128:(t + 1) * 128, :], w=[xk], slot=xk)
                                for half in range(2):
                                    bank = (t * 2 + half) % 2
                                    for c in range(8):
                                        op("pe", lambda e, c=c: e.matmul(pf[bank][:, :], lhsT=YTs[stl % 2][:, c, sub * 128:(sub + 1) * 128], rhs=wo[:, c, half * 512:(half + 1) * 512],
                                                                         start=(c == 0), stop=(c == 7)), r=["YTs%d" % (stl % 2), "wo"], w=["pf%d" % bank])
                                    op("dve", lambda e: e.tensor_tensor(out=xo[t % 2][:, half * 512:(half + 1) * 512], in0=pf[bank][:, :], in1=xs[t % 2][:, half * 512:(half + 1) * 512], op=ALU.add),
                                       r=["pf%d" % bank, xk], w=[ok])
                                dma(x_dst[t * 128:(t + 1) * 128, :], xo[t % 2][:], r=[ok], slot=ok)
                        sc.barrier()
        sc.barrier()
        print("instructions:", sc.n_ins)
    return nc


def host_consts(S):
    bf = ml_dtypes.bfloat16
    c = {}
    c["c_ident"] = np.eye(128, dtype=np.float32).astype(bf)

    def cs(d):
        inv = (10000.0 ** (-np.arange(0, d, 2, dtype=np.float32) / np.float32(d))).astype(np.float32)
        ang = (np.arange(S, dtype=np.float32)[:, None] * inv[None, :]).astype(np.float32)
        return np.concatenate([np.cos(ang), np.sin(ang)], axis=1).astype(np.float32)

    c["c_cs64"] = cs(64)
    c["c_cs32"] = cs(32)
    kk = np.arange(128)[:, None]
    qq = np.arange(512)[None, :]
    m = []
    for r in range(4):
        m.append(((r * 128 + kk) < ((qq // 64) + 1) * 64))
    for r in range(4):
        m.append(((r * 128 + kk) < qq))
    c["c_mask"] = np.concatenate(m, axis=1).astype(np.float32).astype(bf)
    q1 = np.arange(128)[:, None]
    k1 = np.arange(128)[None, :]
    m01 = (k1 < ((q1 // 64) + 1) * 64).astype(np.float32)
    c["c_adm"] = np.concatenate([m01, (1.0 - m01) * NEGBIG], axis=1).astype(np.float32)
    c["c_tri"] = (np.arange(128)[:, None] >= np.arange(128)[None, :]).astype(np.float32).astype(bf)
    c["c_pow2"] = np.tile((2.0 ** -(np.arange(32) + 1.0))[None, :], (128, 1)).astype(np.float32)
    return c


def host_params(inp, DEPTH):
    f = lambda a: np.ascontiguousarray(np.asarray(a, dtype=np.float32)[:DEPTH])
    p = {}
    p["w_in"] = np.ascontiguousarray(f(inp["w_in"])[:, :, PERM])
    p["w_out"] = f(inp["w_out"])
    p["w_uq"] = f(inp["mla_w_uq"])
    p["w_ukv"] = f(inp["mla_w_ukv"])
    p["ln_g"] = np.ascontiguousarray(f(inp["ln_g"]).reshape(-1, 8, 128).transpose(0, 2, 1))
    p["qn_g"] = np.ascontiguousarray(f(inp["mla_q_norm_g"]).reshape(-1, 2, 128).transpose(0, 2, 1))
    p["kvn_g"] = np.ascontiguousarray(f(inp["mla_kv_norm_g"]).reshape(-1, 128, 1))
    dq, dkk = f(inp["dsa_q_g"]), f(inp["dsa_k_g"])
    p["g64"] = np.ascontiguousarray(np.concatenate([dq] * 4 + [dkk], axis=1))
    fq, fk, mk = f(inp["diff_q_g"]), f(inp["diff_k_g"]), f(inp["mla_k_g"])
    p["g32"] = np.ascontiguousarray(np.concatenate([fq] * 8 + [fk] * 8 + [mk[:, 64:96]], axis=1))
    p["gq96"] = f(inp["mla_q_g"])
    p["gk64"] = np.ascontiguousarray(mk[:, :64])
    p["subln"] = np.ascontiguousarray(f(inp["diff_subln_g"]).reshape(-1, 64, 1))
    p["lvec"] = np.ascontiguousarray(np.concatenate([f(inp["diff_lq1"]), f(inp["diff_lk1"]), f(inp["diff_lq2"]), f(inp["diff_lk2"])], axis=1))
    return p


_CACHE = {}


def run(inputs, S, DEPTH, debug=False):
    import math
    key = (S, DEPTH, debug)
    lam_inits = [0.8 - 0.6 * math.exp(-0.3 * l) for l in range(DEPTH)]
    if key not in _CACHE:
        _CACHE[key] = build(S, DEPTH, debug, lam_inits, MIX=globals().get("MIXSEL", "ABCD"))
    nc = _CACHE[key]
    x = np.asarray(inputs["x"], dtype=np.float32)
    B = x.shape[0]
    shared = {}
    shared.update(host_consts(S))
    shared.update(host_params(inputs, DEPTH))
    in_maps = []
    for b in range(B):
        m = dict(shared)
        m["x"] = np.ascontiguousarray(x[b, :S])
        in_maps.append(m)
    res = run_bass_kernel_spmd(nc, in_maps, core_ids=list(range(B)))
    return res


def kernel(**inputs):
    res = run(inputs, 8192, 4)
    return np.stack([np.asarray(r["y"], dtype=np.float32) for r in res.results], axis=0)
```

```python
import numpy as np
import ml_dtypes
from contextlib import ExitStack
import concourse.bass as bass
import concourse.mybir as mybir
from concourse.bass_utils import run_bass_kernel_spmd

F32 = mybir.dt.float32
BF16 = mybir.dt.bfloat16
AF = mybir.ActivationFunctionType
ALU = mybir.AluOpType
AX = mybir.AxisListType

D_MODEL = 1024
IN_COLS = 3684
EPS = 1e-6
NEGBIG = -1.0e30

O_AQ, O_AK, O_AV, O_AG = 0, 256, 512, 768
O_BCQ, O_BCKV, O_BKR, O_BG = 1024, 1280, 1408, 1440
O_CQ, O_CK, O_CV, O_CG, O_CIQ, O_CIK, O_CIW = 1696, 1952, 2016, 2080, 2336, 2592, 2656
O_DQ, O_DK, O_DV, O_DG = 2660, 2916, 3172, 3428


def _perm():
    r = lambda a, n: list(range(a, a + n))
    p = []
    p += r(O_AQ, 256) + r(O_AK, 256)
    p += r(O_AG, 256) + r(O_BG, 256) + r(O_CG, 256) + r(O_DG, 256)
    p += r(O_AV, 256) + r(O_DV, 256) + r(O_CV, 64)
    p += r(O_CQ, 256) + r(O_CK, 64) + r(O_CIQ, 256) + r(O_CIK, 64)
    p += r(O_DQ, 256) + r(O_DK, 256) + r(O_BKR, 32)
    p += r(O_BCQ, 256) + r(O_BCKV, 128)
    p += r(O_CIW, 4)
    assert len(p) == IN_COLS and sorted(p) == list(range(IN_COLS))
    return np.array(p)


PERM = _perm()
T0 = 1536
NTM = IN_COLS - T0
P_AV, P_DV, P_CV = 0, 256, 512
P_X64 = 576
P_Y32 = 1216
P_CQ = 1760
P_CKV = 2016
P_IW = 2144
NB_FT = 26


class Sched:
    ENG = ("pe", "act", "dve", "pool", "sp")

    def __init__(self, nc, es):
        self.nc = nc
        self.es = es
        self.e = dict(pe=nc.tensor, act=nc.scalar, dve=nc.vector, pool=nc.gpsimd, sp=nc.sync)
        self.semobj = {}
        self.cnt = {}
        for k in ("pe", "act", "dve", "pool"):
            self.semobj[k] = es.enter_context(nc.semaphore("s_" + k))
            self.cnt[k] = 0
        self.known = {k: {} for k in self.ENG}
        self.lastw = {}
        self.reads = {}
        self.n_ins = 0

    def _dsem(self, slot):
        name = "d_" + slot
        if name not in self.semobj:
            self.semobj[name] = self.es.enter_context(self.nc.semaphore(name))
            self.cnt[name] = 0
        return name

    def _waits(self, E, r, w, attach_ok=False):
        need = {}

        def add(tok, kind):
            sname, val, prod = tok
            if prod == E:
                if E == "pe" or kind == "war":
                    return
            if self.known[E].get(sname, 0) >= val:
                return
            if need.get(sname, 0) < val:
                need[sname] = val

        for k in r:
            t = self.lastw.get(k)
            if t is not None:
                add(t, "raw")
        for k in w:
            t = self.lastw.get(k)
            if t is not None:
                add(t, "waw")
            for t in self.reads.get(k, {}).values():
                add(t, "war")
        items = list(need.items())
        attach = None
        if attach_ok and items:
            attach = items.pop()
        for sname, val in items:
            self.e[E].wait_ge(self.semobj[sname], val)
            self.known[E][sname] = val
            self.n_ins += 1
        if attach is not None:
            self.known[E][attach[0]] = attach[1]
        return attach

    def _record(self, tok, r, w):
        for k in r:
            d = self.reads.setdefault(k, {})
            d[tok[0]] = tok
        for k in w:
            self.lastw[k] = tok
            self.reads[k] = {}

    def op(self, E, fn, r=(), w=()):
        att = self._waits(E, r, w, attach_ok=True)
        ins = fn(self.e[E])
        if att is not None:
            ins._wait_ge(self.semobj[att[0]], att[1])
        self.cnt[E] += 1
        ins.then_inc(self.semobj[E], 1)
        tok = (E, self.cnt[E], E)
        self._record(tok, r, w)
        self.n_ins += 1
        return tok

    def dma(self, out, in_, r=(), w=(), slot=None, Q="sp"):
        self._waits(Q, r, w)
        sname = self._dsem(slot)
        ins = self.e[Q].dma_start(out=out, in_=in_)
        self.cnt[sname] += 16
        ins.then_inc(self.semobj[sname], 16)
        tok = (sname, self.cnt[sname], "dma")
        self._record(tok, r, w)
        self.n_ins += 1
        return tok

    def barrier(self, engines=None):
        for E in (engines or self.ENG):
            for sname, c in self.cnt.items():
                if c == 0 or self.known[E].get(sname, 0) >= c:
                    continue
                if sname == E and E == "pe":
                    continue
                self.e[E].wait_ge(self.semobj[sname], c)
                self.known[E][sname] = c
                self.n_ins += 1
        if engines is None:
            self.lastw = {}
            self.reads = {}


def bc(ap, shape, axis):
    return ap.unsqueeze(axis).to_broadcast(list(shape))


def build(S, DEPTH, debug=False, lam_inits=None, MIX="ABCD", DO_OUT=True):
    TOPK = min(256, S // 4)
    NT = S // 128
    NST = S // 512
    nc = bass.Bass("TRN2", target_bir_lowering=False)

    def din(name, shape, dt=F32):
        return nc.dram_tensor(name, list(shape), dt, kind="ExternalInput").ap()

    x_in = din("x", [S, D_MODEL])
    w_in = din("w_in", [DEPTH, D_MODEL, IN_COLS])
    w_out = din("w_out", [DEPTH, D_MODEL, D_MODEL])
    w_uq = din("w_uq", [DEPTH, 256, 384])
    w_ukv = din("w_ukv", [DEPTH, 128, 512])
    ln_g = din("ln_g", [DEPTH, 128, 8])
    qn_g = din("qn_g", [DEPTH, 128, 2])
    kvn_g = din("kvn_g", [DEPTH, 128, 1])
    g64 = din("g64", [DEPTH, 5 * 64])
    g32 = din("g32", [DEPTH, 17 * 32])
    gq96 = din("gq96", [DEPTH, 96])
    gk64 = din("gk64", [DEPTH, 64])
    subln = din("subln", [DEPTH, 64, 1])
    lvec = din("lvec", [DEPTH, 4 * 32])
    c_ident = din("c_ident", [128, 128], BF16)
    c_cs64 = din("c_cs64", [S, 64])
    c_cs32 = din("c_cs32", [S, 32])
    c_mask = din("c_mask", [128, 8 * 512], BF16)
    c_adm = din("c_adm", [128, 2 * 128])
    c_tri = din("c_tri", [128, 128], BF16)
    c_pow2 = din("c_pow2", [128, 32])

    okind = "ExternalOutput"
    y_out = nc.dram_tensor("y", [S, D_MODEL], F32, kind=okind).ap()
    dk = okind if debug else "Internal"
    FT = nc.dram_tensor("FT", [NB_FT, 128, S], BF16, kind=dk).ap()
    GT = nc.dram_tensor("GT", [8, 128, S], F32, kind=dk).ap()
    VA = nc.dram_tensor("VA", [S, 13, 128], BF16, kind=dk).ap()
    IW = nc.dram_tensor("IW", [S, 4], F32, kind=dk).ap()
    YT = nc.dram_tensor("YT", [8, 128, S], BF16, kind=dk).ap()
    XR = nc.dram_tensor("XR", [S, D_MODEL], F32, kind="Internal").ap()
    if debug:
        DSEL = nc.dram_tensor("DSEL", [S // 512, 128, S // 128, 512], BF16, kind=okind).ap()
        DSC = nc.dram_tensor("DSC", [S // 128, 128, S], F32, kind=okind).ap()
        DBS = nc.dram_tensor("DBS", [S // 128, 128, 64], F32, kind=okind).ap()

    with ExitStack() as es:
        sc = Sched(nc, es)
        op, dma = sc.op, sc.dma

        uniq = [0]

        def sb(st, name, shape, dt=F32):
            uniq[0] += 1
            return st.enter_context(nc.sbuf_tensor("%s_%d" % (name, uniq[0]), list(shape), dt))

        def ps(st, name, shape, dt=F32):
            return st.enter_context(nc.psum_tensor(name, list(shape), dt))

        ident = sb(es, "ident", [128, 128], BF16)
        masks = sb(es, "masks", [128, 8, 512], BF16)
        adm = sb(es, "adm", [128, 2, 128])
        tri = sb(es, "tri", [128, 128], BF16)
        ones_bf = sb(es, "ones_bf", [128, 128], BF16)
        ones_f = sb(es, "ones_f", [128, 128])
        pow2 = sb(es, "pow2", [128, 32])
        neghalf = sb(es, "neghalf", [128, 32])
        epst = sb(es, "epst", [128, 1])
        nh512 = sb(es, "nh512", [128, 512])
        dma(ident[:], c_ident[:, :], w=["ident"], slot="k_ident")
        dma(masks[:].rearrange("p a b -> p (a b)"), c_mask[:, :], w=["masks"], slot="k_masks")
        dma(adm[:].rearrange("p a b -> p (a b)"), c_adm[:, :], w=["adm"], slot="k_adm")
        dma(tri[:], c_tri[:, :], w=["tri"], slot="k_tri")
        dma(pow2[:], c_pow2[:, :], w=["pow2"], slot="k_pow2")
        op("dve", lambda e: e.memset(ones_bf[:], 1.0), w=["ones_bf"])
        op("dve", lambda e: e.memset(ones_f[:], 1.0), w=["ones_f"])
        op("dve", lambda e: e.memset(neghalf[:], -0.5), w=["neghalf"])
        op("dve", lambda e: e.memset(epst[:], EPS), w=["epst"])
        op("dve", lambda e: e.memset(nh512[:], -0.5), w=["nh512"])

        pf = [ps(es, "pf%d" % i, [128, 512]) for i in range(6)]
        pb = [ps(es, "pb%d" % i, [128, 1024], BF16) for i in range(2)]

        for l in range(DEPTH):
            x_src = x_in if l == 0 else XR
            x_dst = y_out if l == DEPTH - 1 else XR
            lam_init = float(lam_inits[l])
            with ExitStack() as ls:
                sublnt = sb(ls, "sublnt", [64, 1])
                lv = sb(ls, "lv", [1, 4, 32])
                lam_bc = sb(ls, "lam_bc", [128, 2])
                dma(sublnt[:], subln[l, :, :], w=["sublnt"], slot="k_sublnt")
                dma(lv[:].rearrange("p a b -> p (a b)"), lvec[l:l + 1, :], w=["lv"], slot="k_lv")
                op("dve", lambda e: e.tensor_scalar(out=sublnt[:], in0=sublnt[:], scalar1=1.0 - lam_init, scalar2=None,
                                                    op0=ALU.mult), r=["sublnt"], w=["sublnt"])
                lt = sb(ls, "lt", [1, 2, 32])
                l2 = sb(ls, "l2", [1, 4])
                op("dve", lambda e: e.tensor_tensor(out=lt[:, 0, :], in0=lv[:, 0, :], in1=lv[:, 1, :], op=ALU.mult), r=["lv"], w=["lt"])
                op("dve", lambda e: e.tensor_tensor(out=lt[:, 1, :], in0=lv[:, 2, :], in1=lv[:, 3, :], op=ALU.mult), r=["lv", "lt"], w=["lt"])
                op("dve", lambda e: e.tensor_reduce(out=l2[:, 0:2], in_=lt[:], axis=AX.X, op=ALU.add), r=["lt"], w=["l2"])
                op("act", lambda e: e.activation(out=l2[:, 0:2], in_=l2[:, 0:2], func=AF.Exp), r=["l2"], w=["l2"])
                op("dve", lambda e: e.tensor_tensor(out=l2[:, 2:3], in0=l2[:, 0:1], in1=l2[:, 1:2], op=ALU.subtract), r=["l2"], w=["l2"])
                op("dve", lambda e: e.tensor_scalar(out=l2[:, 3:4], in0=l2[:, 2:3], scalar1=lam_init, scalar2=None, op0=ALU.add), r=["l2"], w=["l2"])
                op("dve", lambda e: e.tensor_scalar(out=l2[:, 2:3], in0=l2[:, 3:4], scalar1=-1.0, scalar2=None, op0=ALU.mult), r=["l2"], w=["l2"])
                op("pe", lambda e: e.matmul(pf[0][:, 0:2], lhsT=ones_f[0:1, :], rhs=l2[0:1, 2:4], start=True, stop=True),
                   r=["l2", "ones_f"], w=["pf0"])
                op("dve", lambda e: e.tensor_copy(out=lam_bc[:], in_=pf[0][:, 0:2]), r=["pf0"], w=["lam_bc"])

                with ExitStack() as p1:
                    w_sb = sb(p1, "w_sb", [128, 8, IN_COLS], BF16)
                    g64t = sb(p1, "g64t", [128, 5, 64])
                    g32t = sb(p1, "g32t", [128, 17, 32])
                    gq96t = sb(p1, "gq96t", [128, 96])
                    gk64t = sb(p1, "gk64t", [128, 64])
                    dma(g64t[:].rearrange("p a b -> p (a b)"), g64[l:l + 1, :].to_broadcast([128, 320]), w=["g64t"], slot="k_g64t")
                    dma(g32t[:].rearrange("p a b -> p (a b)"), g32[l:l + 1, :].to_broadcast([128, 544]), w=["g32t"], slot="k_g32t")
                    dma(gq96t[:], gq96[l:l + 1, :].to_broadcast([128, 96]), w=["gq96t"], slot="k_gq96t")
                    dma(gk64t[:], gk64[l:l + 1, :].to_broadcast([128, 64]), w=["gk64t"], slot="k_gk64t")
                    wuq_sb = sb(p1, "wuq_sb", [128, 2, 384], BF16)
                    wukv_sb = sb(p1, "wukv_sb", [128, 512], BF16)
                    lng = sb(p1, "lng", [128, 8])
                    qng = sb(p1, "qng", [128, 2])
                    kvng = sb(p1, "kvng", [128, 1])
                    dma(lng[:], ln_g[l, :, :], w=["lng"], slot="k_lng")
                    dma(qng[:], qn_g[l, :, :], w=["qng"], slot="k_qng")
                    dma(kvng[:], kvn_g[l, :, :], w=["kvng"], slot="k_kvng")
                    with ExitStack() as p0:
                        wst = [sb(p0, "wst%d" % i, [128, IN_COLS]) for i in range(2)]
                        for c in range(8):
                            k = "wst%d" % (c % 2)
                            dma(wst[c % 2][:], w_in[l, c * 128:(c + 1) * 128, :], w=[k], slot=k)
                            eng = "dve" if c % 2 == 0 else "pool"
                            op(eng, lambda e, c=c: e.tensor_scalar(out=w_sb[:, c, :], in0=wst[c % 2][:], scalar1=lng[:, c:c + 1],
                                                                    scalar2=0.0, op0=ALU.mult, op1=ALU.add), r=[k, "lng"], w=["w_sb"])
                        for c in range(2):
                            dma(wst[c][:, 0:384], w_uq[l, c * 128:(c + 1) * 128, :], w=["wst%d" % c], slot="wst%d" % c)
                            op("dve", lambda e, c=c: e.tensor_scalar(out=wuq_sb[:, c, :], in0=wst[c][:, 0:384], scalar1=qng[:, c:c + 1],
                                                                      scalar2=None, op0=ALU.mult), r=["wst%d" % c, "qng"], w=["wuq_sb"])
                        dma(wst[0][:, 0:512], w_ukv[l, :, :], w=["wst0"], slot="wst0")
                        op("dve", lambda e: e.tensor_scalar(out=wukv_sb[:], in0=wst[0][:, 0:512], scalar1=kvng[:, 0:1],
                                                            scalar2=None, op0=ALU.mult), r=["wst0", "kvng"], w=["wukv_sb"])
                        sc.barrier()
                    xt = [sb(p1, "xt%d" % i, [128, D_MODEL]) for i in range(2)]
                    junk = sb(p1, "junk", [128, D_MODEL], BF16)
                    hb = sb(p1, "hb", [128, D_MODEL], BF16)
                    hT = sb(p1, "hT", [128, 8, 512], BF16)
                    proj = sb(p1, "proj", [128, NTM])
                    tmp = sb(p1, "tmp", [128, 640])
                    tmp2 = sb(p1, "tmp2", [128, 640])
                    st8 = sb(p1, "st8", [128, 64])
                    cs64 = [sb(p1, "cs64_%d" % i, [128, 64]) for i in range(2)]
                    cs32 = [sb(p1, "cs32_%d" % i, [128, 32]) for i in range(2)]
                    TT = sb(p1, "TT", [128, 22, 128], BF16)
                    VS = sb(p1, "VS", [128, 13, 128], BF16)
                    IWs = sb(p1, "IWs", [128, 4])
                    QB = sb(p1, "QB", [128, 4, 96])
                    KV = sb(p1, "KV", [128, 4, 128])
                    cqn = sb(p1, "cqn", [128, 384], BF16)
                    cqT = sb(p1, "cqT", [128, 3, 128], BF16)
                    FTs = sb(p1, "FTs", [128, NB_FT, 512], BF16)
                    GS = sb(p1, "GS", [128, 8, 512])
                    op("pool", lambda e: e.memset(TT[:], 0.0), w=["TT"])
                    op("pool", lambda e: e.memset(VS[:], 1.0), w=["VS"])

                    def load_x(t):
                        k = "xt%d" % (t % 2)
                        dma(xt[t % 2][:], x_src[t * 128:(t + 1) * 128, :], w=[k], slot=k)
                        dma(cs64[t % 2][:], c_cs64[t * 128:(t + 1) * 128, :], w=["cs64_%d" % (t % 2)], slot="cs64_%d" % (t % 2))
                        dma(cs32[t % 2][:], c_cs32[t * 128:(t + 1) * 128, :], w=["cs32_%d" % (t % 2)], slot="cs32_%d" % (t % 2))

                    def rstd_of(ss_ap, n, d, key):
                        op("act", lambda e: e.activation(out=ss_ap, in_=ss_ap, func=AF.Ln, scale=1.0 / d, bias=epst[:, 0:1]), r=[key, "epst"], w=[key])
                        op("act", lambda e: e.activation(out=ss_ap, in_=ss_ap, func=AF.Exp, scale=-0.5), r=[key], w=[key])

                    load_x(0)
                    for stl in range(NST):
                        for sub in range(4):
                            t = stl * 4 + sub
                            if t + 1 < NT:
                                load_x(t + 1)
                            xk = "xt%d" % (t % 2)
                            X = xt[t % 2]
                            c64k, c32k = "cs64_%d" % (t % 2), "cs32_%d" % (t % 2)
                            C64, C32 = cs64[t % 2], cs32[t % 2]
                            op("act", lambda e: e.activation(out=junk[:], in_=X[:], func=AF.Square, accum_out=st8[:, 0:1]),
                               r=[xk], w=["junk", "st_x"])
                            rstd_of(st8[:, 0:1], 1, D_MODEL, "st_x")
                            op("dve", lambda e: e.tensor_scalar(out=hb[:], in0=X[:], scalar1=st8[:, 0:1], scalar2=None, op0=ALU.mult),
                               r=[xk, "st_x"], w=["hb"])
                            for c in range(8):
                                op("pe", lambda e, c=c: e.transpose(pb[0][:, c * 128:(c + 1) * 128], hb[:, c * 128:(c + 1) * 128], ident[:]),
                                   r=["hb", "ident"], w=["pb0"])
                            op("act", lambda e: e.activation(out=hT[:, :, sub * 128:(sub + 1) * 128],
                                                             in_=pb[0][:].rearrange("p (c t) -> p c t", c=8), func=AF.Copy),
                               r=["pb0"], w=["hT%d" % sub])
                            col = 0
                            ci = 0
                            while col < NTM:
                                n = min(512, NTM - col)
                                bank = 1 + (ci % 2)
                                for c in range(8):
                                    op("pe", lambda e, c=c, col=col, n=n, bank=bank: e.matmul(
                                        pf[bank][:, 0:n], lhsT=hT[:, c, sub * 128:(sub + 1) * 128], rhs=w_sb[:, c, T0 + col:T0 + col + n],
                                        start=(c == 0), stop=(c == 7)), r=["hT%d" % sub, "w_sb"], w=["pf%d" % bank])
                                eng = "act" if ci % 2 == 0 else "dve"
                                if eng == "act":
                                    op("act", lambda e, col=col, n=n, bank=bank: e.activation(out=proj[:, col:col + n], in_=pf[bank][:, 0:n], func=AF.Copy),
                                       r=["pf%d" % bank], w=["proj%d" % ci])
                                else:
                                    op("dve", lambda e, col=col, n=n, bank=bank: e.tensor_copy(out=proj[:, col:col + n], in_=pf[bank][:, 0:n]),
                                       r=["pf%d" % bank], w=["proj%d" % ci])
                                col += n
                                ci += 1
                            PJ = ["proj%d" % i for i in range(ci)]
                            op("pool", lambda e: e.tensor_copy(out=VS[:, 0:4, 0:64], in_=proj[:, P_AV:P_AV + 256].rearrange("p (h d) -> p h d", h=4)), r=PJ, w=["VS"])
                            op("pool", lambda e: e.tensor_copy(out=VS[:, 9:13, 0:64], in_=proj[:, P_DV:P_DV + 256].rearrange("p (h d) -> p h d", h=4)), r=PJ, w=["VS"])
                            op("pool", lambda e: e.tensor_copy(out=VS[:, 8, 0:64], in_=proj[:, P_CV:P_CV + 64]), r=PJ, w=["VS"])
                            op("pool", lambda e: e.tensor_scalar(out=IWs[:], in0=proj[:, P_IW:P_IW + 4], scalar1=0.0625, scalar2=0.0, op0=ALU.mult, op1=ALU.add), r=PJ, w=["IWs"])
                            X64 = proj[:, P_X64:P_X64 + 640].rearrange("p (h d) -> p h d", h=10)
                            T3 = tmp[:].rearrange("p (h d) -> p h d", h=10)
                            T3b = tmp2[:].rearrange("p (h d) -> p h d", h=10)
                            op("act", lambda e: e.activation(out=T3[:, 0:5, :], in_=X64[:, 0:5, :], func=AF.Square), r=PJ, w=["tmp"])
                            op("dve", lambda e: e.tensor_reduce(out=st8[:, 8:13], in_=T3[:, 0:5, :], axis=AX.X, op=ALU.add), r=["tmp"], w=["st_a"])
                            rstd_of(st8[:, 8:13], 5, 64, "st_a")
                            op("dve", lambda e: e.tensor_tensor(out=X64[:, 0:5, :], in0=X64[:, 0:5, :], in1=bc(st8[:, 8:13], [128, 5, 64], 2), op=ALU.mult),
                               r=PJ + ["st_a"], w=PJ)
                            op("dve", lambda e: e.tensor_tensor(out=X64[:, 0:5, :], in0=X64[:, 0:5, :], in1=g64t[:], op=ALU.mult), r=PJ + ["g64t"], w=PJ)
                            cosb = bc(C64[:, 0:32], [128, 10, 32], 1)
                            sinb = bc(C64[:, 32:64], [128, 10, 32], 1)
                            op("dve", lambda e: e.tensor_tensor(out=T3[:, :, 0:32], in0=X64[:, :, 0:32], in1=cosb, op=ALU.mult), r=PJ + [c64k, "tmp"], w=["tmp"])
                            op("pool", lambda e: e.tensor_tensor(out=T3b[:, :, 0:32], in0=X64[:, :, 32:64], in1=sinb, op=ALU.mult), r=PJ + [c64k], w=["tmp2"])
                            op("dve", lambda e: e.tensor_tensor(out=T3[:, :, 32:64], in0=X64[:, :, 0:32], in1=sinb, op=ALU.mult), r=PJ + [c64k, "tmp"], w=["tmp"])
                            op("pool", lambda e: e.tensor_tensor(out=T3b[:, :, 32:64], in0=X64[:, :, 32:64], in1=cosb, op=ALU.mult), r=PJ + [c64k, "tmp2"], w=["tmp2"])
                            TU = TT[:].rearrange("p b (u d) -> p (b u) d", u=2)
                            for (h0, h1, u0) in ((0, 5, 16), (5, 10, 22)):
                                op("dve", lambda e, h0=h0, h1=h1, u0=u0: e.tensor_tensor(out=TU[:, u0:u0 + 5, 0:32], in0=T3[:, h0:h1, 0:32], in1=T3b[:, h0:h1, 0:32], op=ALU.subtract),
                                   r=["tmp", "tmp2"], w=["TT"])
                                op("dve", lambda e, h0=h0, h1=h1, u0=u0: e.tensor_tensor(out=TU[:, u0:u0 + 5, 32:64], in0=T3[:, h0:h1, 32:64], in1=T3b[:, h0:h1, 32:64], op=ALU.add),
                                   r=["tmp", "tmp2"], w=["TT"])
                            op("pool", lambda e: e.tensor_copy(out=TU[:, 21, :], in_=TU[:, 20, :]), r=["TT"], w=["TT"])
                            op("pool", lambda e: e.tensor_copy(out=TU[:, 27, :], in_=TU[:, 26, :]), r=["TT"], w=["TT"])
                            Y32 = proj[:, P_Y32:P_Y32 + 544].rearrange("p (h d) -> p h d", h=17)
                            U3 = tmp[:, 0:544].rearrange("p (h d) -> p h d", h=17)
                            U3b = tmp2[:, 0:544].rearrange("p (h d) -> p h d", h=17)
                            op("act", lambda e: e.activation(out=U3[:], in_=Y32, func=AF.Square), r=PJ + ["TT"], w=["tmp"])
                            op("dve", lambda e: e.tensor_reduce(out=st8[:, 16:33], in_=U3[:], axis=AX.X, op=ALU.add), r=["tmp"], w=["st_b"])
                            rstd_of(st8[:, 16:33], 17, 32, "st_b")
                            op("dve", lambda e: e.tensor_tensor(out=Y32, in0=Y32, in1=bc(st8[:, 16:33], [128, 17, 32], 2), op=ALU.mult), r=PJ + ["st_b"], w=PJ)
                            op("dve", lambda e: e.tensor_tensor(out=Y32, in0=Y32, in1=g32t[:], op=ALU.mult), r=PJ + ["g32t"], w=PJ)
                            cosb = bc(C32[:, 0:16], [128, 17, 16], 1)
                            sinb = bc(C32[:, 16:32], [128, 17, 16], 1)
                            op("dve", lambda e: e.tensor_tensor(out=U3[:, :, 0:16], in0=Y32[:, :, 0:16], in1=cosb, op=ALU.mult), r=PJ + [c32k, "tmp"], w=["tmp"])
                            op("pool", lambda e: e.tensor_tensor(out=U3b[:, :, 0:16], in0=Y32[:, :, 16:32], in1=sinb, op=ALU.mult), r=PJ + [c32k, "TT"], w=["tmp2"])
                            op("dve", lambda e: e.tensor_tensor(out=U3[:, :, 16:32], in0=Y32[:, :, 0:16], in1=sinb, op=ALU.mult), r=PJ + [c32k, "tmp"], w=["tmp"])
                            op("pool", lambda e: e.tensor_tensor(out=U3b[:, :, 16:32], in0=Y32[:, :, 16:32], in1=cosb, op=ALU.mult), r=PJ + [c32k, "tmp2"], w=["tmp2"])
                            TQ = TT[:].rearrange("p b (u d) -> p (b u) d", u=4)
                            op("dve", lambda e: e.tensor_tensor(out=TQ[:, 56:88:2, 0:16], in0=U3[:, 0:16, 0:16], in1=U3b[:, 0:16, 0:16], op=ALU.subtract), r=["tmp", "tmp2"], w=["TT"])
                            op("dve", lambda e: e.tensor_tensor(out=TQ[:, 56:88:2, 16:32], in0=U3[:, 0:16, 16:32], in1=U3b[:, 0:16, 16:32], op=ALU.add), r=["tmp", "tmp2"], w=["TT"])
                            for hh in range(4):
                                u = (4 + hh) * 4 + 2
                                op("dve", lambda e, u=u: e.tensor_tensor(out=TQ[:, u, 0:16], in0=U3[:, 16, 0:16], in1=U3b[:, 16, 0:16], op=ALU.subtract), r=["tmp", "tmp2"], w=["TT"])
                                op("dve", lambda e, u=u: e.tensor_tensor(out=TQ[:, u, 16:32], in0=U3[:, 16, 16:32], in1=U3b[:, 16, 16:32], op=ALU.add), r=["tmp", "tmp2"], w=["TT"])
                            CQ = proj[:, P_CQ:P_CQ + 256]
                            CKV = proj[:, P_CKV:P_CKV + 128]
                            op("act", lambda e: e.activation(out=tmp[:, 0:256], in_=CQ, func=AF.Square, accum_out=st8[:, 40:41]), r=PJ + ["tmp", "TT"], w=["tmp", "st_c"])
                            op("act", lambda e: e.activation(out=tmp[:, 256:384], in_=CKV, func=AF.Square, accum_out=st8[:, 41:42]), r=PJ + ["tmp"], w=["tmp", "st_c"])
                            op("dve", lambda e: e.tensor_scalar(out=st8[:, 41:42], in0=st8[:, 41:42], scalar1=2.0, scalar2=None, op0=ALU.mult), r=["st_c"], w=["st_c"])
                            rstd_of(st8[:, 40:42], 2, 256, "st_c")
                            op("dve", lambda e: e.tensor_scalar(out=cqn[:, 0:256], in0=CQ, scalar1=st8[:, 40:41], scalar2=None, op0=ALU.mult), r=PJ + ["st_c"], w=["cqn"])
                            op("dve", lambda e: e.tensor_scalar(out=cqn[:, 256:384], in0=CKV, scalar1=st8[:, 41:42], scalar2=None, op0=ALU.mult), r=PJ + ["st_c", "cqn"], w=["cqn"])
                            for c in range(3):
                                op("pe", lambda e, c=c: e.transpose(pb[1][:, c * 128:(c + 1) * 128], cqn[:, c * 128:(c + 1) * 128], ident[:]), r=["cqn", "ident"], w=["pb1"])
                            op("act", lambda e: e.activation(out=cqT[:].rearrange("p c t -> p (c t)"), in_=pb[1][:, 0:384], func=AF.Copy), r=["pb1"], w=["cqT"])
                            for c in range(2):
                                op("pe", lambda e, c=c: e.matmul(pf[3][:, 0:384], lhsT=cqT[:, c, :], rhs=wuq_sb[:, c, :], start=(c == 0), stop=(c == 1)),
                                   r=["cqT", "wuq_sb"], w=["pf3"])
                            op("pe", lambda e: e.matmul(pf[4][:, 0:512], lhsT=cqT[:, 2, :], rhs=wukv_sb[:], start=True, stop=True), r=["cqT", "wukv_sb"], w=["pf4"])
                            op("act", lambda e: e.activation(out=QB[:].rearrange("p h d -> p (h d)"), in_=pf[3][:, 0:384], func=AF.Copy), r=["pf3"], w=["QB"])
                            op("dve", lambda e: e.tensor_copy(out=KV[:].rearrange("p h d -> p (h d)"), in_=pf[4][:, 0:512]), r=["pf4"], w=["KV"])
                            Q3 = tmp[:, 0:384].rearrange("p (h d) -> p h d", h=4)
                            K3 = tmp2[:, 0:256].rearrange("p (h d) -> p h d", h=4)
                            op("act", lambda e: e.activation(out=Q3, in_=QB[:], func=AF.Square), r=["QB", "tmp"], w=["tmp"])
                            op("act", lambda e: e.activation(out=K3, in_=KV[:, :, 0:64], func=AF.Square), r=["KV", "tmp2", "TT"], w=["tmp2"])
                            op("dve", lambda e: e.tensor_reduce(out=st8[:, 44:48], in_=Q3[:, :, 0:64], axis=AX.X, op=ALU.add), r=["tmp"], w=["st_d"])
                            op("dve", lambda e: e.tensor_reduce(out=st8[:, 48:52], in_=K3, axis=AX.X, op=ALU.add), r=["tmp2"], w=["st_d"])
                            op("dve", lambda e: e.tensor_reduce(out=st8[:, 52:56], in_=Q3[:, :, 64:96], axis=AX.X, op=ALU.add), r=["tmp"], w=["st_e"])
                            rstd_of(st8[:, 44:52], 8, 64, "st_d")
                            rstd_of(st8[:, 52:56], 4, 32, "st_e")
                            T128 = TT[:]
                            op("dve", lambda e: e.tensor_tensor(out=QB[:, :, 0:64], in0=QB[:, :, 0:64], in1=bc(st8[:, 44:48], [128, 4, 64], 2), op=ALU.mult), r=["QB", "st_d"], w=["QB"])
                            op("dve", lambda e: e.tensor_tensor(out=T128[:, 0:4, 0:64], in0=QB[:, :, 0:64], in1=bc(gq96t[:, 0:64], [128, 4, 64], 1), op=ALU.mult), r=["QB", "gq96t"], w=["TT"])
                            op("dve", lambda e: e.tensor_tensor(out=KV[:, :, 0:64], in0=KV[:, :, 0:64], in1=bc(st8[:, 48:52], [128, 4, 64], 2), op=ALU.mult), r=["KV", "st_d"], w=["KV"])
                            op("dve", lambda e: e.tensor_tensor(out=T128[:, 4:8, 0:64], in0=KV[:, :, 0:64], in1=bc(gk64t[:], [128, 4, 64], 1), op=ALU.mult), r=["KV", "gk64t"], w=["TT"])
                            op("pool", lambda e: e.tensor_copy(out=VS[:, 4:8, 0:64], in_=KV[:, :, 64:128]), r=["KV"], w=["VS"])
                            op("dve", lambda e: e.tensor_tensor(out=QB[:, :, 64:96], in0=QB[:, :, 64:96], in1=bc(st8[:, 52:56], [128, 4, 32], 2), op=ALU.mult), r=["QB", "st_e"], w=["QB"])
                            op("dve", lambda e: e.tensor_tensor(out=QB[:, :, 64:96], in0=QB[:, :, 64:96], in1=bc(gq96t[:, 64:96], [128, 4, 32], 1), op=ALU.mult), r=["QB", "gq96t"], w=["QB"])
                            cosb = bc(C32[:, 0:16], [128, 4, 16], 1)
                            sinb = bc(C32[:, 16:32], [128, 4, 16], 1)
                            R3 = tmp[:, 0:128].rearrange("p (h d) -> p h d", h=4)
                            R3b = tmp2[:, 0:128].rearrange("p (h d) -> p h d", h=4)
                            op("dve", lambda e: e.tensor_tensor(out=R3[:, :, 0:16], in0=QB[:, :, 64:80], in1=cosb, op=ALU.mult), r=["QB", c32k, "tmp"], w=["tmp"])
                            op("pool", lambda e: e.tensor_tensor(out=R3b[:, :, 0:16], in0=QB[:, :, 80:96], in1=sinb, op=ALU.mult), r=["QB", c32k, "tmp2"], w=["tmp2"])
                            op("dve", lambda e: e.tensor_tensor(out=R3[:, :, 16:32], in0=QB[:, :, 64:80], in1=sinb, op=ALU.mult), r=["QB", c32k, "tmp"], w=["tmp"])
                            op("pool", lambda e: e.tensor_tensor(out=R3b[:, :, 16:32], in0=QB[:, :, 80:96], in1=cosb, op=ALU.mult), r=["QB", c32k, "tmp2"], w=["tmp2"])
                            op("dve", lambda e: e.tensor_tensor(out=T128[:, 0:4, 64:80], in0=R3[:, :, 0:16], in1=R3b[:, :, 0:16], op=ALU.subtract), r=["tmp", "tmp2"], w=["TT"])
                            op("dve", lambda e: e.tensor_tensor(out=T128[:, 0:4, 80:96], in0=R3[:, :, 16:32], in1=R3b[:, :, 16:32], op=ALU.add), r=["tmp", "tmp2"], w=["TT"])
                            for g0, g1, bank in ((0, 8, 0), (8, 16, 1), (16, 22, 0)):
                                for b in range(g0, g1):
                                    op("pe", lambda e, b=b, g0=g0, bank=bank: e.transpose(pb[bank][:, (b - g0) * 128:(b - g0 + 1) * 128], TT[:, b, :], ident[:]),
                                       r=["TT", "ident"], w=["pb%d" % bank])
                                nb = g1 - g0
                                eng = "act" if bank == 0 else "dve"
                                if eng == "act":
                                    op("act", lambda e, g0=g0, g1=g1, nb=nb, bank=bank: e.activation(
                                        out=FTs[:, 4 + g0:4 + g1, sub * 128:(sub + 1) * 128], in_=pb[bank][:, 0:nb * 128].rearrange("p (b t) -> p b t", b=nb), func=AF.Copy),
                                       r=["pb%d" % bank], w=["FTs"])
                                else:
                                    op("dve", lambda e, g0=g0, g1=g1, nb=nb, bank=bank: e.tensor_copy(
                                        out=FTs[:, 4 + g0:4 + g1, sub * 128:(sub + 1) * 128], in_=pb[bank][:, 0:nb * 128].rearrange("p (b t) -> p b t", b=nb)),
                                       r=["pb%d" % bank], w=["FTs"])
                            dma(VA[t * 128:(t + 1) * 128, :, :], VS[:], r=["VS"], slot="VS")
                            dma(IW[t * 128:(t + 1) * 128, :], IWs[:], r=["IWs"], slot="IWs")
                        HT = ["hT%d" % i for i in range(4)]
                        for ct in range(12):
                            bank = 3 + (ct % 2)
                            for c in range(8):
                                op("pe", lambda e, c=c, ct=ct, bank=bank: e.matmul(pf[bank][:, :], lhsT=w_sb[:, c, ct * 128:(ct + 1) * 128], rhs=hT[:, c, :],
                                                                                  start=(c == 0), stop=(c == 7)), r=HT + ["w_sb"], w=["pf%d" % bank])
                            if ct < 4:
                                op("dve", lambda e, ct=ct, bank=bank: e.tensor_copy(out=FTs[:, ct, :], in_=pf[bank][:, :]), r=["pf%d" % bank], w=["FTs"])
                            else:
                                op("act", lambda e, ct=ct, bank=bank: e.activation(out=GS[:, ct - 4, :], in_=pf[bank][:, :], func=AF.Silu), r=["pf%d" % bank], w=["GS"])
                        q0 = stl * 512
                        for g in range(0, NB_FT, 7):
                            g1 = min(NB_FT, g + 7)
                            dma(FT[g:g1, :, q0:q0 + 512].rearrange("b p t -> p b t"), FTs[:, g:g1, :], r=["FTs"], slot="FTs")
                        for g in range(0, 8, 4):
                            dma(GT[g:g + 4, :, q0:q0 + 512].rearrange("b p t -> p b t"), GS[:, g:g + 4, :], r=["GS"], slot="GS")
                    sc.barrier()
                NQ = S // 512
                NIT = 16

                def load_V(st, name, h0, nh):
                    V = sb(st, name, [128, NT, nh, 128], BF16)
                    for g in range(0, NT, 8):
                        g1 = min(NT, g + 8)
                        dma(V[:, g:g1, :, :], VA[g * 128:g1 * 128, h0:h0 + nh, :].rearrange("(k p) h d -> p k h d", p=128), w=[name], slot=name)
                    return V

                def load_K(st, name, b0, nb):
                    Kt = sb(st, name, [128, nb, S], BF16)
                    for b in range(nb):
                        dma(Kt[:, b, :], FT[b0 + b, :, :], w=[name], slot=name)
                    return Kt

                def softmax_mixer(mname, kb0, nkb, qb0, nqb, vh0, nvh, gb0, heads, scale, diag_mask, selT_fn=None, post=None, pre_q=None):
                    with ExitStack() as ms:
                        Kt = load_K(ms, "Kt", kb0, nkb)
                        V = load_V(ms, "Vr", vh0, nvh)
                        Qs = [sb(ms, "Qs%d" % i, [128, nqb, 512], BF16) for i in range(2)]
                        Gs = [sb(ms, "Gs%d" % i, [128, 2, 512]) for i in range(1)]
                        PT = [sb(ms, "PT%d" % i, [128, 512], BF16) for i in range(3)]
                        RC = sb(ms, "RC", [128, 2 if post is not None else 1, 512])
                        OD = sb(ms, "OD", [128, 3 if post is not None else 1, 512])
                        YS = [sb(ms, "YS%d" % i, [128, 2, 512], BF16) for i in range(2)]
                        extra = pre_q(ms) if pre_q is not None else None
                        ctr = {"s": 0, "p": 0, "o": 0}
                        allmaps = []
                        for (mps, _vh) in heads:
                            for m in mps:
                                if m not in allmaps:
                                    allmaps.append(m)
                        use_qz = (selT_fn is None) and any(m[3] < 96 for m in allmaps)
                        Qz = None
                        if use_qz:
                            Qz = [[sb(ms, "Qz%d_%d" % (i, mi), [128, 512], BF16) for mi in range(len(allmaps))] for i in range(2)]
                            for i in range(2):
                                for mi in range(len(allmaps)):
                                    op("pool", lambda e, i=i, mi=mi: e.memset(Qz[i][mi][:], 0.0), w=["Qz%d_%d" % (i, mi)])

                        def load_q(j):
                            q0 = j * 512
                            dma(Qs[j % 2][:], FT[qb0:qb0 + nqb, :, q0:q0 + 512].rearrange("b p t -> p b t"), w=["Qs%d" % (j % 2)], slot="Qs%d" % (j % 2))
                            if use_qz:
                                for mi, (kblk_, qblk_, base_, dk_) in enumerate(allmaps):
                                    op("pool" if mi % 2 else "dve", lambda e, mi=mi, qblk_=qblk_, base_=base_, dk_=dk_: e.tensor_copy(
                                        out=Qz[j % 2][mi][base_:base_ + dk_, :], in_=Qs[j % 2][base_:base_ + dk_, qblk_, :]),
                                       r=["Qs%d" % (j % 2)], w=["Qz%d_%d" % (j % 2, mi)])

                        def load_g(j):
                            q0 = j * 512
                            dma(Gs[0][:], GT[gb0:gb0 + 2, :, q0:q0 + 512].rearrange("b p t -> p b t"), w=["Gs0"], slot="Gs0")

                        def tile_gen(j, selT, selk):
                            load_g(j)
                            Qk, Gk = "Qs%d" % (j % 2), "Gs0"
                            Q, G = Qs[j % 2], Gs[0]
                            nkt = 4 * (j + 1)
                            ysk = "YS%d" % (j % 2)
                            for hi, (maps, vh) in enumerate(heads):
                                obanks = []
                                for m in maps:
                                    obanks.append(2 + (ctr["o"] % (2 if selT_fn is not None else 4)))
                                    ctr["o"] += 1
                                seq = [(kt, mi) for kt in range(nkt) for mi in range(len(maps))]

                                def emit_qk(idx):
                                    kt, mi = seq[idx]
                                    kblk, qblk, base, dkk = maps[mi]
                                    sbank = ctr["s"] % 2
                                    ctr["s"] += 1
                                    if use_qz:
                                        zi = allmaps.index(maps[mi])
                                        op("pe", lambda e: e.matmul(pf[sbank][:, :], lhsT=Kt[:, kblk, kt * 128:(kt + 1) * 128],
                                                                    rhs=Qz[j % 2][zi][:, :], start=True, stop=(selT is None)),
                                           r=["Kt", "Qz%d_%d" % (j % 2, zi)], w=["pf%d" % sbank])
                                    elif dkk >= 96:
                                        op("pe", lambda e: e.matmul(pf[sbank][:, :], lhsT=Kt[:, kblk, kt * 128:(kt + 1) * 128],
                                                                    rhs=Q[:, qblk, :], start=True, stop=(selT is None)),
                                           r=["Kt", Qk], w=["pf%d" % sbank])
                                    else:
                                        op("pe", lambda e: e.matmul(pf[sbank][:, :], lhsT=Kt[base:base + dkk, kblk, kt * 128:(kt + 1) * 128],
                                                                    rhs=Q[base:base + dkk, qblk, :], start=True, stop=(selT is None)),
                                           r=["Kt", Qk], w=["pf%d" % sbank])
                                    if selT is not None:
                                        op("pe", lambda e: e.matmul(pf[sbank][:, :], lhsT=ident[:], rhs=selT[:, kt, :], start=False, stop=True),
                                           r=["ident", selk], w=["pf%d" % sbank])
                                    return sbank

                                pend = emit_qk(0)
                                for idx in range(len(seq)):
                                    kt, mi = seq[idx]
                                    sbank = pend
                                    if idx + 1 < len(seq):
                                        pend = emit_qk(idx + 1)
                                    pi = ctr["p"] % 3
                                    ctr["p"] += 1
                                    P = PT[pi]
                                    pk = "PT%d" % pi
                                    op("act", lambda e: e.activation(out=P[:], in_=pf[sbank][:, :], func=AF.Exp, scale=scale), r=["pf%d" % sbank], w=[pk])
                                    r_ = kt - (nkt - 4)
                                    if diag_mask is not None and r_ >= 0:
                                        op("dve", lambda e: e.tensor_tensor(out=P[:], in0=P[:], in1=masks[:, diag_mask + r_, :], op=ALU.mult), r=[pk, "masks"], w=[pk])
                                    ob = obanks[mi]
                                    op("pe", lambda e: e.matmul(pf[ob][:, :], lhsT=V[:, kt, vh, :], rhs=P[:], start=(kt == 0), stop=(kt == nkt - 1)),
                                       r=["Vr", pk], w=["pf%d" % ob])
                                    yield
                                blk, hb_ = hi // 2, (hi % 2) * 64
                                for mi, ob in enumerate(obanks):
                                    op("dve", lambda e, mi=mi, ob=ob: e.reciprocal(out=RC[64:128, mi, :], in_=pf[ob][64:128, :]), r=["pf%d" % ob], w=["RC"])
                                if post is None:
                                    ob = obanks[0]
                                    op("dve", lambda e: e.tensor_tensor(out=OD[hb_:hb_ + 64, 0, :], in0=pf[ob][0:64, :], in1=RC[64:128, 0, :], op=ALU.mult),
                                       r=["pf%d" % ob, "RC"], w=["OD"])
                                    op("dve", lambda e: e.tensor_tensor(out=YS[j % 2][hb_:hb_ + 64, blk, :], in0=OD[hb_:hb_ + 64, 0, :], in1=G[hb_:hb_ + 64, blk, :], op=ALU.mult),
                                       r=["OD", Gk], w=[ysk])
                                else:
                                    post(obanks, RC, OD, G, Gk, YS[j % 2], ysk, blk, hb_)
                            q0 = j * 512
                            dma(YT[gb0:gb0 + 2, :, q0:q0 + 512].rearrange("b p t -> p b t"), YS[j % 2][:], r=[ysk], slot=ysk)
                            yield

                        load_q(0)
                        if selT_fn is None:
                            for j in range(NQ):
                                if j + 1 < NQ:
                                    load_q(j + 1)
                                for _ in tile_gen(j, None, None):
                                    pass
                        else:
                            cur = selT_fn(0, extra, None)
                            for j in range(NQ):
                                if j + 1 < NQ:
                                    load_q(j + 1)
                                g = tile_gen(j, cur[0], cur[1])
                                nxt = None
                                if j + 1 < NQ:
                                    nxt = selT_fn(j + 1, extra, lambda g=g: next(g, None))
                                for _ in g:
                                    pass
                                cur = nxt
                        sc.barrier()

                if "B" in MIX:
                    softmax_mixer("B", 8, 4, 4, 4, 4, 4, 2, [([(h, h, 0, 96)], h) for h in range(4)], 96.0 ** -0.5, 0)

                def post_D(obanks, RC, OD, G, Gk, YSt, ysk, blk, hb_):
                    a, b = obanks
                    op("dve", lambda e: e.tensor_tensor(out=OD[0:64, 0, :], in0=pf[a][0:64, :], in1=RC[64:128, 0, :], op=ALU.mult), r=["pf%d" % a, "RC"], w=["OD"])
                    op("dve", lambda e: e.tensor_tensor(out=OD[0:64, 1, :], in0=pf[b][0:64, :], in1=RC[64:128, 1, :], op=ALU.mult), r=["pf%d" % b, "RC", "OD"], w=["OD"])
                    op("dve", lambda e: e.scalar_tensor_tensor(out=OD[0:64, 0, :], in0=OD[0:64, 1, :], scalar=lam_bc[0:64, 0:1], in1=OD[0:64, 0, :], op0=ALU.mult, op1=ALU.add),
                       r=["OD", "lam_bc"], w=["OD"])
                    op("act", lambda e: e.activation(out=OD[0:64, 1, :], in_=OD[0:64, 0, :], func=AF.Square), r=["OD"], w=["OD"])
                    op("pe", lambda e: e.matmul(pf[a][0:64, :], lhsT=ones_f[0:64, 0:64], rhs=OD[0:64, 1, :], start=True, stop=True), r=["OD", "ones_f"], w=["pf%d" % a])
                    op("act", lambda e: e.activation(out=OD[0:64, 1, :], in_=pf[a][0:64, :], func=AF.Ln, scale=1.0 / 64, bias=epst[0:64, 0:1]), r=["pf%d" % a, "OD", "epst"], w=["OD"])
                    op("act", lambda e: e.activation(out=OD[0:64, 1, :], in_=OD[0:64, 1, :], func=AF.Exp, scale=-0.5), r=["OD"], w=["OD"])
                    op("dve", lambda e: e.scalar_tensor_tensor(out=OD[hb_:hb_ + 64, 2, :], in0=OD[0:64, 0, :], scalar=sublnt[0:64, 0:1], in1=OD[0:64, 1, :], op0=ALU.mult, op1=ALU.mult),
                       r=["OD", "sublnt"], w=["OD"])
                    op("dve", lambda e: e.tensor_tensor(out=YSt[hb_:hb_ + 64, blk, :], in0=OD[hb_:hb_ + 64, 2, :], in1=G[hb_:hb_ + 64, blk, :], op=ALU.mult), r=["OD", Gk], w=[ysk])

                if "D" in MIX:
                    softmax_mixer("D", 22, 4, 18, 4, 9, 4, 6,
                                  [([(h, h, c * 64, 32) for c in range(2)], h) for h in range(4)], 32.0 ** -0.5, 0, post=post_D)

                def pre_C(ms):
                    ex = {}
                    ex["IKc"] = [sb(ms, "IKc%d" % i, [128, 512], BF16) for i in range(3)]
                    ex["IQ"] = [sb(ms, "IQ%d" % i, [128, 2, 512], BF16) for i in range(1)]
                    ex["IWt"] = [sb(ms, "IWt%d" % i, [128, 4, 4]) for i in range(2)]
                    ex["SC"] = sb(ms, "SC", [128, S])
                    ex["JK"] = sb(ms, "JK", [128, S], BF16)
                    ex["BD"] = sb(ms, "BD", [128, S], BF16)
                    ex["selT"] = [sb(ms, "selT" + str(i), [128, NT, 512], mybir.dt.float8e4) for i in range(2)]
                    ex["RL"] = [sb(ms, "RL%d" % i, [128, 512]) for i in range(2)]
                    ex["bs"] = sb(ms, "bs", [128, 64])
                    return ex

                def selT_C(j, ex, pump):
                    pump = pump if pump is not None else (lambda: None)
                    q0 = j * 512
                    IQ, IWt = ex["IQ"][0], ex["IWt"][j % 2]
                    iqk, iwk = "IQ0", "IWt%d" % (j % 2)
                    SC, JK, selT, RL, bs, IKc = ex["SC"], ex["JK"], ex["selT"][j % 2], ex["RL"], ex["bs"], ex["IKc"]
                    selk = "selT" + str(j % 2)
                    BD = ex["BD"]
                    dma(IQ[:], FT[15:17, :, q0:q0 + 512].rearrange("b p t -> p b t"), w=[iqk], slot=iqk)
                    dma(IWt[:], IW[q0:q0 + 512, :].rearrange("(a p) h -> p a h", p=128), w=[iwk], slot=iwk)
                    op("pool", lambda e: e.memset(selT[:, 4 * j:4 * j + 4, :], -240.0), w=[selk])
                    rl = 0
                    chunks = [(qi, c0) for qi in range(4) for c0 in range(0, (4 * j + qi + 1) * 128, 512)]

                    def load_ik(ci):
                        qi_, c0_ = chunks[ci]
                        n_ = min(512, (4 * j + qi_ + 1) * 128 - c0_)
                        kk_ = "IKc%d" % (ci % 3)
                        dma(IKc[ci % 3][:, 0:n_], FT[17, :, c0_:c0_ + n_], w=[kk_], slot=kk_)

                    load_ik(0)
                    ci = 0
                    for qi in range(4):
                        i = 4 * j + qi
                        Lk = (i + 1) * 128
                        for c0 in range(0, Lk, 512):
                            n = min(512, Lk - c0)
                            if ci + 1 < len(chunks):
                                load_ik(ci + 1)
                            IKb, ikk = IKc[ci % 3], "IKc%d" % (ci % 3)
                            ci += 1
                            for h in range(4):
                                blk, base = h // 2, (h % 2) * 64
                                bank = 4 + (h % 2)
                                op("pe", lambda e: e.matmul(pf[bank][:, 0:n], lhsT=IQ[base:base + 64, blk, qi * 128:(qi + 1) * 128], rhs=IKb[base:base + 64, 0:n],
                                                            start=True, stop=True), r=[iqk, ikk], w=["pf%d" % bank])
                                R = RL[rl % 2]
                                rk = "RL%d" % (rl % 2)
                                rl += 1
                                op("act", lambda e: e.activation(out=R[:, 0:n], in_=pf[bank][:, 0:n], func=AF.Relu), r=["pf%d" % bank], w=[rk])
                                if h == 0:
                                    op("dve", lambda e: e.tensor_scalar(out=SC[:, c0:c0 + n], in0=R[:, 0:n], scalar1=IWt[:, qi, 0:1], scalar2=None, op0=ALU.mult),
                                       r=[rk, iwk], w=["SC"])
                                else:
                                    op("dve", lambda e: e.scalar_tensor_tensor(out=SC[:, c0:c0 + n], in0=R[:, 0:n], scalar=IWt[:, qi, h:h + 1], in1=SC[:, c0:c0 + n],
                                                                                op0=ALU.mult, op1=ALU.add), r=[rk, iwk, "SC"], w=["SC"])
                                pump()
                        op("dve", lambda e: e.tensor_tensor(out=SC[:, Lk - 128:Lk], in0=SC[:, Lk - 128:Lk], in1=adm[:, 0, :], op=ALU.mult), r=["SC", "adm"], w=["SC"])
                        op("dve", lambda e: e.tensor_tensor(out=SC[:, Lk - 128:Lk], in0=SC[:, Lk - 128:Lk], in1=adm[:, 1, :], op=ALU.add), r=["SC", "adm"], w=["SC"])
                        if Lk <= TOPK:
                            op("dve", lambda e: e.tensor_scalar(out=JK[:, 0:Lk], in0=SC[:, 0:Lk], scalar1=-1.0e29, scalar2=None, op0=ALU.is_ge), r=["SC"], w=["JK"])
                        else:
                            op("dve", lambda e: e.max(out=bs[:, 0:8], in_=SC[:, 0:Lk]), r=["SC"], w=["bs_hi"])
                            op("dve", lambda e: e.tensor_reduce(out=bs[:, 8:9], in_=SC[:, 0:Lk - 128], axis=AX.X, op=ALU.min), r=["SC"], w=["bs_lo"])
                            op("dve", lambda e: e.tensor_tensor(out=bs[:, 9:10], in0=bs[:, 0:1], in1=bs[:, 8:9], op=ALU.subtract), r=["bs_hi", "bs_lo"], w=["bs_d"])
                            op("dve", lambda e: e.tensor_scalar(out=bs[:, 9:10], in0=bs[:, 9:10], scalar1=1.001, scalar2=1.0e-20, op0=ALU.mult, op1=ALU.add), r=["bs_d"], w=["bs_d"])
                            op("dve", lambda e: e.tensor_scalar(out=bs[:, 32:32 + NIT], in0=pow2[:, 0:NIT], scalar1=bs[:, 9:10], scalar2=None, op0=ALU.mult), r=["bs_d", "pow2"], w=["bs_dl"])
                            for k in range(NIT):
                                op("dve", lambda e, k=k: e.tensor_tensor(out=bs[:, 10:11], in0=bs[:, 8:9], in1=bs[:, 32 + k:33 + k], op=ALU.add), r=["bs_lo", "bs_dl"], w=["bs_mid"])
                                op("dve", lambda e: e.tensor_scalar(out=JK[:, 0:Lk], in0=SC[:, 0:Lk], scalar1=bs[:, 10:11], scalar2=0.0, op0=ALU.is_ge, op1=ALU.add,
                                                                    accum_out=bs[:, 11:12]), r=["SC", "bs_mid"], w=["JK", "bs_cnt"])
                                op("dve", lambda e, k=k: e.tensor_scalar(out=bs[:, 12:13], in0=bs[:, 11:12], scalar1=TOPK - 0.5, scalar2=bs[:, 32 + k:33 + k], op0=ALU.is_ge, op1=ALU.mult),
                                   r=["bs_cnt", "bs_dl"], w=["bs_t"])
                                op("dve", lambda e: e.tensor_tensor(out=bs[:, 8:9], in0=bs[:, 8:9], in1=bs[:, 12:13], op=ALU.add), r=["bs_lo", "bs_t"], w=["bs_lo"])
                                pump()
                                pump()
                            op("dve", lambda e: e.tensor_tensor(out=bs[:, 13:14], in0=bs[:, 8:9], in1=bs[:, 31 + NIT:32 + NIT], op=ALU.add), r=["bs_lo", "bs_dl"], w=["bs_hf"])
                            op("dve", lambda e: e.tensor_scalar(out=JK[:, 0:Lk], in0=SC[:, 0:Lk], scalar1=bs[:, 8:9], scalar2=0.0, op0=ALU.is_ge, op1=ALU.add,
                                                                accum_out=bs[:, 14:15]), r=["SC", "bs_lo"], w=["JK", "bs_cl"])
                            op("dve", lambda e: e.scalar_tensor_tensor(out=BD[:, 0:Lk], in0=SC[:, 0:Lk], scalar=bs[:, 13:14], in1=JK[:, 0:Lk], op0=ALU.is_lt, op1=ALU.mult,
                                                                        accum_out=bs[:, 15:16]), r=["SC", "JK", "bs_hf"], w=["BD", "bs_nb"])
                            op("dve", lambda e: e.tensor_tensor(out=bs[:, 16:17], in0=bs[:, 15:16], in1=bs[:, 14:15], op=ALU.subtract), r=["bs_nb", "bs_cl"], w=["bs_m"])
                            op("dve", lambda e: e.tensor_scalar(out=bs[:, 16:17], in0=bs[:, 16:17], scalar1=float(TOPK) + 0.5, scalar2=None, op0=ALU.add), r=["bs_m"], w=["bs_m"])
                            op("dve", lambda e: e.tensor_tensor_scan(out=SC[:, 0:Lk], data0=ones_bf[:, 0:1].to_broadcast([128, Lk]), data1=BD[:, 0:Lk], initial=0.0, op0=ALU.mult, op1=ALU.add),
                               r=["BD", "ones_bf", "SC"], w=["SC"])
                            op("dve", lambda e: e.scalar_tensor_tensor(out=BD[:, 0:Lk], in0=SC[:, 0:Lk], scalar=bs[:, 16:17], in1=BD[:, 0:Lk], op0=ALU.is_gt, op1=ALU.mult),
                               r=["SC", "BD", "bs_m"], w=["BD"])
                            op("pool", lambda e: e.tensor_tensor(out=JK[:, 0:Lk], in0=JK[:, 0:Lk], in1=BD[:, 0:Lk], op=ALU.subtract), r=["JK", "BD"], w=["JK"])
                        if debug:
                            dma(DSC[i, :, :], SC[:, :], r=["SC"], slot="dbg1")
                            dma(DBS[i, :, :], bs[:, :], r=["bs_lo", "bs_hi", "bs_d", "bs_dl", "bs_mid", "bs_cnt", "bs_t", "bs_m", "bs_cl", "bs_nb", "bs_hf"], slot="dbg2")
                        for g in range(0, i + 1, 8):
                            g1 = min(i + 1, g + 8)
                            bank = (g // 8) % 2
                            for kb in range(g, g1):
                                op("pe", lambda e, kb=kb: e.transpose(pb[bank][:, (kb - g) * 128:(kb - g + 1) * 128], JK[:, kb * 128:(kb + 1) * 128], ident[:]),
                                   r=["JK", "ident"], w=["pb%d" % bank])
                            nb_ = g1 - g
                            op("dve", lambda e: e.tensor_scalar(out=selT[:, g:g1, qi * 128:(qi + 1) * 128], in0=pb[bank][:, 0:nb_ * 128].rearrange("p (b t) -> p b t", b=nb_),
                                                                scalar1=-1.0, scalar2=240.0, op0=ALU.add, op1=ALU.mult, saturate=False), r=["pb%d" % bank], w=[selk])
                            pump()
                    return selT, selk

                if "C" in MIX:
                    softmax_mixer("C", 14, 1, 12, 2, 8, 1, 4, [([(0, h // 2, (h % 2) * 64, 64)], 0) for h in range(4)], 0.125, None, selT_fn=selT_C, pre_q=pre_C)

                if "A" in MIX:
                    with ExitStack() as ms:
                        Kt = load_K(ms, "Kt", 2, 2)
                        V = load_V(ms, "Vr", 0, 4)
                        nK = sb(ms, "nK", [128, 2, S], BF16)
                        for b in range(2):
                            op("dve" if b == 0 else "pool", lambda e, b=b: e.tensor_scalar(out=nK[:, b, :], in0=Kt[:, b, :], scalar1=-0.125, scalar2=0.0, op0=ALU.mult, op1=ALU.add), r=["Kt"], w=["nK"])
                        Qs = [sb(ms, "Qs%d" % i, [128, 2, 512], BF16) for i in range(2)]
                        Gs = [sb(ms, "Gs%d" % i, [128, 2, 512]) for i in range(2)]
                        NBUF = 3
                        Et = [sb(ms, "Et%d" % i, [128, 512]) for i in range(NBUF)]
                        SPt = [sb(ms, "SPt%d" % i, [128, 512]) for i in range(NBUF)]
                        SPh = [sb(ms, "SPh%d" % i, [128, 512], BF16) for i in range(NBUF)]
                        SPl = [sb(ms, "SPl%d" % i, [128, 512], BF16) for i in range(NBUF)]
                        Cf = [sb(ms, "Cf%d" % i, [128, 512]) for i in range(2)]
                        Ch = [[sb(ms, "Ch%d_%d" % (i, k), [128, 512], BF16) for k in range(2)] for i in range(2)]
                        Cl = [[sb(ms, "Cl%d_%d" % (i, k), [128, 512], BF16) for k in range(2)] for i in range(2)]
                        PT = [sb(ms, "PT%d" % i, [128, 512], BF16) for i in range(3)]
                        YS = [sb(ms, "YS%d" % i, [128, 2, 512], BF16) for i in range(2)]
                        gc = {"g1": 0, "g2": 0}

                        QzA = [[sb(ms, "QzA%d_%d" % (i, h), [128, 512], BF16) for h in range(4)] for i in range(2)]
                        for i in range(2):
                            for h in range(4):
                                op("pool", lambda e, i=i, h=h: e.memset(QzA[i][h][:], 0.0), w=["QzA%d_%d" % (i, h)])

                        def load_qA(j):
                            q0 = j * 512
                            dma(Qs[j % 2][:], FT[0:2, :, q0:q0 + 512].rearrange("b p t -> p b t"), w=["Qs%d" % (j % 2)], slot="Qs%d" % (j % 2))
                            for h in range(4):
                                b_, bs_ = h // 2, (h % 2) * 64
                                op("pool" if h % 2 else "dve", lambda e, h=h, b_=b_, bs_=bs_: e.tensor_copy(out=QzA[j % 2][h][bs_:bs_ + 64, :], in_=Qs[j % 2][bs_:bs_ + 64, b_, :]),
                                   r=["Qs%d" % (j % 2)], w=["QzA%d_%d" % (j % 2, h)])
                            dma(Gs[j % 2][:], GT[0:2, :, q0:q0 + 512].rearrange("b p t -> p b t"), w=["Gs%d" % (j % 2)], slot="Gs%d" % (j % 2))

                        load_qA(0)
                        for j in range(NQ):
                            if j + 1 < NQ:
                                load_qA(j + 1)
                            Qk, Gk = "Qs%d" % (j % 2), "Gs%d" % (j % 2)
                            Q, G = Qs[j % 2], Gs[j % 2]
                            nkt = 4 * (j + 1)
                            ysk = "YS%d" % (j % 2)
                            for pair in range(2):
                                seq = [(kt, sl) for kt in range(nkt - 1, -1, -1) for sl in range(2)]
                                info = {}
                                cidx = [0, 0]

                                def stage1(idx):
                                    kt, sl = seq[idx]
                                    h = 2 * pair + sl
                                    blk, base = h // 2, (h % 2) * 64
                                    b1 = gc["g1"] % NBUF
                                    zb = gc["g1"] % 2
                                    gc["g1"] += 1
                                    info[idx] = b1
                                    r_ = kt - (nkt - 4)
                                    ksl = slice(kt * 128, (kt + 1) * 128)
                                    op("pe", lambda e: e.matmul(pf[zb][:, :], lhsT=Kt[:, blk, ksl], rhs=QzA[j % 2][h][:, :], start=True, stop=True),
                                       r=["Kt", "QzA%d_%d" % (j % 2, h)], w=["pf%d" % zb])
                                    op("act", lambda e: e.activation(out=Et[b1][:], in_=pf[zb][:, :], func=AF.Exp, scale=0.125), r=["pf%d" % zb], w=["Et%d" % b1])
                                    op("act", lambda e: e.activation(out=SPt[b1][:], in_=Et[b1][:], func=AF.Ln, bias=1.0, scale=1.0), r=["Et%d" % b1], w=["SPt%d" % b1])
                                    if r_ >= 0:
                                        op("dve", lambda e: e.tensor_tensor(out=SPt[b1][:], in0=SPt[b1][:], in1=masks[:, 4 + r_, :], op=ALU.mult), r=["SPt%d" % b1, "masks"], w=["SPt%d" % b1])
                                    op("dve", lambda e: e.tensor_copy(out=SPh[b1][:], in_=SPt[b1][:]), r=["SPt%d" % b1], w=["SPh%d" % b1])
                                    op("dve", lambda e: e.tensor_tensor(out=SPl[b1][:], in0=SPt[b1][:], in1=SPh[b1][:], op=ALU.subtract), r=["SPt%d" % b1, "SPh%d" % b1], w=["SPl%d" % b1])

                                def stage2(idx):
                                    kt, sl = seq[idx]
                                    h = 2 * pair + sl
                                    blk, base = h // 2, (h % 2) * 64
                                    b1 = info.pop(idx)
                                    rb = 2 + gc["g2"] % 2
                                    p2 = gc["g2"] % 3
                                    gc["g2"] += 1
                                    ob = 4 + sl
                                    first = (kt == nkt - 1)
                                    r_ = kt - (nkt - 4)
                                    ksl = slice(kt * 128, (kt + 1) * 128)
                                    op("pe", lambda e: e.matmul(pf[rb][:, :], lhsT=tri[:], rhs=SPh[b1][:], start=True, stop=False), r=["tri", "SPh%d" % b1], w=["pf%d" % rb])
                                    op("pe", lambda e: e.matmul(pf[rb][:, :], lhsT=tri[:], rhs=SPl[b1][:], start=False, stop=False), r=["tri", "SPl%d" % b1], w=["pf%d" % rb])
                                    if not first:
                                        cu = cidx[sl] % 2
                                        op("pe", lambda e: e.matmul(pf[rb][:, :], lhsT=ones_bf[:], rhs=Ch[sl][cu][:], start=False, stop=False), r=["ones_bf", "Ch%d_%d" % (sl, cu)], w=["pf%d" % rb])
                                        op("pe", lambda e: e.matmul(pf[rb][:, :], lhsT=ones_bf[:], rhs=Cl[sl][cu][:], start=False, stop=False), r=["ones_bf", "Cl%d_%d" % (sl, cu)], w=["pf%d" % rb])
                                    op("pe", lambda e: e.matmul(pf[rb][:, :], lhsT=nK[:, blk, ksl], rhs=QzA[j % 2][h][:, :], start=False, stop=True),
                                       r=["nK", "QzA%d_%d" % (j % 2, h)], w=["pf%d" % rb])
                                    if kt > 0:
                                        cidx[sl] += 1
                                        cw = cidx[sl] % 2
                                        if first:
                                            op("pool", lambda e: e.tensor_copy(out=Cf[sl][:], in_=SPt[b1][:]), r=["SPt%d" % b1], w=["Cf%d" % sl])
                                        else:
                                            op("pool", lambda e: e.tensor_tensor(out=Cf[sl][:], in0=Cf[sl][:], in1=SPt[b1][:], op=ALU.add), r=["SPt%d" % b1, "Cf%d" % sl], w=["Cf%d" % sl])
                                        op("act", lambda e: e.activation(out=Ch[sl][cw][:], in_=Cf[sl][:], func=AF.Copy), r=["Cf%d" % sl], w=["Ch%d_%d" % (sl, cw)])
                                        op("pool", lambda e: e.tensor_tensor(out=Cl[sl][cw][:], in0=Cf[sl][:], in1=Ch[sl][cw][:], op=ALU.subtract), r=["Cf%d" % sl, "Ch%d_%d" % (sl, cw)], w=["Cl%d_%d" % (sl, cw)])
                                    op("act", lambda e: e.activation(out=PT[p2][:], in_=pf[rb][:, :], func=AF.Exp, scale=-1.0), r=["pf%d" % rb], w=["PT%d" % p2])
                                    if r_ >= 0:
                                        op("dve", lambda e: e.tensor_tensor(out=PT[p2][:], in0=PT[p2][:], in1=masks[:, 4 + r_, :], op=ALU.mult), r=["PT%d" % p2, "masks"], w=["PT%d" % p2])
                                    op("pe", lambda e: e.matmul(pf[ob][:, :], lhsT=V[:, kt, h, :], rhs=PT[p2][:], start=first, stop=(kt == 0)), r=["Vr", "PT%d" % p2], w=["pf%d" % ob])

                                LA = 2
                                for idx in range(len(seq) + LA):
                                    if idx < len(seq):
                                        stage1(idx)
                                    if idx >= LA:
                                        stage2(idx - LA)
                                for sl in range(2):
                                    h = 2 * pair + sl
                                    blk, base = h // 2, (h % 2) * 64
                                    ob = 4 + sl
                                    op("dve", lambda e: e.tensor_tensor(out=YS[j % 2][base:base + 64, blk, :], in0=pf[ob][0:64, :], in1=G[base:base + 64, blk, :], op=ALU.mult),
                                       r=["pf%d" % ob, Gk], w=[ysk])
                            q0 = j * 512
                            dma(YT[0:2, :, q0:q0 + 512].rearrange("b p t -> p b t"), YS[j % 2][:], r=[ysk], slot=ysk)
                        sc.barrier()

                if DO_OUT:
                    with ExitStack() as ms:
                        wo = sb(ms, "wo", [128, 8, D_MODEL], BF16)
                        wos = [sb(ms, "wos%d" % i, [128, D_MODEL]) for i in range(2)]
                        for c in range(8):
                            k = "wos%d" % (c % 2)
                            dma(wos[c % 2][:], w_out[l, c * 128:(c + 1) * 128, :], w=[k], slot=k)
                            op("dve" if c % 2 == 0 else "pool", lambda e, c=c: e.tensor_copy(out=wo[:, c, :], in_=wos[c % 2][:]), r=[k], w=["wo"])
                        YTs = [sb(ms, "YTs%d" % i, [128, 8, 512], BF16) for i in range(2)]
                        xs = [sb(ms, "xs%d" % i, [128, D_MODEL]) for i in range(2)]
                        xo = [sb(ms, "xo%d" % i, [128, D_MODEL]) for i in range(2)]

                        def load_y(stl):
                            q0 = stl * 512
                            for g in range(0, 8, 4):
                                dma(YTs[stl % 2][:, g:g + 4, :], YT[g:g + 4, :, q0:q0 + 512].rearrange("b p t -> p b t"), w=["YTs%d" % (stl % 2)], slot="YTs%d" % (stl % 2))

                        load_y(0)
                        for stl in range(NST):
                            if stl + 1 < NST:
                                load_y(stl + 1)
                            for sub in range(4):
                                t = stl * 4 + sub
                                xk, ok = "xs%d" % (t % 2), "xo%d" % (t % 2)
                                dma(xs[t % 2][:], x_src[t * 128:(t + 1) * 128, :], w=[xk], slot=xk)
                                for half in range(2):
                                    bank = (t * 2 + half) % 2
                                    for c in range(8):
                                        op("pe", lambda e, c=c: e.matmul(pf[bank][:, :], lhsT=YTs[stl % 2][:, c, sub * 128:(sub + 1) * 128], rhs=wo[:, c, half * 512:(half + 1) * 512],
                                                                         start=(c == 0), stop=(c == 7)), r=["YTs%d" % (stl % 2), "wo"], w=["pf%d" % bank])
                                    op("dve", lambda e: e.tensor_tensor(out=xo[t % 2][:, half * 512:(half + 1) * 512], in0=pf[bank][:, :], in1=xs[t % 2][:, half * 512:(half + 1) * 512], op=ALU.add),
                                       r=["pf%d" % bank, xk], w=[ok])
                                dma(x_dst[t * 128:(t + 1) * 128, :], xo[t % 2][:], r=[ok], slot=ok)
                        sc.barrier()
        sc.barrier()
        print("instructions:", sc.n_ins)
    return nc


def host_consts(S):
    bf = ml_dtypes.bfloat16
    c = {}
    c["c_ident"] = np.eye(128, dtype=np.float32).astype(bf)

    def cs(d):
        inv = (10000.0 ** (-np.arange(0, d, 2, dtype=np.float32) / np.float32(d))).astype(np.float32)
        ang = (np.arange(S, dtype=np.float32)[:, None] * inv[None, :]).astype(np.float32)
        return np.concatenate([np.cos(ang), np.sin(ang)], axis=1).astype(np.float32)

    c["c_cs64"] = cs(64)
    c["c_cs32"] = cs(32)
    kk = np.arange(128)[:, None]
    qq = np.arange(512)[None, :]
    m = []
    for r in range(4):
        m.append(((r * 128 + kk) < ((qq // 64) + 1) * 64))
    for r in range(4):
        m.append(((r * 128 + kk) < qq))
    c["c_mask"] = np.concatenate(m, axis=1).astype(np.float32).astype(bf)
    q1 = np.arange(128)[:, None]
    k1 = np.arange(128)[None, :]
    m01 = (k1 < ((q1 // 64) + 1) * 64).astype(np.float32)
    c["c_adm"] = np.concatenate([m01, (1.0 - m01) * NEGBIG], axis=1).astype(np.float32)
    c["c_tri"] = (np.arange(128)[:, None] >= np.arange(128)[None, :]).astype(np.float32).astype(bf)
    c["c_pow2"] = np.tile((2.0 ** -(np.arange(32) + 1.0))[None, :], (128, 1)).astype(np.float32)
    return c


def host_params(inp, DEPTH):
    f = lambda a: np.ascontiguousarray(np.asarray(a, dtype=np.float32)[:DEPTH])
    p = {}
    p["w_in"] = np.ascontiguousarray(f(inp["w_in"])[:, :, PERM])
    p["w_out"] = f(inp["w_out"])
    p["w_uq"] = f(inp["mla_w_uq"])
    p["w_ukv"] = f(inp["mla_w_ukv"])
    p["ln_g"] = np.ascontiguousarray(f(inp["ln_g"]).reshape(-1, 8, 128).transpose(0, 2, 1))
    p["qn_g"] = np.ascontiguousarray(f(inp["mla_q_norm_g"]).reshape(-1, 2, 128).transpose(0, 2, 1))
    p["kvn_g"] = np.ascontiguousarray(f(inp["mla_kv_norm_g"]).reshape(-1, 128, 1))
    dq, dkk = f(inp["dsa_q_g"]), f(inp["dsa_k_g"])
    p["g64"] = np.ascontiguousarray(np.concatenate([dq] * 4 + [dkk], axis=1))
    fq, fk, mk = f(inp["diff_q_g"]), f(inp["diff_k_g"]), f(inp["mla_k_g"])
    p["g32"] = np.ascontiguousarray(np.concatenate([fq] * 8 + [fk] * 8 + [mk[:, 64:96]], axis=1))
    p["gq96"] = f(inp["mla_q_g"])
    p["gk64"] = np.ascontiguousarray(mk[:, :64])
    p["subln"] = np.ascontiguousarray(f(inp["diff_subln_g"]).reshape(-1, 64, 1))
    p["lvec"] = np.ascontiguousarray(np.concatenate([f(inp["diff_lq1"]), f(inp["diff_lk1"]), f(inp["diff_lq2"]), f(inp["diff_lk2"])], axis=1))
    return p


_CACHE = {}


def run(inputs, S, DEPTH, debug=False):
    import math
    key = (S, DEPTH, debug)
    lam_inits = [0.8 - 0.6 * math.exp(-0.3 * l) for l in range(DEPTH)]
    if key not in _CACHE:
        _CACHE[key] = build(S, DEPTH, debug, lam_inits, MIX=globals().get("MIXSEL", "ABCD"))
    nc = _CACHE[key]
    x = np.asarray(inputs["x"], dtype=np.float32)
    B = x.shape[0]
    shared = {}
    shared.update(host_consts(S))
    shared.update(host_params(inputs, DEPTH))
    in_maps = []
    for b in range(B):
        m = dict(shared)
        m["x"] = np.ascontiguousarray(x[b, :S])
        in_maps.append(m)
    res = run_bass_kernel_spmd(nc, in_maps, core_ids=list(range(B)))
    return res


def kernel(**inputs):
    res = run(inputs, 8192, 4)
    return np.stack([np.asarray(r["y"], dtype=np.float32) for r in res.results], axis=0)
```
